# Optimizing a Trainium2 kernel written in Bass

```python
import math
import jax
import jax.numpy as jnp
from jax import lax
import numpy as np


D_MODEL = 2048
BATCH = 1
SEQ = 16384
DEPTH = 1

CHUNK = 64
PLE_DIM = 256
EPS = 1e-6

RET_HEADS = 8
RET_HEAD_DIM = 128
RET_WIDTH = RET_HEADS * RET_HEAD_DIM
ROPE_THETA = 10000.0

SSM_WIDTH = 1024
SSM_GROUP_SIZE = 16
SSM_GROUPS = SSM_WIDTH // SSM_GROUP_SIZE
SSM_STATE = 64
DT_MIN = 1e-3
DT_MAX = 1e-1

IN_COLS = 4 * RET_WIDTH + SSM_WIDTH

N_EXPERT_GROUPS = 4
EXPERTS_PER_GROUP = 8
N_EXPERTS = N_EXPERT_GROUPS * EXPERTS_PER_GROUP
TOP_K_INNER = 2
D_EXPERT = 512
MOE_BLOCK = 128

kernel_name = 'hybrid_retention_s5_hmoe_block'


def _rmsnorm(x, w):
    xf = x.astype(jnp.float32)
    y = xf * lax.rsqrt(jnp.mean(xf * xf, axis=-1, keepdims=True) + EPS)
    return (y * w.astype(jnp.float32)).astype(x.dtype)


def _rope(t):
    seq = t.shape[1]
    half = t.shape[-1] // 2
    inv_freq = ROPE_THETA ** (-jnp.arange(half, dtype=jnp.float32) / half)
    ang = jnp.arange(seq, dtype=jnp.float32)[:, None] * inv_freq[None, :]
    cos = jnp.cos(ang)[None, :, None, :]
    sin = jnp.sin(ang)[None, :, None, :]
    t1, t2 = t[..., :half], t[..., half:]
    return jnp.concatenate([t1 * cos - t2 * sin, t1 * sin + t2 * cos], axis=-1)


def _retention(q, k, v, g, gn_w):
    bsz, seq, nh, dh = q.shape
    nc = seq // CHUNK
    q = _rope(q)
    k = _rope(k) * (dh ** -0.5)
    log_gamma = jnp.log1p(-jnp.exp2(-5.0 - jnp.arange(nh, dtype=jnp.float32)))
    pos = jnp.arange(CHUNK, dtype=jnp.float32)
    intra_decay = jnp.exp(log_gamma[:, None, None] * jnp.abs(pos[:, None] - pos[None, :]))
    key_decay = jnp.exp(log_gamma[:, None] * (CHUNK - 1.0 - pos)[None, :])
    query_decay = jnp.exp(log_gamma[:, None] * (pos + 1.0)[None, :])
    chunk_decay = jnp.exp(log_gamma * CHUNK)
    qc = q.reshape(bsz, nc, CHUNK, nh, dh)
    kc = k.reshape(bsz, nc, CHUNK, nh, dh)
    vc = v.reshape(bsz, nc, CHUNK, nh, dh)
    scores = jnp.einsum('bnqhd,bnkhd->bnhqk', qc, kc) * intra_decay
    y_intra = jnp.einsum('bnhqk,bnkhe->bnqhe', scores, vc)
    kv = jnp.einsum('bnkhd,bnkhe,hk->nbhde', kc, vc, key_decay)

    def step(state, kv_n):
        return state * chunk_decay[None, :, None, None] + kv_n, state

    _, state_prev = lax.scan(step, jnp.zeros((bsz, nh, dh, dh), jnp.float32), kv)
    y_cross = jnp.einsum('bnqhd,nbhde,hq->bnqhe', qc, state_prev, query_decay)
    y = (y_intra + y_cross).reshape(bsz, seq, nh, dh)
    mu = jnp.mean(y, axis=-1, keepdims=True)
    var = jnp.mean(jnp.square(y - mu), axis=-1, keepdims=True)
    y = ((y - mu) * lax.rsqrt(var + EPS)).reshape(bsz, seq, nh * dh) * gn_w
    return jax.nn.silu(g) * y


def _s5(u, lam_re, lam_im, log_dt, b_re, b_im, c_re, c_im, d, w_glu):
    bsz, seq, _ = u.shape
    f32 = jnp.float32
    ug = u.reshape(bsz, seq, SSM_GROUPS, SSM_GROUP_SIZE)
    lam = lax.complex(jnp.minimum(lam_re.astype(f32), -1e-4), lam_im.astype(f32))
    dt = jnp.exp(log_dt.astype(f32))[:, None]
    log_lam_bar = lam * dt
    lam_bar = jnp.exp(log_lam_bar)
    b_mat = lax.complex(b_re.astype(f32), b_im.astype(f32))
    b_bar = ((lam_bar - 1.0) / lam)[..., None] * b_mat
    bu = jnp.einsum('gpc,bsgc->bsgp', b_bar, ug.astype(jnp.complex64))
    steps = jnp.ones((1, seq, 1, 1), f32)

    def combine(left, right):
        n_l, s_l = left
        n_r, s_r = right
        return n_l + n_r, jnp.exp(n_r.astype(jnp.complex64) * log_lam_bar) * s_l + s_r

    _, states = lax.associative_scan(combine, (steps, bu), axis=1)
    c_mat = lax.complex(c_re.astype(f32), c_im.astype(f32))
    y = jnp.real(jnp.einsum('gcp,bsgp->bsgc', c_mat, states)).reshape(bsz, seq, SSM_WIDTH)
    y = y + d.astype(f32) * u
    y = jax.nn.gelu(y)
    return y * jax.nn.sigmoid(y @ w_glu.astype(f32))


def _hybrid_mixer(h, w_in, ret_gn_w, lam_re, lam_im, log_dt, b_re, b_im, c_re, c_im,
                  ssm_d, w_glu, w_branch_a, w_branch_b, w_merge, w_out):
    bsz, seq, _ = h.shape
    f32 = jnp.float32
    proj = h @ w_in
    q, k, v, g, u = jnp.split(proj, [RET_WIDTH, 2 * RET_WIDTH, 3 * RET_WIDTH, 4 * RET_WIDTH], axis=-1)

    def heads(t):
        return t.astype(f32).reshape(bsz, seq, RET_HEADS, RET_HEAD_DIM)

    y_ret = _retention(heads(q), heads(k), heads(v), g.astype(f32), ret_gn_w.astype(f32)).astype(h.dtype)
    y_ssm = _s5(u.astype(f32), lam_re, lam_im, log_dt, b_re, b_im, c_re, c_im, ssm_d, w_glu).astype(h.dtype)
    gates = jax.nn.sigmoid(h @ w_merge).reshape(bsz, seq, 2, D_MODEL)
    merged = gates[:, :, 0] * (y_ret @ w_branch_a) + gates[:, :, 1] * (y_ssm @ w_branch_b)
    return merged @ w_out


def _hier_moe(h, w_rg, b_rg, w_re, b_re, w_gate, w_up, w_down):
    bsz, seq, dm = h.shape
    n_tok = bsz * seq
    ht = h.reshape(n_tok, dm)
    g_prob = jax.nn.softmax((ht @ w_rg).astype(jnp.float32) + b_rg.astype(jnp.float32), axis=-1)
    g_w, g_idx = lax.top_k(g_prob, 1)
    e_logits = ((ht @ w_re).astype(jnp.float32) + b_re.astype(jnp.float32)).reshape(
        n_tok, N_EXPERT_GROUPS, EXPERTS_PER_GROUP)
    e_sel = e_logits[jnp.arange(n_tok), g_idx[:, 0]]
    top_logit, top_j = lax.top_k(e_sel, TOP_K_INNER)
    e_w = jax.nn.softmax(top_logit, axis=-1) * g_w
    expert = g_idx * EXPERTS_PER_GROUP + top_j
    n_assign = n_tok * TOP_K_INNER
    flat_e = expert.reshape(-1).astype(jnp.int32)
    flat_tok = jnp.repeat(jnp.arange(n_tok, dtype=jnp.int32), TOP_K_INNER)
    flat_w = e_w.reshape(-1)
    order = jnp.argsort(flat_e)
    se, stok, sw = flat_e[order], flat_tok[order], flat_w[order]
    counts = jnp.bincount(flat_e, length=N_EXPERTS).astype(jnp.int32)
    start = jnp.cumsum(counts) - counts
    padded = (counts + MOE_BLOCK - 1) // MOE_BLOCK * MOE_BLOCK
    pad_end = jnp.cumsum(padded)
    pad_start = pad_end - padded
    dest = pad_start[se] + (jnp.arange(n_assign, dtype=jnp.int32) - start[se])
    n_rows = n_assign + N_EXPERTS * MOE_BLOCK
    n_blocks = n_rows // MOE_BLOCK
    row_tok = jnp.zeros((n_rows,), jnp.int32).at[dest].set(stok)
    row_w = jnp.zeros((n_rows,), jnp.float32).at[dest].set(sw)
    block_start = jnp.arange(n_blocks, dtype=jnp.int32) * MOE_BLOCK
    block_expert = jnp.minimum(jnp.searchsorted(pad_end, block_start, side='right'), N_EXPERTS - 1)

    def expert_block(args):
        e, toks = args
        xb = ht[toks]
        a = jax.nn.silu(xb @ w_gate[e]) * (xb @ w_up[e])
        return a @ w_down[e]

    y_rows = lax.map(expert_block, (block_expert, row_tok.reshape(n_blocks, MOE_BLOCK)))
    y_rows = y_rows.reshape(n_rows, dm) * row_w[:, None].astype(h.dtype)
    out = jax.ops.segment_sum(y_rows, row_tok, num_segments=n_tok)
    return out.reshape(bsz, seq, dm)


def setup_inputs(seed: int = 0) -> dict:
    key = jax.random.key(seed)
    ks = jax.random.split(key, 32)
    f32 = jnp.float32

    def nrm(k, shape, scale):
        return jax.random.normal(k, shape, f32) * scale

    def gain(k, shape):
        return 1.0 + 0.02 * jax.random.normal(k, shape, f32)

    L, D = DEPTH, D_MODEL
    R, W, G, P, Cg = RET_WIDTH, SSM_WIDTH, SSM_GROUPS, SSM_STATE, SSM_GROUP_SIZE
    E, F = N_EXPERTS, D_EXPERT
    return {
        'x': nrm(ks[0], (BATCH, SEQ, D), 1.0),
        'p': nrm(ks[1], (L, BATCH, SEQ, PLE_DIM), 1.0),
        'norm_mix': gain(ks[2], (L, D)),
        'w_in': nrm(ks[3], (L, D, IN_COLS), D ** -0.5),
        'ret_gn_w': gain(ks[4], (L, R)),
        'ssm_lam_re': -0.5 + nrm(ks[5], (L, G, P), 0.01),
        'ssm_lam_im': math.pi * jnp.arange(P, dtype=f32) + nrm(ks[6], (L, G, P), 0.01),
        'ssm_log_dt': jax.random.uniform(ks[7], (L, G), f32, math.log(DT_MIN), math.log(DT_MAX)),
        'ssm_b_re': nrm(ks[8], (L, G, P, Cg), (2 * Cg) ** -0.5),
        'ssm_b_im': nrm(ks[9], (L, G, P, Cg), (2 * Cg) ** -0.5),
        'ssm_c_re': nrm(ks[10], (L, G, Cg, P), P ** -0.5),
        'ssm_c_im': nrm(ks[11], (L, G, Cg, P), P ** -0.5),
        'ssm_d': nrm(ks[12], (L, W), 1.0),
        'w_glu': nrm(ks[13], (L, W, W), W ** -0.5),
        'w_branch_a': nrm(ks[14], (L, R, D), R ** -0.5),
        'w_branch_b': nrm(ks[15], (L, W, D), W ** -0.5),
        'w_merge': nrm(ks[16], (L, D, 2 * D), D ** -0.5),
        'w_out': nrm(ks[17], (L, D, D), D ** -0.5),
        'norm_ffn': gain(ks[18], (L, D)),
        'w_router_group': nrm(ks[19], (L, D, N_EXPERT_GROUPS), D ** -0.5),
        'b_router_group': nrm(ks[20], (L, N_EXPERT_GROUPS), 0.01),
        'w_router_expert': nrm(ks[21], (L, D, E), D ** -0.5),
        'b_router_expert': nrm(ks[22], (L, E), 0.01),
        'w_exp_gate': nrm(ks[23], (L, E, D, F), D ** -0.5),
        'w_exp_up': nrm(ks[24], (L, E, D, F), D ** -0.5),
        'w_exp_down': nrm(ks[25], (L, E, F, D), F ** -0.5),
        'norm_ple': gain(ks[26], (L, D)),
        'w_ple_gate': nrm(ks[27], (L, D, D), D ** -0.5),
        'w_ple': nrm(ks[28], (L, PLE_DIM, D), PLE_DIM ** -0.5),
        'norm_f': gain(ks[29], (D,)),
    }


def reference(x, p, norm_mix, w_in, ret_gn_w, ssm_lam_re, ssm_lam_im, ssm_log_dt,
              ssm_b_re, ssm_b_im, ssm_c_re, ssm_c_im, ssm_d, w_glu, w_branch_a, w_branch_b,
              w_merge, w_out, norm_ffn, w_router_group, b_router_group, w_router_expert,
              b_router_expert, w_exp_gate, w_exp_up, w_exp_down, norm_ple, w_ple_gate,
              w_ple, norm_f):
    for i in range(DEPTH):
        h = _rmsnorm(x, norm_mix[i])
        x = x + _hybrid_mixer(h, w_in[i], ret_gn_w[i], ssm_lam_re[i], ssm_lam_im[i], ssm_log_dt[i],
                              ssm_b_re[i], ssm_b_im[i], ssm_c_re[i], ssm_c_im[i], ssm_d[i], w_glu[i],
                              w_branch_a[i], w_branch_b[i], w_merge[i], w_out[i])
        h = _rmsnorm(x, norm_ffn[i])
        x = x + _hier_moe(h, w_router_group[i], b_router_group[i], w_router_expert[i],
                          b_router_expert[i], w_exp_gate[i], w_exp_up[i], w_exp_down[i])
        h = _rmsnorm(x, norm_ple[i])
        x = x + jax.nn.sigmoid(h @ w_ple_gate[i]) * (p[i] @ w_ple[i])
    return _rmsnorm(x, norm_f)
```

```python
import numpy as np
import ml_dtypes
from contextlib import ExitStack
import concourse.bass as bass
import concourse.mybir as mybir
from concourse.bass_utils import run_bass_kernel_spmd

F32 = mybir.dt.float32
BF16 = mybir.dt.bfloat16
AF = mybir.ActivationFunctionType
ALU = mybir.AluOpType

NCORES = 8
SEQ = 16384
T = SEQ // NCORES
NT = T // 128
D = 2048
KC = D // 128
H = 8
DH = 128
RW = 1024
EPS = 1e-6
PI = float(np.pi)


class Buf:
    __slots__ = ("name", "w", "r")

    def __init__(self, name):
        self.name = name
        self.w = None
        self.r = {}


class K:
    NDSEM = 24

    def __init__(self, nc, es):
        self.nc = nc
        self.es = es
        self.eng = {"pe": nc.tensor, "act": nc.scalar, "dve": nc.vector, "pool": nc.gpsimd, "sp": nc.sync}
        self.sem = {n: es.enter_context(nc.semaphore("s_" + n)) for n in self.eng}
        self.cnt = {n: 0 for n in self.eng}
        self.known = {n: {} for n in self.eng}
        self.dsem = {q: [es.enter_context(nc.semaphore("d_%s%d" % (q, i))) for i in range(self.NDSEM)]
                     for q in ("sp", "pool", "act")}
        self.duse = {q: [0] * self.NDSEM for q in self.dsem}
        self.dnext = {q: 0 for q in self.dsem}
        self.nbuf = 0

    def buf(self, name=None):
        self.nbuf += 1
        return Buf(name or ("b%d" % self.nbuf))

    def bufs(self, n, name="b"):
        return [self.buf("%s%d" % (name, i)) for i in range(n)]

    def _semof(self, key):
        return self.sem[key] if isinstance(key, str) else self.dsem[key[1]][key[2]]

    def _wait(self, e, deps):
        need = {}
        for t in deps:
            if t is None:
                continue
            k, v = t
            if need.get(k, 0) < v:
                need[k] = v
        kn = self.known[e]
        for k, v in need.items():
            if kn.get(k, 0) >= v:
                continue
            self.eng[e].wait_ge(self._semof(k), v)
            kn[k] = v

    SAME_ENGINE_WAIT = True

    def _deps(self, e, reads, writes):
        deps = []
        skip_same = (e == "pe") or (not self.SAME_ENGINE_WAIT)
        for b in reads:
            if b.w is not None and not (skip_same and b.w[0] == e):
                deps.append(b.w)
        for b in writes:
            if b.w is not None and not (skip_same and b.w[0] == e):
                deps.append(b.w)
            for k, v in b.r.items():
                if k == e:
                    continue
                deps.append((k, v))
        return deps

    def op(self, e, fn, reads=(), writes=()):
        self._wait(e, self._deps(e, reads, writes))
        ins = fn(self.eng[e])
        self.cnt[e] += 1
        c = self.cnt[e]
        ins.then_inc(self.sem[e], 1)
        for b in reads:
            if b.r.get(e, 0) < c:
                b.r[e] = c
        for b in writes:
            b.w = (e, c)
            b.r = {}
        return ins

    def dma(self, q, out, in_, reads=(), writes=()):
        deps = self._deps(q, reads, writes)
        idx = self.dnext[q]
        self.dnext[q] = (idx + 1) % self.NDSEM
        key = ("d", q, idx)
        if self.duse[q][idx] > 0:
            deps.append((key, 16 * self.duse[q][idx]))
        self._wait(q, deps)
        ins = self.eng[q].dma_start(out=out, in_=in_)
        self.duse[q][idx] += 1
        v = 16 * self.duse[q][idx]
        ins.then_inc(self.dsem[q][idx], 16)
        for b in reads:
            if b.r.get(key, 0) < v:
                b.r[key] = v
        for b in writes:
            b.w = (key, v)
            b.r = {}
        return ins

    def qop(self, q, fn, reads=(), writes=()):
        deps = self._deps(q, reads, writes)
        idx = self.dnext[q]
        self.dnext[q] = (idx + 1) % self.NDSEM
        key = ("d", q, idx)
        if self.duse[q][idx] > 0:
            deps.append((key, 16 * self.duse[q][idx]))
        self._wait(q, deps)
        ins = fn(self.eng[q])
        self.duse[q][idx] += 1
        v = 16 * self.duse[q][idx]
        ins.then_inc(self.dsem[q][idx], 16)
        for b in reads:
            if b.r.get(key, 0) < v:
                b.r[key] = v
        for b in writes:
            b.w = (key, v)
            b.r = {}
        return ins

    def barrier(self):
        deps = [(n, c) for n, c in self.cnt.items() if c > 0]
        for q in self.dsem:
            for i, u in enumerate(self.duse[q]):
                if u > 0:
                    deps.append((("d", q, i), 16 * u))
        for e in self.eng:
            self._wait(e, [d for d in deps if d[0] != e])

    def finish(self):
        deps = [(n, c) for n, c in self.cnt.items() if c > 0 and n != "sp"]
        for q in self.dsem:
            for i, u in enumerate(self.duse[q]):
                if u > 0:
                    deps.append((("d", q, i), 16 * u))
        self._wait("sp", deps)


def _gammas():
    return 1.0 - np.exp2(-5.0 - np.arange(H, dtype=np.float64))


def host_tables(core):
    half = DH // 2
    inv_freq = (np.float32(10000.0) ** (-np.arange(half, dtype=np.float32) / np.float32(half))).astype(np.float32)
    pos = (core * T + np.arange(T)).astype(np.float32)
    ang = (pos[:, None] * inv_freq[None, :]).astype(np.float32).astype(np.float64)
    cos = np.cos(ang).astype(np.float32).reshape(NT, 128, half).transpose(1, 0, 2)
    sin = np.sin(ang).astype(np.float32).reshape(NT, 128, half).transpose(1, 0, 2)
    g = _gammas()
    a = np.arange(128)
    ka, qb = a[:, None], a[None, :]
    same = (ka // 64) == (qb // 64)
    earlier = (ka // 64) < (qb // 64)
    mask = np.zeros((128, H, 128), np.float64)
    for h in range(H):
        m = np.where(same, g[h] ** np.abs(qb - ka), np.where(earlier, g[h] ** (qb - ka).clip(0), 0.0))
        mask[:, h, :] = m * DH ** -0.5
    qdec = np.stack([g[h] ** (a + 1.0) for h in range(H)], axis=1)
    kdec = np.stack([g[h] ** (127.0 - a) * DH ** -0.5 for h in range(H)], axis=1)
    onehot = np.zeros((128, NCORES), np.float64)
    onehot[:, core] = 1.0
    tabs = np.concatenate([cos.reshape(128, -1), sin.reshape(128, -1), mask.reshape(128, -1), qdec, kdec, onehot],
                          axis=1).astype(np.float32)
    return np.ascontiguousarray(tabs)


TAB_COS = 0
TAB_SIN = TAB_COS + NT * 64
TAB_MASK = TAB_SIN + NT * 64
TAB_QDEC = TAB_MASK + H * 128
TAB_KDEC = TAB_QDEC + H
TAB_ONEHOT = TAB_KDEC + H
TAB_W = TAB_ONEHOT + NCORES
S5P_W = 96 + 4 * 512
NQ = 32
AGW = 1024 + 64
SMALL_W = 16 + 1024 + 16 + 16 + 36
NE = 32
NPT = (NCORES - 1) * NT
CG = 384
FE = 512


class _Done(Exception):
    pass


def build(debug=(), mode="single"):
    with_carry = mode in ("states", "main", "fused")
    nc = bass.Bass("TRN2", target_bir_lowering=False)
    es = ExitStack()
    k = K(nc, es)
    G128 = [float(x) for x in _gammas() ** 128]
    G2048 = [float(x) for x in _gammas() ** 2048]

    def din(name, shape, dt=F32):
        return nc.dram_tensor(name, list(shape), dt, kind="ExternalInput").ap()

    def dout(name, shape, dt=F32):
        return nc.dram_tensor(name, list(shape), dt, kind="ExternalOutput").ap()

    def dscr(name, shape, dt=F32):
        return nc.dram_tensor(name, list(shape), dt, kind="Internal").ap()

    def sb(name, shape, dt=F32, stack=None):
        return (stack or es).enter_context(nc.sbuf_tensor("sb_" + name, list(shape), dt))

    x_d = din("x", [T, D])
    if mode == "fused":
        xfull_d = din("xfull", [SEQ, D])
        ropep_d = din("ropep", [NPT, 128, 128])
        kdecp_d = din("kdecp", [128, NPT * H])
    w_in_d = din("w_in", [D, 5120])
    tabs_d = din("tabs", [128, TAB_W])
    small_d = din("small", [128, SMALL_W])
    identb_d = din("identb", [128, 128])
    s5p_d = din("s5p", [128, S5P_W])
    s5d_d = din("s5d", [128, 8 + 32])
    w_glu_d = din("w_glu", [RW, RW])
    w_merge_d = din("w_merge", [D, 2 * D])
    w_a_d = din("w_a", [RW, D])
    w_b_d = din("w_b", [RW, D])
    w_out_d = din("w_out", [D, D])
    w_rt_d = din("w_rt", [D, 36])
    w_eg_d = din("w_eg", [NE, D, FE])
    w_eu_d = din("w_eu", [NE, D, FE])
    w_ed_d = din("w_ed", [NE, FE, D])
    w_pg_d = din("w_pg", [D, D])
    w_ple_d = din("w_ple", [256, D])
    p_d = din("p", [T, 256])
    nfb_d = din("nfb", [128, D])
    moec_d = din("moec", [128, 256 + CG])
    dbg = {}
    for nm, shp in debug:
        dbg[nm] = dout("dbg_" + nm, shp)

    yretT_d = dscr("yretT", [RW, T], BF16)
    yssmT_d = dscr("yssmT", [RW, T], BF16)
    x1_d = dscr("x1", [T, D])
    x1_w = [k.bufs(8, "x1w%d_" % i) for i in range(NT)]
    dcount = [0]
    dtmps = [sb("dump%d" % i, [128, 512], F32) for i in range(len(debug))]

    def dump(name, src, src_bufs, cols):
        if name not in dbg or dbg[name] is None:
            return
        tmp = dtmps[dcount[0]][:, 0:cols]
        dcount[0] += 1
        tb = k.buf()
        k.op("act", lambda e: e.copy(out=tmp, in_=src), reads=src_bufs, writes=[tb])
        k.dma("sp", dbg[name][:, :], tmp, reads=[tb])
        dbg[name] = None

    ps = [es.enter_context(nc.psum_tensor("ps%d" % i, [128, 512], F32)) for i in range(8)]
    psb = k.bufs(8, "ps")

    identb = sb("identb", [128, 128], BF16)
    identb_b = k.buf("identb")
    small = sb("small", [128, SMALL_W])
    small_b = k.buf("small")
    k.dma("pool", identb[:], identb_d[:, :], writes=[identb_b])
    k.dma("sp", small[:], small_d[:, :], writes=[small_b])
    nmixT = small[:, 0:16]
    gnw = small[:, 16:16 + 1024]
    nffnT = small[:, 1040:1056]
    npleT = small[:, 1056:1072]
    rbias = small[:, 1072:1108]
    def s5_setup(ss, with_cp=True, tag=""):
        s5p = sb("s5p" + tag, [128, S5P_W], F32, ss)
        s5p_b = k.buf("s5p")
        k.dma("sp", s5p[:], s5p_d[:, :], writes=[s5p_b])
        lamre, lamim, logdt = s5p[:, 0:32], s5p[:, 32:64], s5p[:, 64:96]
        Bre = s5p[:, 96:608].rearrange("p (q c) -> p q c", c=16)
        Bim = s5p[:, 608:1120].rearrange("p (q c) -> p q c", c=16)
        Cre = s5p[:, 1120:1632].rearrange("p (q c) -> p q c", c=16)
        Cim = s5p[:, 1632:2144].rearrange("p (q c) -> p q c", c=16)
        W = sb("s5w" + tag, [128, 20, 32], F32, ss)
        wb = k.buf("s5w")

        def tt(o, a, b_, op, eng="dve"):
            k.op(eng, lambda e: e.tensor_tensor(out=o, in0=a, in1=b_, op=op), reads=[wb, s5p_b], writes=[wb])

        def ts(o, a, s1, s2, op0, op1=None, eng="dve"):
            if op1 is None:
                k.op(eng, lambda e: e.tensor_scalar(out=o, in0=a, scalar1=s1, scalar2=None, op0=op0), reads=[wb, s5p_b], writes=[wb])
            else:
                k.op(eng, lambda e: e.tensor_scalar(out=o, in0=a, scalar1=s1, scalar2=s2, op0=op0, op1=op1), reads=[wb, s5p_b], writes=[wb])

        def act(o, a, f):
            k.op("act", lambda e: e.activation(out=o, in_=a, func=f), reads=[wb, s5p_b], writes=[wb])

        lr, dt, a_, b_, ea, r1, r2, sbn, cbn, Lre, Lim, nr, den, t0, t1_, cr, ci, t2_, t3_ = [W[:, i, :] for i in range(19)]
        ts(lr, lamre, -1e-4, None, ALU.min)
        act(dt, logdt, AF.Exp)
        tt(a_, lr, dt, ALU.mult)
        tt(b_, lamim, dt, ALU.mult)
        act(ea, a_, AF.Exp)
        for (dst, off) in ((r1, PI), (r2, 1.5 * PI)):
            ts(dst, b_, off, None, ALU.add)
            ts(t0, dst, 2 * PI, -2 * PI, ALU.is_ge, ALU.mult)
            tt(t1_, dst, t0, ALU.add)
            ts(t0, dst, 4 * PI, -2 * PI, ALU.is_ge, ALU.mult)
            tt(t1_, t1_, t0, ALU.add)
            ts(t0, dst, 6 * PI, -2 * PI, ALU.is_ge, ALU.mult)
            tt(t1_, t1_, t0, ALU.add)
            ts(dst, t1_, -PI, None, ALU.add)
        act(sbn, r1, AF.Sin)
        act(cbn, r2, AF.Sin)
        tt(Lre, ea, cbn, ALU.mult)
        tt(Lim, ea, sbn, ALU.mult)
        ts(nr, Lre, -1.0, None, ALU.add)
        tt(den, lr, lr, ALU.mult)
        tt(t0, lamim, lamim, ALU.mult)
        tt(den, den, t0, ALU.add)
        k.op("dve", lambda e: e.reciprocal(out=den, in_=den), reads=[wb], writes=[wb])
        tt(t0, nr, lr, ALU.mult)
        tt(t1_, Lim, lamim, ALU.mult)
        tt(t0, t0, t1_, ALU.add)
        tt(cr, t0, den, ALU.mult)
        tt(t0, Lim, lr, ALU.mult)
        tt(t1_, nr, lamim, ALU.mult)
        tt(t0, t0, t1_, ALU.subtract)
        tt(ci, t0, den, ALU.mult)
        Bb = sb("s5Bb" + tag, [128, 2, 32, 16], F32, ss)
        T3 = sb("s5T3" + tag, [128, 2, 32, 16], F32, ss)
        bc = lambda v: v.unsqueeze(2).broadcast_to([128, 32, 16])
        tt(T3[:, 0], Bre, bc(cr), ALU.mult)
        tt(T3[:, 1], Bim, bc(ci), ALU.mult)
        tt(Bb[:, 0], T3[:, 0], T3[:, 1], ALU.subtract)
        tt(T3[:, 0], Bim, bc(cr), ALU.mult)
        tt(T3[:, 1], Bre, bc(ci), ALU.mult)
        tt(Bb[:, 1], T3[:, 0], T3[:, 1], ALU.add)
        k.op("dve", lambda e: e.memset(Pre[:, 0, :], 1.0), reads=[wb], writes=[wb])
        k.op("dve", lambda e: e.memset(Pim[:, 0, :], 0.0), reads=[wb], writes=[wb])
        for kk in range(8):
            tt(t0, Pre[:, kk, :], Lre, ALU.mult)
            tt(t1_, Pim[:, kk, :], Lim, ALU.mult)
            tt(Pre[:, kk + 1, :], t0, t1_, ALU.subtract)
            tt(t0, Pre[:, kk, :], Lim, ALU.mult)
            tt(t1_, Pim[:, kk, :], Lre, ALU.mult)
            tt(Pim[:, kk + 1, :], t0, t1_, ALU.add)
        k.op("dve", lambda e: e.tensor_copy(out=Dre[:, 0, :], in_=Pre[:, 8, :]), reads=[wb], writes=[wb])
        k.op("dve", lambda e: e.tensor_copy(out=Dim[:, 0, :], in_=Pim[:, 8, :]), reads=[wb], writes=[wb])
        for i in range(8):
            tt(t0, Dre[:, i, :], Dre[:, i, :], ALU.mult)
            tt(t1_, Dim[:, i, :], Dim[:, i, :], ALU.mult)
            tt(Dre[:, i + 1, :], t0, t1_, ALU.subtract)
            tt(t0, Dre[:, i, :], Dim[:, i, :], ALU.mult)
            ts(Dim[:, i + 1, :], t0, 2.0, None, ALU.mult)
        ts(nDim[:, :, :], Dim[:, :, :], -1.0, None, ALU.mult)
        for sx in range(8):
            pr = bc(Pre[:, 7 - sx, :])
            pi_ = bc(Pim[:, 7 - sx, :])
            tt(T3[:, 0], Bb[:, 0], pr, ALU.mult)
            tt(T3[:, 1], Bb[:, 1], pi_, ALU.mult)
            tt(RBc[:, :, sx, 0, :], T3[:, 0], T3[:, 1], ALU.subtract)
            tt(T3[:, 0], Bb[:, 1], pr, ALU.mult)
            tt(T3[:, 1], Bb[:, 0], pi_, ALU.mult)
            tt(RBc[:, :, sx, 1, :], T3[:, 0], T3[:, 1], ALU.add)
        for kk in (range(9) if with_cp else ()):
            pr = bc(Pre[:, kk, :])
            pi_ = bc(Pim[:, kk, :])
            tt(T3[:, 0], Cre, pr, ALU.mult)
            tt(T3[:, 1], Cim, pi_, ALU.mult)
            tt(CPc[:, :, kk, 0, :], T3[:, 0], T3[:, 1], ALU.subtract)
            tt(T3[:, 0], Cre, pi_, ALU.mult)
            tt(T3[:, 1], Cim, pr, ALU.mult)
            tt(T3[:, 0], T3[:, 0], T3[:, 1], ALU.add)
            ts(CPc[:, :, kk, 1, :], T3[:, 0], -1.0, None, ALU.mult)
        return wb

    cs = ExitStack()
    S = sb("S", [128, H, 128], F32, cs)
    Sbf = sb("Sbf", [128, H, 128], BF16, cs)
    S_b = k.bufs(H, "S")
    Sbf_b = k.bufs(H, "Sbf")
    carryP = sb("carryP", [128, 32, 2], F32, cs)
    carryP_b = k.buf("carryP")
    tabs1 = sb("tabs1", [128, NCORES], F32, cs)
    tabs1_b = k.buf("tabs1")
    k.dma("sp", tabs1[:], tabs_d[:, TAB_ONEHOT:TAB_W], writes=[tabs1_b])

    def stage1_tile(src_rows, dst3, dst_b, nT, xt_, xt_b_, xs_, xs_b_, st_, st_b_, pbanks, part="both"):
        if part in ("both", "a"):
            k.dma("sp", xt_[:], src_rows, writes=[xt_b_])
            k.op("act", lambda e: e.activation(out=xs_[:], in_=xt_[:], func=AF.Square, accum_out=st_[:, 0:1]), reads=[xt_b_], writes=[xs_b_, st_b_])
            k.op("act", lambda e: e.activation(out=st_[:, 1:2], in_=st_[:, 0:1], func=AF.Sqrt, scale=1.0 / D, bias=EPS), reads=[st_b_], writes=[st_b_])
            k.op("dve", lambda e: e.reciprocal(out=st_[:, 1:2], in_=st_[:, 1:2]), reads=[st_b_], writes=[st_b_])
            k.op("act", lambda e: e.activation(out=xs_[:], in_=xt_[:], func=AF.Copy, scale=st_[:, 1:2]), reads=[xt_b_, st_b_, xs_b_], writes=[xs_b_])
        if part in ("both", "b"):
            for half in range(2):
                pbank = ps[pbanks[half]].bitcast(BF16)
                for j in range(8):
                    kc = half * 8 + j
                    k.op("pe", lambda e: e.transpose(out=pbank[:, j * 128:(j + 1) * 128], in_=xs_[:, kc * 128:(kc + 1) * 128], identity=identb[:]),
                         reads=[xs_b_, identb_b], writes=[psb[pbanks[half]]])
                k.op("dve", lambda e: e.tensor_tensor(out=dst3[:, half * 8:(half + 1) * 8, :], in0=pbank[:, :].rearrange("p (j t) -> p j t", t=128),
                                                      in1=nT[:, half * 8:(half + 1) * 8].unsqueeze(2).broadcast_to([128, 8, 128]), op=ALU.mult),
                     reads=[psb[pbanks[half]], small_b], writes=[dst_b])

    if mode == "fused":
        uTp_d = dscr("uTp", [RW, NPT * 128], BF16)
        uTp_b = k.bufs(NPT, "uTp")
        with ExitStack() as pa:
            Wkv = sb("pWkv", [128, KC, 2048], BF16, pa)
            Wkv_b = k.bufs(4, "pWkv")
            for cb in range(4):
                c0 = RW + cb * 512
                k.dma("pool", Wkv[:, :, cb * 512:(cb + 1) * 512], w_in_d[:, c0:c0 + 512].rearrange("(kc p) c -> p kc c", p=128), writes=[Wkv_b[cb]])
            kdecP = sb("kdecP", [128, NPT, H], F32, pa)
            kdecP_b = k.buf("kdecP")
            k.dma("sp", kdecP[:], kdecp_d[:, :].rearrange("p (g h) -> p g h", h=H), writes=[kdecP_b])
            ropeP = [sb("ropeP%d" % i, [128, 2, 64], F32, pa) for i in range(2)]
            ropeP_b = k.bufs(2, "ropeP")
            xtA = [sb("pxt%d" % i, [128, D], F32, pa) for i in range(3)]
            xsA = [sb("pxs%d" % i, [128, D], BF16, pa) for i in range(3)]
            stA = [sb("pst%d" % i, [128, 2], F32, pa) for i in range(3)]
            xtA_b, xsA_b, stA_b = k.bufs(3, "pxt"), k.bufs(3, "pxs"), k.bufs(3, "pst")
            hTt = [sb("phTt%d" % i, [128, KC, 128], BF16, pa) for i in range(2)]
            hTt_b = k.bufs(2, "phTt")
            tt4 = [sb("ptt%d" % i, [128, 4, 64], F32, pa) for i in range(4)]
            tt4_b = k.bufs(4, "ptt")
            krr = [sb("pkr%d" % i, [128, 4, 2, 64], F32, pa) for i in range(2)]
            krr1_b, krr2_b = k.bufs(2, "pkr1"), k.bufs(2, "pkr2")
            ktdA = [sb("pktd%d" % i, [128, H, 128], BF16, pa) for i in range(2)]
            ktdA_b = [k.bufs(2, "pktd%d_" % i) for i in range(2)]
            vbA = [sb("pvb%d" % i, [128, 1024], BF16, pa) for i in range(2)]
            vbA_b = [k.bufs(2, "pvb%d_" % i) for i in range(2)]
            KV0, KV1 = 6, 7
            Wu = sb("pWu", [128, KC, RW], BF16, pa)
            Wu_b = k.bufs(2, "pWu")
            for cb in range(2):
                c0 = 4 * RW + cb * 512
                k.dma("pool", Wu[:, :, cb * 512:(cb + 1) * 512], w_in_d[:, c0:c0 + 512].rearrange("(kc p) c -> p kc c", p=128), writes=[Wu_b[cb]])
            uTt = [sb("puTt%d" % i, [128, 8, 128], BF16, pa) for i in range(2)]
            uTt_b = k.bufs(2, "puTt")
            for g in range(NPT):
                b = g % 2
                k.dma("sp", ropeP[b][:], ropep_d[g].rearrange("p (c f) -> p c f", c=2), writes=[ropeP_b[b]])

                def s1(gg, part):
                    b3 = gg % 3
                    stage1_tile(xfull_d[gg * 128:(gg + 1) * 128, :], hTt[gg % 2][:, :, :], hTt_b[gg % 2], nmixT, xtA[b3], xtA_b[b3], xsA[b3], xsA_b[b3],
                                stA[b3], stA_b[b3], (0, 1), part=part)
                if g == 0:
                    s1(0, "a")
                    s1(1, "a")
                    s1(0, "b")
                if g + 2 < NPT:
                    s1(g + 2, "a")
                if g + 1 < NPT:
                    s1(g + 1, "b")
                for cb in range(4):
                    pb = 2 + cb % 2
                    for kc in range(KC):
                        k.op("pe", lambda e: e.matmul(ps[pb][:, :], hTt[b][:, kc, :], Wkv[:, kc, cb * 512:(cb + 1) * 512], start=(kc == 0), stop=(kc == KC - 1)),
                             reads=[hTt_b[b], Wkv_b[cb]], writes=[psb[pb]])
                    if cb < 2:
                        X4 = ps[pb][:, :].rearrange("p (h c f) -> p h c f", c=2, f=64)
                        A, B = X4[:, :, 0, :], X4[:, :, 1, :]
                        C = ropeP[b][:, 0, :].unsqueeze(1).broadcast_to([128, 4, 64])
                        Sn = ropeP[b][:, 1, :].unsqueeze(1).broadcast_to([128, 4, 64])
                        kb = cb % 2
                        k.op("dve", lambda e: e.tensor_tensor(out=tt4[0][:], in0=A, in1=C, op=ALU.mult), reads=[psb[pb], ropeP_b[b]], writes=[tt4_b[0]])
                        k.op("dve", lambda e: e.tensor_tensor(out=tt4[1][:], in0=B, in1=Sn, op=ALU.mult), reads=[psb[pb], ropeP_b[b]], writes=[tt4_b[1]])
                        k.op("dve", lambda e: e.tensor_tensor(out=tt4[2][:], in0=A, in1=Sn, op=ALU.mult), reads=[psb[pb], ropeP_b[b]], writes=[tt4_b[2]])
                        k.op("dve", lambda e: e.tensor_tensor(out=tt4[3][:], in0=B, in1=C, op=ALU.mult), reads=[psb[pb], ropeP_b[b]], writes=[tt4_b[3]])
                        k.op("pool", lambda e: e.tensor_tensor(out=krr[kb][:, :, 0, :], in0=tt4[0][:], in1=tt4[1][:], op=ALU.subtract),
                             reads=[tt4_b[0], tt4_b[1]], writes=[krr1_b[kb]])
                        k.op("pool", lambda e: e.tensor_tensor(out=krr[kb][:, :, 1, :], in0=tt4[2][:], in1=tt4[3][:], op=ALU.add),
                             reads=[tt4_b[2], tt4_b[3]], writes=[krr2_b[kb]])
                        k.op("pool", lambda e: e.tensor_tensor(
                            out=ktdA[b][:, cb * 4:(cb + 1) * 4, :], in0=krr[kb][:, :, :, :].rearrange("p h c f -> p h (c f)"),
                            in1=kdecP[:, g, cb * 4:(cb + 1) * 4].unsqueeze(2).broadcast_to([128, 4, 128]), op=ALU.mult),
                            reads=[krr1_b[kb], krr2_b[kb], kdecP_b], writes=[ktdA_b[b][cb]])
                    else:
                        k.op("act", lambda e: e.copy(out=vbA[b][:, (cb - 2) * 512:(cb - 1) * 512], in_=ps[pb][:, :]), reads=[psb[pb]], writes=[vbA_b[b][cb - 2]])
                for ct in range(8):
                    ub_ = 4 + ct // 4
                    for kc in range(KC):
                        k.op("pe", lambda e: e.matmul(ps[ub_][:, (ct % 4) * 128:(ct % 4 + 1) * 128], Wu[:, kc, ct * 128:(ct + 1) * 128], hTt[b][:, kc, :],
                                                      start=(kc == 0), stop=(kc == KC - 1)), reads=[Wu_b[ct // 4], hTt_b[b]], writes=[psb[ub_]])
                    k.op("act", lambda e: e.copy(out=uTt[b][:, ct, :].rearrange("p (s m) -> p s m", s=8),
                                                 in_=ps[ub_][:, (ct % 4) * 128:(ct % 4 + 1) * 128].rearrange("p (m s) -> p s m", s=8)),
                         reads=[psb[ub_], uTt_b[b]], writes=[uTt_b[b]])
                k.dma("sp", uTp_d[:, g * 128:(g + 1) * 128].rearrange("(ct p) t -> p ct t", p=128), uTt[b][:], reads=[uTt_b[b]], writes=[uTp_b[g]])
                for h in range(H):
                    kvb = KV0 if h < 4 else KV1
                    k.op("pe", lambda e: e.matmul(ps[kvb][:, (h % 4) * 128:(h % 4 + 1) * 128], ktdA[b][:, h, :], vbA[b][:, h * 128:(h + 1) * 128],
                                                  start=(g == 0 and h % 4 == 0), stop=(g == NPT - 1 and h % 4 == 3)), reads=[ktdA_b[b][h // 4], vbA_b[b][h // 4]], writes=[psb[kvb]])
            for h in range(H):
                kvb = KV0 if h < 4 else KV1
                k.op("act", lambda e: e.copy(out=S[:, h, :], in_=ps[kvb][:, (h % 4) * 128:(h % 4 + 1) * 128]), reads=[psb[kvb]], writes=[S_b[h]])
                k.op("act", lambda e: e.copy(out=Sbf[:, h, :], in_=S[:, h, :]), reads=[S_b[h]], writes=[Sbf_b[h]])
        k.barrier()
        with ExitStack() as pb_:
            Pre = sb("qPre", [128, 9, 32], F32, pb_)
            Pim = sb("qPim", [128, 9, 32], F32, pb_)
            Dre = sb("qDre", [128, 9, 32], F32, pb_)
            Dim = sb("qDim", [128, 9, 32], F32, pb_)
            nDim = sb("qnDim", [128, 9, 32], F32, pb_)
            RBc = sb("qRBc", [128, 32, 8, 2, 16], BF16, pb_)
            CPc = None
            with ExitStack() as ss:
                mats_b = s5_setup(ss, with_cp=False, tag="q")
            k.barrier()
            NSEG = NCORES - 1
            NCH = NSEG * 256
            RBm = [sb("qRBm%d" % i, [128, 8, 2, 32], BF16, pb_) for i in range(2)]
            RBm_b = k.bufs(2, "qRBm")
            RBT = [sb("qRBT%d" % i, [32, 16, 128], BF16, pb_) for i in range(2)]
            RBT_b = k.bufs(2, "qRBT")
            uq = [sb("quq%d" % i, [32, NCH * 8], BF16, pb_) for i in range(2)]
            uq_b = k.bufs(2, "quq")
            wA = [sb("qwA%d" % i, [128, 2, NSEG, 256], F32, pb_) for i in range(2)]
            wA_b = k.bufs(2, "qwA")
            wB = [sb("qwB%d" % i, [128, 2, NSEG, 128], F32, pb_) for i in range(2)]
            wB_b = k.bufs(2, "qwB")
            Eall = sb("qEall", [128, 32, 2, NSEG], F32, pb_)
            Eall_b = k.bufs(32, "qEall")
            for i in range(2):
                k.op("pool", lambda e: e.memset(RBm[i][:], 0.0), writes=[RBm_b[i]])
            PTA, PTB = 0, 1
            NBK = 4
            CW = NCH // NBK
            def pfx_pair(q):
                b = q % 2
                for gi in range(2):
                    prt = slice(gi * 64, gi * 64 + 64)
                    csl = slice(gi * 16, gi * 16 + 16)
                    k.op("pool", lambda e: e.tensor_copy(out=RBm[b][prt, :, :, csl], in_=RBc[prt, q, :, :, :]), reads=[mats_b], writes=[RBm_b[b]])
                pTA = ps[PTA].bitcast(BF16)
                pTB = ps[PTB].bitcast(BF16)
                for sx in range(8):
                    for ri in range(2):
                        idx = sx * 2 + ri
                        pt, pbb = (pTA, psb[PTA]) if idx < 8 else (pTB, psb[PTB])
                        k.op("pe", lambda e: e.transpose(out=pt[0:32, (idx % 8) * 128:(idx % 8 + 1) * 128], in_=RBm[b][:, sx, ri, :], identity=identb[:]),
                             reads=[RBm_b[b], identb_b], writes=[pbb])
                k.op("act", lambda e: e.copy(out=RBT[b][:, 0:8, :], in_=pTA[0:32, :].rearrange("p (a c) -> p a c", c=128)), reads=[psb[PTA]], writes=[RBT_b[b]])
                k.op("act", lambda e: e.copy(out=RBT[b][:, 8:16, :], in_=pTB[0:32, :].rearrange("p (a c) -> p a c", c=128)), reads=[psb[PTB], RBT_b[b]], writes=[RBT_b[b]])
                yield
                k.dma("sp", uq[b][:], uTp_d[q * 32:(q + 1) * 32, :], reads=uTp_b, writes=[uq_b[b]])
                uq4 = uq[b][:, :].rearrange("p (g s m) -> p g s m", s=8, m=16)
                TPB = CW // 16
                wflat = wA[b][:, :, :, :].rearrange("p r g m -> p r (g m)")
                for ri in range(2):
                    for nbk in range(NBK):
                        pbk = 2 + (ri * NBK + nbk) % 6
                        for sx in range(8):
                            k.op("pe", lambda e: e.matmul(ps[pbk][:, 0:CW], RBT[b][:, sx * 2 + ri, :], uq4[:, nbk * TPB:(nbk + 1) * TPB, sx, :],
                                                          start=(sx == 0), stop=(sx == 7)), reads=[RBT_b[b], uq_b[b]], writes=[psb[pbk]])
                        k.op("act", lambda e: e.copy(out=wflat[:, ri, nbk * CW:(nbk + 1) * CW], in_=ps[pbk][:, 0:CW]), reads=[psb[pbk]], writes=[wA_b[b]])
                yield
                src, src_b2 = wA[b], wA_b[b]
                dst, dst_b2 = wB[b], wB_b[b]
                n = 256
                for lv in range(8):
                    hn = n // 2
                    v = src[:, :, :, 0:n].rearrange("p r g (m two) -> p r g m two", two=2)
                    dre, dim_, ndim = Dre[:, lv, q:q + 1], Dim[:, lv, q:q + 1], nDim[:, lv, q:q + 1]
                    o_re, o_im = dst[:, 0, :, 0:hn], dst[:, 1, :, 0:hn]
                    k.op("dve", lambda e: e.scalar_tensor_tensor(out=o_re, in0=v[:, 0, :, :, 0], scalar=dre, in1=v[:, 0, :, :, 1], op0=ALU.mult, op1=ALU.add),
                         reads=[src_b2, mats_b, dst_b2], writes=[dst_b2])
                    k.op("dve", lambda e: e.scalar_tensor_tensor(out=o_im, in0=v[:, 1, :, :, 0], scalar=dre, in1=v[:, 1, :, :, 1], op0=ALU.mult, op1=ALU.add),
                         reads=[src_b2, mats_b, dst_b2], writes=[dst_b2])
                    yield
                    k.op("dve", lambda e: e.scalar_tensor_tensor(out=o_re, in0=v[:, 1, :, :, 0], scalar=ndim, in1=o_re, op0=ALU.mult, op1=ALU.add),
                         reads=[src_b2, mats_b, dst_b2], writes=[dst_b2])
                    k.op("dve", lambda e: e.scalar_tensor_tensor(out=o_im, in0=v[:, 0, :, :, 0], scalar=dim_, in1=o_im, op0=ALU.mult, op1=ALU.add),
                         reads=[src_b2, mats_b, dst_b2], writes=[dst_b2])
                    yield
                    src, src_b2, dst, dst_b2 = dst, dst_b2, src, src_b2
                    n = hn
                k.op("act", lambda e: e.copy(out=Eall[:, q, :, :], in_=src[:, :, :, 0]), reads=[src_b2], writes=[Eall_b[q]])

            for q0 in range(0, NQ, 2):
                gens = [pfx_pair(q0), pfx_pair(q0 + 1)]
                while gens:
                    for gnr in list(gens):
                        try:
                            next(gnr)
                        except StopIteration:
                            gens.remove(gnr)
            Xc = sb("qXc", [128, 32, 2], F32, pb_)
            tq = sb("qtq", [128, 4, 32], F32, pb_)
            xb = k.buf("qXc")
            k.op("dve", lambda e: e.memset(Xc[:], 0.0), writes=[xb])
            k.op("dve", lambda e: e.memset(carryP[:], 0.0), writes=[carryP_b])
            d8r, d8i = Dre[:, 8, :], Dim[:, 8, :]
            for r in range(NSEG):
                tt_ = lambda o, a, b2, op: k.op("dve", lambda e: e.tensor_tensor(out=o, in0=a, in1=b2, op=op), reads=[xb, mats_b] + Eall_b, writes=[xb])
                tt_(tq[:, 0, :], Xc[:, :, 0], d8r, ALU.mult)
                tt_(tq[:, 1, :], Xc[:, :, 1], d8i, ALU.mult)
                tt_(tq[:, 2, :], Xc[:, :, 1], d8r, ALU.mult)
                tt_(tq[:, 3, :], Xc[:, :, 0], d8i, ALU.mult)
                tt_(tq[:, 0, :], tq[:, 0, :], tq[:, 1, :], ALU.subtract)
                tt_(tq[:, 2, :], tq[:, 2, :], tq[:, 3, :], ALU.add)
                tt_(Xc[:, :, 0], tq[:, 0, :], Eall[:, :, 0, r], ALU.add)
                tt_(Xc[:, :, 1], tq[:, 2, :], Eall[:, :, 1, r], ALU.add)
                k.op("dve", lambda e: e.scalar_tensor_tensor(out=carryP[:, :, :].rearrange("p q r -> p (q r)"), in0=Xc[:, :, :].rearrange("p q r -> p (q r)"),
                                                             scalar=tabs1[:, r + 1:r + 2], in1=carryP[:, :, :].rearrange("p q r -> p (q r)"),
                                                             op0=ALU.mult, op1=ALU.add), reads=[xb, tabs1_b, carryP_b], writes=[carryP_b])
        k.barrier()

    hs = ExitStack()
    hT = sb("hT", [128, KC, T], BF16, hs)
    hT_b = k.bufs(NT, "hT")
    ms = ExitStack()
    tabs = sb("tabs", [128, TAB_W], F32, ms)
    tabs_b = k.buf("tabs")
    k.dma("sp", tabs[:], tabs_d[:, :], writes=[tabs_b])
    cosT = tabs[:, TAB_COS:TAB_SIN].rearrange("p (i f) -> p i f", f=64)
    sinT = tabs[:, TAB_SIN:TAB_MASK].rearrange("p (i f) -> p i f", f=64)
    maskT = tabs[:, TAB_MASK:TAB_QDEC].rearrange("p (h f) -> p h f", f=128)
    qdec = tabs[:, TAB_QDEC:TAB_KDEC]
    kdec = tabs[:, TAB_KDEC:TAB_ONEHOT]
    onehot = tabs[:, TAB_ONEHOT:TAB_W]


    with ExitStack() as s1:
        xt = [sb("xt%d" % i, [128, D], F32, s1) for i in range(2)]
        xt_b = k.bufs(2, "xt")
        xs = [sb("xs%d" % i, [128, D], BF16, s1) for i in range(2)]
        xs_b = k.bufs(2, "xs")
        junk = sb("junk", [128, D], BF16, s1)
        junk_b = k.buf("junk")
        ss = sb("ss", [128, NT], F32, s1)
        rstd = sb("rstd", [128, NT], F32, s1)
        ss_b = k.bufs(NT, "ss")
        rstd_b = k.bufs(NT, "rstd")
        for i in range(NT):
            b = i % 2
            k.dma("sp", xt[b][:], x_d[i * 128:(i + 1) * 128, :], writes=[xt_b[b]])
            k.op("act", lambda e: e.activation(out=junk[:], in_=xt[b][:], func=AF.Square, accum_out=ss[:, i:i + 1]),
                 reads=[xt_b[b]], writes=[junk_b, ss_b[i]])
            k.op("act", lambda e: e.activation(out=rstd[:, i:i + 1], in_=ss[:, i:i + 1], func=AF.Sqrt, scale=1.0 / D, bias=EPS),
                 reads=[ss_b[i]], writes=[rstd_b[i]])
            k.op("dve", lambda e: e.reciprocal(out=rstd[:, i:i + 1], in_=rstd[:, i:i + 1]), reads=[rstd_b[i]], writes=[rstd_b[i]])
            k.op("act", lambda e: e.activation(out=xs[b][:], in_=xt[b][:], func=AF.Copy, scale=rstd[:, i:i + 1]),
                 reads=[xt_b[b], rstd_b[i]], writes=[xs_b[b]])
            for half in range(2):
                pbank = ps[half].bitcast(BF16)
                for j in range(8):
                    kc = half * 8 + j
                    k.op("pe", lambda e: e.transpose(out=pbank[:, j * 128:(j + 1) * 128],
                                                     in_=xs[b][:, kc * 128:(kc + 1) * 128], identity=identb[:]),
                         reads=[xs_b[b], identb_b], writes=[psb[half]])
                k.op("dve", lambda e: e.tensor_tensor(
                    out=hT[:, half * 8:(half + 1) * 8, i * 128:(i + 1) * 128],
                    in0=pbank[:, :].rearrange("p (j t) -> p j t", t=128),
                    in1=nmixT[:, half * 8:(half + 1) * 8].unsqueeze(2).broadcast_to([128, 8, 128]),
                    op=ALU.mult), reads=[psb[half], small_b], writes=[hT_b[i]])

    k.barrier()

    def retention(full, rs):
        Wh = [sb("Wh%d_%d" % (i, full), [128, KC, 512], BF16, rs) for i in range(2)]
        Wh_b = k.bufs(2, "Wh")
        xqk = [sb("xqk%d_%d" % (i, full), [128, 256], F32, rs) for i in range(4)]
        xqk_b = k.bufs(4, "xqk")
        t1 = [sb("t1_%d_%d" % (i, full), [128, 2, 64], F32, rs) for i in range(4)]
        t2 = [sb("t2_%d_%d" % (i, full), [128, 2, 64], F32, rs) for i in range(4)]
        t3 = [sb("t3_%d_%d" % (i, full), [128, 2, 64], F32, rs) for i in range(4)]
        t4 = [sb("t4_%d_%d" % (i, full), [128, 2, 64], F32, rs) for i in range(4)]
        t1_b, t2_b, t3_b, t4_b = k.bufs(4, "t1"), k.bufs(4, "t2"), k.bufs(4, "t3"), k.bufs(4, "t4")
        qkr = [sb("qkr%d_%d" % (i, full), [128, 2, 2, 64], BF16, rs) for i in range(4)]
        qkr1_b, qkr2_b = k.bufs(4, "qkr1"), k.bufs(4, "qkr2")
        vb = [sb("vb%d_%d" % (i, full), [128, 128], BF16, rs) for i in range(4)]
        vb_b = k.bufs(4, "vb")
        sg = [sb("sg%d_%d" % (i, full), [128, 128], F32, rs) for i in range(4)]
        sg_b = k.bufs(4, "sg")
        qd = [sb("qd%d_%d" % (i, full), [128, 128], BF16, rs) for i in range(4)]
        qd_b = k.bufs(4, "qd")
        ktd = [sb("ktd%d_%d" % (i, full), [128, 128], BF16, rs) for i in range(4)]
        ktd_b = k.bufs(4, "ktd")
        qkT = [sb("qkT%d_%d" % (i, full), [128, 384], BF16, rs) for i in range(4)]
        qkT_b = k.bufs(4, "qkT")
        Pm = [sb("Pm%d_%d" % (i, full), [128, 128], BF16, rs) for i in range(4)]
        Pm_b = k.bufs(4, "Pm")
        st6 = [sb("st6_%d_%d" % (i, full), [128, 6], F32, rs) for i in range(4)]
        mv = [sb("mv%d_%d" % (i, full), [128, 4], F32, rs) for i in range(4)]
        st6_b, mv_b = k.bufs(4, "st6"), k.bufs(4, "mv")
        yn = [sb("yn%d_%d" % (i, full), [128, 128], F32, rs) for i in range(4)]
        yn_b = k.bufs(4, "yn")
        yr = [sb("yr%d_%d" % (i, full), [128, 128], BF16, rs) for i in range(4)]
        yr_b = k.bufs(4, "yr")
        yst = [sb("yst%d_%d" % (i, full), [128, T], BF16, rs) for i in range(2)]
        yst_b = k.bufs(2, "yst")
        PJb, PTb, MB = (0, 3), (1, 4), (2, 5)
        t_sc, t_py, t_kv, t_t2 = k.bufs(2, "r_sc"), k.bufs(2, "r_py"), k.bufs(2, "r_kv"), k.bufs(2, "r_t2")

        def load_w(h):
            hb = h % 2
            for j in range(4):
                c0 = j * RW + h * DH
                k.dma("pool", Wh[hb][:, :, j * 128:(j + 1) * 128],
                      w_in_d[:, c0:c0 + 128].rearrange("(kc p) c -> p kc c", p=128), writes=[Wh_b[hb]] if j == 3 else [])
        def load_w_tracked(h):
            hb = h % 2
            for j in range(4):
                if not full and j in (0, 3):
                    continue
                c0 = j * RW + h * DH
                k.dma("pool", Wh[hb][:, :, j * 128:(j + 1) * 128],
                      w_in_d[:, c0:c0 + 128].rearrange("(kc p) c -> p kc c", p=128), writes=[Whj_b[hb][j]])

        Whj_b = [k.bufs(4, "Whj%d_" % i) for i in range(2)]
        def head_body(h):
            hb = h % 2
            wreads = [Whj_b[hb][j] for j in ((0, 1, 2, 3) if full else (1, 2))]
            for i in range(NT):
                b = (h % 2) * 2 + i % 2
                hp = h % 2
                pj = ps[PJb[hp]]
                pjb = psb[PJb[hp]]
                tok = slice(i * 128, (i + 1) * 128)
                c_lo, c_hi = (0, 512) if full else (128, 384)
                for kc in range(KC):
                    k.op("pe", lambda e: e.matmul(pj[:, c_lo:c_hi], hT[:, kc, tok], Wh[hb][:, kc, c_lo:c_hi],
                                                  start=(kc == 0), stop=(kc == KC - 1)),
                         reads=[hT_b[i]] + wreads, writes=[pjb])
                if h == 0 and i == 0 and full:
                    dump("Wh", Wh[hb][:, 3, :], wreads, 512)
                    dump("pj", pj[:, :], [pjb], 512)
                yield
                k.op("act", lambda e: e.copy(out=xqk[b][:, c_lo:256], in_=pj[:, c_lo:256]), reads=[pjb], writes=[xqk_b[b]])
                k.op("act", lambda e: e.copy(out=vb[b][:], in_=pj[:, 256:384]), reads=[pjb], writes=[vb_b[b]])
                if full:
                    k.op("act", lambda e: e.activation(out=sg[b][:], in_=pj[:, 384:512], func=AF.Silu), reads=[pjb], writes=[sg_b[b]])
                yield
                X = xqk[b][:, :].rearrange("p (a c f) -> p a c f", a=2, c=2)
                a0 = 0 if full else 1
                A = X[:, a0:2, 0, :]
                B = X[:, a0:2, 1, :]
                na = 2 - a0
                C = cosT[:, i, :].unsqueeze(1).broadcast_to([128, na, 64])
                Sn = sinT[:, i, :].unsqueeze(1).broadcast_to([128, na, 64])
                k.op("dve", lambda e: e.tensor_tensor(out=t1[b][:, a0:2, :], in0=A, in1=C, op=ALU.mult), reads=[xqk_b[b], tabs_b], writes=[t1_b[b]])
                k.op("dve", lambda e: e.tensor_tensor(out=t2[b][:, a0:2, :], in0=B, in1=Sn, op=ALU.mult), reads=[xqk_b[b], tabs_b], writes=[t2_b[b]])
                k.op("dve", lambda e: e.tensor_tensor(out=qkr[b][:, a0:2, 0, :], in0=t1[b][:, a0:2, :], in1=t2[b][:, a0:2, :], op=ALU.subtract),
                     reads=[t1_b[b], t2_b[b]], writes=[qkr1_b[b]])
                k.op("pool", lambda e: e.tensor_tensor(out=t3[b][:, a0:2, :], in0=A, in1=Sn, op=ALU.mult), reads=[xqk_b[b], tabs_b], writes=[t3_b[b]])
                k.op("pool", lambda e: e.tensor_tensor(out=t4[b][:, a0:2, :], in0=B, in1=C, op=ALU.mult), reads=[xqk_b[b], tabs_b], writes=[t4_b[b]])
                k.op("pool", lambda e: e.tensor_tensor(out=qkr[b][:, a0:2, 1, :], in0=t3[b][:, a0:2, :], in1=t4[b][:, a0:2, :], op=ALU.add),
                     reads=[t3_b[b], t4_b[b]], writes=[qkr2_b[b]])
                if h == 0 and i == 0 and full:
                    dump("xqk", xqk[b][:, :], [xqk_b[b]], 256)
                    dump("qkr", qkr[b][:, :, :, :].rearrange("p a c f -> p (a c f)"), [qkr1_b[b], qkr2_b[b]], 256)
                yield
                qr = qkr[b][:, 0, :, :].rearrange("p c f -> p (c f)")
                kr = qkr[b][:, 1, :, :].rearrange("p c f -> p (c f)")
                k.op("pool", lambda e: e.tensor_scalar(out=ktd[b][:], in0=kr, scalar1=kdec[:, h:h + 1], scalar2=None, op0=ALU.mult),
                     reads=[qkr1_b[b], qkr2_b[b], tabs_b], writes=[ktd_b[b]])
                if full:
                    k.op("act", lambda e: e.activation(out=qd[b][:], in_=qr, func=AF.Copy, scale=qdec[:, h:h + 1]),
                         reads=[qkr1_b[b], qkr2_b[b], tabs_b], writes=[qd_b[b]])
                    pT = ps[PTb[hp]].bitcast(BF16)
                    k.op("pe", lambda e: e.transpose(out=pT[:, 0:128], in_=qr, identity=identb[:]),
                         reads=[qkr1_b[b], qkr2_b[b], identb_b], writes=[psb[PTb[hp]]])
                    k.op("pe", lambda e: e.transpose(out=pT[:, 128:256], in_=qd[b][:], identity=identb[:]),
                         reads=[qd_b[b], identb_b], writes=[psb[PTb[hp]]])
                    k.op("pe", lambda e: e.transpose(out=pT[:, 256:384], in_=kr, identity=identb[:]),
                         reads=[qkr1_b[b], qkr2_b[b], identb_b], writes=[psb[PTb[hp]]])
                    k.op("act", lambda e: e.copy(out=qkT[b][:], in_=pT[:, 0:384]), reads=[psb[PTb[hp]]], writes=[qkT_b[b]])
                    yield
                    k.op("pe", lambda e: e.matmul(ps[MB[hp]][:, 0:128], qkT[b][:, 256:384], qkT[b][:, 0:128], start=True, stop=True),
                         reads=[qkT_b[b]], writes=[t_sc[hp]])
                    k.op("dve", lambda e: e.tensor_tensor(out=Pm[b][:], in0=ps[MB[hp]][:, 0:128], in1=maskT[:, h, :], op=ALU.mult),
                         reads=[t_sc[hp], tabs_b], writes=[Pm_b[b]])
                    if h == 0 and i == 0:
                        dump("qkT", qkT[b][:, :], [qkT_b[b]], 384)
                        dump("Pm", Pm[b][:, :], [Pm_b[b]], 128)
                    k.op("pe", lambda e: e.matmul(ps[MB[hp]][:, 128:256], Pm[b][:], vb[b][:], start=True, stop=False),
                         reads=[Pm_b[b], vb_b[b]], writes=[t_py[hp]])
                    k.op("pe", lambda e: e.matmul(ps[MB[hp]][:, 128:256], qkT[b][:, 128:256], Sbf[:, h, :], start=False, stop=True),
                         reads=[qkT_b[b], Sbf_b[h]], writes=[t_py[hp]])
                yield
                k.op("pe", lambda e: e.matmul(ps[MB[hp]][:, 256:384], ktd[b][:], vb[b][:], start=True, stop=True),
                     reads=[ktd_b[b], vb_b[b]], writes=[t_kv[hp]])
                k.op("dve", lambda e: e.scalar_tensor_tensor(out=S[:, h, :], in0=S[:, h, :], scalar=G128[h], in1=ps[MB[hp]][:, 256:384],
                                                             op0=ALU.mult, op1=ALU.add), reads=[S_b[h], t_kv[hp]], writes=[S_b[h]])
                if full:
                    k.op("act", lambda e: e.copy(out=Sbf[:, h, :], in_=S[:, h, :]), reads=[S_b[h]], writes=[Sbf_b[h]])
                    yield
                    k.op("dve", lambda e: e.bn_stats(out=st6[b][:], in_=ps[MB[hp]][:, 128:256]), reads=[t_py[hp]], writes=[st6_b[b]])
                    k.op("dve", lambda e: e.bn_aggr(out=mv[b][:, 0:2], in_=st6[b][:]), reads=[st6_b[b]], writes=[mv_b[b]])
                    k.op("act", lambda e: e.activation(out=mv[b][:, 2:3], in_=mv[b][:, 1:2], func=AF.Sqrt, bias=EPS, scale=1.0),
                         reads=[mv_b[b]], writes=[mv_b[b]])
                    k.op("dve", lambda e: e.reciprocal(out=mv[b][:, 2:3], in_=mv[b][:, 2:3]), reads=[mv_b[b]], writes=[mv_b[b]])
                    k.op("dve", lambda e: e.tensor_scalar(out=mv[b][:, 3:4], in0=mv[b][:, 0:1], scalar1=mv[b][:, 2:3], scalar2=-1.0,
                                                          op0=ALU.mult, op1=ALU.mult), reads=[mv_b[b]], writes=[mv_b[b]])
                    k.op("act", lambda e: e.activation(out=yn[b][:], in_=ps[MB[hp]][:, 128:256], func=AF.Identity,
                                                       scale=mv[b][:, 2:3], bias=mv[b][:, 3:4]), reads=[t_py[hp], mv_b[b]], writes=[yn_b[b]])
                    if h == 0 and i == 0:
                        dump("mv", mv[b][:, :], [mv_b[b]], 4)
                        dump("yn", yn[b][:, :], [yn_b[b]], 128)
                    k.op("pool", lambda e: e.tensor_tensor(out=yn[b][:], in0=yn[b][:], in1=gnw[:, h * 128:(h + 1) * 128], op=ALU.mult),
                         reads=[yn_b[b], small_b], writes=[yn_b[b]])
                    k.op("pool", lambda e: e.tensor_tensor(out=yr[b][:], in0=yn[b][:], in1=sg[b][:], op=ALU.mult),
                         reads=[yn_b[b], sg_b[b]], writes=[yr_b[b]])
                    yield
                    pT2 = ps[MB[hp]].bitcast(BF16)
                    if h == 0 and i == 0:
                        dump("yr", yr[b][:, :], [yr_b[b]], 128)
                    k.op("pe", lambda e: e.transpose(out=pT2[:, 768:896], in_=yr[b][:], identity=identb[:]),
                         reads=[yr_b[b], identb_b], writes=[t_t2[hp]])
                    k.op("act", lambda e: e.copy(out=yst[hb][:, tok], in_=pT2[:, 768:896]), reads=[t_t2[hp]], writes=[yst_b[hb]])
            if full:
                k.dma("sp", yretT_d[h * 128:(h + 1) * 128, :], yst[hb][:], reads=[yst_b[hb]], writes=[yretT_b[h]])

        load_w_tracked(0)
        load_w_tracked(1)
        for h0 in range(0, H, 2):
            gens = [head_body(h0), head_body(h0 + 1)]
            while gens:
                for gnr in list(gens):
                    try:
                        next(gnr)
                    except StopIteration:
                        gens.remove(gnr)
            if h0 + 2 < H:
                load_w_tracked(h0 + 2)
                load_w_tracked(h0 + 3)

    yretT_b = k.bufs(H, "yretT_d")
    for h in (range(H) if mode != "fused" else ()):
        k.op("dve", lambda e: e.memset(S[:, h, :], 0.0), writes=[S_b[h]])
        k.op("pool", lambda e: e.memset(Sbf[:, h, :], 0.0), writes=[Sbf_b[h]])
    if mode == "states":
        with ExitStack() as rs:
            retention(False, rs)
        k.barrier()


    k.barrier()

    def s5_uproj(us):
        Wu = [sb("Wu%d" % i, [128, KC, 128], BF16, us) for i in range(2)]
        Wu_b = k.bufs(2, "Wu")
        for ct in range(8):
            b = ct % 2
            c0 = 4 * RW + ct * 128
            k.dma("pool", Wu[b][:], w_in_d[:, c0:c0 + 128].rearrange("(kc p) c -> p kc c", p=128), writes=[Wu_b[b]])
            for tb in range(4):
                pb = tb % 2
                for kc in range(KC):
                    k.op("pe", lambda e: e.matmul(ps[pb][:, :], Wu[b][:, kc, :], hT[:, kc, tb * 512:(tb + 1) * 512],
                                                  start=(kc == 0), stop=(kc == KC - 1)),
                         reads=[Wu_b[b]] + hT_b[tb * 4:(tb + 1) * 4], writes=[psb[pb]])
                k.op("act", lambda e: e.copy(out=uT[:, ct, :].rearrange("p (hf s m) -> p hf s m", hf=2, s=8)[:, tb // 2, :, (tb % 2) * 64:(tb % 2) * 64 + 64],
                                             in_=ps[pb][:, :].rearrange("p (m s) -> p s m", s=8)), reads=[psb[pb]],
                     writes=[uT_b[ct * 4 + j] for j in range(4)])

    def s5_pairs(full, ps_):
        RBm = [sb("RBm%d_%d" % (i, full), [128, 8, 2, 32], BF16, ps_) for i in range(2)]
        CPm = [sb("CPm%d_%d" % (i, full), [128, 9, 2, 32], BF16, ps_) for i in range(2)]
        RBm_b, CPm_b = k.bufs(2, "RBm"), k.bufs(2, "CPm")
        RBT = [sb("RBT%d_%d" % (i, full), [32, 16, 128], BF16, ps_) for i in range(2)]
        RBT_b = k.bufs(2, "RBT")
        KT = [sb("KT%d_%d" % (i, full), [32, 8, 32], BF16, ps_) for i in range(2)]
        KT_b = k.bufs(2, "KT")
        uq = [sb("uq%d_%d" % (i, full), [32, T], BF16, ps_) for i in range(2)]
        uq_b = k.bufs(2, "uq")
        XA = [sb("XA%d_%d" % (i, full), [128, 2, 129], F32, ps_) for i in range(2)]
        XB = [sb("XB%d_%d" % (i, full), [128, 2, 129], F32, ps_) for i in range(2)]
        XA_b, XB_b = k.bufs(2, "XA"), k.bufs(2, "XB")
        xp = [sb("xp%d_%d" % (i, full), [128, 2, 128], BF16, ps_) for i in range(2)]
        xp_b = k.bufs(2, "xp")
        yq = [sb("yq%d_%d" % (i, full), [32, 1024], BF16, ps_) for i in range(2)]
        yq_b = k.bufs(2, "yq")
        for i in range(2):
            k.op("pool", lambda e: e.memset(RBm[i][:], 0.0), writes=[RBm_b[i]])
            k.op("pool", lambda e: e.memset(CPm[i][:], 0.0), writes=[CPm_b[i]])
        PTA, PTB, PK, PYA, PYB = 0, 1, 2, 4, 5

        def pair_body(q):
            b = q % 2
            PW = 3 if b == 0 else 6
            ct, ql = q // 4, q % 4
            for gi in range(2):
                prt = slice(gi * 64, gi * 64 + 64)
                csl = slice(gi * 16, gi * 16 + 16)
                k.op("pool", lambda e: e.tensor_copy(out=RBm[b][prt, :, :, csl], in_=RBc[prt, q, :, :, :]), reads=[mats_b], writes=[RBm_b[b]])
                if full:
                    k.op("pool", lambda e: e.tensor_copy(out=CPm[b][prt, :, :, csl], in_=CPc[prt, q, :, :, :]), reads=[mats_b], writes=[CPm_b[b]])
            pTA = ps[PTA].bitcast(BF16)
            pTB = ps[PTB].bitcast(BF16)
            for sx in range(8):
                for ri in range(2):
                    idx = sx * 2 + ri
                    pt, pbb = (pTA, psb[PTA]) if idx < 8 else (pTB, psb[PTB])
                    k.op("pe", lambda e: e.transpose(out=pt[0:32, (idx % 8) * 128:(idx % 8 + 1) * 128], in_=RBm[b][:, sx, ri, :], identity=identb[:]),
                         reads=[RBm_b[b], identb_b], writes=[pbb])
            k.op("act", lambda e: e.copy(out=RBT[b][:, 0:8, :], in_=pTA[0:32, :].rearrange("p (a c) -> p a c", c=128)), reads=[psb[PTA]], writes=[RBT_b[b]])
            k.op("act", lambda e: e.copy(out=RBT[b][:, 8:16, :], in_=pTB[0:32, :].rearrange("p (a c) -> p a c", c=128)), reads=[psb[PTB], RBT_b[b]], writes=[RBT_b[b]])
            if full:
                for kk in range(8):
                    k.op("pe", lambda e: e.matmul(ps[PK][0:32, kk * 32:(kk + 1) * 32], RBm[b][:, 7, 0, :], CPm[b][:, kk, 0, :], start=True, stop=False),
                         reads=[RBm_b[b], CPm_b[b]], writes=[psb[PK]])
                    k.op("pe", lambda e: e.matmul(ps[PK][0:32, kk * 32:(kk + 1) * 32], RBm[b][:, 7, 1, :], CPm[b][:, kk, 1, :], start=False, stop=True),
                         reads=[RBm_b[b], CPm_b[b]], writes=[psb[PK]])
                k.op("act", lambda e: e.copy(out=KT[b][:], in_=ps[PK][0:32, 0:256].rearrange("p (a c) -> p a c", c=32)), reads=[psb[PK]], writes=[KT_b[b]])
            yield
            k.dma("sp", uq[b][:], uT[32 * ql:32 * ql + 32, ct, :], reads=[uT_b[ct * 4 + j] for j in range(4)], writes=[uq_b[b]])
            uqs = uq[b][:, :].rearrange("p (hf s m) -> p hf s m", hf=2, s=8)
            for hf in range(2):
                for ri in range(2):
                    for sx in range(8):
                        k.op("pe", lambda e: e.matmul(ps[PW][:, ri * 128:(ri + 1) * 128], RBT[b][:, sx * 2 + ri, :], uqs[:, hf, sx, :],
                                                      start=(sx == 0), stop=(sx == 7)), reads=[RBT_b[b], uq_b[b]], writes=[psb[PW]])
                yield
                k.op("act", lambda e: e.copy(out=XA[b][:, :, 1:129], in_=ps[PW][:, 0:256].rearrange("p (r m) -> p r m", r=2)),
                     reads=[psb[PW]], writes=[XA_b[b]])
                k.op("dve", lambda e: e.tensor_copy(out=XA[b][:, :, 0], in_=carry[:, q, :]), reads=[carry_b[q], XA_b[b]], writes=[XA_b[b]])
                src, dst, src_b, dst_b = XA[b], XB[b], XA_b[b], XB_b[b]
                N = 129
                for st in range(8):
                    d = 1 << st
                    k.op("pool", lambda e: e.tensor_copy(out=dst[:, :, 0:d], in_=src[:, :, 0:d]), reads=[src_b], writes=[dst_b])
                    dre, dim_, ndim = Dre[:, st, q:q + 1], Dim[:, st, q:q + 1], nDim[:, st, q:q + 1]
                    k.op("dve", lambda e: e.scalar_tensor_tensor(out=dst[:, 0, d:N], in0=src[:, 0, 0:N - d], scalar=dre, in1=src[:, 0, d:N],
                                                                 op0=ALU.mult, op1=ALU.add), reads=[src_b, mats_b, dst_b], writes=[dst_b])
                    k.op("dve", lambda e: e.scalar_tensor_tensor(out=dst[:, 1, d:N], in0=src[:, 1, 0:N - d], scalar=dre, in1=src[:, 1, d:N],
                                                                 op0=ALU.mult, op1=ALU.add), reads=[src_b, mats_b, dst_b], writes=[dst_b])
                    yield
                    k.op("dve", lambda e: e.scalar_tensor_tensor(out=dst[:, 0, d:N], in0=src[:, 1, 0:N - d], scalar=ndim, in1=dst[:, 0, d:N],
                                                                 op0=ALU.mult, op1=ALU.add), reads=[src_b, mats_b, dst_b], writes=[dst_b])
                    k.op("dve", lambda e: e.scalar_tensor_tensor(out=dst[:, 1, d:N], in0=src[:, 0, 0:N - d], scalar=dim_, in1=dst[:, 1, d:N],
                                                                 op0=ALU.mult, op1=ALU.add), reads=[src_b, mats_b, dst_b], writes=[dst_b])
                    yield
                    src, dst, src_b, dst_b = dst, src, dst_b, src_b
                X, X_b = src, src_b
                k.op("dve", lambda e: e.tensor_copy(out=carry[:, q, :], in_=X[:, :, 128]), reads=[X_b], writes=[carry_b[q]])
                yield
                if full:
                    k.op("act", lambda e: e.copy(out=xp[b][:], in_=X[:, :, 0:128]), reads=[X_b], writes=[xp_b[b]])
                    for j in range(8):
                        pyi = PYA if j < 4 else PYB
                        o = ps[pyi][0:32, (j % 4) * 128:(j % 4 + 1) * 128]
                        k.op("pe", lambda e: e.matmul(o, CPm[b][:, j + 1, 0, :], xp[b][:, 0, :], start=True, stop=False),
                             reads=[CPm_b[b], xp_b[b]], writes=[psb[pyi]])
                        k.op("pe", lambda e: e.matmul(o, CPm[b][:, j + 1, 1, :], xp[b][:, 1, :], start=False, stop=False),
                             reads=[CPm_b[b], xp_b[b]], writes=[psb[pyi]])
                        for sx in range(j + 1):
                            k.op("pe", lambda e: e.matmul(o, KT[b][:, j - sx, :], uqs[:, hf, sx, :], start=False, stop=(sx == j)),
                                 reads=[KT_b[b], uq_b[b]], writes=[psb[pyi]])
                    yb = b
                    yq3 = yq[yb][:, :].rearrange("p (m s) -> p m s", s=8)
                    for jj, pyi in ((0, PYA), (1, PYB)):
                        k.op("dve", lambda e: e.scalar_tensor_tensor(
                            out=yq3[:, :, jj * 4:(jj + 1) * 4], in0=uqs[:, hf, jj * 4:(jj + 1) * 4, :].rearrange("p j m -> p m j"), scalar=s5d[0:32, 8 + q:9 + q],
                            in1=ps[pyi][0:32, :].rearrange("p (j m) -> p m j", j=4), op0=ALU.mult, op1=ALU.add),
                            reads=[uq_b[b], psb[pyi], s5d_b, yq_b[yb]], writes=[yq_b[yb]])
                    k.dma("sp", uT[32 * ql:32 * ql + 32, ct, hf * 1024:(hf + 1) * 1024], yq[yb][:], reads=[yq_b[yb]],
                          writes=[uT_b[ct * 4 + hf * 2], uT_b[ct * 4 + hf * 2 + 1]])

        for q0 in range(0, NQ, 2):
            gens = [pair_body(q0), pair_body(q0 + 1)]
            while gens:
                for gnr in list(gens):
                    try:
                        next(gnr)
                    except StopIteration:
                        gens.remove(gnr)

    def s5_post(gs):
        f1 = [sb("g1_%d" % i, [128, 512], F32, gs) for i in range(2)]
        f2 = [sb("g2_%d" % i, [128, 512], F32, gs) for i in range(2)]
        f1_b, f2_b = k.bufs(2, "f1"), k.bufs(2, "f2")
        for ct in range(8):
            for tb in range(4):
                b = (ct * 4 + tb) % 2
                y = uT[:, ct, tb * 512:(tb + 1) * 512]
                yb_ = uT_b[ct * 4 + tb]
                k.op("dve", lambda e: e.tensor_tensor(out=f1[b][:], in0=y, in1=y, op=ALU.mult), reads=[yb_], writes=[f1_b[b]])
                k.op("dve", lambda e: e.tensor_scalar(out=f1[b][:], in0=f1[b][:], scalar1=0.044715, scalar2=1.0, op0=ALU.mult, op1=ALU.add),
                     reads=[f1_b[b]], writes=[f1_b[b]])
                k.op("pool", lambda e: e.tensor_tensor(out=f1[b][:], in0=f1[b][:], in1=y, op=ALU.mult), reads=[f1_b[b], yb_], writes=[f1_b[b]])
                k.op("act", lambda e: e.activation(out=f2[b][:], in_=f1[b][:], func=AF.Sigmoid, scale=1.5957691216057308),
                     reads=[f1_b[b]], writes=[f2_b[b]])
                k.op("pool", lambda e: e.tensor_tensor(out=y, in0=f2[b][:], in1=y, op=ALU.mult), reads=[f2_b[b], yb_], writes=[yb_])
        Wg = [sb("Wg%d" % i, [128, 8, 128], BF16, gs) for i in range(2)]
        Wg_b = k.bufs(2, "Wg")
        og = [sb("og%d" % i, [128, 512], BF16, gs) for i in range(2)]
        og_b = k.bufs(2, "og")
        for co in range(8):
            wbi = co % 2
            k.dma("pool", Wg[wbi][:], w_glu_d[:, co * 128:(co + 1) * 128].rearrange("(kc p) c -> p kc c", p=128), writes=[Wg_b[wbi]])
            for tb in range(4):
                b = (co * 4 + tb) % 2
                for ct in range(8):
                    k.op("pe", lambda e: e.matmul(ps[b][:, :], Wg[wbi][:, ct, :], uT[:, ct, tb * 512:(tb + 1) * 512], start=(ct == 0), stop=(ct == 7)),
                         reads=[Wg_b[wbi], uT_b[ct * 4 + tb]], writes=[psb[b]])
                k.op("act", lambda e: e.activation(out=f2[b][:], in_=ps[b][:, :], func=AF.Sigmoid), reads=[psb[b]], writes=[f2_b[b]])
                k.op("dve", lambda e: e.tensor_tensor(out=og[b][:], in0=f2[b][:], in1=uT[:, co, tb * 512:(tb + 1) * 512], op=ALU.mult),
                     reads=[f2_b[b], uT_b[co * 4 + tb]], writes=[og_b[b]])
                k.dma("sp", yssmT_d[co * 128:(co + 1) * 128, tb * 512:(tb + 1) * 512], og[b][:], reads=[og_b[b]], writes=[yssmT_b[co * 4 + tb]])

    yssmT_b = k.bufs(32, "yssmT")
    k.barrier()
    with ExitStack() as s5s:
        uT = sb("uT", [128, 8, T], BF16, s5s)
        uT_b = k.bufs(32, "uT")
        s5d = sb("s5d", [128, 40], F32, s5s)
        s5d_b = k.buf("s5d")
        k.dma("sp", s5d[:], s5d_d[:, :], writes=[s5d_b])
        Pre = sb("s5Pre", [128, 9, 32], F32, s5s)
        Pim = sb("s5Pim", [128, 9, 32], F32, s5s)
        Dre = sb("s5Dre", [128, 9, 32], F32, s5s)
        Dim = sb("s5Dim", [128, 9, 32], F32, s5s)
        nDim = sb("s5nDim", [128, 9, 32], F32, s5s)
        RBc = sb("s5RBc", [128, 32, 8, 2, 16], BF16, s5s)
        CPc = sb("s5CPc", [128, 32, 9, 2, 16], BF16, s5s)
        carry = sb("s5carry", [128, 32, 2], F32, s5s)
        carry_b = k.bufs(NQ, "carry")
        with ExitStack() as ss:
            mats_b = s5_setup(ss)
        k.barrier()
        if "s5mats" in dbg:
            dump("Pre", Pre[:, :, :].rearrange("p a b -> p (a b)"), [mats_b], 288)
            dump("Pim", Pim[:, :, :].rearrange("p a b -> p (a b)"), [mats_b], 288)
        with ExitStack() as us:
            s5_uproj(us)
        k.barrier()
        if "uT" in dbg:
            dump("uT", uT[:, 0, 0:512], [uT_b[0]], 512)
        for q in range(NQ):
            k.op("dve", lambda e: e.memset(carry[:, q, :], 0.0), writes=[carry_b[q]])
        if mode == "states":
            with ExitStack() as p1:
                s5_pairs(False, p1)
            k.barrier()
            bounce_o = dout("bounce", [128, AGW])
            k.dma("sp", bounce_o[:, 0:1024], S[:, :, :].rearrange("p h e -> p (h e)"), reads=S_b)
            k.dma("sp", bounce_o[:, 1024:AGW], carry[:, :, :].rearrange("p q r -> p (q r)"), reads=carry_b)
        if mode != "states":
            if mode == "fused":
                for q in range(NQ):
                    k.op("dve", lambda e: e.tensor_copy(out=carry[:, q, :], in_=carryP[:, q, :]), reads=[carryP_b], writes=[carry_b[q]])
            elif with_carry:
                with ExitStack() as gsx:
                    gath_d = din("gath", [NCORES * 128, AGW])
                    gb_ = k.buf("gath")
                    G = sb("G", [128, NCORES, AGW], F32, gsx)
                    G_b = k.buf("G")
                    k.dma("sp", G[:], gath_d[:, :].rearrange("(r p) w -> p r w", p=128), reads=[gb_], writes=[G_b])
                    X = sb("Xpre", [128, 1024], F32, gsx)
                    Xc = sb("Xcpre", [128, 32, 2], F32, gsx)
                    tq = sb("tqpre", [128, 4, 32], F32, gsx)
                    xb = k.buf("Xpre")
                    k.op("dve", lambda e: e.memset(X[:], 0.0), writes=[xb])
                    k.op("dve", lambda e: e.memset(Xc[:], 0.0), reads=[xb], writes=[xb])
                    for h in range(H):
                        k.op("dve", lambda e: e.memset(S[:, h, :], 0.0), writes=[S_b[h]])
                    for q in range(NQ):
                        k.op("dve", lambda e: e.memset(carry[:, q, :], 0.0), writes=[carry_b[q]])
                    d8r, d8i = Dre[:, 8, :], Dim[:, 8, :]
                    for r in range(NCORES - 1):
                        for h in range(H):
                            k.op("dve", lambda e: e.scalar_tensor_tensor(out=X[:, h * 128:(h + 1) * 128], in0=X[:, h * 128:(h + 1) * 128], scalar=G2048[h],
                                                                         in1=G[:, r, h * 128:(h + 1) * 128], op0=ALU.mult, op1=ALU.add),
                                 reads=[xb, G_b], writes=[xb])
                        k.op("dve", lambda e: e.scalar_tensor_tensor(out=S[:, :, :].rearrange("p h e -> p (h e)"), in0=X[:], scalar=onehot[:, r + 1:r + 2],
                                                                     in1=S[:, :, :].rearrange("p h e -> p (h e)"), op0=ALU.mult, op1=ALU.add),
                             reads=[xb, tabs_b] + S_b, writes=S_b)
                        Er = G[:, r, 1024:AGW].rearrange("p (q r) -> p q r", r=2)
                        tt_ = lambda o, a, b2, op: k.op("dve", lambda e: e.tensor_tensor(out=o, in0=a, in1=b2, op=op), reads=[xb, G_b, mats_b], writes=[xb])
                        tt_(tq[:, 0, :], Xc[:, :, 0], d8r, ALU.mult)
                        tt_(tq[:, 1, :], Xc[:, :, 1], d8i, ALU.mult)
                        tt_(tq[:, 2, :], Xc[:, :, 1], d8r, ALU.mult)
                        tt_(tq[:, 3, :], Xc[:, :, 0], d8i, ALU.mult)
                        tt_(tq[:, 0, :], tq[:, 0, :], tq[:, 1, :], ALU.subtract)
                        tt_(tq[:, 2, :], tq[:, 2, :], tq[:, 3, :], ALU.add)
                        tt_(Xc[:, :, 0], tq[:, 0, :], Er[:, :, 0], ALU.add)
                        tt_(Xc[:, :, 1], tq[:, 2, :], Er[:, :, 1], ALU.add)
                        k.op("dve", lambda e: e.scalar_tensor_tensor(out=carry[:, :, :].rearrange("p q r -> p (q r)"), in0=Xc[:, :, :].rearrange("p q r -> p (q r)"),
                                                                     scalar=onehot[:, r + 1:r + 2], in1=carry[:, :, :].rearrange("p q r -> p (q r)"),
                                                                     op0=ALU.mult, op1=ALU.add), reads=[xb, tabs_b] + carry_b, writes=carry_b)
                    for h in range(H):
                        k.op("act", lambda e: e.copy(out=Sbf[:, h, :], in_=S[:, h, :]), reads=[S_b[h]], writes=[Sbf_b[h]])
                k.barrier()
            with ExitStack() as p2:
                s5_pairs(True, p2)
            k.barrier()
            if "ypre" in dbg:
                dump("ypre", uT[:, 0, 0:512], [uT_b[0]], 512)
            with ExitStack() as gs:
                s5_post(gs)
            k.barrier()

    if mode != "states":
        with ExitStack() as rs:
            retention(True, rs)
        k.barrier()

        if "yret" in dbg:
            with ExitStack() as sd:
                for h in range(H):
                    tmpb = sb("dbgy_b%d" % h, [128, T], BF16, sd)
                    tmpf = sb("dbgy_f%d" % h, [128, T], F32, sd)
                    tb, tb2 = k.buf(), k.buf()
                    k.dma("sp", tmpb[:], yretT_d[h * 128:(h + 1) * 128, :], reads=[yretT_b[h]], writes=[tb])
                    k.op("act", lambda e: e.copy(out=tmpf[:], in_=tmpb[:]), reads=[tb], writes=[tb2])
                    k.dma("sp", dbg["yret"][h * 128:(h + 1) * 128, :], tmpf[:], reads=[tb2])
            k.barrier()

        if "yssm" in dbg:
            with ExitStack() as sd:
                for co in range(8):
                    tmpb = sb("dbgs_b%d" % co, [128, T], BF16, sd)
                    tmpf = sb("dbgs_f%d" % co, [128, T], F32, sd)
                    tb_, tb2 = k.buf(), k.buf()
                    k.dma("sp", tmpb[:], yssmT_d[co * 128:(co + 1) * 128, :], reads=yssmT_b[co * 4:(co + 1) * 4], writes=[tb_])
                    k.op("act", lambda e: e.copy(out=tmpf[:], in_=tmpb[:]), reads=[tb_], writes=[tb2])
                    k.dma("sp", dbg["yssm"][co * 128:(co + 1) * 128, :], tmpf[:], reads=[tb2])
            k.barrier()


        ms.close()
        k.barrier()
        x1_b = k.bufs(NT, "x1")
        with ExitStack() as gs4:
            yr = sb("m_yr", [128, 8, 1024], BF16, gs4)
            ys = sb("m_ys", [128, 8, 1024], BF16, gs4)
            yr_b4, ys_b4 = k.buf("m_yr"), k.buf("m_ys")
            mT = sb("m_mT", [128, KC, 1024], BF16, gs4)
            mT_b = k.bufs(2, "m_mT")
            wma = [sb("m_wma%d" % i, [128, KC, 128], BF16, gs4) for i in range(2)]
            wmb = [sb("m_wmb%d" % i, [128, KC, 128], BF16, gs4) for i in range(2)]
            wa = [sb("m_wa%d" % i, [128, 8, 128], BF16, gs4) for i in range(2)]
            wb_ = [sb("m_wb%d" % i, [128, 8, 128], BF16, gs4) for i in range(2)]
            wma_b, wmb_b, wa_b, wbb_b = k.bufs(2, "wma"), k.bufs(2, "wmb"), k.bufs(2, "wa"), k.bufs(2, "wb")
            wo = [sb("m_wo%d" % i, [128, KC, 256], BF16, gs4) for i in range(2)]
            wo_b = k.bufs(2, "wo")
            sga = [sb("m_sga%d" % i, [128, 512], F32, gs4) for i in range(2)]
            sgb = [sb("m_sgb%d" % i, [128, 512], F32, gs4) for i in range(2)]
            sga_b, sgb_b = k.bufs(2, "sga"), k.bufs(2, "sgb")
            xo = [sb("m_xo%d" % i, [128, 256], F32, gs4) for i in range(2)]
            oo = [sb("m_oo%d" % i, [128, 256], F32, gs4) for i in range(2)]
            xo_b, oo_b = k.bufs(2, "xo"), k.bufs(2, "oo")

            def load_mw(j):
                b = j % 2
                k.dma("pool", wma[b][:], w_merge_d[:, j * 128:(j + 1) * 128].rearrange("(kc p) c -> p kc c", p=128), writes=[wma_b[b]])
                k.dma("pool", wmb[b][:], w_merge_d[:, D + j * 128:D + (j + 1) * 128].rearrange("(kc p) c -> p kc c", p=128), writes=[wmb_b[b]])
                k.dma("pool", wa[b][:], w_a_d[:, j * 128:(j + 1) * 128].rearrange("(kc p) c -> p kc c", p=128), writes=[wa_b[b]])
                k.dma("pool", wb_[b][:], w_b_d[:, j * 128:(j + 1) * 128].rearrange("(kc p) c -> p kc c", p=128), writes=[wbb_b[b]])

            it = 0
            for hf in range(2):
                t0 = hf * 1024
                k.dma("sp", yr[:], yretT_d[:, t0:t0 + 1024].rearrange("(hc p) t -> p hc t", p=128), reads=yretT_b, writes=[yr_b4])
                k.dma("sp", ys[:], yssmT_d[:, t0:t0 + 1024].rearrange("(hc p) t -> p hc t", p=128), reads=yssmT_b, writes=[ys_b4])
                load_mw(0)
                for j in range(KC):
                    b = j % 2
                    if j + 1 < KC:
                        load_mw(j + 1)
                    for tb in range(2):
                        pbase = 4 * (it % 2)
                        it += 1
                        tsl = slice(t0 + tb * 512, t0 + (tb + 1) * 512)
                        lsl = slice(tb * 512, (tb + 1) * 512)
                        hb_ = hT_b[(t0 + tb * 512) // 128:(t0 + tb * 512) // 128 + 4]
                        for kc in range(KC):
                            k.op("pe", lambda e: e.matmul(ps[pbase][:, :], wma[b][:, kc, :], hT[:, kc, tsl], start=(kc == 0), stop=(kc == KC - 1)),
                                 reads=[wma_b[b]] + hb_, writes=[psb[pbase]])
                        for kc in range(KC):
                            k.op("pe", lambda e: e.matmul(ps[pbase + 1][:, :], wmb[b][:, kc, :], hT[:, kc, tsl], start=(kc == 0), stop=(kc == KC - 1)),
                                 reads=[wmb_b[b]] + hb_, writes=[psb[pbase + 1]])
                        for hc in range(8):
                            k.op("pe", lambda e: e.matmul(ps[pbase + 2][:, :], wa[b][:, hc, :], yr[:, hc, lsl], start=(hc == 0), stop=(hc == 7)),
                                 reads=[wa_b[b], yr_b4], writes=[psb[pbase + 2]])
                        for hc in range(8):
                            k.op("pe", lambda e: e.matmul(ps[pbase + 3][:, :], wb_[b][:, hc, :], ys[:, hc, lsl], start=(hc == 0), stop=(hc == 7)),
                                 reads=[wbb_b[b], ys_b4], writes=[psb[pbase + 3]])
                        sb_i = it % 2
                        k.op("act", lambda e: e.activation(out=sga[sb_i][:], in_=ps[pbase][:, :], func=AF.Sigmoid), reads=[psb[pbase]], writes=[sga_b[sb_i]])
                        k.op("act", lambda e: e.activation(out=sgb[sb_i][:], in_=ps[pbase + 1][:, :], func=AF.Sigmoid), reads=[psb[pbase + 1]], writes=[sgb_b[sb_i]])
                        k.op("dve", lambda e: e.tensor_tensor(out=sga[sb_i][:], in0=sga[sb_i][:], in1=ps[pbase + 2][:, :], op=ALU.mult),
                             reads=[sga_b[sb_i], psb[pbase + 2]], writes=[sga_b[sb_i]])
                        k.op("dve", lambda e: e.tensor_tensor(out=sgb[sb_i][:], in0=sgb[sb_i][:], in1=ps[pbase + 3][:, :], op=ALU.mult),
                             reads=[sgb_b[sb_i], psb[pbase + 3]], writes=[sgb_b[sb_i]])
                        k.op("pool", lambda e: e.tensor_tensor(out=mT[:, j, lsl], in0=sga[sb_i][:], in1=sgb[sb_i][:], op=ALU.add),
                             reads=[sga_b[sb_i], sgb_b[sb_i]], writes=[mT_b[tb]])
                for nb in range(8):
                    wbi = nb % 2
                    k.dma("pool", wo[wbi][:], w_out_d[:, nb * 256:(nb + 1) * 256].rearrange("(kc p) c -> p kc c", p=128), writes=[wo_b[wbi]])
                    for tl in range(8):
                        pb = 2 * ((nb * 8 + tl) % 2)
                        xb_ = (nb * 8 + tl) % 2
                        row0 = t0 + tl * 128
                        k.dma("sp", xo[xb_][:], x_d[row0:row0 + 128, nb * 256:(nb + 1) * 256], writes=[xo_b[xb_]])
                        for j in range(KC):
                            k.op("pe", lambda e: e.matmul(ps[pb][:, 0:256], mT[:, j, tl * 128:(tl + 1) * 128], wo[wbi][:, j, :], start=(j == 0), stop=(j == KC - 1)),
                                 reads=[mT_b[tl // 4], wo_b[wbi]], writes=[psb[pb]])
                        k.op("dve", lambda e: e.tensor_tensor(out=oo[xb_][:], in0=xo[xb_][:], in1=ps[pb][:, 0:256], op=ALU.add),
                             reads=[xo_b[xb_], psb[pb]], writes=[oo_b[xb_]])
                        k.dma("sp", x1_d[row0:row0 + 128, nb * 256:(nb + 1) * 256], oo[xb_][:], reads=[oo_b[xb_]], writes=[x1_w[row0 // 128][nb]])
        hs.close()
        k.barrier()
        if "x1" in dbg:
            with ExitStack() as sd:
                for i in range(NT):
                    tmpf = sb("dbgx1_%d" % i, [128, D], F32, sd)
                    tb_ = k.buf()
                    k.dma("sp", tmpf[:], x1_d[i * 128:(i + 1) * 128, :], reads=x1_w[i], writes=[tb_])
                    k.dma("sp", dbg["x1"][i * 128:(i + 1) * 128, :], tmpf[:], reads=[tb_])
            k.barrier()


        out_d = dout("out", [T, D])
        with ExitStack() as g5:
            st5 = sb("st5", [128, 8, 4], F32, g5)
            st5_b = k.bufs(8, "st5")
            wr = sb("wr", [128, KC, 36], BF16, g5)
            wr_b = k.buf("wr")
            k.dma("pool", wr[:], w_rt_d[:, :].rearrange("(kc p) c -> p kc c", p=128), writes=[wr_b])
            wts = sb("wts", [128, 8, NE], F32, g5)
            wts_b = k.bufs(8, "wts")
            wtsb = sb("wtsb", [128, 8, NE], BF16, g5)
            wtsb_b = k.bufs(8, "wtsb")
            Ab = sb("Ab", [128, 8, 4], BF16, g5)
            Ab_b = k.bufs(8, "Ab")
            rank = sb("rank", [128, 8, 4], F32, g5)
            rank_b = k.bufs(8, "rank")
            rt = sb("rt", [128, 8, 40], F32, g5)
            rl = sb("rl", [128, 4, 36], F32, g5)
            rt_b = k.buf("rt")
            ltso = sb("ltso", [128, 256], BF16, g5)
            iota = sb("iota", [128, CG], F32, g5)
            cst_b = k.buf("moec")
            k.dma("pool", ltso[:], moec_d[:, 0:256], writes=[cst_b])
            k.dma("sp", iota[:], moec_d[:, 256:256 + CG], reads=[cst_b], writes=[cst_b])

            def norm_tile(src, src_b, xs_ap, xs_b, junk_ap, junk_b, stc, stc_b, dst3, dst_b, nT, pbanks):
                k.op("act", lambda e: e.activation(out=junk_ap, in_=src, func=AF.Square, accum_out=stc[:, 0:1]), reads=[src_b], writes=[junk_b, stc_b])
                k.op("act", lambda e: e.activation(out=stc[:, 1:2], in_=stc[:, 0:1], func=AF.Sqrt, scale=1.0 / D, bias=EPS), reads=[stc_b], writes=[stc_b])
                k.op("dve", lambda e: e.reciprocal(out=stc[:, 1:2], in_=stc[:, 1:2]), reads=[stc_b], writes=[stc_b])
                k.op("act", lambda e: e.activation(out=xs_ap, in_=src, func=AF.Copy, scale=stc[:, 1:2]), reads=[src_b, stc_b], writes=[xs_b])
                for half in range(2):
                    pbank = ps[pbanks[half]].bitcast(BF16)
                    for j in range(8):
                        kc = half * 8 + j
                        k.op("pe", lambda e: e.transpose(out=pbank[:, j * 128:(j + 1) * 128], in_=xs_ap[:, kc * 128:(kc + 1) * 128], identity=identb[:]),
                             reads=[xs_b, identb_b], writes=[psb[pbanks[half]]])
                    k.op("dve", lambda e: e.tensor_tensor(out=dst3[:, half * 8:(half + 1) * 8, :], in0=pbank[:, :].rearrange("p (j t) -> p j t", t=128),
                                                          in1=nT[:, half * 8:(half + 1) * 8].unsqueeze(2).broadcast_to([128, 8, 128]), op=ALU.mult),
                         reads=[psb[pbanks[half]], small_b], writes=[dst_b])

            for hf in range(2):
                t0 = hf * 1024
                with ExitStack() as gm:
                    hn = sb("hn%d" % hf, [128, 8, D], BF16, gm)
                    hn_b = k.bufs(8, "hn")
                    hrt = [sb("hrt%d_%d" % (hf, i), [128, KC, 128], BF16, gm) for i in range(2)]
                    hrt_b = k.bufs(2, "hrt")
                    xt5 = [sb("xt5_%d_%d" % (hf, i), [128, D], F32, gm) for i in range(2)]
                    xt5_b = k.bufs(2, "xt5")
                    Gw = sb("Gw%d" % hf, [128, KC, FE], BF16, gm)
                    Uw = sb("Uw%d" % hf, [128, KC, FE], BF16, gm)
                    Dw = sb("Dw%d" % hf, [128, 4, D], BF16, gm)
                    Gw_b, Uw_b, Dw_b = k.buf("Gw"), k.buf("Uw"), k.buf("Dw")
                    sgT = sb("sgT%d" % hf, [128, 4, CG], BF16, gm)
                    aT = sb("aT%d" % hf, [128, 4, CG], BF16, gm)
                    sgT_b, aT_b = k.bufs(4, "sgT"), k.bufs(4, "aT")
                    xg = sb("xg%d" % hf, [128, KC * CG], BF16, gm)
                    xg3 = xg[:, :].rearrange("p (kc c) -> p kc c", c=CG)
                    accgb = xg[:, :].rearrange("p (b d) -> p b d", d=D)
                    xg_b = k.buf("xg")
                    Selg = sb("Selg%d" % hf, [128, 8, CG], BF16, gm)
                    Selg_b = k.bufs(8, "Selg")
                    SelgT = sb("SelgT%d" % hf, [128, 3, 8, 128], BF16, gm)
                    SelgT_b = k.bufs(3, "SelgT")
                    accg = sb("accg%d" % hf, [128, 3, D], F32, gm)
                    accg_b = k.bufs(3, "accg")
                    wtsg = sb("wtsg%d" % hf, [128, 3, 8], F32, gm)
                    wtsg_b = k.buf("wtsg")
                    for tl in range(8):
                        b = tl % 2
                        gt = t0 // 128 + tl
                        k.dma("sp", xt5[b][:], x1_d[gt * 128:(gt + 1) * 128, :], reads=x1_w[gt], writes=[xt5_b[b]])
                        norm_tile(xt5[b][:], xt5_b[b], hn[:, tl, :], hn_b[tl], xg[:, 0:D], xg_b, st5[:, tl, :], st5_b[tl], hrt[b][:, :, :], hrt_b[b], nffnT, (6, 7))
                        R = rt[:, tl, :]
                        for kc in range(KC):
                            k.op("pe", lambda e: e.matmul(ps[5][:, 0:36], hrt[b][:, kc, :], wr[:, kc, :], start=(kc == 0), stop=(kc == KC - 1)),
                                 reads=[hrt_b[b], wr_b], writes=[psb[5]])
                        L = rl[:, tl % 4, :]
                        rb_ = [rt_b]
                        k.op("dve", lambda e: e.tensor_tensor(out=L, in0=ps[5][:, 0:36], in1=rbias, op=ALU.add), reads=[psb[5], small_b] + rb_, writes=rb_)
                        lg, le = L[:, 0:4], L[:, 4:36]
                        gmax, ngmax, gsum, gw, m1, m2, d12, w1, w2 = [R[:, i:i + 1] for i in range(9)]
                        ohg, pen = R[:, 12:16], R[:, 16:20]
                        k.op("dve", lambda e: e.reduce_max(out=gmax, in_=lg, axis=mybir.AxisListType.X), reads=rb_, writes=rb_)
                        k.op("dve", lambda e: e.tensor_scalar(out=ngmax, in0=gmax, scalar1=-1.0, scalar2=None, op0=ALU.mult), reads=rb_, writes=rb_)
                        k.op("act", lambda e: e.activation(out=R[:, 20:24], in_=lg, func=AF.Exp, bias=ngmax, scale=1.0, accum_out=gsum), reads=rb_, writes=rb_)
                        k.op("dve", lambda e: e.reciprocal(out=gw, in_=gsum), reads=rb_, writes=rb_)
                        k.op("dve", lambda e: e.tensor_scalar(out=ohg, in0=lg, scalar1=gmax, scalar2=None, op0=ALU.is_equal), reads=rb_, writes=rb_)
                        k.op("dve", lambda e: e.tensor_scalar(out=pen, in0=ohg, scalar1=-1.0, scalar2=1e30, op0=ALU.add, op1=ALU.mult), reads=rb_, writes=rb_)
                        le3 = le.rearrange("p (g x) -> p g x", x=8)
                        k.op("dve", lambda e: e.tensor_tensor(out=le3, in0=le3, in1=pen.unsqueeze(2).broadcast_to([128, 4, 8]), op=ALU.add), reads=rb_, writes=rb_)
                        k.op("dve", lambda e: e.reduce_max(out=m1, in_=le, axis=mybir.AxisListType.X), reads=rb_, writes=rb_)
                        mk1, mk2 = wts[:, tl, :], L[:, 4:36]
                        k.op("dve", lambda e: e.tensor_scalar(out=mk1, in0=le, scalar1=m1, scalar2=None, op0=ALU.is_equal), reads=rb_, writes=rb_ + [wts_b[tl]])
                        k.op("dve", lambda e: e.scalar_tensor_tensor(out=le, in0=mk1, scalar=-1e30, in1=le, op0=ALU.mult, op1=ALU.add), reads=rb_ + [wts_b[tl]], writes=rb_)
                        k.op("dve", lambda e: e.reduce_max(out=m2, in_=le, axis=mybir.AxisListType.X), reads=rb_, writes=rb_)
                        k.op("dve", lambda e: e.tensor_scalar(out=mk2, in0=le, scalar1=m2, scalar2=None, op0=ALU.is_equal), reads=rb_, writes=rb_)
                        k.op("dve", lambda e: e.tensor_tensor(out=d12, in0=m1, in1=m2, op=ALU.subtract), reads=rb_, writes=rb_)
                        k.op("act", lambda e: e.activation(out=w1, in_=d12, func=AF.Sigmoid), reads=rb_, writes=rb_)
                        k.op("act", lambda e: e.activation(out=w2, in_=d12, func=AF.Sigmoid, scale=-1.0), reads=rb_, writes=rb_)
                        k.op("dve", lambda e: e.tensor_tensor(out=w1, in0=w1, in1=gw, op=ALU.mult), reads=rb_, writes=rb_)
                        k.op("dve", lambda e: e.tensor_tensor(out=w2, in0=w2, in1=gw, op=ALU.mult), reads=rb_, writes=rb_)
                        k.op("dve", lambda e: e.tensor_scalar(out=mk1, in0=mk1, scalar1=w1, scalar2=None, op0=ALU.mult), reads=rb_ + [wts_b[tl]], writes=[wts_b[tl]])
                        k.op("dve", lambda e: e.scalar_tensor_tensor(out=mk1, in0=mk2, scalar=w2, in1=mk1, op0=ALU.mult, op1=ALU.add), reads=rb_ + [wts_b[tl]], writes=[wts_b[tl]])
                        k.op("act", lambda e: e.copy(out=wtsb[:, tl, :], in_=wts[:, tl, :]), reads=[wts_b[tl]], writes=[wtsb_b[tl]])
                        k.op("act", lambda e: e.copy(out=Ab[:, tl, :], in_=ohg), reads=rb_, writes=[Ab_b[tl]])
                    if hf == 0 and "wts" in dbg:
                        dump("wts", wts[:, :, :].rearrange("p a b -> p (a b)"), wts_b, 256)
                    for tl in range(8):
                        k.op("pe", lambda e: e.matmul(ps[5][:, 0:4], ltso[:, 0:128], Ab[:, tl, :], start=True, stop=(tl == 0)), reads=[Ab_b[tl], cst_b], writes=[psb[5]])
                        for tp in range(tl):
                            k.op("pe", lambda e: e.matmul(ps[5][:, 0:4], ltso[:, 128:256], Ab[:, tp, :], start=False, stop=(tp == tl - 1)), reads=[Ab_b[tp], cst_b], writes=[psb[5]])
                        k.op("act", lambda e: e.copy(out=rank[:, tl, :], in_=ps[5][:, 0:4]), reads=[psb[5]], writes=[rank_b[tl]])
                    for g in range(4):
                        for tl in range(8):
                            eng = "dve" if tl % 2 == 0 else "pool"
                            k.op(eng, lambda e: e.tensor_scalar(out=Selg[:, tl, :], in0=iota[:], scalar1=rank[:, tl, g:g + 1], scalar2=rt[:, tl, 12 + g:13 + g],
                                                                op0=ALU.is_equal, op1=ALU.mult), reads=[rank_b[tl], rt_b, cst_b], writes=[Selg_b[tl]])
                        for blk in range(3):
                            bank = ps[blk].bitcast(BF16)
                            for tl in range(8):
                                k.op("pe", lambda e: e.transpose(out=bank[:, tl * 128:(tl + 1) * 128], in_=Selg[:, tl, blk * 128:(blk + 1) * 128], identity=identb[:]),
                                     reads=[Selg_b[tl], identb_b], writes=[psb[blk]])
                            k.op("act", lambda e: e.copy(out=SelgT[:, blk, :, :], in_=bank[:, :].rearrange("p (t c) -> p t c", c=128)), reads=[psb[blk]], writes=[SelgT_b[blk]])
                        for blk in range(3):
                            for tl in range(8):
                                k.op("pe", lambda e: e.matmul(ps[3][:, blk * 8:(blk + 1) * 8], Selg[:, tl, blk * 128:(blk + 1) * 128], wtsb[:, tl, g * 8:(g + 1) * 8],
                                                              start=(tl == 0), stop=(tl == 7)), reads=[Selg_b[tl], wtsb_b[tl]], writes=[psb[3]])
                        k.op("act", lambda e: e.copy(out=wtsg[:, :, :], in_=ps[3][:, 0:24].rearrange("p (b x) -> p b x", x=8)), reads=[psb[3]], writes=[wtsg_b])
                        for r4 in range(4):
                            for kcl in range(4):
                                kc = r4 * 4 + kcl
                                bank = 4 + kcl
                                for tl in range(8):
                                    k.op("pe", lambda e: e.matmul(ps[bank][:, 0:CG], hn[:, tl, kc * 128:(kc + 1) * 128], Selg[:, tl, :], start=(tl == 0), stop=(tl == 7)),
                                         reads=[hn_b[tl], Selg_b[tl]], writes=[psb[bank]])
                                if kcl % 2 == 0:
                                    k.op("act", lambda e: e.activation(out=xg3[:, kc, :], in_=ps[bank][:, 0:CG], func=AF.Copy, scale=nffnT[:, kc:kc + 1]),
                                         reads=[psb[bank], small_b, xg_b], writes=[xg_b])
                                else:
                                    k.op("dve", lambda e: e.tensor_scalar(out=xg3[:, kc, :], in0=ps[bank][:, 0:CG], scalar1=nffnT[:, kc:kc + 1], scalar2=None, op0=ALU.mult),
                                         reads=[psb[bank], small_b, xg_b], writes=[xg_b])
                        for blk in range(3):
                            k.op("pool", lambda e: e.memset(accg[:, blk, :], 0.0), writes=[accg_b[blk]])
                        for el in range(8):
                            ex = g * 8 + el
                            k.dma("pool", Gw[:], w_eg_d[ex].rearrange("(kc p) f -> p kc f", p=128), writes=[Gw_b])
                            k.dma("pool", Uw[:], w_eu_d[ex].rearrange("(kc p) f -> p kc f", p=128), writes=[Uw_b])
                            k.dma("pool", Dw[:], w_ed_d[ex].rearrange("(fc p) d -> p fc d", p=128), writes=[Dw_b])
                            for ft in range(4):
                                for kc in range(KC):
                                    k.op("pe", lambda e: e.matmul(ps[ft][:, 0:CG], Gw[:, kc, ft * 128:(ft + 1) * 128], xg3[:, kc, :], start=(kc == 0), stop=(kc == KC - 1)),
                                         reads=[Gw_b, xg_b], writes=[psb[ft]])
                                k.op("act", lambda e: e.activation(out=sgT[:, ft, :], in_=ps[ft][:, 0:CG], func=AF.Silu), reads=[psb[ft]], writes=[sgT_b[ft]])
                            for ft in range(4):
                                for kc in range(KC):
                                    k.op("pe", lambda e: e.matmul(ps[4 + ft][:, 0:CG], Uw[:, kc, ft * 128:(ft + 1) * 128], xg3[:, kc, :], start=(kc == 0), stop=(kc == KC - 1)),
                                         reads=[Uw_b, xg_b], writes=[psb[4 + ft]])
                                k.op("dve", lambda e: e.tensor_tensor(out=aT[:, ft, :], in0=sgT[:, ft, :], in1=ps[4 + ft][:, 0:CG], op=ALU.mult),
                                     reads=[psb[4 + ft], sgT_b[ft]], writes=[aT_b[ft]])
                            cnt = 0
                            for blk in range(3):
                                for nb in range(4):
                                    pb = cnt % 4
                                    cnt += 1
                                    for fc in range(4):
                                        k.op("pe", lambda e: e.matmul(ps[pb][:, :], aT[:, fc, blk * 128:(blk + 1) * 128], Dw[:, fc, nb * 512:(nb + 1) * 512],
                                                                      start=(fc == 0), stop=(fc == 3)), reads=[Dw_b] + aT_b, writes=[psb[pb]])
                                    k.op("dve", lambda e: e.scalar_tensor_tensor(out=accg[:, blk, nb * 512:(nb + 1) * 512], in0=ps[pb][:, :], scalar=wtsg[:, blk, el:el + 1],
                                                                                 in1=accg[:, blk, nb * 512:(nb + 1) * 512], op0=ALU.mult, op1=ALU.add),
                                         reads=[psb[pb], wtsg_b, accg_b[blk]], writes=[accg_b[blk]])
                        for blk in range(3):
                            k.op("act", lambda e: e.copy(out=accgb[:, blk, :], in_=accg[:, blk, :]), reads=[accg_b[blk], xg_b], writes=[xg_b])
                        cnt = 0
                        for tl in range(8):
                            b = tl % 2
                            gt = t0 // 128 + tl
                            k.dma("sp", xt5[b][:], x1_d[gt * 128:(gt + 1) * 128, :], reads=x1_w[gt], writes=[xt5_b[b]])
                            for nb in range(4):
                                pb = 4 + cnt % 4
                                cnt += 1
                                for blk in range(3):
                                    k.op("pe", lambda e: e.matmul(ps[pb][:, :], SelgT[:, blk, tl, :], accgb[:, blk, nb * 512:(nb + 1) * 512], start=(blk == 0), stop=(blk == 2)),
                                         reads=[SelgT_b[blk], xg_b], writes=[psb[pb]])
                                k.op("dve", lambda e: e.tensor_tensor(out=xt5[b][:, nb * 512:(nb + 1) * 512], in0=xt5[b][:, nb * 512:(nb + 1) * 512], in1=ps[pb][:, :], op=ALU.add),
                                     reads=[psb[pb], xt5_b[b]], writes=[xt5_b[b]])
                            k.dma("sp", x1_d[gt * 128:(gt + 1) * 128, :], xt5[b][:], reads=[xt5_b[b]], writes=x1_w[gt])
                k.barrier()
                gpx = ExitStack()
                acc = sb("acc%d" % hf, [128, 8, D], F32, gpx)
                acc_b = k.bufs(8, "acc")
                hh = sb("hh%d" % hf, [128, KC, 1024], BF16, gpx)
                hh_b = k.bufs(8, "hh")
                xs5 = sb("xs5_%d" % hf, [128, D], BF16, gpx)
                junk5 = sb("junk5_%d" % hf, [128, D], BF16, gpx)
                xs5_b, junk5_b = k.buf("xs5"), k.buf("junk5")
                for tl in range(8):
                    k.dma("sp", acc[:, tl, :], x1_d[t0 + tl * 128:t0 + (tl + 1) * 128, :], reads=x1_w[(t0 // 128) + tl], writes=[acc_b[tl]])
                if hf == 0 and "x2" in dbg:
                    for tl in range(8):
                        k.dma("sp", dbg["x2"][tl * 128:(tl + 1) * 128, :], acc[:, tl, :], reads=[acc_b[tl]])

                def norm_to_hh(tl, nT):
                    norm_tile(acc[:, tl, :], acc_b[tl], xs5[:], xs5_b, junk5[:], junk5_b, st5[:, tl, :], st5_b[tl],
                              hh[:, :, tl * 128:(tl + 1) * 128], hh_b[tl], nT, (6, 7))
                with ExitStack() as gp:
                    pT = sb("pT%d" % hf, [128, 2, 1024], BF16, gp)
                    pT_b = k.bufs(8, "pT")
                    pld = [sb("pld%d_%d" % (hf, i), [128, 256], F32, gp) for i in range(2)]
                    plb = [sb("plb%d_%d" % (hf, i), [128, 256], BF16, gp) for i in range(2)]
                    pld_b, plb_b = k.bufs(2, "pld"), k.bufs(2, "plb")
                    wpg = [sb("wpg%d_%d" % (hf, i), [128, KC, 256], BF16, gp) for i in range(2)]
                    wpl = [sb("wpl%d_%d" % (hf, i), [128, 2, 256], BF16, gp) for i in range(2)]
                    wpg_b, wpl_b = k.bufs(2, "wpg"), k.bufs(2, "wpl")
                    sg5 = [sb("sg5_%d_%d" % (hf, i), [128, 256], F32, gp) for i in range(2)]
                    sg5_b = k.bufs(2, "sg5")
                    nfb = sb("nfb%d" % hf, [128, D], F32, gp)
                    nfb_b = k.buf("nfb")
                    k.dma("sp", nfb[:], nfb_d[:, :], writes=[nfb_b])
                    ot = [sb("ot%d_%d" % (hf, i), [128, D], F32, gp) for i in range(2)]
                    ot_b = k.bufs(2, "ot")
                    for tl in range(8):
                        b = tl % 2
                        norm_to_hh(tl, npleT)
                        k.dma("sp", pld[b][:], p_d[t0 + tl * 128:t0 + (tl + 1) * 128, :], writes=[pld_b[b]])
                        k.op("act", lambda e: e.copy(out=plb[b][:], in_=pld[b][:]), reads=[pld_b[b]], writes=[plb_b[b]])
                        pbank = ps[5].bitcast(BF16)
                        for c2 in range(2):
                            k.op("pe", lambda e: e.transpose(out=pbank[:, c2 * 128:(c2 + 1) * 128], in_=plb[b][:, c2 * 128:(c2 + 1) * 128], identity=identb[:]),
                                 reads=[plb_b[b], identb_b], writes=[psb[5]])
                        k.op("act", lambda e: e.copy(out=pT[:, :, tl * 128:(tl + 1) * 128], in_=pbank[:, 0:256].rearrange("p (c t) -> p c t", t=128)),
                             reads=[psb[5]], writes=[pT_b[tl]])
                    cnt = 0
                    for nb in range(8):
                        wbi = nb % 2
                        k.dma("pool", wpg[wbi][:], w_pg_d[:, nb * 256:(nb + 1) * 256].rearrange("(kc p) c -> p kc c", p=128), writes=[wpg_b[wbi]])
                        k.dma("pool", wpl[wbi][:], w_ple_d[:, nb * 256:(nb + 1) * 256].rearrange("(kc p) c -> p kc c", p=128), writes=[wpl_b[wbi]])
                        for tl in range(8):
                            pb = 2 * (cnt % 2)
                            sb_i = cnt % 2
                            cnt += 1
                            for kc in range(KC):
                                k.op("pe", lambda e: e.matmul(ps[pb][:, 0:256], hh[:, kc, tl * 128:(tl + 1) * 128], wpg[wbi][:, kc, :], start=(kc == 0), stop=(kc == KC - 1)),
                                     reads=[hh_b[tl], wpg_b[wbi]], writes=[psb[pb]])
                            for c2 in range(2):
                                k.op("pe", lambda e: e.matmul(ps[pb + 1][:, 0:256], pT[:, c2, tl * 128:(tl + 1) * 128], wpl[wbi][:, c2, :], start=(c2 == 0), stop=(c2 == 1)),
                                     reads=[pT_b[tl], wpl_b[wbi]], writes=[psb[pb + 1]])
                            k.op("act", lambda e: e.activation(out=sg5[sb_i][:], in_=ps[pb][:, 0:256], func=AF.Sigmoid), reads=[psb[pb]], writes=[sg5_b[sb_i]])
                            k.op("dve", lambda e: e.tensor_tensor(out=sg5[sb_i][:], in0=sg5[sb_i][:], in1=ps[pb + 1][:, 0:256], op=ALU.mult),
                                 reads=[sg5_b[sb_i], psb[pb + 1]], writes=[sg5_b[sb_i]])
                            k.op("pool", lambda e: e.tensor_tensor(out=acc[:, tl, nb * 256:(nb + 1) * 256], in0=acc[:, tl, nb * 256:(nb + 1) * 256], in1=sg5[sb_i][:], op=ALU.add),
                                 reads=[sg5_b[sb_i], acc_b[tl]], writes=[acc_b[tl]])
                    for tl in range(8):
                        b = tl % 2
                        k.op("act", lambda e: e.activation(out=junk5[:], in_=acc[:, tl, :], func=AF.Square, accum_out=st5[:, tl, 2:3]),
                             reads=[acc_b[tl]], writes=[junk5_b, st5_b[tl]])
                        k.op("act", lambda e: e.activation(out=st5[:, tl, 3:4], in_=st5[:, tl, 2:3], func=AF.Sqrt, scale=1.0 / D, bias=EPS),
                             reads=[st5_b[tl]], writes=[st5_b[tl]])
                        k.op("dve", lambda e: e.reciprocal(out=st5[:, tl, 3:4], in_=st5[:, tl, 3:4]), reads=[st5_b[tl]], writes=[st5_b[tl]])
                        k.op("dve", lambda e: e.scalar_tensor_tensor(out=ot[b][:], in0=acc[:, tl, :], scalar=st5[:, tl, 3:4], in1=nfb[:], op0=ALU.mult, op1=ALU.mult),
                             reads=[acc_b[tl], st5_b[tl], nfb_b], writes=[ot_b[b]])
                        k.dma("sp", out_d[t0 + tl * 128:t0 + (tl + 1) * 128, :], ot[b][:], reads=[ot_b[b]])
                k.barrier()
                gpx.close()

    else:
        ms.close()
        hs.close()
    k.finish()
    cs.close()
    es.close()
    return nc


def prefix_tables(core):
    half = DH // 2
    inv_freq = (np.float32(10000.0) ** (-np.arange(half, dtype=np.float32) / np.float32(half))).astype(np.float32)
    npos = NPT * 128
    pos = np.arange(npos).astype(np.float32)
    ang = (pos[:, None] * inv_freq[None, :]).astype(np.float32).astype(np.float64)
    ropep = np.concatenate([np.cos(ang), np.sin(ang)], axis=1).astype(np.float32).reshape(NPT, 128, 128)
    g = _gammas()
    t0 = core * T
    t = np.arange(npos)
    dist = (t0 - 1 - t).astype(np.float64)
    kd = np.zeros((npos, H), np.float64)
    valid = t < t0
    for h in range(H):
        kd[valid, h] = g[h] ** dist[valid] * DH ** -0.5
    kdecp = kd.reshape(NPT, 128, H).transpose(1, 0, 2).reshape(128, NPT * H).astype(np.float32)
    return np.ascontiguousarray(ropep), np.ascontiguousarray(kdecp)


def s5_host_layout(inp):
    def st(a):
        return a.reshape(32, 2, 64).transpose(1, 2, 0).reshape(128, 32)
    lamre = st(inp["ssm_lam_re"][0])
    lamim = st(inp["ssm_lam_im"][0])
    logdt = st(np.broadcast_to(inp["ssm_log_dt"][0][:, None], (64, 64)))
    def stb(a):
        return a.reshape(32, 2, 64, 16).transpose(1, 2, 0, 3).reshape(128, 32 * 16)
    bre = stb(inp["ssm_b_re"][0])
    bim = stb(inp["ssm_b_im"][0])
    cre = stb(inp["ssm_c_re"][0].transpose(0, 2, 1))
    cim = stb(inp["ssm_c_im"][0].transpose(0, 2, 1))
    s5p = np.ascontiguousarray(np.concatenate([lamre, lamim, logdt, bre, bim, cre, cim], axis=1).astype(np.float32))
    d = inp["ssm_d"][0]
    dfull = d.reshape(8, 128).T
    dpair = np.zeros((128, 32), np.float32)
    dpair[0:32, :] = d.reshape(32, 32).T
    s5d = np.ascontiguousarray(np.concatenate([dfull, dpair], axis=1).astype(np.float32))
    return s5p, s5d


def _bf16(a):
    return np.ascontiguousarray(a).astype(ml_dtypes.bfloat16)


def make_in_maps(inp, fused=False):
    x = np.ascontiguousarray(inp["x"][0])
    nmixT = np.ascontiguousarray(inp["norm_mix"][0].reshape(KC, 128).T)
    gnw_b = np.broadcast_to(inp["ret_gn_w"][0][None, :], (128, RW))
    nffnT = inp["norm_ffn"][0].reshape(KC, 128).T
    npleT = inp["norm_ple"][0].reshape(KC, 128).T
    rb = np.broadcast_to(np.concatenate([inp["b_router_group"][0], inp["b_router_expert"][0]])[None, :], (128, 36))
    small = np.ascontiguousarray(np.concatenate([nmixT, gnw_b, nffnT, npleT, rb], axis=1).astype(np.float32))
    ident = np.eye(128, dtype=np.float32)
    s5p, s5d = s5_host_layout(inp)
    ar = np.arange(128)
    lts = (ar[:, None] < ar[None, :]).astype(np.float32)
    moec = np.ascontiguousarray(np.concatenate([lts, np.ones((128, 128), np.float32),
                                                np.broadcast_to(np.arange(CG, dtype=np.float32)[None, :], (128, CG))], axis=1))
    maps = []
    for c in range(NCORES):
        maps.append({
            "x": np.ascontiguousarray(x[c * T:(c + 1) * T]),
            "w_in": np.ascontiguousarray(inp["w_in"][0]),
            "tabs": host_tables(c),
            "small": small,
            "identb": ident,
            "s5p": s5p,
            "s5d": s5d,
            "w_glu": np.ascontiguousarray(inp["w_glu"][0]),
            "w_merge": np.ascontiguousarray(inp["w_merge"][0]),
            "w_a": np.ascontiguousarray(inp["w_branch_a"][0]),
            "w_b": np.ascontiguousarray(inp["w_branch_b"][0]),
            "w_out": np.ascontiguousarray(inp["w_out"][0]),
            "w_rt": np.ascontiguousarray(np.concatenate([inp["w_router_group"][0], inp["w_router_expert"][0]], axis=1)),
            "w_eg": np.ascontiguousarray(inp["w_exp_gate"][0]),
            "w_eu": np.ascontiguousarray(inp["w_exp_up"][0]),
            "w_ed": np.ascontiguousarray(inp["w_exp_down"][0]),
            "w_pg": np.ascontiguousarray(inp["w_ple_gate"][0]),
            "w_ple": np.ascontiguousarray(inp["w_ple"][0]),
            "p": np.ascontiguousarray(inp["p"][0, 0][c * T:(c + 1) * T]),
            "nfb": np.ascontiguousarray(np.broadcast_to(inp["norm_f"][None, :], (128, D))),
            "moec": moec,
        })
        if fused:
            ropep, kdecp = prefix_tables(c)
            maps[-1]["xfull"] = x
            maps[-1]["ropep"] = ropep
            maps[-1]["kdecp"] = kdecp
    return maps


def kernel(**inp):
    maps = make_in_maps(inp, fused=True)
    nc = build(mode="fused")
    res = run_bass_kernel_spmd(nc, maps, core_ids=list(range(NCORES)))
    return np.concatenate([r["out"] for r in res.results], axis=0)[None]
```

```python
import numpy as np
import ml_dtypes
from contextlib import ExitStack
import concourse.bass as bass
import concourse.mybir as mybir
from concourse.bass_utils import run_bass_kernel_spmd

F32 = mybir.dt.float32
BF16 = mybir.dt.bfloat16
AF = mybir.ActivationFunctionType
ALU = mybir.AluOpType

NCORES = 8
SEQ = 16384
T = SEQ // NCORES
NT = T // 128
D = 2048
KC = D // 128
H = 8
DH = 128
RW = 1024
EPS = 1e-6
PI = float(np.pi)


class Buf:
    __slots__ = ("name", "w", "r")

    def __init__(self, name):
        self.name = name
        self.w = None
        self.r = {}


class K:
    NDSEM = 24

    def __init__(self, nc, es):
        self.nc = nc
        self.es = es
        self.eng = {"pe": nc.tensor, "act": nc.scalar, "dve": nc.vector, "pool": nc.gpsimd, "sp": nc.sync}
        self.sem = {n: es.enter_context(nc.semaphore("s_" + n)) for n in self.eng}
        self.cnt = {n: 0 for n in self.eng}
        self.known = {n: {} for n in self.eng}
        self.dsem = {q: [es.enter_context(nc.semaphore("d_%s%d" % (q, i))) for i in range(self.NDSEM)]
                     for q in ("sp", "pool", "act")}
        self.duse = {q: [0] * self.NDSEM for q in self.dsem}
        self.dnext = {q: 0 for q in self.dsem}
        self.nbuf = 0

    def buf(self, name=None):
        self.nbuf += 1
        return Buf(name or ("b%d" % self.nbuf))

    def bufs(self, n, name="b"):
        return [self.buf("%s%d" % (name, i)) for i in range(n)]

    def _semof(self, key):
        return self.sem[key] if isinstance(key, str) else self.dsem[key[1]][key[2]]

    def _wait(self, e, deps):
        need = {}
        for t in deps:
            if t is None:
                continue
            k, v = t
            if need.get(k, 0) < v:
                need[k] = v
        kn = self.known[e]
        for k, v in need.items():
            if kn.get(k, 0) >= v:
                continue
            self.eng[e].wait_ge(self._semof(k), v)
            kn[k] = v

    SAME_ENGINE_WAIT = True

    def _deps(self, e, reads, writes):
        deps = []
        skip_same = (e == "pe") or (not self.SAME_ENGINE_WAIT)
        for b in reads:
            if b.w is not None and not (skip_same and b.w[0] == e):
                deps.append(b.w)
        for b in writes:
            if b.w is not None and not (skip_same and b.w[0] == e):
                deps.append(b.w)
            for k, v in b.r.items():
                if k == e:
                    continue
                deps.append((k, v))
        return deps

    def op(self, e, fn, reads=(), writes=()):
        self._wait(e, self._deps(e, reads, writes))
        ins = fn(self.eng[e])
        self.cnt[e] += 1
        c = self.cnt[e]
        ins.then_inc(self.sem[e], 1)
        for b in reads:
            if b.r.get(e, 0) < c:
                b.r[e] = c
        for b in writes:
            b.w = (e, c)
            b.r = {}
        return ins

    def dma(self, q, out, in_, reads=(), writes=()):
        deps = self._deps(q, reads, writes)
        idx = self.dnext[q]
        self.dnext[q] = (idx + 1) % self.NDSEM
        key = ("d", q, idx)
        if self.duse[q][idx] > 0:
            deps.append((key, 16 * self.duse[q][idx]))
        self._wait(q, deps)
        ins = self.eng[q].dma_start(out=out, in_=in_)
        self.duse[q][idx] += 1
        v = 16 * self.duse[q][idx]
        ins.then_inc(self.dsem[q][idx], 16)
        for b in reads:
            if b.r.get(key, 0) < v:
                b.r[key] = v
        for b in writes:
            b.w = (key, v)
            b.r = {}
        return ins

    def qop(self, q, fn, reads=(), writes=()):
        deps = self._deps(q, reads, writes)
        idx = self.dnext[q]
        self.dnext[q] = (idx + 1) % self.NDSEM
        key = ("d", q, idx)
        if self.duse[q][idx] > 0:
            deps.append((key, 16 * self.duse[q][idx]))
        self._wait(q, deps)
        ins = fn(self.eng[q])
        self.duse[q][idx] += 1
        v = 16 * self.duse[q][idx]
        ins.then_inc(self.dsem[q][idx], 16)
        for b in reads:
            if b.r.get(key, 0) < v:
                b.r[key] = v
        for b in writes:
            b.w = (key, v)
            b.r = {}
        return ins

    def barrier(self):
        deps = [(n, c) for n, c in self.cnt.items() if c > 0]
        for q in self.dsem:
            for i, u in enumerate(self.duse[q]):
                if u > 0:
                    deps.append((("d", q, i), 16 * u))
        for e in self.eng:
            self._wait(e, [d for d in deps if d[0] != e])

    def finish(self):
        deps = [(n, c) for n, c in self.cnt.items() if c > 0 and n != "sp"]
        for q in self.dsem:
            for i, u in enumerate(self.duse[q]):
                if u > 0:
                    deps.append((("d", q, i), 16 * u))
        self._wait("sp", deps)


def _gammas():
    return 1.0 - np.exp2(-5.0 - np.arange(H, dtype=np.float64))


def host_tables(core):
    half = DH // 2
    inv_freq = (np.float32(10000.0) ** (-np.arange(half, dtype=np.float32) / np.float32(half))).astype(np.float32)
    pos = (core * T + np.arange(T)).astype(np.float32)
    ang = (pos[:, None] * inv_freq[None, :]).astype(np.float32).astype(np.float64)
    cos = np.cos(ang).astype(np.float32).reshape(NT, 128, half).transpose(1, 0, 2)
    sin = np.sin(ang).astype(np.float32).reshape(NT, 128, half).transpose(1, 0, 2)
    g = _gammas()
    a = np.arange(128)
    ka, qb = a[:, None], a[None, :]
    same = (ka // 64) == (qb // 64)
    earlier = (ka // 64) < (qb // 64)
    mask = np.zeros((128, H, 128), np.float64)
    for h in range(H):
        m = np.where(same, g[h] ** np.abs(qb - ka), np.where(earlier, g[h] ** (qb - ka).clip(0), 0.0))
        mask[:, h, :] = m * DH ** -0.5
    qdec = np.stack([g[h] ** (a + 1.0) for h in range(H)], axis=1)
    kdec = np.stack([g[h] ** (127.0 - a) * DH ** -0.5 for h in range(H)], axis=1)
    onehot = np.zeros((128, NCORES), np.float64)
    onehot[:, core] = 1.0
    tabs = np.concatenate([cos.reshape(128, -1), sin.reshape(128, -1), mask.reshape(128, -1), qdec, kdec, onehot],
                          axis=1).astype(np.float32)
    return np.ascontiguousarray(tabs)


TAB_COS = 0
TAB_SIN = TAB_COS + NT * 64
TAB_MASK = TAB_SIN + NT * 64
TAB_QDEC = TAB_MASK + H * 128
TAB_KDEC = TAB_QDEC + H
TAB_ONEHOT = TAB_KDEC + H
TAB_W = TAB_ONEHOT + NCORES
S5P_W = 96 + 4 * 512
NQ = 32
AGW = 1024 + 64
SMALL_W = 16 + 1024 + 16 + 16 + 36
NE = 32
NPT = (NCORES - 1) * NT
CG = 384
FE = 512


class _Done(Exception):
    pass


def build(debug=(), mode="single"):
    with_carry = mode in ("states", "main", "fused")
    nc = bass.Bass("TRN2", target_bir_lowering=False)
    es = ExitStack()
    k = K(nc, es)
    G128 = [float(x) for x in _gammas() ** 128]
    G2048 = [float(x) for x in _gammas() ** 2048]

    def din(name, shape, dt=F32):
        return nc.dram_tensor(name, list(shape), dt, kind="ExternalInput").ap()

    def dout(name, shape, dt=F32):
        return nc.dram_tensor(name, list(shape), dt, kind="ExternalOutput").ap()

    def dscr(name, shape, dt=F32):
        return nc.dram_tensor(name, list(shape), dt, kind="Internal").ap()

    def sb(name, shape, dt=F32, stack=None):
        return (stack or es).enter_context(nc.sbuf_tensor("sb_" + name, list(shape), dt))

    x_d = din("x", [T, D])
    if mode == "fused":
        xfull_d = din("xfull", [SEQ, D])
        ropep_d = din("ropep", [NPT, 128, 128])
        kdecp_d = din("kdecp", [128, NPT * H])
    w_in_d = din("w_in", [D, 5120])
    tabs_d = din("tabs", [128, TAB_W])
    small_d = din("small", [128, SMALL_W])
    identb_d = din("identb", [128, 128])
    s5p_d = din("s5p", [128, S5P_W])
    s5d_d = din("s5d", [128, 8 + 32])
    w_glu_d = din("w_glu", [RW, RW])
    w_merge_d = din("w_merge", [D, 2 * D])
    w_a_d = din("w_a", [RW, D])
    w_b_d = din("w_b", [RW, D])
    w_out_d = din("w_out", [D, D])
    w_rt_d = din("w_rt", [D, 36])
    w_eg_d = din("w_eg", [NE, D, FE])
    w_eu_d = din("w_eu", [NE, D, FE])
    w_ed_d = din("w_ed", [NE, FE, D])
    w_pg_d = din("w_pg", [D, D])
    w_ple_d = din("w_ple", [256, D])
    p_d = din("p", [T, 256])
    nfb_d = din("nfb", [128, D])
    moec_d = din("moec", [128, 256 + CG])
    dbg = {}
    for nm, shp in debug:
        dbg[nm] = dout("dbg_" + nm, shp)

    yretT_d = dscr("yretT", [RW, T], BF16)
    yssmT_d = dscr("yssmT", [RW, T], BF16)
    x1_d = dscr("x1", [T, D])
    x1_w = [k.bufs(8, "x1w%d_" % i) for i in range(NT)]
    dcount = [0]
    dtmps = [sb("dump%d" % i, [128, 512], F32) for i in range(len(debug))]

    def dump(name, src, src_bufs, cols):
        if name not in dbg or dbg[name] is None:
            return
        tmp = dtmps[dcount[0]][:, 0:cols]
        dcount[0] += 1
        tb = k.buf()
        k.op("act", lambda e: e.copy(out=tmp, in_=src), reads=src_bufs, writes=[tb])
        k.dma("sp", dbg[name][:, :], tmp, reads=[tb])
        dbg[name] = None

    ps = [es.enter_context(nc.psum_tensor("ps%d" % i, [128, 512], F32)) for i in range(8)]
    psb = k.bufs(8, "ps")

    identb = sb("identb", [128, 128], BF16)
    identb_b = k.buf("identb")
    small = sb("small", [128, SMALL_W])
    small_b = k.buf("small")
    k.dma("pool", identb[:], identb_d[:, :], writes=[identb_b])
    k.dma("sp", small[:], small_d[:, :], writes=[small_b])
    nmixT = small[:, 0:16]
    gnw = small[:, 16:16 + 1024]
    nffnT = small[:, 1040:1056]
    npleT = small[:, 1056:1072]
    rbias = small[:, 1072:1108]
    def s5_setup(ss, with_cp=True, tag=""):
        s5p = sb("s5p" + tag, [128, S5P_W], F32, ss)
        s5p_b = k.buf("s5p")
        k.dma("sp", s5p[:], s5p_d[:, :], writes=[s5p_b])
        lamre, lamim, logdt = s5p[:, 0:32], s5p[:, 32:64], s5p[:, 64:96]
        Bre = s5p[:, 96:608].rearrange("p (q c) -> p q c", c=16)
        Bim = s5p[:, 608:1120].rearrange("p (q c) -> p q c", c=16)
        Cre = s5p[:, 1120:1632].rearrange("p (q c) -> p q c", c=16)
        Cim = s5p[:, 1632:2144].rearrange("p (q c) -> p q c", c=16)
        W = sb("s5w" + tag, [128, 20, 32], F32, ss)
        wb = k.buf("s5w")

        def tt(o, a, b_, op, eng="dve"):
            k.op(eng, lambda e: e.tensor_tensor(out=o, in0=a, in1=b_, op=op), reads=[wb, s5p_b], writes=[wb])

        def ts(o, a, s1, s2, op0, op1=None, eng="dve"):
            if op1 is None:
                k.op(eng, lambda e: e.tensor_scalar(out=o, in0=a, scalar1=s1, scalar2=None, op0=op0), reads=[wb, s5p_b], writes=[wb])
            else:
                k.op(eng, lambda e: e.tensor_scalar(out=o, in0=a, scalar1=s1, scalar2=s2, op0=op0, op1=op1), reads=[wb, s5p_b], writes=[wb])

        def act(o, a, f):
            k.op("act", lambda e: e.activation(out=o, in_=a, func=f), reads=[wb, s5p_b], writes=[wb])

        lr, dt, a_, b_, ea, r1, r2, sbn, cbn, Lre, Lim, nr, den, t0, t1_, cr, ci, t2_, t3_ = [W[:, i, :] for i in range(19)]
        ts(lr, lamre, -1e-4, None, ALU.min)
        act(dt, logdt, AF.Exp)
        tt(a_, lr, dt, ALU.mult)
        tt(b_, lamim, dt, ALU.mult)
        act(ea, a_, AF.Exp)
        for (dst, off) in ((r1, PI), (r2, 1.5 * PI)):
            ts(dst, b_, off, None, ALU.add)
            ts(t0, dst, 2 * PI, -2 * PI, ALU.is_ge, ALU.mult)
            tt(t1_, dst, t0, ALU.add)
            ts(t0, dst, 4 * PI, -2 * PI, ALU.is_ge, ALU.mult)
            tt(t1_, t1_, t0, ALU.add)
            ts(t0, dst, 6 * PI, -2 * PI, ALU.is_ge, ALU.mult)
            tt(t1_, t1_, t0, ALU.add)
            ts(dst, t1_, -PI, None, ALU.add)
        act(sbn, r1, AF.Sin)
        act(cbn, r2, AF.Sin)
        tt(Lre, ea, cbn, ALU.mult)
        tt(Lim, ea, sbn, ALU.mult)
        ts(nr, Lre, -1.0, None, ALU.add)
        tt(den, lr, lr, ALU.mult)
        tt(t0, lamim, lamim, ALU.mult)
        tt(den, den, t0, ALU.add)
        k.op("dve", lambda e: e.reciprocal(out=den, in_=den), reads=[wb], writes=[wb])
        tt(t0, nr, lr, ALU.mult)
        tt(t1_, Lim, lamim, ALU.mult)
        tt(t0, t0, t1_, ALU.add)
        tt(cr, t0, den, ALU.mult)
        tt(t0, Lim, lr, ALU.mult)
        tt(t1_, nr, lamim, ALU.mult)
        tt(t0, t0, t1_, ALU.subtract)
        tt(ci, t0, den, ALU.mult)
        Bb = sb("s5Bb" + tag, [128, 2, 32, 16], F32, ss)
        T3 = sb("s5T3" + tag, [128, 2, 32, 16], F32, ss)
        bc = lambda v: v.unsqueeze(2).broadcast_to([128, 32, 16])
        tt(T3[:, 0], Bre, bc(cr), ALU.mult)
        tt(T3[:, 1], Bim, bc(ci), ALU.mult)
        tt(Bb[:, 0], T3[:, 0], T3[:, 1], ALU.subtract)
        tt(T3[:, 0], Bim, bc(cr), ALU.mult)
        tt(T3[:, 1], Bre, bc(ci), ALU.mult)
        tt(Bb[:, 1], T3[:, 0], T3[:, 1], ALU.add)
        k.op("dve", lambda e: e.memset(Pre[:, 0, :], 1.0), reads=[wb], writes=[wb])
        k.op("dve", lambda e: e.memset(Pim[:, 0, :], 0.0), reads=[wb], writes=[wb])
        for kk in range(8):
            tt(t0, Pre[:, kk, :], Lre, ALU.mult)
            tt(t1_, Pim[:, kk, :], Lim, ALU.mult)
            tt(Pre[:, kk + 1, :], t0, t1_, ALU.subtract)
            tt(t0, Pre[:, kk, :], Lim, ALU.mult)
            tt(t1_, Pim[:, kk, :], Lre, ALU.mult)
            tt(Pim[:, kk + 1, :], t0, t1_, ALU.add)
        k.op("dve", lambda e: e.tensor_copy(out=Dre[:, 0, :], in_=Pre[:, 8, :]), reads=[wb], writes=[wb])
        k.op("dve", lambda e: e.tensor_copy(out=Dim[:, 0, :], in_=Pim[:, 8, :]), reads=[wb], writes=[wb])
        for i in range(8):
            tt(t0, Dre[:, i, :], Dre[:, i, :], ALU.mult)
            tt(t1_, Dim[:, i, :], Dim[:, i, :], ALU.mult)
            tt(Dre[:, i + 1, :], t0, t1_, ALU.subtract)
            tt(t0, Dre[:, i, :], Dim[:, i, :], ALU.mult)
            ts(Dim[:, i + 1, :], t0, 2.0, None, ALU.mult)
        ts(nDim[:, :, :], Dim[:, :, :], -1.0, None, ALU.mult)
        for sx in range(8):
            pr = bc(Pre[:, 7 - sx, :])
            pi_ = bc(Pim[:, 7 - sx, :])
            tt(T3[:, 0], Bb[:, 0], pr, ALU.mult)
            tt(T3[:, 1], Bb[:, 1], pi_, ALU.mult)
            tt(RBc[:, :, sx, 0, :], T3[:, 0], T3[:, 1], ALU.subtract)
            tt(T3[:, 0], Bb[:, 1], pr, ALU.mult)
            tt(T3[:, 1], Bb[:, 0], pi_, ALU.mult)
            tt(RBc[:, :, sx, 1, :], T3[:, 0], T3[:, 1], ALU.add)
        for kk in (range(9) if with_cp else ()):
            pr = bc(Pre[:, kk, :])
            pi_ = bc(Pim[:, kk, :])
            tt(T3[:, 0], Cre, pr, ALU.mult)
            tt(T3[:, 1], Cim, pi_, ALU.mult)
            tt(CPc[:, :, kk, 0, :], T3[:, 0], T3[:, 1], ALU.subtract)
            tt(T3[:, 0], Cre, pi_, ALU.mult)
            tt(T3[:, 1], Cim, pr, ALU.mult)
            tt(T3[:, 0], T3[:, 0], T3[:, 1], ALU.add)
            ts(CPc[:, :, kk, 1, :], T3[:, 0], -1.0, None, ALU.mult)
        return wb

    cs = ExitStack()
    S = sb("S", [128, H, 128], F32, cs)
    Sbf = sb("Sbf", [128, H, 128], BF16, cs)
    S_b = k.bufs(H, "S")
    Sbf_b = k.bufs(H, "Sbf")
    carryP = sb("carryP", [128, 32, 2], F32, cs)
    carryP_b = k.buf("carryP")
    tabs1 = sb("tabs1", [128, NCORES], F32, cs)
    tabs1_b = k.buf("tabs1")
    k.dma("sp", tabs1[:], tabs_d[:, TAB_ONEHOT:TAB_W], writes=[tabs1_b])

    def stage1_tile(src_rows, dst3, dst_b, nT, xt_, xt_b_, xs_, xs_b_, st_, st_b_, pbanks, part="both"):
        if part in ("both", "a"):
            k.dma("sp", xt_[:], src_rows, writes=[xt_b_])
            k.op("act", lambda e: e.activation(out=xs_[:], in_=xt_[:], func=AF.Square, accum_out=st_[:, 0:1]), reads=[xt_b_], writes=[xs_b_, st_b_])
            k.op("act", lambda e: e.activation(out=st_[:, 1:2], in_=st_[:, 0:1], func=AF.Sqrt, scale=1.0 / D, bias=EPS), reads=[st_b_], writes=[st_b_])
            k.op("dve", lambda e: e.reciprocal(out=st_[:, 1:2], in_=st_[:, 1:2]), reads=[st_b_], writes=[st_b_])
            k.op("act", lambda e: e.activation(out=xs_[:], in_=xt_[:], func=AF.Copy, scale=st_[:, 1:2]), reads=[xt_b_, st_b_, xs_b_], writes=[xs_b_])
        if part in ("both", "b"):
            for half in range(2):
                pbank = ps[pbanks[half]].bitcast(BF16)
                for j in range(8):
                    kc = half * 8 + j
                    k.op("pe", lambda e: e.transpose(out=pbank[:, j * 128:(j + 1) * 128], in_=xs_[:, kc * 128:(kc + 1) * 128], identity=identb[:]),
                         reads=[xs_b_, identb_b], writes=[psb[pbanks[half]]])
                k.op("dve", lambda e: e.tensor_tensor(out=dst3[:, half * 8:(half + 1) * 8, :], in0=pbank[:, :].rearrange("p (j t) -> p j t", t=128),
                                                      in1=nT[:, half * 8:(half + 1) * 8].unsqueeze(2).broadcast_to([128, 8, 128]), op=ALU.mult),
                     reads=[psb[pbanks[half]], small_b], writes=[dst_b])

    if mode == "fused":
        uTp_d = dscr("uTp", [RW, NPT * 128], BF16)
        uTp_b = k.bufs(NPT, "uTp")
        with ExitStack() as pa:
            Wkv = sb("pWkv", [128, KC, 2048], BF16, pa)
            Wkv_b = k.bufs(4, "pWkv")
            for cb in range(4):
                c0 = RW + cb * 512
                k.dma("pool", Wkv[:, :, cb * 512:(cb + 1) * 512], w_in_d[:, c0:c0 + 512].rearrange("(kc p) c -> p kc c", p=128), writes=[Wkv_b[cb]])
            kdecP = sb("kdecP", [128, NPT, H], F32, pa)
            kdecP_b = k.buf("kdecP")
            k.dma("sp", kdecP[:], kdecp_d[:, :].rearrange("p (g h) -> p g h", h=H), writes=[kdecP_b])
            ropeP = [sb("ropeP%d" % i, [128, 2, 64], F32, pa) for i in range(2)]
            ropeP_b = k.bufs(2, "ropeP")
            xtA = [sb("pxt%d" % i, [128, D], F32, pa) for i in range(3)]
            xsA = [sb("pxs%d" % i, [128, D], BF16, pa) for i in range(3)]
            stA = [sb("pst%d" % i, [128, 2], F32, pa) for i in range(3)]
            xtA_b, xsA_b, stA_b = k.bufs(3, "pxt"), k.bufs(3, "pxs"), k.bufs(3, "pst")
            hTt = [sb("phTt%d" % i, [128, KC, 128], BF16, pa) for i in range(2)]
            hTt_b = k.bufs(2, "phTt")
            tt4 = [sb("ptt%d" % i, [128, 4, 64], F32, pa) for i in range(4)]
            tt4_b = k.bufs(4, "ptt")
            krr = [sb("pkr%d" % i, [128, 4, 2, 64], F32, pa) for i in range(2)]
            krr1_b, krr2_b = k.bufs(2, "pkr1"), k.bufs(2, "pkr2")
            ktdA = [sb("pktd%d" % i, [128, H, 128], BF16, pa) for i in range(2)]
            ktdA_b = [k.bufs(2, "pktd%d_" % i) for i in range(2)]
            vbA = [sb("pvb%d" % i, [128, 1024], BF16, pa) for i in range(2)]
            vbA_b = [k.bufs(2, "pvb%d_" % i) for i in range(2)]
            KV0, KV1 = 6, 7
            Wu = sb("pWu", [128, KC, RW], BF16, pa)
            Wu_b = k.bufs(2, "pWu")
            for cb in range(2):
                c0 = 4 * RW + cb * 512
                k.dma("pool", Wu[:, :, cb * 512:(cb + 1) * 512], w_in_d[:, c0:c0 + 512].rearrange("(kc p) c -> p kc c", p=128), writes=[Wu_b[cb]])
            uTt = [sb("puTt%d" % i, [128, 8, 128], BF16, pa) for i in range(2)]
            uTt_b = k.bufs(2, "puTt")
            utok = [sb("putok%d" % i, [128, RW], BF16, pa) for i in range(2)]
            utok_b = [k.bufs(2, "putok%d_" % i) for i in range(2)]
            for g in range(NPT):
                b = g % 2
                k.dma("sp", ropeP[b][:], ropep_d[g].rearrange("p (c f) -> p c f", c=2), writes=[ropeP_b[b]])

                def s1(gg, part):
                    b3 = gg % 3
                    stage1_tile(xfull_d[gg * 128:(gg + 1) * 128, :], hTt[gg % 2][:, :, :], hTt_b[gg % 2], nmixT, xtA[b3], xtA_b[b3], xsA[b3], xsA_b[b3],
                                stA[b3], stA_b[b3], (0, 1), part=part)
                if g == 0:
                    s1(0, "a")
                    s1(1, "a")
                    s1(0, "b")
                if g + 2 < NPT:
                    s1(g + 2, "a")
                if g + 1 < NPT:
                    s1(g + 1, "b")
                for cb in range(4):
                    pb = 2 + cb % 2
                    for kc in range(KC):
                        k.op("pe", lambda e: e.matmul(ps[pb][:, :], hTt[b][:, kc, :], Wkv[:, kc, cb * 512:(cb + 1) * 512], start=(kc == 0), stop=(kc == KC - 1)),
                             reads=[hTt_b[b], Wkv_b[cb]], writes=[psb[pb]])
                    if cb < 2:
                        X4 = ps[pb][:, :].rearrange("p (h c f) -> p h c f", c=2, f=64)
                        A, B = X4[:, :, 0, :], X4[:, :, 1, :]
                        C = ropeP[b][:, 0, :].unsqueeze(1).broadcast_to([128, 4, 64])
                        Sn = ropeP[b][:, 1, :].unsqueeze(1).broadcast_to([128, 4, 64])
                        kb = cb % 2
                        k.op("dve", lambda e: e.tensor_tensor(out=tt4[0][:], in0=A, in1=C, op=ALU.mult), reads=[psb[pb], ropeP_b[b]], writes=[tt4_b[0]])
                        k.op("dve", lambda e: e.tensor_tensor(out=tt4[1][:], in0=B, in1=Sn, op=ALU.mult), reads=[psb[pb], ropeP_b[b]], writes=[tt4_b[1]])
                        k.op("dve", lambda e: e.tensor_tensor(out=tt4[2][:], in0=A, in1=Sn, op=ALU.mult), reads=[psb[pb], ropeP_b[b]], writes=[tt4_b[2]])
                        k.op("dve", lambda e: e.tensor_tensor(out=tt4[3][:], in0=B, in1=C, op=ALU.mult), reads=[psb[pb], ropeP_b[b]], writes=[tt4_b[3]])
                        k.op("pool", lambda e: e.tensor_tensor(out=krr[kb][:, :, 0, :], in0=tt4[0][:], in1=tt4[1][:], op=ALU.subtract),
                             reads=[tt4_b[0], tt4_b[1]], writes=[krr1_b[kb]])
                        k.op("pool", lambda e: e.tensor_tensor(out=krr[kb][:, :, 1, :], in0=tt4[2][:], in1=tt4[3][:], op=ALU.add),
                             reads=[tt4_b[2], tt4_b[3]], writes=[krr2_b[kb]])
                        k.op("pool", lambda e: e.tensor_tensor(
                            out=ktdA[b][:, cb * 4:(cb + 1) * 4, :], in0=krr[kb][:, :, :, :].rearrange("p h c f -> p h (c f)"),
                            in1=kdecP[:, g, cb * 4:(cb + 1) * 4].unsqueeze(2).broadcast_to([128, 4, 128]), op=ALU.mult),
                            reads=[krr1_b[kb], krr2_b[kb], kdecP_b], writes=[ktdA_b[b][cb]])
                    else:
                        k.op("act", lambda e: e.copy(out=vbA[b][:, (cb - 2) * 512:(cb - 1) * 512], in_=ps[pb][:, :]), reads=[psb[pb]], writes=[vbA_b[b][cb - 2]])
                for cb in range(2):
                    pbk = 4 + cb
                    for kc in range(KC):
                        k.op("pe", lambda e: e.matmul(ps[pbk][:, :], hTt[b][:, kc, :], Wu[:, kc, cb * 512:(cb + 1) * 512], start=(kc == 0), stop=(kc == KC - 1)),
                             reads=[hTt_b[b], Wu_b[cb]], writes=[psb[pbk]])
                    k.op("act", lambda e: e.copy(out=utok[b][:, cb * 512:(cb + 1) * 512], in_=ps[pbk][:, :]), reads=[psb[pbk]], writes=[utok_b[b][cb]])
                for cb in range(2):
                    pbk = 4 + cb
                    ubank = ps[pbk].bitcast(BF16)
                    for c4 in range(4):
                        ct = cb * 4 + c4
                        k.op("pe", lambda e: e.transpose(out=ubank[:, c4 * 128:(c4 + 1) * 128], in_=utok[b][:, ct * 128:(ct + 1) * 128], identity=identb[:]),
                             reads=[utok_b[b][cb], identb_b], writes=[psb[pbk]])
                    for c4 in range(4):
                        ct = cb * 4 + c4
                        eng = "act" if c4 % 2 == 0 else "dve"
                        fnc = (lambda e: e.copy(out=uTt[b][:, ct, :].rearrange("p (s m) -> p s m", s=8),
                                                in_=ubank[:, c4 * 128:(c4 + 1) * 128].rearrange("p (m s) -> p s m", s=8))) if eng == "act" else \
                              (lambda e: e.tensor_copy(out=uTt[b][:, ct, :].rearrange("p (s m) -> p s m", s=8),
                                                       in_=ubank[:, c4 * 128:(c4 + 1) * 128].rearrange("p (m s) -> p s m", s=8)))
                        k.op(eng, fnc, reads=[psb[pbk], uTt_b[b]], writes=[uTt_b[b]])
                k.dma("sp", uTp_d[:, g * 128:(g + 1) * 128].rearrange("(ct p) t -> p ct t", p=128), uTt[b][:], reads=[uTt_b[b]], writes=[uTp_b[g]])
                for h in range(H):
                    kvb = KV0 if h < 4 else KV1
                    k.op("pe", lambda e: e.matmul(ps[kvb][:, (h % 4) * 128:(h % 4 + 1) * 128], ktdA[b][:, h, :], vbA[b][:, h * 128:(h + 1) * 128],
                                                  start=(g == 0 and h % 4 == 0), stop=(g == NPT - 1 and h % 4 == 3)), reads=[ktdA_b[b][h // 4], vbA_b[b][h // 4]], writes=[psb[kvb]])
            for h in range(H):
                kvb = KV0 if h < 4 else KV1
                k.op("act", lambda e: e.copy(out=S[:, h, :], in_=ps[kvb][:, (h % 4) * 128:(h % 4 + 1) * 128]), reads=[psb[kvb]], writes=[S_b[h]])
                k.op("act", lambda e: e.copy(out=Sbf[:, h, :], in_=S[:, h, :]), reads=[S_b[h]], writes=[Sbf_b[h]])
        k.barrier()
        with ExitStack() as pb_:
            Pre = sb("qPre", [128, 9, 32], F32, pb_)
            Pim = sb("qPim", [128, 9, 32], F32, pb_)
            Dre = sb("qDre", [128, 9, 32], F32, pb_)
            Dim = sb("qDim", [128, 9, 32], F32, pb_)
            nDim = sb("qnDim", [128, 9, 32], F32, pb_)
            RBc = sb("qRBc", [128, 32, 8, 2, 16], BF16, pb_)
            CPc = None
            with ExitStack() as ss:
                mats_b = s5_setup(ss, with_cp=False, tag="q")
            k.barrier()
            NSEG = NCORES - 1
            NCH = NSEG * 256
            RBm = [sb("qRBm%d" % i, [128, 8, 2, 32], BF16, pb_) for i in range(2)]
            RBm_b = k.bufs(2, "qRBm")
            RBT = [sb("qRBT%d" % i, [32, 16, 128], BF16, pb_) for i in range(2)]
            RBT_b = k.bufs(2, "qRBT")
            uq = [sb("quq%d" % i, [32, NCH * 8], BF16, pb_) for i in range(2)]
            uq_b = k.bufs(2, "quq")
            wA = [sb("qwA%d" % i, [128, 2, NSEG, 256], F32, pb_) for i in range(2)]
            wA_b = k.bufs(2, "qwA")
            wB = [sb("qwB%d" % i, [128, 2, NSEG, 128], F32, pb_) for i in range(2)]
            wB_b = k.bufs(2, "qwB")
            Eall = sb("qEall", [128, 32, 2, NSEG], F32, pb_)
            Eall_b = k.bufs(32, "qEall")
            for i in range(2):
                k.op("pool", lambda e: e.memset(RBm[i][:], 0.0), writes=[RBm_b[i]])
            PTA, PTB = 0, 1
            NBK = 4
            CW = NCH // NBK
            def pfx_pair(q):
                b = q % 2
                for gi in range(2):
                    prt = slice(gi * 64, gi * 64 + 64)
                    csl = slice(gi * 16, gi * 16 + 16)
                    k.op("pool", lambda e: e.tensor_copy(out=RBm[b][prt, :, :, csl], in_=RBc[prt, q, :, :, :]), reads=[mats_b], writes=[RBm_b[b]])
                pTA = ps[PTA].bitcast(BF16)
                pTB = ps[PTB].bitcast(BF16)
                for sx in range(8):
                    for ri in range(2):
                        idx = sx * 2 + ri
                        pt, pbb = (pTA, psb[PTA]) if idx < 8 else (pTB, psb[PTB])
                        k.op("pe", lambda e: e.transpose(out=pt[0:32, (idx % 8) * 128:(idx % 8 + 1) * 128], in_=RBm[b][:, sx, ri, :], identity=identb[:]),
                             reads=[RBm_b[b], identb_b], writes=[pbb])
                k.op("act", lambda e: e.copy(out=RBT[b][:, 0:8, :], in_=pTA[0:32, :].rearrange("p (a c) -> p a c", c=128)), reads=[psb[PTA]], writes=[RBT_b[b]])
                k.op("act", lambda e: e.copy(out=RBT[b][:, 8:16, :], in_=pTB[0:32, :].rearrange("p (a c) -> p a c", c=128)), reads=[psb[PTB], RBT_b[b]], writes=[RBT_b[b]])
                yield
                k.dma("sp", uq[b][:], uTp_d[q * 32:(q + 1) * 32, :], reads=uTp_b, writes=[uq_b[b]])
                uq4 = uq[b][:, :].rearrange("p (g s m) -> p g s m", s=8, m=16)
                TPB = CW // 16
                wflat = wA[b][:, :, :, :].rearrange("p r g m -> p r (g m)")
                for ri in range(2):
                    for nbk in range(NBK):
                        pbk = 2 + (ri * NBK + nbk) % 6
                        for sx in range(8):
                            k.op("pe", lambda e: e.matmul(ps[pbk][:, 0:CW], RBT[b][:, sx * 2 + ri, :], uq4[:, nbk * TPB:(nbk + 1) * TPB, sx, :],
                                                          start=(sx == 0), stop=(sx == 7)), reads=[RBT_b[b], uq_b[b]], writes=[psb[pbk]])
                        k.op("act", lambda e: e.copy(out=wflat[:, ri, nbk * CW:(nbk + 1) * CW], in_=ps[pbk][:, 0:CW]), reads=[psb[pbk]], writes=[wA_b[b]])
                yield
                src, src_b2 = wA[b], wA_b[b]
                dst, dst_b2 = wB[b], wB_b[b]
                n = 256
                for lv in range(8):
                    hn = n // 2
                    v = src[:, :, :, 0:n].rearrange("p r g (m two) -> p r g m two", two=2)
                    dre, dim_, ndim = Dre[:, lv, q:q + 1], Dim[:, lv, q:q + 1], nDim[:, lv, q:q + 1]
                    o_re, o_im = dst[:, 0, :, 0:hn], dst[:, 1, :, 0:hn]
                    k.op("dve", lambda e: e.scalar_tensor_tensor(out=o_re, in0=v[:, 0, :, :, 0], scalar=dre, in1=v[:, 0, :, :, 1], op0=ALU.mult, op1=ALU.add),
                         reads=[src_b2, mats_b, dst_b2], writes=[dst_b2])
                    k.op("dve", lambda e: e.scalar_tensor_tensor(out=o_im, in0=v[:, 1, :, :, 0], scalar=dre, in1=v[:, 1, :, :, 1], op0=ALU.mult, op1=ALU.add),
                         reads=[src_b2, mats_b, dst_b2], writes=[dst_b2])
                    yield
                    k.op("dve", lambda e: e.scalar_tensor_tensor(out=o_re, in0=v[:, 1, :, :, 0], scalar=ndim, in1=o_re, op0=ALU.mult, op1=ALU.add),
                         reads=[src_b2, mats_b, dst_b2], writes=[dst_b2])
                    k.op("dve", lambda e: e.scalar_tensor_tensor(out=o_im, in0=v[:, 0, :, :, 0], scalar=dim_, in1=o_im, op0=ALU.mult, op1=ALU.add),
                         reads=[src_b2, mats_b, dst_b2], writes=[dst_b2])
                    yield
                    src, src_b2, dst, dst_b2 = dst, dst_b2, src, src_b2
                    n = hn
                k.op("act", lambda e: e.copy(out=Eall[:, q, :, :], in_=src[:, :, :, 0]), reads=[src_b2], writes=[Eall_b[q]])

            for q0 in range(0, NQ, 2):
                gens = [pfx_pair(q0), pfx_pair(q0 + 1)]
                while gens:
                    for gnr in list(gens):
                        try:
                            next(gnr)
                        except StopIteration:
                            gens.remove(gnr)
            Xc = sb("qXc", [128, 32, 2], F32, pb_)
            tq = sb("qtq", [128, 4, 32], F32, pb_)
            xb = k.buf("qXc")
            k.op("dve", lambda e: e.memset(Xc[:], 0.0), writes=[xb])
            k.op("dve", lambda e: e.memset(carryP[:], 0.0), writes=[carryP_b])
            d8r, d8i = Dre[:, 8, :], Dim[:, 8, :]
            for r in range(NSEG):
                tt_ = lambda o, a, b2, op: k.op("dve", lambda e: e.tensor_tensor(out=o, in0=a, in1=b2, op=op), reads=[xb, mats_b] + Eall_b, writes=[xb])
                tt_(tq[:, 0, :], Xc[:, :, 0], d8r, ALU.mult)
                tt_(tq[:, 1, :], Xc[:, :, 1], d8i, ALU.mult)
                tt_(tq[:, 2, :], Xc[:, :, 1], d8r, ALU.mult)
                tt_(tq[:, 3, :], Xc[:, :, 0], d8i, ALU.mult)
                tt_(tq[:, 0, :], tq[:, 0, :], tq[:, 1, :], ALU.subtract)
                tt_(tq[:, 2, :], tq[:, 2, :], tq[:, 3, :], ALU.add)
                tt_(Xc[:, :, 0], tq[:, 0, :], Eall[:, :, 0, r], ALU.add)
                tt_(Xc[:, :, 1], tq[:, 2, :], Eall[:, :, 1, r], ALU.add)
                k.op("dve", lambda e: e.scalar_tensor_tensor(out=carryP[:, :, :].rearrange("p q r -> p (q r)"), in0=Xc[:, :, :].rearrange("p q r -> p (q r)"),
                                                             scalar=tabs1[:, r + 1:r + 2], in1=carryP[:, :, :].rearrange("p q r -> p (q r)"),
                                                             op0=ALU.mult, op1=ALU.add), reads=[xb, tabs1_b, carryP_b], writes=[carryP_b])
        k.barrier()

    hs = ExitStack()
    hT = sb("hT", [128, KC, T], BF16, hs)
    hT_b = k.bufs(NT, "hT")
    ms = ExitStack()
    tabs = sb("tabs", [128, TAB_W], F32, ms)
    tabs_b = k.buf("tabs")
    k.dma("sp", tabs[:], tabs_d[:, :], writes=[tabs_b])
    cosT = tabs[:, TAB_COS:TAB_SIN].rearrange("p (i f) -> p i f", f=64)
    sinT = tabs[:, TAB_SIN:TAB_MASK].rearrange("p (i f) -> p i f", f=64)
    maskT = tabs[:, TAB_MASK:TAB_QDEC].rearrange("p (h f) -> p h f", f=128)
    qdec = tabs[:, TAB_QDEC:TAB_KDEC]
    kdec = tabs[:, TAB_KDEC:TAB_ONEHOT]
    onehot = tabs[:, TAB_ONEHOT:TAB_W]


    with ExitStack() as s1:
        xt = [sb("xt%d" % i, [128, D], F32, s1) for i in range(2)]
        xt_b = k.bufs(2, "xt")
        xs = [sb("xs%d" % i, [128, D], BF16, s1) for i in range(2)]
        xs_b = k.bufs(2, "xs")
        junk = sb("junk", [128, D], BF16, s1)
        junk_b = k.buf("junk")
        ss = sb("ss", [128, NT], F32, s1)
        rstd = sb("rstd", [128, NT], F32, s1)
        ss_b = k.bufs(NT, "ss")
        rstd_b = k.bufs(NT, "rstd")
        for i in range(NT):
            b = i % 2
            k.dma("sp", xt[b][:], x_d[i * 128:(i + 1) * 128, :], writes=[xt_b[b]])
            k.op("act", lambda e: e.activation(out=junk[:], in_=xt[b][:], func=AF.Square, accum_out=ss[:, i:i + 1]),
                 reads=[xt_b[b]], writes=[junk_b, ss_b[i]])
            k.op("act", lambda e: e.activation(out=rstd[:, i:i + 1], in_=ss[:, i:i + 1], func=AF.Sqrt, scale=1.0 / D, bias=EPS),
                 reads=[ss_b[i]], writes=[rstd_b[i]])
            k.op("dve", lambda e: e.reciprocal(out=rstd[:, i:i + 1], in_=rstd[:, i:i + 1]), reads=[rstd_b[i]], writes=[rstd_b[i]])
            k.op("act", lambda e: e.activation(out=xs[b][:], in_=xt[b][:], func=AF.Copy, scale=rstd[:, i:i + 1]),
                 reads=[xt_b[b], rstd_b[i]], writes=[xs_b[b]])
            for half in range(2):
                pbank = ps[half].bitcast(BF16)
                for j in range(8):
                    kc = half * 8 + j
                    k.op("pe", lambda e: e.transpose(out=pbank[:, j * 128:(j + 1) * 128],
                                                     in_=xs[b][:, kc * 128:(kc + 1) * 128], identity=identb[:]),
                         reads=[xs_b[b], identb_b], writes=[psb[half]])
                k.op("dve", lambda e: e.tensor_tensor(
                    out=hT[:, half * 8:(half + 1) * 8, i * 128:(i + 1) * 128],
                    in0=pbank[:, :].rearrange("p (j t) -> p j t", t=128),
                    in1=nmixT[:, half * 8:(half + 1) * 8].unsqueeze(2).broadcast_to([128, 8, 128]),
                    op=ALU.mult), reads=[psb[half], small_b], writes=[hT_b[i]])

    k.barrier()

    def retention(full, rs):
        Wh = [sb("Wh%d_%d" % (i, full), [128, KC, 512], BF16, rs) for i in range(2)]
        Wh_b = k.bufs(2, "Wh")
        xqk = [sb("xqk%d_%d" % (i, full), [128, 256], F32, rs) for i in range(4)]
        xqk_b = k.bufs(4, "xqk")
        t1 = [sb("t1_%d_%d" % (i, full), [128, 2, 64], F32, rs) for i in range(4)]
        t2 = [sb("t2_%d_%d" % (i, full), [128, 2, 64], F32, rs) for i in range(4)]
        t3 = [sb("t3_%d_%d" % (i, full), [128, 2, 64], F32, rs) for i in range(4)]
        t4 = [sb("t4_%d_%d" % (i, full), [128, 2, 64], F32, rs) for i in range(4)]
        t1_b, t2_b, t3_b, t4_b = k.bufs(4, "t1"), k.bufs(4, "t2"), k.bufs(4, "t3"), k.bufs(4, "t4")
        qkr = [sb("qkr%d_%d" % (i, full), [128, 2, 2, 64], BF16, rs) for i in range(4)]
        qkr1_b, qkr2_b = k.bufs(4, "qkr1"), k.bufs(4, "qkr2")
        vb = [sb("vb%d_%d" % (i, full), [128, 128], BF16, rs) for i in range(4)]
        vb_b = k.bufs(4, "vb")
        sg = [sb("sg%d_%d" % (i, full), [128, 128], F32, rs) for i in range(4)]
        sg_b = k.bufs(4, "sg")
        qd = [sb("qd%d_%d" % (i, full), [128, 128], BF16, rs) for i in range(4)]
        qd_b = k.bufs(4, "qd")
        ktd = [sb("ktd%d_%d" % (i, full), [128, 128], BF16, rs) for i in range(4)]
        ktd_b = k.bufs(4, "ktd")
        qkT = [sb("qkT%d_%d" % (i, full), [128, 384], BF16, rs) for i in range(4)]
        qkT_b = k.bufs(4, "qkT")
        Pm = [sb("Pm%d_%d" % (i, full), [128, 128], BF16, rs) for i in range(4)]
        Pm_b = k.bufs(4, "Pm")
        st6 = [sb("st6_%d_%d" % (i, full), [128, 6], F32, rs) for i in range(4)]
        mv = [sb("mv%d_%d" % (i, full), [128, 4], F32, rs) for i in range(4)]
        st6_b, mv_b = k.bufs(4, "st6"), k.bufs(4, "mv")
        yn = [sb("yn%d_%d" % (i, full), [128, 128], F32, rs) for i in range(4)]
        yn_b = k.bufs(4, "yn")
        yr = [sb("yr%d_%d" % (i, full), [128, 128], BF16, rs) for i in range(4)]
        yr_b = k.bufs(4, "yr")
        yst = [sb("yst%d_%d" % (i, full), [128, T], BF16, rs) for i in range(2)]
        yst_b = k.bufs(2, "yst")
        PJb, PTb, MB = (0, 3), (1, 4), (2, 5)
        t_sc, t_py, t_kv, t_t2 = k.bufs(2, "r_sc"), k.bufs(2, "r_py"), k.bufs(2, "r_kv"), k.bufs(2, "r_t2")

        def load_w(h):
            hb = h % 2
            for j in range(4):
                c0 = j * RW + h * DH
                k.dma("pool", Wh[hb][:, :, j * 128:(j + 1) * 128],
                      w_in_d[:, c0:c0 + 128].rearrange("(kc p) c -> p kc c", p=128), writes=[Wh_b[hb]] if j == 3 else [])
        def load_w_tracked(h):
            hb = h % 2
            for j in range(4):
                if not full and j in (0, 3):
                    continue
                c0 = j * RW + h * DH
                k.dma("pool", Wh[hb][:, :, j * 128:(j + 1) * 128],
                      w_in_d[:, c0:c0 + 128].rearrange("(kc p) c -> p kc c", p=128), writes=[Whj_b[hb][j]])

        Whj_b = [k.bufs(4, "Whj%d_" % i) for i in range(2)]
        def head_body(h):
            hb = h % 2
            wreads = [Whj_b[hb][j] for j in ((0, 1, 2, 3) if full else (1, 2))]
            for i in range(NT):
                b = (h % 2) * 2 + i % 2
                hp = h % 2
                pj = ps[PJb[hp]]
                pjb = psb[PJb[hp]]
                tok = slice(i * 128, (i + 1) * 128)
                c_lo, c_hi = (0, 512) if full else (128, 384)
                for kc in range(KC):
                    k.op("pe", lambda e: e.matmul(pj[:, c_lo:c_hi], hT[:, kc, tok], Wh[hb][:, kc, c_lo:c_hi],
                                                  start=(kc == 0), stop=(kc == KC - 1)),
                         reads=[hT_b[i]] + wreads, writes=[pjb])
                if h == 0 and i == 0 and full:
                    dump("Wh", Wh[hb][:, 3, :], wreads, 512)
                    dump("pj", pj[:, :], [pjb], 512)
                yield
                k.op("act", lambda e: e.copy(out=xqk[b][:, c_lo:256], in_=pj[:, c_lo:256]), reads=[pjb], writes=[xqk_b[b]])
                k.op("act", lambda e: e.copy(out=vb[b][:], in_=pj[:, 256:384]), reads=[pjb], writes=[vb_b[b]])
                if full:
                    k.op("act", lambda e: e.activation(out=sg[b][:], in_=pj[:, 384:512], func=AF.Silu), reads=[pjb], writes=[sg_b[b]])
                yield
                X = xqk[b][:, :].rearrange("p (a c f) -> p a c f", a=2, c=2)
                a0 = 0 if full else 1
                A = X[:, a0:2, 0, :]
                B = X[:, a0:2, 1, :]
                na = 2 - a0
                C = cosT[:, i, :].unsqueeze(1).broadcast_to([128, na, 64])
                Sn = sinT[:, i, :].unsqueeze(1).broadcast_to([128, na, 64])
                k.op("dve", lambda e: e.tensor_tensor(out=t1[b][:, a0:2, :], in0=A, in1=C, op=ALU.mult), reads=[xqk_b[b], tabs_b], writes=[t1_b[b]])
                k.op("dve", lambda e: e.tensor_tensor(out=t2[b][:, a0:2, :], in0=B, in1=Sn, op=ALU.mult), reads=[xqk_b[b], tabs_b], writes=[t2_b[b]])
                k.op("dve", lambda e: e.tensor_tensor(out=qkr[b][:, a0:2, 0, :], in0=t1[b][:, a0:2, :], in1=t2[b][:, a0:2, :], op=ALU.subtract),
                     reads=[t1_b[b], t2_b[b]], writes=[qkr1_b[b]])
                k.op("pool", lambda e: e.tensor_tensor(out=t3[b][:, a0:2, :], in0=A, in1=Sn, op=ALU.mult), reads=[xqk_b[b], tabs_b], writes=[t3_b[b]])
                k.op("pool", lambda e: e.tensor_tensor(out=t4[b][:, a0:2, :], in0=B, in1=C, op=ALU.mult), reads=[xqk_b[b], tabs_b], writes=[t4_b[b]])
                k.op("pool", lambda e: e.tensor_tensor(out=qkr[b][:, a0:2, 1, :], in0=t3[b][:, a0:2, :], in1=t4[b][:, a0:2, :], op=ALU.add),
                     reads=[t3_b[b], t4_b[b]], writes=[qkr2_b[b]])
                if h == 0 and i == 0 and full:
                    dump("xqk", xqk[b][:, :], [xqk_b[b]], 256)
                    dump("qkr", qkr[b][:, :, :, :].rearrange("p a c f -> p (a c f)"), [qkr1_b[b], qkr2_b[b]], 256)
                yield
                qr = qkr[b][:, 0, :, :].rearrange("p c f -> p (c f)")
                kr = qkr[b][:, 1, :, :].rearrange("p c f -> p (c f)")
                k.op("pool", lambda e: e.tensor_scalar(out=ktd[b][:], in0=kr, scalar1=kdec[:, h:h + 1], scalar2=None, op0=ALU.mult),
                     reads=[qkr1_b[b], qkr2_b[b], tabs_b], writes=[ktd_b[b]])
                if full:
                    k.op("act", lambda e: e.activation(out=qd[b][:], in_=qr, func=AF.Copy, scale=qdec[:, h:h + 1]),
                         reads=[qkr1_b[b], qkr2_b[b], tabs_b], writes=[qd_b[b]])
                    pT = ps[PTb[hp]].bitcast(BF16)
                    k.op("pe", lambda e: e.transpose(out=pT[:, 0:128], in_=qr, identity=identb[:]),
                         reads=[qkr1_b[b], qkr2_b[b], identb_b], writes=[psb[PTb[hp]]])
                    k.op("pe", lambda e: e.transpose(out=pT[:, 128:256], in_=qd[b][:], identity=identb[:]),
                         reads=[qd_b[b], identb_b], writes=[psb[PTb[hp]]])
                    k.op("pe", lambda e: e.transpose(out=pT[:, 256:384], in_=kr, identity=identb[:]),
                         reads=[qkr1_b[b], qkr2_b[b], identb_b], writes=[psb[PTb[hp]]])
                    k.op("act", lambda e: e.copy(out=qkT[b][:], in_=pT[:, 0:384]), reads=[psb[PTb[hp]]], writes=[qkT_b[b]])
                    yield
                    k.op("pe", lambda e: e.matmul(ps[MB[hp]][:, 0:128], qkT[b][:, 256:384], qkT[b][:, 0:128], start=True, stop=True),
                         reads=[qkT_b[b]], writes=[t_sc[hp]])
                    k.op("dve", lambda e: e.tensor_tensor(out=Pm[b][:], in0=ps[MB[hp]][:, 0:128], in1=maskT[:, h, :], op=ALU.mult),
                         reads=[t_sc[hp], tabs_b], writes=[Pm_b[b]])
                    if h == 0 and i == 0:
                        dump("qkT", qkT[b][:, :], [qkT_b[b]], 384)
                        dump("Pm", Pm[b][:, :], [Pm_b[b]], 128)
                    k.op("pe", lambda e: e.matmul(ps[MB[hp]][:, 128:256], Pm[b][:], vb[b][:], start=True, stop=False),
                         reads=[Pm_b[b], vb_b[b]], writes=[t_py[hp]])
                    k.op("pe", lambda e: e.matmul(ps[MB[hp]][:, 128:256], qkT[b][:, 128:256], Sbf[:, h, :], start=False, stop=True),
                         reads=[qkT_b[b], Sbf_b[h]], writes=[t_py[hp]])
                yield
                k.op("pe", lambda e: e.matmul(ps[MB[hp]][:, 256:384], ktd[b][:], vb[b][:], start=True, stop=True),
                     reads=[ktd_b[b], vb_b[b]], writes=[t_kv[hp]])
                k.op("dve", lambda e: e.scalar_tensor_tensor(out=S[:, h, :], in0=S[:, h, :], scalar=G128[h], in1=ps[MB[hp]][:, 256:384],
                                                             op0=ALU.mult, op1=ALU.add), reads=[S_b[h], t_kv[hp]], writes=[S_b[h]])
                if full:
                    k.op("act", lambda e: e.copy(out=Sbf[:, h, :], in_=S[:, h, :]), reads=[S_b[h]], writes=[Sbf_b[h]])
                    yield
                    k.op("dve", lambda e: e.bn_stats(out=st6[b][:], in_=ps[MB[hp]][:, 128:256]), reads=[t_py[hp]], writes=[st6_b[b]])
                    k.op("dve", lambda e: e.bn_aggr(out=mv[b][:, 0:2], in_=st6[b][:]), reads=[st6_b[b]], writes=[mv_b[b]])
                    k.op("act", lambda e: e.activation(out=mv[b][:, 2:3], in_=mv[b][:, 1:2], func=AF.Sqrt, bias=EPS, scale=1.0),
                         reads=[mv_b[b]], writes=[mv_b[b]])
                    k.op("dve", lambda e: e.reciprocal(out=mv[b][:, 2:3], in_=mv[b][:, 2:3]), reads=[mv_b[b]], writes=[mv_b[b]])
                    k.op("dve", lambda e: e.tensor_scalar(out=mv[b][:, 3:4], in0=mv[b][:, 0:1], scalar1=mv[b][:, 2:3], scalar2=-1.0,
                                                          op0=ALU.mult, op1=ALU.mult), reads=[mv_b[b]], writes=[mv_b[b]])
                    k.op("act", lambda e: e.activation(out=yn[b][:], in_=ps[MB[hp]][:, 128:256], func=AF.Identity,
                                                       scale=mv[b][:, 2:3], bias=mv[b][:, 3:4]), reads=[t_py[hp], mv_b[b]], writes=[yn_b[b]])
                    if h == 0 and i == 0:
                        dump("mv", mv[b][:, :], [mv_b[b]], 4)
                        dump("yn", yn[b][:, :], [yn_b[b]], 128)
                    k.op("pool", lambda e: e.tensor_tensor(out=yn[b][:], in0=yn[b][:], in1=gnw[:, h * 128:(h + 1) * 128], op=ALU.mult),
                         reads=[yn_b[b], small_b], writes=[yn_b[b]])
                    k.op("pool", lambda e: e.tensor_tensor(out=yr[b][:], in0=yn[b][:], in1=sg[b][:], op=ALU.mult),
                         reads=[yn_b[b], sg_b[b]], writes=[yr_b[b]])
                    yield
                    pT2 = ps[MB[hp]].bitcast(BF16)
                    if h == 0 and i == 0:
                        dump("yr", yr[b][:, :], [yr_b[b]], 128)
                    k.op("pe", lambda e: e.transpose(out=pT2[:, 768:896], in_=yr[b][:], identity=identb[:]),
                         reads=[yr_b[b], identb_b], writes=[t_t2[hp]])
                    k.op("act", lambda e: e.copy(out=yst[hb][:, tok], in_=pT2[:, 768:896]), reads=[t_t2[hp]], writes=[yst_b[hb]])
            if full:
                k.dma("sp", yretT_d[h * 128:(h + 1) * 128, :], yst[hb][:], reads=[yst_b[hb]], writes=[yretT_b[h]])

        load_w_tracked(0)
        load_w_tracked(1)
        for h0 in range(0, H, 2):
            gens = [head_body(h0), head_body(h0 + 1)]
            while gens:
                for gnr in list(gens):
                    try:
                        next(gnr)
                    except StopIteration:
                        gens.remove(gnr)
            if h0 + 2 < H:
                load_w_tracked(h0 + 2)
                load_w_tracked(h0 + 3)

    yretT_b = k.bufs(H, "yretT_d")
    for h in (range(H) if mode != "fused" else ()):
        k.op("dve", lambda e: e.memset(S[:, h, :], 0.0), writes=[S_b[h]])
        k.op("pool", lambda e: e.memset(Sbf[:, h, :], 0.0), writes=[Sbf_b[h]])
    if mode == "states":
        with ExitStack() as rs:
            retention(False, rs)
        k.barrier()


    k.barrier()

    def s5_uproj(us):
        Wu = [sb("Wu%d" % i, [128, KC, 128], BF16, us) for i in range(2)]
        Wu_b = k.bufs(2, "Wu")
        for ct in range(8):
            b = ct % 2
            c0 = 4 * RW + ct * 128
            k.dma("pool", Wu[b][:], w_in_d[:, c0:c0 + 128].rearrange("(kc p) c -> p kc c", p=128), writes=[Wu_b[b]])
            for tb in range(4):
                pb = tb % 2
                for kc in range(KC):
                    k.op("pe", lambda e: e.matmul(ps[pb][:, :], Wu[b][:, kc, :], hT[:, kc, tb * 512:(tb + 1) * 512],
                                                  start=(kc == 0), stop=(kc == KC - 1)),
                         reads=[Wu_b[b]] + hT_b[tb * 4:(tb + 1) * 4], writes=[psb[pb]])
                k.op("act", lambda e: e.copy(out=uT[:, ct, :].rearrange("p (hf s m) -> p hf s m", hf=2, s=8)[:, tb // 2, :, (tb % 2) * 64:(tb % 2) * 64 + 64],
                                             in_=ps[pb][:, :].rearrange("p (m s) -> p s m", s=8)), reads=[psb[pb]],
                     writes=[uT_b[ct * 4 + j] for j in range(4)])

    def s5_pairs(full, ps_):
        RBm = [sb("RBm%d_%d" % (i, full), [128, 8, 2, 32], BF16, ps_) for i in range(2)]
        CPm = [sb("CPm%d_%d" % (i, full), [128, 9, 2, 32], BF16, ps_) for i in range(2)]
        RBm_b, CPm_b = k.bufs(2, "RBm"), k.bufs(2, "CPm")
        RBT = [sb("RBT%d_%d" % (i, full), [32, 16, 128], BF16, ps_) for i in range(2)]
        RBT_b = k.bufs(2, "RBT")
        KT = [sb("KT%d_%d" % (i, full), [32, 8, 32], BF16, ps_) for i in range(2)]
        KT_b = k.bufs(2, "KT")
        uq = [sb("uq%d_%d" % (i, full), [32, T], BF16, ps_) for i in range(2)]
        uq_b = k.bufs(2, "uq")
        XA = [sb("XA%d_%d" % (i, full), [128, 2, 129], F32, ps_) for i in range(2)]
        XB = [sb("XB%d_%d" % (i, full), [128, 2, 129], F32, ps_) for i in range(2)]
        XA_b, XB_b = k.bufs(2, "XA"), k.bufs(2, "XB")
        xp = [sb("xp%d_%d" % (i, full), [128, 2, 128], BF16, ps_) for i in range(2)]
        xp_b = k.bufs(2, "xp")
        yq = [sb("yq%d_%d" % (i, full), [32, 1024], BF16, ps_) for i in range(2)]
        yq_b = k.bufs(2, "yq")
        for i in range(2):
            k.op("pool", lambda e: e.memset(RBm[i][:], 0.0), writes=[RBm_b[i]])
            k.op("pool", lambda e: e.memset(CPm[i][:], 0.0), writes=[CPm_b[i]])
        PTA, PTB, PK, PYA, PYB = 0, 1, 2, 4, 5

        def pair_body(q):
            b = q % 2
            PW = 3 if b == 0 else 6
            ct, ql = q // 4, q % 4
            for gi in range(2):
                prt = slice(gi * 64, gi * 64 + 64)
                csl = slice(gi * 16, gi * 16 + 16)
                k.op("pool", lambda e: e.tensor_copy(out=RBm[b][prt, :, :, csl], in_=RBc[prt, q, :, :, :]), reads=[mats_b], writes=[RBm_b[b]])
                if full:
                    k.op("pool", lambda e: e.tensor_copy(out=CPm[b][prt, :, :, csl], in_=CPc[prt, q, :, :, :]), reads=[mats_b], writes=[CPm_b[b]])
            pTA = ps[PTA].bitcast(BF16)
            pTB = ps[PTB].bitcast(BF16)
            for sx in range(8):
                for ri in range(2):
                    idx = sx * 2 + ri
                    pt, pbb = (pTA, psb[PTA]) if idx < 8 else (pTB, psb[PTB])
                    k.op("pe", lambda e: e.transpose(out=pt[0:32, (idx % 8) * 128:(idx % 8 + 1) * 128], in_=RBm[b][:, sx, ri, :], identity=identb[:]),
                         reads=[RBm_b[b], identb_b], writes=[pbb])
            k.op("act", lambda e: e.copy(out=RBT[b][:, 0:8, :], in_=pTA[0:32, :].rearrange("p (a c) -> p a c", c=128)), reads=[psb[PTA]], writes=[RBT_b[b]])
            k.op("act", lambda e: e.copy(out=RBT[b][:, 8:16, :], in_=pTB[0:32, :].rearrange("p (a c) -> p a c", c=128)), reads=[psb[PTB], RBT_b[b]], writes=[RBT_b[b]])
            if full:
                for kk in range(8):
                    k.op("pe", lambda e: e.matmul(ps[PK][0:32, kk * 32:(kk + 1) * 32], RBm[b][:, 7, 0, :], CPm[b][:, kk, 0, :], start=True, stop=False),
                         reads=[RBm_b[b], CPm_b[b]], writes=[psb[PK]])
                    k.op("pe", lambda e: e.matmul(ps[PK][0:32, kk * 32:(kk + 1) * 32], RBm[b][:, 7, 1, :], CPm[b][:, kk, 1, :], start=False, stop=True),
                         reads=[RBm_b[b], CPm_b[b]], writes=[psb[PK]])
                k.op("act", lambda e: e.copy(out=KT[b][:], in_=ps[PK][0:32, 0:256].rearrange("p (a c) -> p a c", c=32)), reads=[psb[PK]], writes=[KT_b[b]])
            yield
            k.dma("sp", uq[b][:], uT[32 * ql:32 * ql + 32, ct, :], reads=[uT_b[ct * 4 + j] for j in range(4)], writes=[uq_b[b]])
            uqs = uq[b][:, :].rearrange("p (hf s m) -> p hf s m", hf=2, s=8)
            for hf in range(2):
                for ri in range(2):
                    for sx in range(8):
                        k.op("pe", lambda e: e.matmul(ps[PW][:, ri * 128:(ri + 1) * 128], RBT[b][:, sx * 2 + ri, :], uqs[:, hf, sx, :],
                                                      start=(sx == 0), stop=(sx == 7)), reads=[RBT_b[b], uq_b[b]], writes=[psb[PW]])
                yield
                k.op("act", lambda e: e.copy(out=XA[b][:, :, 1:129], in_=ps[PW][:, 0:256].rearrange("p (r m) -> p r m", r=2)),
                     reads=[psb[PW]], writes=[XA_b[b]])
                k.op("dve", lambda e: e.tensor_copy(out=XA[b][:, :, 0], in_=carry[:, q, :]), reads=[carry_b[q], XA_b[b]], writes=[XA_b[b]])
                src, dst, src_b, dst_b = XA[b], XB[b], XA_b[b], XB_b[b]
                N = 129
                for st in range(8):
                    d = 1 << st
                    k.op("pool", lambda e: e.tensor_copy(out=dst[:, :, 0:d], in_=src[:, :, 0:d]), reads=[src_b], writes=[dst_b])
                    dre, dim_, ndim = Dre[:, st, q:q + 1], Dim[:, st, q:q + 1], nDim[:, st, q:q + 1]
                    k.op("dve", lambda e: e.scalar_tensor_tensor(out=dst[:, 0, d:N], in0=src[:, 0, 0:N - d], scalar=dre, in1=src[:, 0, d:N],
                                                                 op0=ALU.mult, op1=ALU.add), reads=[src_b, mats_b, dst_b], writes=[dst_b])
                    k.op("dve", lambda e: e.scalar_tensor_tensor(out=dst[:, 1, d:N], in0=src[:, 1, 0:N - d], scalar=dre, in1=src[:, 1, d:N],
                                                                 op0=ALU.mult, op1=ALU.add), reads=[src_b, mats_b, dst_b], writes=[dst_b])
                    yield
                    k.op("dve", lambda e: e.scalar_tensor_tensor(out=dst[:, 0, d:N], in0=src[:, 1, 0:N - d], scalar=ndim, in1=dst[:, 0, d:N],
                                                                 op0=ALU.mult, op1=ALU.add), reads=[src_b, mats_b, dst_b], writes=[dst_b])
                    k.op("dve", lambda e: e.scalar_tensor_tensor(out=dst[:, 1, d:N], in0=src[:, 0, 0:N - d], scalar=dim_, in1=dst[:, 1, d:N],
                                                                 op0=ALU.mult, op1=ALU.add), reads=[src_b, mats_b, dst_b], writes=[dst_b])
                    yield
                    src, dst, src_b, dst_b = dst, src, dst_b, src_b
                X, X_b = src, src_b
                k.op("dve", lambda e: e.tensor_copy(out=carry[:, q, :], in_=X[:, :, 128]), reads=[X_b], writes=[carry_b[q]])
                yield
                if full:
                    k.op("act", lambda e: e.copy(out=xp[b][:], in_=X[:, :, 0:128]), reads=[X_b], writes=[xp_b[b]])
                    for j in range(8):
                        pyi = PYA if j < 4 else PYB
                        o = ps[pyi][0:32, (j % 4) * 128:(j % 4 + 1) * 128]
                        k.op("pe", lambda e: e.matmul(o, CPm[b][:, j + 1, 0, :], xp[b][:, 0, :], start=True, stop=False),
                             reads=[CPm_b[b], xp_b[b]], writes=[psb[pyi]])
                        k.op("pe", lambda e: e.matmul(o, CPm[b][:, j + 1, 1, :], xp[b][:, 1, :], start=False, stop=False),
                             reads=[CPm_b[b], xp_b[b]], writes=[psb[pyi]])
                        for sx in range(j + 1):
                            k.op("pe", lambda e: e.matmul(o, KT[b][:, j - sx, :], uqs[:, hf, sx, :], start=False, stop=(sx == j)),
                                 reads=[KT_b[b], uq_b[b]], writes=[psb[pyi]])
                    yb = b
                    yq3 = yq[yb][:, :].rearrange("p (m s) -> p m s", s=8)
                    for jj, pyi in ((0, PYA), (1, PYB)):
                        k.op("dve", lambda e: e.scalar_tensor_tensor(
                            out=yq3[:, :, jj * 4:(jj + 1) * 4], in0=uqs[:, hf, jj * 4:(jj + 1) * 4, :].rearrange("p j m -> p m j"), scalar=s5d[0:32, 8 + q:9 + q],
                            in1=ps[pyi][0:32, :].rearrange("p (j m) -> p m j", j=4), op0=ALU.mult, op1=ALU.add),
                            reads=[uq_b[b], psb[pyi], s5d_b, yq_b[yb]], writes=[yq_b[yb]])
                    k.dma("sp", uT[32 * ql:32 * ql + 32, ct, hf * 1024:(hf + 1) * 1024], yq[yb][:], reads=[yq_b[yb]],
                          writes=[uT_b[ct * 4 + hf * 2], uT_b[ct * 4 + hf * 2 + 1]])

        for q0 in range(0, NQ, 2):
            gens = [pair_body(q0), pair_body(q0 + 1)]
            while gens:
                for gnr in list(gens):
                    try:
                        next(gnr)
                    except StopIteration:
                        gens.remove(gnr)

    def s5_post(gs):
        f1 = [sb("g1_%d" % i, [128, 512], F32, gs) for i in range(2)]
        f2 = [sb("g2_%d" % i, [128, 512], F32, gs) for i in range(2)]
        f1_b, f2_b = k.bufs(2, "f1"), k.bufs(2, "f2")
        for ct in range(8):
            for tb in range(4):
                b = (ct * 4 + tb) % 2
                y = uT[:, ct, tb * 512:(tb + 1) * 512]
                yb_ = uT_b[ct * 4 + tb]
                k.op("dve", lambda e: e.tensor_tensor(out=f1[b][:], in0=y, in1=y, op=ALU.mult), reads=[yb_], writes=[f1_b[b]])
                k.op("dve", lambda e: e.tensor_scalar(out=f1[b][:], in0=f1[b][:], scalar1=0.044715, scalar2=1.0, op0=ALU.mult, op1=ALU.add),
                     reads=[f1_b[b]], writes=[f1_b[b]])
                k.op("pool", lambda e: e.tensor_tensor(out=f1[b][:], in0=f1[b][:], in1=y, op=ALU.mult), reads=[f1_b[b], yb_], writes=[f1_b[b]])
                k.op("act", lambda e: e.activation(out=f2[b][:], in_=f1[b][:], func=AF.Sigmoid, scale=1.5957691216057308),
                     reads=[f1_b[b]], writes=[f2_b[b]])
                k.op("pool", lambda e: e.tensor_tensor(out=y, in0=f2[b][:], in1=y, op=ALU.mult), reads=[f2_b[b], yb_], writes=[yb_])
        Wg = [sb("Wg%d" % i, [128, 8, 128], BF16, gs) for i in range(2)]
        Wg_b = k.bufs(2, "Wg")
        og = [sb("og%d" % i, [128, 512], BF16, gs) for i in range(2)]
        og_b = k.bufs(2, "og")
        for co in range(8):
            wbi = co % 2
            k.dma("pool", Wg[wbi][:], w_glu_d[:, co * 128:(co + 1) * 128].rearrange("(kc p) c -> p kc c", p=128), writes=[Wg_b[wbi]])
            for tb in range(4):
                b = (co * 4 + tb) % 2
                for ct in range(8):
                    k.op("pe", lambda e: e.matmul(ps[b][:, :], Wg[wbi][:, ct, :], uT[:, ct, tb * 512:(tb + 1) * 512], start=(ct == 0), stop=(ct == 7)),
                         reads=[Wg_b[wbi], uT_b[ct * 4 + tb]], writes=[psb[b]])
                k.op("act", lambda e: e.activation(out=f2[b][:], in_=ps[b][:, :], func=AF.Sigmoid), reads=[psb[b]], writes=[f2_b[b]])
                k.op("dve", lambda e: e.tensor_tensor(out=og[b][:], in0=f2[b][:], in1=uT[:, co, tb * 512:(tb + 1) * 512], op=ALU.mult),
                     reads=[f2_b[b], uT_b[co * 4 + tb]], writes=[og_b[b]])
                k.dma("sp", yssmT_d[co * 128:(co + 1) * 128, tb * 512:(tb + 1) * 512], og[b][:], reads=[og_b[b]], writes=[yssmT_b[co * 4 + tb]])

    yssmT_b = k.bufs(32, "yssmT")
    k.barrier()
    with ExitStack() as s5s:
        uT = sb("uT", [128, 8, T], BF16, s5s)
        uT_b = k.bufs(32, "uT")
        s5d = sb("s5d", [128, 40], F32, s5s)
        s5d_b = k.buf("s5d")
        k.dma("sp", s5d[:], s5d_d[:, :], writes=[s5d_b])
        Pre = sb("s5Pre", [128, 9, 32], F32, s5s)
        Pim = sb("s5Pim", [128, 9, 32], F32, s5s)
        Dre = sb("s5Dre", [128, 9, 32], F32, s5s)
        Dim = sb("s5Dim", [128, 9, 32], F32, s5s)
        nDim = sb("s5nDim", [128, 9, 32], F32, s5s)
        RBc = sb("s5RBc", [128, 32, 8, 2, 16], BF16, s5s)
        CPc = sb("s5CPc", [128, 32, 9, 2, 16], BF16, s5s)
        carry = sb("s5carry", [128, 32, 2], F32, s5s)
        carry_b = k.bufs(NQ, "carry")
        with ExitStack() as ss:
            mats_b = s5_setup(ss)
        k.barrier()
        if "s5mats" in dbg:
            dump("Pre", Pre[:, :, :].rearrange("p a b -> p (a b)"), [mats_b], 288)
            dump("Pim", Pim[:, :, :].rearrange("p a b -> p (a b)"), [mats_b], 288)
        with ExitStack() as us:
            s5_uproj(us)
        k.barrier()
        if "uT" in dbg:
            dump("uT", uT[:, 0, 0:512], [uT_b[0]], 512)
        for q in range(NQ):
            k.op("dve", lambda e: e.memset(carry[:, q, :], 0.0), writes=[carry_b[q]])
        if mode == "states":
            with ExitStack() as p1:
                s5_pairs(False, p1)
            k.barrier()
            bounce_o = dout("bounce", [128, AGW])
            k.dma("sp", bounce_o[:, 0:1024], S[:, :, :].rearrange("p h e -> p (h e)"), reads=S_b)
            k.dma("sp", bounce_o[:, 1024:AGW], carry[:, :, :].rearrange("p q r -> p (q r)"), reads=carry_b)
        if mode != "states":
            if mode == "fused":
                for q in range(NQ):
                    k.op("dve", lambda e: e.tensor_copy(out=carry[:, q, :], in_=carryP[:, q, :]), reads=[carryP_b], writes=[carry_b[q]])
            elif with_carry:
                with ExitStack() as gsx:
                    gath_d = din("gath", [NCORES * 128, AGW])
                    gb_ = k.buf("gath")
                    G = sb("G", [128, NCORES, AGW], F32, gsx)
                    G_b = k.buf("G")
                    k.dma("sp", G[:], gath_d[:, :].rearrange("(r p) w -> p r w", p=128), reads=[gb_], writes=[G_b])
                    X = sb("Xpre", [128, 1024], F32, gsx)
                    Xc = sb("Xcpre", [128, 32, 2], F32, gsx)
                    tq = sb("tqpre", [128, 4, 32], F32, gsx)
                    xb = k.buf("Xpre")
                    k.op("dve", lambda e: e.memset(X[:], 0.0), writes=[xb])
                    k.op("dve", lambda e: e.memset(Xc[:], 0.0), reads=[xb], writes=[xb])
                    for h in range(H):
                        k.op("dve", lambda e: e.memset(S[:, h, :], 0.0), writes=[S_b[h]])
                    for q in range(NQ):
                        k.op("dve", lambda e: e.memset(carry[:, q, :], 0.0), writes=[carry_b[q]])
                    d8r, d8i = Dre[:, 8, :], Dim[:, 8, :]
                    for r in range(NCORES - 1):
                        for h in range(H):
                            k.op("dve", lambda e: e.scalar_tensor_tensor(out=X[:, h * 128:(h + 1) * 128], in0=X[:, h * 128:(h + 1) * 128], scalar=G2048[h],
                                                                         in1=G[:, r, h * 128:(h + 1) * 128], op0=ALU.mult, op1=ALU.add),
                                 reads=[xb, G_b], writes=[xb])
                        k.op("dve", lambda e: e.scalar_tensor_tensor(out=S[:, :, :].rearrange("p h e -> p (h e)"), in0=X[:], scalar=onehot[:, r + 1:r + 2],
                                                                     in1=S[:, :, :].rearrange("p h e -> p (h e)"), op0=ALU.mult, op1=ALU.add),
                             reads=[xb, tabs_b] + S_b, writes=S_b)
                        Er = G[:, r, 1024:AGW].rearrange("p (q r) -> p q r", r=2)
                        tt_ = lambda o, a, b2, op: k.op("dve", lambda e: e.tensor_tensor(out=o, in0=a, in1=b2, op=op), reads=[xb, G_b, mats_b], writes=[xb])
                        tt_(tq[:, 0, :], Xc[:, :, 0], d8r, ALU.mult)
                        tt_(tq[:, 1, :], Xc[:, :, 1], d8i, ALU.mult)
                        tt_(tq[:, 2, :], Xc[:, :, 1], d8r, ALU.mult)
                        tt_(tq[:, 3, :], Xc[:, :, 0], d8i, ALU.mult)
                        tt_(tq[:, 0, :], tq[:, 0, :], tq[:, 1, :], ALU.subtract)
                        tt_(tq[:, 2, :], tq[:, 2, :], tq[:, 3, :], ALU.add)
                        tt_(Xc[:, :, 0], tq[:, 0, :], Er[:, :, 0], ALU.add)
                        tt_(Xc[:, :, 1], tq[:, 2, :], Er[:, :, 1], ALU.add)
                        k.op("dve", lambda e: e.scalar_tensor_tensor(out=carry[:, :, :].rearrange("p q r -> p (q r)"), in0=Xc[:, :, :].rearrange("p q r -> p (q r)"),
                                                                     scalar=onehot[:, r + 1:r + 2], in1=carry[:, :, :].rearrange("p q r -> p (q r)"),
                                                                     op0=ALU.mult, op1=ALU.add), reads=[xb, tabs_b] + carry_b, writes=carry_b)
                    for h in range(H):
                        k.op("act", lambda e: e.copy(out=Sbf[:, h, :], in_=S[:, h, :]), reads=[S_b[h]], writes=[Sbf_b[h]])
                k.barrier()
            with ExitStack() as p2:
                s5_pairs(True, p2)
            k.barrier()
            if "ypre" in dbg:
                dump("ypre", uT[:, 0, 0:512], [uT_b[0]], 512)
            with ExitStack() as gs:
                s5_post(gs)
            k.barrier()

    if mode != "states":
        with ExitStack() as rs:
            retention(True, rs)
        k.barrier()

        if "yret" in dbg:
            with ExitStack() as sd:
                for h in range(H):
                    tmpb = sb("dbgy_b%d" % h, [128, T], BF16, sd)
                    tmpf = sb("dbgy_f%d" % h, [128, T], F32, sd)
                    tb, tb2 = k.buf(), k.buf()
                    k.dma("sp", tmpb[:], yretT_d[h * 128:(h + 1) * 128, :], reads=[yretT_b[h]], writes=[tb])
                    k.op("act", lambda e: e.copy(out=tmpf[:], in_=tmpb[:]), reads=[tb], writes=[tb2])
                    k.dma("sp", dbg["yret"][h * 128:(h + 1) * 128, :], tmpf[:], reads=[tb2])
            k.barrier()

        if "yssm" in dbg:
            with ExitStack() as sd:
                for co in range(8):
                    tmpb = sb("dbgs_b%d" % co, [128, T], BF16, sd)
                    tmpf = sb("dbgs_f%d" % co, [128, T], F32, sd)
                    tb_, tb2 = k.buf(), k.buf()
                    k.dma("sp", tmpb[:], yssmT_d[co * 128:(co + 1) * 128, :], reads=yssmT_b[co * 4:(co + 1) * 4], writes=[tb_])
                    k.op("act", lambda e: e.copy(out=tmpf[:], in_=tmpb[:]), reads=[tb_], writes=[tb2])
                    k.dma("sp", dbg["yssm"][co * 128:(co + 1) * 128, :], tmpf[:], reads=[tb2])
            k.barrier()


        ms.close()
        k.barrier()
        x1_b = k.bufs(NT, "x1")
        with ExitStack() as gs4:
            yr = sb("m_yr", [128, 8, 1024], BF16, gs4)
            ys = sb("m_ys", [128, 8, 1024], BF16, gs4)
            yr_b4, ys_b4 = k.buf("m_yr"), k.buf("m_ys")
            mT = sb("m_mT", [128, KC, 1024], BF16, gs4)
            mT_b = k.bufs(2, "m_mT")
            wma = [sb("m_wma%d" % i, [128, KC, 128], BF16, gs4) for i in range(2)]
            wmb = [sb("m_wmb%d" % i, [128, KC, 128], BF16, gs4) for i in range(2)]
            wa = [sb("m_wa%d" % i, [128, 8, 128], BF16, gs4) for i in range(2)]
            wb_ = [sb("m_wb%d" % i, [128, 8, 128], BF16, gs4) for i in range(2)]
            wma_b, wmb_b, wa_b, wbb_b = k.bufs(2, "wma"), k.bufs(2, "wmb"), k.bufs(2, "wa"), k.bufs(2, "wb")
            wo = [sb("m_wo%d" % i, [128, KC, 256], BF16, gs4) for i in range(2)]
            wo_b = k.bufs(2, "wo")
            sga = [sb("m_sga%d" % i, [128, 512], F32, gs4) for i in range(2)]
            sgb = [sb("m_sgb%d" % i, [128, 512], F32, gs4) for i in range(2)]
            sga_b, sgb_b = k.bufs(2, "sga"), k.bufs(2, "sgb")
            xo = [sb("m_xo%d" % i, [128, 256], F32, gs4) for i in range(2)]
            oo = [sb("m_oo%d" % i, [128, 256], F32, gs4) for i in range(2)]
            xo_b, oo_b = k.bufs(2, "xo"), k.bufs(2, "oo")

            def load_mw(j):
                b = j % 2
                k.dma("pool", wma[b][:], w_merge_d[:, j * 128:(j + 1) * 128].rearrange("(kc p) c -> p kc c", p=128), writes=[wma_b[b]])
                k.dma("pool", wmb[b][:], w_merge_d[:, D + j * 128:D + (j + 1) * 128].rearrange("(kc p) c -> p kc c", p=128), writes=[wmb_b[b]])
                k.dma("pool", wa[b][:], w_a_d[:, j * 128:(j + 1) * 128].rearrange("(kc p) c -> p kc c", p=128), writes=[wa_b[b]])
                k.dma("pool", wb_[b][:], w_b_d[:, j * 128:(j + 1) * 128].rearrange("(kc p) c -> p kc c", p=128), writes=[wbb_b[b]])

            it = 0
            for hf in range(2):
                t0 = hf * 1024
                k.dma("sp", yr[:], yretT_d[:, t0:t0 + 1024].rearrange("(hc p) t -> p hc t", p=128), reads=yretT_b, writes=[yr_b4])
                k.dma("sp", ys[:], yssmT_d[:, t0:t0 + 1024].rearrange("(hc p) t -> p hc t", p=128), reads=yssmT_b, writes=[ys_b4])
                load_mw(0)
                for j in range(KC):
                    b = j % 2
                    if j + 1 < KC:
                        load_mw(j + 1)
                    for tb in range(2):
                        pbase = 4 * (it % 2)
                        it += 1
                        tsl = slice(t0 + tb * 512, t0 + (tb + 1) * 512)
                        lsl = slice(tb * 512, (tb + 1) * 512)
                        hb_ = hT_b[(t0 + tb * 512) // 128:(t0 + tb * 512) // 128 + 4]
                        for kc in range(KC):
                            k.op("pe", lambda e: e.matmul(ps[pbase][:, :], wma[b][:, kc, :], hT[:, kc, tsl], start=(kc == 0), stop=(kc == KC - 1)),
                                 reads=[wma_b[b]] + hb_, writes=[psb[pbase]])
                        for kc in range(KC):
                            k.op("pe", lambda e: e.matmul(ps[pbase + 1][:, :], wmb[b][:, kc, :], hT[:, kc, tsl], start=(kc == 0), stop=(kc == KC - 1)),
                                 reads=[wmb_b[b]] + hb_, writes=[psb[pbase + 1]])
                        for hc in range(8):
                            k.op("pe", lambda e: e.matmul(ps[pbase + 2][:, :], wa[b][:, hc, :], yr[:, hc, lsl], start=(hc == 0), stop=(hc == 7)),
                                 reads=[wa_b[b], yr_b4], writes=[psb[pbase + 2]])
                        for hc in range(8):
                            k.op("pe", lambda e: e.matmul(ps[pbase + 3][:, :], wb_[b][:, hc, :], ys[:, hc, lsl], start=(hc == 0), stop=(hc == 7)),
                                 reads=[wbb_b[b], ys_b4], writes=[psb[pbase + 3]])
                        sb_i = it % 2
                        k.op("act", lambda e: e.activation(out=sga[sb_i][:], in_=ps[pbase][:, :], func=AF.Sigmoid), reads=[psb[pbase]], writes=[sga_b[sb_i]])
                        k.op("act", lambda e: e.activation(out=sgb[sb_i][:], in_=ps[pbase + 1][:, :], func=AF.Sigmoid), reads=[psb[pbase + 1]], writes=[sgb_b[sb_i]])
                        k.op("dve", lambda e: e.tensor_tensor(out=sga[sb_i][:], in0=sga[sb_i][:], in1=ps[pbase + 2][:, :], op=ALU.mult),
                             reads=[sga_b[sb_i], psb[pbase + 2]], writes=[sga_b[sb_i]])
                        k.op("dve", lambda e: e.tensor_tensor(out=sgb[sb_i][:], in0=sgb[sb_i][:], in1=ps[pbase + 3][:, :], op=ALU.mult),
                             reads=[sgb_b[sb_i], psb[pbase + 3]], writes=[sgb_b[sb_i]])
                        k.op("pool", lambda e: e.tensor_tensor(out=mT[:, j, lsl], in0=sga[sb_i][:], in1=sgb[sb_i][:], op=ALU.add),
                             reads=[sga_b[sb_i], sgb_b[sb_i]], writes=[mT_b[tb]])
                for nb in range(8):
                    wbi = nb % 2
                    k.dma("pool", wo[wbi][:], w_out_d[:, nb * 256:(nb + 1) * 256].rearrange("(kc p) c -> p kc c", p=128), writes=[wo_b[wbi]])
                    for tl in range(8):
                        pb = 2 * ((nb * 8 + tl) % 2)
                        xb_ = (nb * 8 + tl) % 2
                        row0 = t0 + tl * 128
                        k.dma("sp", xo[xb_][:], x_d[row0:row0 + 128, nb * 256:(nb + 1) * 256], writes=[xo_b[xb_]])
                        for j in range(KC):
                            k.op("pe", lambda e: e.matmul(ps[pb][:, 0:256], mT[:, j, tl * 128:(tl + 1) * 128], wo[wbi][:, j, :], start=(j == 0), stop=(j == KC - 1)),
                                 reads=[mT_b[tl // 4], wo_b[wbi]], writes=[psb[pb]])
                        k.op("dve", lambda e: e.tensor_tensor(out=oo[xb_][:], in0=xo[xb_][:], in1=ps[pb][:, 0:256], op=ALU.add),
                             reads=[xo_b[xb_], psb[pb]], writes=[oo_b[xb_]])
                        k.dma("sp", x1_d[row0:row0 + 128, nb * 256:(nb + 1) * 256], oo[xb_][:], reads=[oo_b[xb_]], writes=[x1_w[row0 // 128][nb]])
        hs.close()
        k.barrier()
        if "x1" in dbg:
            with ExitStack() as sd:
                for i in range(NT):
                    tmpf = sb("dbgx1_%d" % i, [128, D], F32, sd)
                    tb_ = k.buf()
                    k.dma("sp", tmpf[:], x1_d[i * 128:(i + 1) * 128, :], reads=x1_w[i], writes=[tb_])
                    k.dma("sp", dbg["x1"][i * 128:(i + 1) * 128, :], tmpf[:], reads=[tb_])
            k.barrier()


        out_d = dout("out", [T, D])
        with ExitStack() as g5:
            st5 = sb("st5", [128, 8, 4], F32, g5)
            st5_b = k.bufs(8, "st5")
            wr = sb("wr", [128, KC, 36], BF16, g5)
            wr_b = k.buf("wr")
            k.dma("pool", wr[:], w_rt_d[:, :].rearrange("(kc p) c -> p kc c", p=128), writes=[wr_b])
            wts = sb("wts", [128, 8, NE], F32, g5)
            wts_b = k.bufs(8, "wts")
            wtsb = sb("wtsb", [128, 8, NE], BF16, g5)
            wtsb_b = k.bufs(8, "wtsb")
            Ab = sb("Ab", [128, 8, 4], BF16, g5)
            Ab_b = k.bufs(8, "Ab")
            rank = sb("rank", [128, 8, 4], F32, g5)
            rank_b = k.bufs(8, "rank")
            rt = sb("rt", [128, 8, 40], F32, g5)
            rl = sb("rl", [128, 4, 36], F32, g5)
            rt_b = k.buf("rt")
            ltso = sb("ltso", [128, 256], BF16, g5)
            iota = sb("iota", [128, CG], F32, g5)
            cst_b = k.buf("moec")
            k.dma("pool", ltso[:], moec_d[:, 0:256], writes=[cst_b])
            k.dma("sp", iota[:], moec_d[:, 256:256 + CG], reads=[cst_b], writes=[cst_b])

            def norm_tile(src, src_b, xs_ap, xs_b, junk_ap, junk_b, stc, stc_b, dst3, dst_b, nT, pbanks):
                k.op("act", lambda e: e.activation(out=junk_ap, in_=src, func=AF.Square, accum_out=stc[:, 0:1]), reads=[src_b], writes=[junk_b, stc_b])
                k.op("act", lambda e: e.activation(out=stc[:, 1:2], in_=stc[:, 0:1], func=AF.Sqrt, scale=1.0 / D, bias=EPS), reads=[stc_b], writes=[stc_b])
                k.op("dve", lambda e: e.reciprocal(out=stc[:, 1:2], in_=stc[:, 1:2]), reads=[stc_b], writes=[stc_b])
                k.op("act", lambda e: e.activation(out=xs_ap, in_=src, func=AF.Copy, scale=stc[:, 1:2]), reads=[src_b, stc_b], writes=[xs_b])
                for half in range(2):
                    pbank = ps[pbanks[half]].bitcast(BF16)
                    for j in range(8):
                        kc = half * 8 + j
                        k.op("pe", lambda e: e.transpose(out=pbank[:, j * 128:(j + 1) * 128], in_=xs_ap[:, kc * 128:(kc + 1) * 128], identity=identb[:]),
                             reads=[xs_b, identb_b], writes=[psb[pbanks[half]]])
                    k.op("dve", lambda e: e.tensor_tensor(out=dst3[:, half * 8:(half + 1) * 8, :], in0=pbank[:, :].rearrange("p (j t) -> p j t", t=128),
                                                          in1=nT[:, half * 8:(half + 1) * 8].unsqueeze(2).broadcast_to([128, 8, 128]), op=ALU.mult),
                         reads=[psb[pbanks[half]], small_b], writes=[dst_b])

            for hf in range(2):
                t0 = hf * 1024
                with ExitStack() as gm:
                    hn = sb("hn%d" % hf, [128, 8, D], BF16, gm)
                    hn_b = k.bufs(8, "hn")
                    hrt = [sb("hrt%d_%d" % (hf, i), [128, KC, 128], BF16, gm) for i in range(2)]
                    hrt_b = k.bufs(2, "hrt")
                    xt5 = [sb("xt5_%d_%d" % (hf, i), [128, D], F32, gm) for i in range(2)]
                    xt5_b = k.bufs(2, "xt5")
                    Gw = sb("Gw%d" % hf, [128, KC, FE], BF16, gm)
                    Uw = sb("Uw%d" % hf, [128, KC, FE], BF16, gm)
                    Dw = sb("Dw%d" % hf, [128, 4, D], BF16, gm)
                    Gw_b, Uw_b, Dw_b = k.buf("Gw"), k.buf("Uw"), k.buf("Dw")
                    sgT = sb("sgT%d" % hf, [128, 4, CG], BF16, gm)
                    aT = sb("aT%d" % hf, [128, 4, CG], BF16, gm)
                    sgT_b, aT_b = k.bufs(4, "sgT"), k.bufs(4, "aT")
                    xg = sb("xg%d" % hf, [128, KC * CG], BF16, gm)
                    xg3 = xg[:, :].rearrange("p (kc c) -> p kc c", c=CG)
                    accgb = xg[:, :].rearrange("p (b d) -> p b d", d=D)
                    xg_b = k.buf("xg")
                    Selg = sb("Selg%d" % hf, [128, 8, CG], BF16, gm)
                    Selg_b = k.bufs(8, "Selg")
                    SelgT = sb("SelgT%d" % hf, [128, 3, 8, 128], BF16, gm)
                    SelgT_b = k.bufs(3, "SelgT")
                    accg = sb("accg%d" % hf, [128, 3, D], F32, gm)
                    accg_b = k.bufs(3, "accg")
                    wtsg = sb("wtsg%d" % hf, [128, 3, 8], F32, gm)
                    wtsg_b = k.buf("wtsg")
                    for tl in range(8):
                        b = tl % 2
                        gt = t0 // 128 + tl
                        k.dma("sp", xt5[b][:], x1_d[gt * 128:(gt + 1) * 128, :], reads=x1_w[gt], writes=[xt5_b[b]])
                        norm_tile(xt5[b][:], xt5_b[b], hn[:, tl, :], hn_b[tl], xg[:, 0:D], xg_b, st5[:, tl, :], st5_b[tl], hrt[b][:, :, :], hrt_b[b], nffnT, (6, 7))
                        R = rt[:, tl, :]
                        for kc in range(KC):
                            k.op("pe", lambda e: e.matmul(ps[5][:, 0:36], hrt[b][:, kc, :], wr[:, kc, :], start=(kc == 0), stop=(kc == KC - 1)),
                                 reads=[hrt_b[b], wr_b], writes=[psb[5]])
                        L = rl[:, tl % 4, :]
                        rb_ = [rt_b]
                        k.op("dve", lambda e: e.tensor_tensor(out=L, in0=ps[5][:, 0:36], in1=rbias, op=ALU.add), reads=[psb[5], small_b] + rb_, writes=rb_)
                        lg, le = L[:, 0:4], L[:, 4:36]
                        gmax, ngmax, gsum, gw, m1, m2, d12, w1, w2 = [R[:, i:i + 1] for i in range(9)]
                        ohg, pen = R[:, 12:16], R[:, 16:20]
                        k.op("dve", lambda e: e.reduce_max(out=gmax, in_=lg, axis=mybir.AxisListType.X), reads=rb_, writes=rb_)
                        k.op("dve", lambda e: e.tensor_scalar(out=ngmax, in0=gmax, scalar1=-1.0, scalar2=None, op0=ALU.mult), reads=rb_, writes=rb_)
                        k.op("act", lambda e: e.activation(out=R[:, 20:24], in_=lg, func=AF.Exp, bias=ngmax, scale=1.0, accum_out=gsum), reads=rb_, writes=rb_)
                        k.op("dve", lambda e: e.reciprocal(out=gw, in_=gsum), reads=rb_, writes=rb_)
                        k.op("dve", lambda e: e.tensor_scalar(out=ohg, in0=lg, scalar1=gmax, scalar2=None, op0=ALU.is_equal), reads=rb_, writes=rb_)
                        k.op("dve", lambda e: e.tensor_scalar(out=pen, in0=ohg, scalar1=-1.0, scalar2=1e30, op0=ALU.add, op1=ALU.mult), reads=rb_, writes=rb_)
                        le3 = le.rearrange("p (g x) -> p g x", x=8)
                        k.op("dve", lambda e: e.tensor_tensor(out=le3, in0=le3, in1=pen.unsqueeze(2).broadcast_to([128, 4, 8]), op=ALU.add), reads=rb_, writes=rb_)
                        k.op("dve", lambda e: e.reduce_max(out=m1, in_=le, axis=mybir.AxisListType.X), reads=rb_, writes=rb_)
                        mk1, mk2 = wts[:, tl, :], L[:, 4:36]
                        k.op("dve", lambda e: e.tensor_scalar(out=mk1, in0=le, scalar1=m1, scalar2=None, op0=ALU.is_equal), reads=rb_, writes=rb_ + [wts_b[tl]])
                        k.op("dve", lambda e: e.scalar_tensor_tensor(out=le, in0=mk1, scalar=-1e30, in1=le, op0=ALU.mult, op1=ALU.add), reads=rb_ + [wts_b[tl]], writes=rb_)
                        k.op("dve", lambda e: e.reduce_max(out=m2, in_=le, axis=mybir.AxisListType.X), reads=rb_, writes=rb_)
                        k.op("dve", lambda e: e.tensor_scalar(out=mk2, in0=le, scalar1=m2, scalar2=None, op0=ALU.is_equal), reads=rb_, writes=rb_)
                        k.op("dve", lambda e: e.tensor_tensor(out=d12, in0=m1, in1=m2, op=ALU.subtract), reads=rb_, writes=rb_)
                        k.op("act", lambda e: e.activation(out=w1, in_=d12, func=AF.Sigmoid), reads=rb_, writes=rb_)
                        k.op("act", lambda e: e.activation(out=w2, in_=d12, func=AF.Sigmoid, scale=-1.0), reads=rb_, writes=rb_)
                        k.op("dve", lambda e: e.tensor_tensor(out=w1, in0=w1, in1=gw, op=ALU.mult), reads=rb_, writes=rb_)
                        k.op("dve", lambda e: e.tensor_tensor(out=w2, in0=w2, in1=gw, op=ALU.mult), reads=rb_, writes=rb_)
                        k.op("dve", lambda e: e.tensor_scalar(out=mk1, in0=mk1, scalar1=w1, scalar2=None, op0=ALU.mult), reads=rb_ + [wts_b[tl]], writes=[wts_b[tl]])
                        k.op("dve", lambda e: e.scalar_tensor_tensor(out=mk1, in0=mk2, scalar=w2, in1=mk1, op0=ALU.mult, op1=ALU.add), reads=rb_ + [wts_b[tl]], writes=[wts_b[tl]])
                        k.op("act", lambda e: e.copy(out=wtsb[:, tl, :], in_=wts[:, tl, :]), reads=[wts_b[tl]], writes=[wtsb_b[tl]])
                        k.op("act", lambda e: e.copy(out=Ab[:, tl, :], in_=ohg), reads=rb_, writes=[Ab_b[tl]])
                    if hf == 0 and "wts" in dbg:
                        dump("wts", wts[:, :, :].rearrange("p a b -> p (a b)"), wts_b, 256)
                    for tl in range(8):
                        k.op("pe", lambda e: e.matmul(ps[5][:, 0:4], ltso[:, 0:128], Ab[:, tl, :], start=True, stop=(tl == 0)), reads=[Ab_b[tl], cst_b], writes=[psb[5]])
                        for tp in range(tl):
                            k.op("pe", lambda e: e.matmul(ps[5][:, 0:4], ltso[:, 128:256], Ab[:, tp, :], start=False, stop=(tp == tl - 1)), reads=[Ab_b[tp], cst_b], writes=[psb[5]])
                        k.op("act", lambda e: e.copy(out=rank[:, tl, :], in_=ps[5][:, 0:4]), reads=[psb[5]], writes=[rank_b[tl]])
                    for g in range(4):
                        for tl in range(8):
                            eng = "dve" if tl % 2 == 0 else "pool"
                            k.op(eng, lambda e: e.tensor_scalar(out=Selg[:, tl, :], in0=iota[:], scalar1=rank[:, tl, g:g + 1], scalar2=rt[:, tl, 12 + g:13 + g],
                                                                op0=ALU.is_equal, op1=ALU.mult), reads=[rank_b[tl], rt_b, cst_b], writes=[Selg_b[tl]])
                        for blk in range(3):
                            bank = ps[blk].bitcast(BF16)
                            for tl in range(8):
                                k.op("pe", lambda e: e.transpose(out=bank[:, tl * 128:(tl + 1) * 128], in_=Selg[:, tl, blk * 128:(blk + 1) * 128], identity=identb[:]),
                                     reads=[Selg_b[tl], identb_b], writes=[psb[blk]])
                            k.op("act", lambda e: e.copy(out=SelgT[:, blk, :, :], in_=bank[:, :].rearrange("p (t c) -> p t c", c=128)), reads=[psb[blk]], writes=[SelgT_b[blk]])
                        for blk in range(3):
                            for tl in range(8):
                                k.op("pe", lambda e: e.matmul(ps[3][:, blk * 8:(blk + 1) * 8], Selg[:, tl, blk * 128:(blk + 1) * 128], wtsb[:, tl, g * 8:(g + 1) * 8],
                                                              start=(tl == 0), stop=(tl == 7)), reads=[Selg_b[tl], wtsb_b[tl]], writes=[psb[3]])
                        k.op("act", lambda e: e.copy(out=wtsg[:, :, :], in_=ps[3][:, 0:24].rearrange("p (b x) -> p b x", x=8)), reads=[psb[3]], writes=[wtsg_b])
                        for r4 in range(4):
                            for kcl in range(4):
                                kc = r4 * 4 + kcl
                                bank = 4 + kcl
                                for tl in range(8):
                                    k.op("pe", lambda e: e.matmul(ps[bank][:, 0:CG], hn[:, tl, kc * 128:(kc + 1) * 128], Selg[:, tl, :], start=(tl == 0), stop=(tl == 7)),
                                         reads=[hn_b[tl], Selg_b[tl]], writes=[psb[bank]])
                                if kcl % 2 == 0:
                                    k.op("act", lambda e: e.activation(out=xg3[:, kc, :], in_=ps[bank][:, 0:CG], func=AF.Copy, scale=nffnT[:, kc:kc + 1]),
                                         reads=[psb[bank], small_b, xg_b], writes=[xg_b])
                                else:
                                    k.op("dve", lambda e: e.tensor_scalar(out=xg3[:, kc, :], in0=ps[bank][:, 0:CG], scalar1=nffnT[:, kc:kc + 1], scalar2=None, op0=ALU.mult),
                                         reads=[psb[bank], small_b, xg_b], writes=[xg_b])
                        for blk in range(3):
                            k.op("pool", lambda e: e.memset(accg[:, blk, :], 0.0), writes=[accg_b[blk]])
                        for el in range(8):
                            ex = g * 8 + el
                            k.dma("pool", Gw[:], w_eg_d[ex].rearrange("(kc p) f -> p kc f", p=128), writes=[Gw_b])
                            k.dma("pool", Uw[:], w_eu_d[ex].rearrange("(kc p) f -> p kc f", p=128), writes=[Uw_b])
                            k.dma("pool", Dw[:], w_ed_d[ex].rearrange("(fc p) d -> p fc d", p=128), writes=[Dw_b])
                            for ft in range(4):
                                for kc in range(KC):
                                    k.op("pe", lambda e: e.matmul(ps[ft][:, 0:CG], Gw[:, kc, ft * 128:(ft + 1) * 128], xg3[:, kc, :], start=(kc == 0), stop=(kc == KC - 1)),
                                         reads=[Gw_b, xg_b], writes=[psb[ft]])
                                k.op("act", lambda e: e.activation(out=sgT[:, ft, :], in_=ps[ft][:, 0:CG], func=AF.Silu), reads=[psb[ft]], writes=[sgT_b[ft]])
                            for ft in range(4):
                                for kc in range(KC):
                                    k.op("pe", lambda e: e.matmul(ps[4 + ft][:, 0:CG], Uw[:, kc, ft * 128:(ft + 1) * 128], xg3[:, kc, :], start=(kc == 0), stop=(kc == KC - 1)),
                                         reads=[Uw_b, xg_b], writes=[psb[4 + ft]])
                                k.op("dve", lambda e: e.tensor_tensor(out=aT[:, ft, :], in0=sgT[:, ft, :], in1=ps[4 + ft][:, 0:CG], op=ALU.mult),
                                     reads=[psb[4 + ft], sgT_b[ft]], writes=[aT_b[ft]])
                            cnt = 0
                            for blk in range(3):
                                for nb in range(4):
                                    pb = cnt % 4
                                    cnt += 1
                                    for fc in range(4):
                                        k.op("pe", lambda e: e.matmul(ps[pb][:, :], aT[:, fc, blk * 128:(blk + 1) * 128], Dw[:, fc, nb * 512:(nb + 1) * 512],
                                                                      start=(fc == 0), stop=(fc == 3)), reads=[Dw_b] + aT_b, writes=[psb[pb]])
                                    k.op("dve", lambda e: e.scalar_tensor_tensor(out=accg[:, blk, nb * 512:(nb + 1) * 512], in0=ps[pb][:, :], scalar=wtsg[:, blk, el:el + 1],
                                                                                 in1=accg[:, blk, nb * 512:(nb + 1) * 512], op0=ALU.mult, op1=ALU.add),
                                         reads=[psb[pb], wtsg_b, accg_b[blk]], writes=[accg_b[blk]])
                        for blk in range(3):
                            k.op("act", lambda e: e.copy(out=accgb[:, blk, :], in_=accg[:, blk, :]), reads=[accg_b[blk], xg_b], writes=[xg_b])
                        cnt = 0
                        for tl in range(8):
                            b = tl % 2
                            gt = t0 // 128 + tl
                            k.dma("sp", xt5[b][:], x1_d[gt * 128:(gt + 1) * 128, :], reads=x1_w[gt], writes=[xt5_b[b]])
                            for nb in range(4):
                                pb = 4 + cnt % 4
                                cnt += 1
                                for blk in range(3):
                                    k.op("pe", lambda e: e.matmul(ps[pb][:, :], SelgT[:, blk, tl, :], accgb[:, blk, nb * 512:(nb + 1) * 512], start=(blk == 0), stop=(blk == 2)),
                                         reads=[SelgT_b[blk], xg_b], writes=[psb[pb]])
                                k.op("dve", lambda e: e.tensor_tensor(out=xt5[b][:, nb * 512:(nb + 1) * 512], in0=xt5[b][:, nb * 512:(nb + 1) * 512], in1=ps[pb][:, :], op=ALU.add),
                                     reads=[psb[pb], xt5_b[b]], writes=[xt5_b[b]])
                            k.dma("sp", x1_d[gt * 128:(gt + 1) * 128, :], xt5[b][:], reads=[xt5_b[b]], writes=x1_w[gt])
                k.barrier()
                gpx = ExitStack()
                acc = sb("acc%d" % hf, [128, 8, D], F32, gpx)
                acc_b = k.bufs(8, "acc")
                hh = sb("hh%d" % hf, [128, KC, 1024], BF16, gpx)
                hh_b = k.bufs(8, "hh")
                xs5 = sb("xs5_%d" % hf, [128, D], BF16, gpx)
                junk5 = sb("junk5_%d" % hf, [128, D], BF16, gpx)
                xs5_b, junk5_b = k.buf("xs5"), k.buf("junk5")
                for tl in range(8):
                    k.dma("sp", acc[:, tl, :], x1_d[t0 + tl * 128:t0 + (tl + 1) * 128, :], reads=x1_w[(t0 // 128) + tl], writes=[acc_b[tl]])
                if hf == 0 and "x2" in dbg:
                    for tl in range(8):
                        k.dma("sp", dbg["x2"][tl * 128:(tl + 1) * 128, :], acc[:, tl, :], reads=[acc_b[tl]])

                def norm_to_hh(tl, nT):
                    norm_tile(acc[:, tl, :], acc_b[tl], xs5[:], xs5_b, junk5[:], junk5_b, st5[:, tl, :], st5_b[tl],
                              hh[:, :, tl * 128:(tl + 1) * 128], hh_b[tl], nT, (6, 7))
                with ExitStack() as gp:
                    pT = sb("pT%d" % hf, [128, 2, 1024], BF16, gp)
                    pT_b = k.bufs(8, "pT")
                    pld = [sb("pld%d_%d" % (hf, i), [128, 256], F32, gp) for i in range(2)]
                    plb = [sb("plb%d_%d" % (hf, i), [128, 256], BF16, gp) for i in range(2)]
                    pld_b, plb_b = k.bufs(2, "pld"), k.bufs(2, "plb")
                    wpg = [sb("wpg%d_%d" % (hf, i), [128, KC, 256], BF16, gp) for i in range(2)]
                    wpl = [sb("wpl%d_%d" % (hf, i), [128, 2, 256], BF16, gp) for i in range(2)]
                    wpg_b, wpl_b = k.bufs(2, "wpg"), k.bufs(2, "wpl")
                    sg5 = [sb("sg5_%d_%d" % (hf, i), [128, 256], F32, gp) for i in range(2)]
                    sg5_b = k.bufs(2, "sg5")
                    nfb = sb("nfb%d" % hf, [128, D], F32, gp)
                    nfb_b = k.buf("nfb")
                    k.dma("sp", nfb[:], nfb_d[:, :], writes=[nfb_b])
                    ot = [sb("ot%d_%d" % (hf, i), [128, D], F32, gp) for i in range(2)]
                    ot_b = k.bufs(2, "ot")
                    for tl in range(8):
                        b = tl % 2
                        norm_to_hh(tl, npleT)
                        k.dma("sp", pld[b][:], p_d[t0 + tl * 128:t0 + (tl + 1) * 128, :], writes=[pld_b[b]])
                        k.op("act", lambda e: e.copy(out=plb[b][:], in_=pld[b][:]), reads=[pld_b[b]], writes=[plb_b[b]])
                        pbank = ps[5].bitcast(BF16)
                        for c2 in range(2):
                            k.op("pe", lambda e: e.transpose(out=pbank[:, c2 * 128:(c2 + 1) * 128], in_=plb[b][:, c2 * 128:(c2 + 1) * 128], identity=identb[:]),
                                 reads=[plb_b[b], identb_b], writes=[psb[5]])
                        k.op("act", lambda e: e.copy(out=pT[:, :, tl * 128:(tl + 1) * 128], in_=pbank[:, 0:256].rearrange("p (c t) -> p c t", t=128)),
                             reads=[psb[5]], writes=[pT_b[tl]])
                    cnt = 0
                    for nb in range(8):
                        wbi = nb % 2
                        k.dma("pool", wpg[wbi][:], w_pg_d[:, nb * 256:(nb + 1) * 256].rearrange("(kc p) c -> p kc c", p=128), writes=[wpg_b[wbi]])
                        k.dma("pool", wpl[wbi][:], w_ple_d[:, nb * 256:(nb + 1) * 256].rearrange("(kc p) c -> p kc c", p=128), writes=[wpl_b[wbi]])
                        for tl in range(8):
                            pb = 2 * (cnt % 2)
                            sb_i = cnt % 2
                            cnt += 1
                            for kc in range(KC):
                                k.op("pe", lambda e: e.matmul(ps[pb][:, 0:256], hh[:, kc, tl * 128:(tl + 1) * 128], wpg[wbi][:, kc, :], start=(kc == 0), stop=(kc == KC - 1)),
                                     reads=[hh_b[tl], wpg_b[wbi]], writes=[psb[pb]])
                            for c2 in range(2):
                                k.op("pe", lambda e: e.matmul(ps[pb + 1][:, 0:256], pT[:, c2, tl * 128:(tl + 1) * 128], wpl[wbi][:, c2, :], start=(c2 == 0), stop=(c2 == 1)),
                                     reads=[pT_b[tl], wpl_b[wbi]], writes=[psb[pb + 1]])
                            k.op("act", lambda e: e.activation(out=sg5[sb_i][:], in_=ps[pb][:, 0:256], func=AF.Sigmoid), reads=[psb[pb]], writes=[sg5_b[sb_i]])
                            k.op("dve", lambda e: e.tensor_tensor(out=sg5[sb_i][:], in0=sg5[sb_i][:], in1=ps[pb + 1][:, 0:256], op=ALU.mult),
                                 reads=[sg5_b[sb_i], psb[pb + 1]], writes=[sg5_b[sb_i]])
                            k.op("pool", lambda e: e.tensor_tensor(out=acc[:, tl, nb * 256:(nb + 1) * 256], in0=acc[:, tl, nb * 256:(nb + 1) * 256], in1=sg5[sb_i][:], op=ALU.add),
                                 reads=[sg5_b[sb_i], acc_b[tl]], writes=[acc_b[tl]])
                    for tl in range(8):
                        b = tl % 2
                        k.op("act", lambda e: e.activation(out=junk5[:], in_=acc[:, tl, :], func=AF.Square, accum_out=st5[:, tl, 2:3]),
                             reads=[acc_b[tl]], writes=[junk5_b, st5_b[tl]])
                        k.op("act", lambda e: e.activation(out=st5[:, tl, 3:4], in_=st5[:, tl, 2:3], func=AF.Sqrt, scale=1.0 / D, bias=EPS),
                             reads=[st5_b[tl]], writes=[st5_b[tl]])
                        k.op("dve", lambda e: e.reciprocal(out=st5[:, tl, 3:4], in_=st5[:, tl, 3:4]), reads=[st5_b[tl]], writes=[st5_b[tl]])
                        k.op("dve", lambda e: e.scalar_tensor_tensor(out=ot[b][:], in0=acc[:, tl, :], scalar=st5[:, tl, 3:4], in1=nfb[:], op0=ALU.mult, op1=ALU.mult),
                             reads=[acc_b[tl], st5_b[tl], nfb_b], writes=[ot_b[b]])
                        k.dma("sp", out_d[t0 + tl * 128:t0 + (tl + 1) * 128, :], ot[b][:], reads=[ot_b[b]])
                k.barrier()
                gpx.close()

    else:
        ms.close()
        hs.close()
    k.finish()
    cs.close()
    es.close()
    return nc


def prefix_tables(core):
    half = DH // 2
    inv_freq = (np.float32(10000.0) ** (-np.arange(half, dtype=np.float32) / np.float32(half))).astype(np.float32)
    npos = NPT * 128
    pos = np.arange(npos).astype(np.float32)
    ang = (pos[:, None] * inv_freq[None, :]).astype(np.float32).astype(np.float64)
    ropep = np.concatenate([np.cos(ang), np.sin(ang)], axis=1).astype(np.float32).reshape(NPT, 128, 128)
    g = _gammas()
    t0 = core * T
    t = np.arange(npos)
    dist = (t0 - 1 - t).astype(np.float64)
    kd = np.zeros((npos, H), np.float64)
    valid = t < t0
    for h in range(H):
        kd[valid, h] = g[h] ** dist[valid] * DH ** -0.5
    kdecp = kd.reshape(NPT, 128, H).transpose(1, 0, 2).reshape(128, NPT * H).astype(np.float32)
    return np.ascontiguousarray(ropep), np.ascontiguousarray(kdecp)


def s5_host_layout(inp):
    def st(a):
        return a.reshape(32, 2, 64).transpose(1, 2, 0).reshape(128, 32)
    lamre = st(inp["ssm_lam_re"][0])
    lamim = st(inp["ssm_lam_im"][0])
    logdt = st(np.broadcast_to(inp["ssm_log_dt"][0][:, None], (64, 64)))
    def stb(a):
        return a.reshape(32, 2, 64, 16).transpose(1, 2, 0, 3).reshape(128, 32 * 16)
    bre = stb(inp["ssm_b_re"][0])
    bim = stb(inp["ssm_b_im"][0])
    cre = stb(inp["ssm_c_re"][0].transpose(0, 2, 1))
    cim = stb(inp["ssm_c_im"][0].transpose(0, 2, 1))
    s5p = np.ascontiguousarray(np.concatenate([lamre, lamim, logdt, bre, bim, cre, cim], axis=1).astype(np.float32))
    d = inp["ssm_d"][0]
    dfull = d.reshape(8, 128).T
    dpair = np.zeros((128, 32), np.float32)
    dpair[0:32, :] = d.reshape(32, 32).T
    s5d = np.ascontiguousarray(np.concatenate([dfull, dpair], axis=1).astype(np.float32))
    return s5p, s5d


def _bf16(a):
    return np.ascontiguousarray(a).astype(ml_dtypes.bfloat16)


def make_in_maps(inp, fused=False):
    x = np.ascontiguousarray(inp["x"][0])
    nmixT = np.ascontiguousarray(inp["norm_mix"][0].reshape(KC, 128).T)
    gnw_b = np.broadcast_to(inp["ret_gn_w"][0][None, :], (128, RW))
    nffnT = inp["norm_ffn"][0].reshape(KC, 128).T
    npleT = inp["norm_ple"][0].reshape(KC, 128).T
    rb = np.broadcast_to(np.concatenate([inp["b_router_group"][0], inp["b_router_expert"][0]])[None, :], (128, 36))
    small = np.ascontiguousarray(np.concatenate([nmixT, gnw_b, nffnT, npleT, rb], axis=1).astype(np.float32))
    ident = np.eye(128, dtype=np.float32)
    s5p, s5d = s5_host_layout(inp)
    ar = np.arange(128)
    lts = (ar[:, None] < ar[None, :]).astype(np.float32)
    moec = np.ascontiguousarray(np.concatenate([lts, np.ones((128, 128), np.float32),
                                                np.broadcast_to(np.arange(CG, dtype=np.float32)[None, :], (128, CG))], axis=1))
    maps = []
    for c in range(NCORES):
        maps.append({
            "x": np.ascontiguousarray(x[c * T:(c + 1) * T]),
            "w_in": np.ascontiguousarray(inp["w_in"][0]),
            "tabs": host_tables(c),
            "small": small,
            "identb": ident,
            "s5p": s5p,
            "s5d": s5d,
            "w_glu": np.ascontiguousarray(inp["w_glu"][0]),
            "w_merge": np.ascontiguousarray(inp["w_merge"][0]),
            "w_a": np.ascontiguousarray(inp["w_branch_a"][0]),
            "w_b": np.ascontiguousarray(inp["w_branch_b"][0]),
            "w_out": np.ascontiguousarray(inp["w_out"][0]),
            "w_rt": np.ascontiguousarray(np.concatenate([inp["w_router_group"][0], inp["w_router_expert"][0]], axis=1)),
            "w_eg": np.ascontiguousarray(inp["w_exp_gate"][0]),
            "w_eu": np.ascontiguousarray(inp["w_exp_up"][0]),
            "w_ed": np.ascontiguousarray(inp["w_exp_down"][0]),
            "w_pg": np.ascontiguousarray(inp["w_ple_gate"][0]),
            "w_ple": np.ascontiguousarray(inp["w_ple"][0]),
            "p": np.ascontiguousarray(inp["p"][0, 0][c * T:(c + 1) * T]),
            "nfb": np.ascontiguousarray(np.broadcast_to(inp["norm_f"][None, :], (128, D))),
            "moec": moec,
        })
        if fused:
            ropep, kdecp = prefix_tables(c)
            maps[-1]["xfull"] = x
            maps[-1]["ropep"] = ropep
            maps[-1]["kdecp"] = kdecp
    return maps


def kernel(**inp):
    maps = make_in_maps(inp, fused=True)
    nc = build(mode="fused")
    res = run_bass_kernel_spmd(nc, maps, core_ids=list(range(NCORES)))
    return np.concatenate([r["out"] for r in res.results], axis=0)[None]
```

```python
import numpy as np
import ml_dtypes
from contextlib import ExitStack
import concourse.bass as bass
import concourse.mybir as mybir
from concourse.bass_utils import run_bass_kernel_spmd

F32 = mybir.dt.float32
BF16 = mybir.dt.bfloat16
AF = mybir.ActivationFunctionType
ALU = mybir.AluOpType

NCORES = 8
SEQ = 16384
T = SEQ // NCORES
NT = T // 128
D = 2048
KC = D // 128
H = 8
DH = 128
RW = 1024
EPS = 1e-6
PI = float(np.pi)


class Buf:
    __slots__ = ("name", "w", "r")

    def __init__(self, name):
        self.name = name
        self.w = None
        self.r = {}


class K:
    NDSEM = 24

    def __init__(self, nc, es):
        self.nc = nc
        self.es = es
        self.eng = {"pe": nc.tensor, "act": nc.scalar, "dve": nc.vector, "pool": nc.gpsimd, "sp": nc.sync}
        self.sem = {n: es.enter_context(nc.semaphore("s_" + n)) for n in self.eng}
        self.cnt = {n: 0 for n in self.eng}
        self.known = {n: {} for n in self.eng}
        self.dsem = {q: [es.enter_context(nc.semaphore("d_%s%d" % (q, i))) for i in range(self.NDSEM)]
                     for q in ("sp", "pool", "act")}
        self.duse = {q: [0] * self.NDSEM for q in self.dsem}
        self.dnext = {q: 0 for q in self.dsem}
        self.nbuf = 0

    def buf(self, name=None):
        self.nbuf += 1
        return Buf(name or ("b%d" % self.nbuf))

    def bufs(self, n, name="b"):
        return [self.buf("%s%d" % (name, i)) for i in range(n)]

    def _semof(self, key):
        return self.sem[key] if isinstance(key, str) else self.dsem[key[1]][key[2]]

    def _wait(self, e, deps):
        need = {}
        for t in deps:
            if t is None:
                continue
            k, v = t
            if need.get(k, 0) < v:
                need[k] = v
        kn = self.known[e]
        for k, v in need.items():
            if kn.get(k, 0) >= v:
                continue
            self.eng[e].wait_ge(self._semof(k), v)
            kn[k] = v

    SAME_ENGINE_WAIT = True

    def _deps(self, e, reads, writes):
        deps = []
        skip_same = (e == "pe") or (not self.SAME_ENGINE_WAIT)
        for b in reads:
            if b.w is not None and not (skip_same and b.w[0] == e):
                deps.append(b.w)
        for b in writes:
            if b.w is not None and not (skip_same and b.w[0] == e):
                deps.append(b.w)
            for k, v in b.r.items():
                if k == e:
                    continue
                deps.append((k, v))
        return deps

    def op(self, e, fn, reads=(), writes=()):
        self._wait(e, self._deps(e, reads, writes))
        ins = fn(self.eng[e])
        self.cnt[e] += 1
        c = self.cnt[e]
        ins.then_inc(self.sem[e], 1)
        for b in reads:
            if b.r.get(e, 0) < c:
                b.r[e] = c
        for b in writes:
            b.w = (e, c)
            b.r = {}
        return ins

    def dma(self, q, out, in_, reads=(), writes=()):
        deps = self._deps(q, reads, writes)
        idx = self.dnext[q]
        self.dnext[q] = (idx + 1) % self.NDSEM
        key = ("d", q, idx)
        if self.duse[q][idx] > 0:
            deps.append((key, 16 * self.duse[q][idx]))
        self._wait(q, deps)
        ins = self.eng[q].dma_start(out=out, in_=in_)
        self.duse[q][idx] += 1
        v = 16 * self.duse[q][idx]
        ins.then_inc(self.dsem[q][idx], 16)
        for b in reads:
            if b.r.get(key, 0) < v:
                b.r[key] = v
        for b in writes:
            b.w = (key, v)
            b.r = {}
        return ins

    def qop(self, q, fn, reads=(), writes=()):
        deps = self._deps(q, reads, writes)
        idx = self.dnext[q]
        self.dnext[q] = (idx + 1) % self.NDSEM
        key = ("d", q, idx)
        if self.duse[q][idx] > 0:
            deps.append((key, 16 * self.duse[q][idx]))
        self._wait(q, deps)
        ins = fn(self.eng[q])
        self.duse[q][idx] += 1
        v = 16 * self.duse[q][idx]
        ins.then_inc(self.dsem[q][idx], 16)
        for b in reads:
            if b.r.get(key, 0) < v:
                b.r[key] = v
        for b in writes:
            b.w = (key, v)
            b.r = {}
        return ins

    def barrier(self):
        deps = [(n, c) for n, c in self.cnt.items() if c > 0]
        for q in self.dsem:
            for i, u in enumerate(self.duse[q]):
                if u > 0:
                    deps.append((("d", q, i), 16 * u))
        for e in self.eng:
            self._wait(e, [d for d in deps if d[0] != e])

    def finish(self):
        deps = [(n, c) for n, c in self.cnt.items() if c > 0 and n != "sp"]
        for q in self.dsem:
            for i, u in enumerate(self.duse[q]):
                if u > 0:
                    deps.append((("d", q, i), 16 * u))
        self._wait("sp", deps)


def _gammas():
    return 1.0 - np.exp2(-5.0 - np.arange(H, dtype=np.float64))


def host_tables(core):
    half = DH // 2
    inv_freq = (np.float32(10000.0) ** (-np.arange(half, dtype=np.float32) / np.float32(half))).astype(np.float32)
    pos = (core * T + np.arange(T)).astype(np.float32)
    ang = (pos[:, None] * inv_freq[None, :]).astype(np.float32).astype(np.float64)
    cos = np.cos(ang).astype(np.float32).reshape(NT, 128, half).transpose(1, 0, 2)
    sin = np.sin(ang).astype(np.float32).reshape(NT, 128, half).transpose(1, 0, 2)
    g = _gammas()
    a = np.arange(128)
    ka, qb = a[:, None], a[None, :]
    same = (ka // 64) == (qb // 64)
    earlier = (ka // 64) < (qb // 64)
    mask = np.zeros((128, H, 128), np.float64)
    for h in range(H):
        m = np.where(same, g[h] ** np.abs(qb - ka), np.where(earlier, g[h] ** (qb - ka).clip(0), 0.0))
        mask[:, h, :] = m * DH ** -0.5
    qdec = np.stack([g[h] ** (a + 1.0) for h in range(H)], axis=1)
    kdec = np.stack([g[h] ** (127.0 - a) * DH ** -0.5 for h in range(H)], axis=1)
    onehot = np.zeros((128, NCORES), np.float64)
    onehot[:, core] = 1.0
    tabs = np.concatenate([cos.reshape(128, -1), sin.reshape(128, -1), mask.reshape(128, -1), qdec, kdec, onehot],
                          axis=1).astype(np.float32)
    return np.ascontiguousarray(tabs)


TAB_COS = 0
TAB_SIN = TAB_COS + NT * 64
TAB_MASK = TAB_SIN + NT * 64
TAB_QDEC = TAB_MASK + H * 128
TAB_KDEC = TAB_QDEC + H
TAB_ONEHOT = TAB_KDEC + H
TAB_W = TAB_ONEHOT + NCORES
S5P_W = 96 + 4 * 512
NQ = 32
AGW = 1024 + 64
SMALL_W = 16 + 1024 + 16 + 16 + 36
NE = 32
NPT = (NCORES - 1) * NT
CG = 384
FE = 512


class _Done(Exception):
    pass


def build(debug=(), mode="single"):
    with_carry = mode in ("states", "main", "fused")
    nc = bass.Bass("TRN2", target_bir_lowering=False)
    es = ExitStack()
    k = K(nc, es)
    G128 = [float(x) for x in _gammas() ** 128]
    G2048 = [float(x) for x in _gammas() ** 2048]

    def din(name, shape, dt=F32):
        return nc.dram_tensor(name, list(shape), dt, kind="ExternalInput").ap()

    def dout(name, shape, dt=F32):
        return nc.dram_tensor(name, list(shape), dt, kind="ExternalOutput").ap()

    def dscr(name, shape, dt=F32):
        return nc.dram_tensor(name, list(shape), dt, kind="Internal").ap()

    def sb(name, shape, dt=F32, stack=None):
        return (stack or es).enter_context(nc.sbuf_tensor("sb_" + name, list(shape), dt))

    x_d = din("x", [T, D])
    if mode == "fused":
        xfull_d = din("xfull", [SEQ, D])
        ropep_d = din("ropep", [NPT, 128, 128])
        kdecp_d = din("kdecp", [128, NPT * H])
    w_in_d = din("w_in", [D, 5120])
    tabs_d = din("tabs", [128, TAB_W])
    small_d = din("small", [128, SMALL_W])
    identb_d = din("identb", [128, 128])
    s5p_d = din("s5p", [128, S5P_W])
    s5d_d = din("s5d", [128, 8 + 32])
    w_glu_d = din("w_glu", [RW, RW])
    w_merge_d = din("w_merge", [D, 2 * D])
    w_a_d = din("w_a", [RW, D])
    w_b_d = din("w_b", [RW, D])
    w_out_d = din("w_out", [D, D])
    w_rt_d = din("w_rt", [D, 36])
    w_eg_d = din("w_eg", [NE, D, FE])
    w_eu_d = din("w_eu", [NE, D, FE])
    w_ed_d = din("w_ed", [NE, FE, D])
    w_pg_d = din("w_pg", [D, D])
    w_ple_d = din("w_ple", [256, D])
    p_d = din("p", [T, 256])
    nfb_d = din("nfb", [128, D])
    moec_d = din("moec", [128, 256 + CG])
    dbg = {}
    for nm, shp in debug:
        dbg[nm] = dout("dbg_" + nm, shp)

    yretT_d = dscr("yretT", [RW, T], BF16)
    yssmT_d = dscr("yssmT", [RW, T], BF16)
    x1_d = dscr("x1", [T, D])
    x1_w = [k.bufs(8, "x1w%d_" % i) for i in range(NT)]
    dcount = [0]
    dtmps = [sb("dump%d" % i, [128, 512], F32) for i in range(len(debug))]

    def dump(name, src, src_bufs, cols):
        if name not in dbg or dbg[name] is None:
            return
        tmp = dtmps[dcount[0]][:, 0:cols]
        dcount[0] += 1
        tb = k.buf()
        k.op("act", lambda e: e.copy(out=tmp, in_=src), reads=src_bufs, writes=[tb])
        k.dma("sp", dbg[name][:, :], tmp, reads=[tb])
        dbg[name] = None

    ps = [es.enter_context(nc.psum_tensor("ps%d" % i, [128, 512], F32)) for i in range(8)]
    psb = k.bufs(8, "ps")

    identb = sb("identb", [128, 128], BF16)
    identb_b = k.buf("identb")
    small = sb("small", [128, SMALL_W])
    small_b = k.buf("small")
    k.dma("pool", identb[:], identb_d[:, :], writes=[identb_b])
    k.dma("sp", small[:], small_d[:, :], writes=[small_b])
    nmixT = small[:, 0:16]
    gnw = small[:, 16:16 + 1024]
    nffnT = small[:, 1040:1056]
    npleT = small[:, 1056:1072]
    rbias = small[:, 1072:1108]
    def s5_setup(ss, with_cp=True, tag=""):
        s5p = sb("s5p" + tag, [128, S5P_W], F32, ss)
        s5p_b = k.buf("s5p")
        k.dma("sp", s5p[:], s5p_d[:, :], writes=[s5p_b])
        lamre, lamim, logdt = s5p[:, 0:32], s5p[:, 32:64], s5p[:, 64:96]
        Bre = s5p[:, 96:608].rearrange("p (q c) -> p q c", c=16)
        Bim = s5p[:, 608:1120].rearrange("p (q c) -> p q c", c=16)
        Cre = s5p[:, 1120:1632].rearrange("p (q c) -> p q c", c=16)
        Cim = s5p[:, 1632:2144].rearrange("p (q c) -> p q c", c=16)
        W = sb("s5w" + tag, [128, 20, 32], F32, ss)
        wb = k.buf("s5w")

        def tt(o, a, b_, op, eng="dve"):
            k.op(eng, lambda e: e.tensor_tensor(out=o, in0=a, in1=b_, op=op), reads=[wb, s5p_b], writes=[wb])

        def ts(o, a, s1, s2, op0, op1=None, eng="dve"):
            if op1 is None:
                k.op(eng, lambda e: e.tensor_scalar(out=o, in0=a, scalar1=s1, scalar2=None, op0=op0), reads=[wb, s5p_b], writes=[wb])
            else:
                k.op(eng, lambda e: e.tensor_scalar(out=o, in0=a, scalar1=s1, scalar2=s2, op0=op0, op1=op1), reads=[wb, s5p_b], writes=[wb])

        def act(o, a, f):
            k.op("act", lambda e: e.activation(out=o, in_=a, func=f), reads=[wb, s5p_b], writes=[wb])

        lr, dt, a_, b_, ea, r1, r2, sbn, cbn, Lre, Lim, nr, den, t0, t1_, cr, ci, t2_, t3_ = [W[:, i, :] for i in range(19)]
        ts(lr, lamre, -1e-4, None, ALU.min)
        act(dt, logdt, AF.Exp)
        tt(a_, lr, dt, ALU.mult)
        tt(b_, lamim, dt, ALU.mult)
        act(ea, a_, AF.Exp)
        for (dst, off) in ((r1, PI), (r2, 1.5 * PI)):
            ts(dst, b_, off, None, ALU.add)
            ts(t0, dst, 2 * PI, -2 * PI, ALU.is_ge, ALU.mult)
            tt(t1_, dst, t0, ALU.add)
            ts(t0, dst, 4 * PI, -2 * PI, ALU.is_ge, ALU.mult)
            tt(t1_, t1_, t0, ALU.add)
            ts(t0, dst, 6 * PI, -2 * PI, ALU.is_ge, ALU.mult)
            tt(t1_, t1_, t0, ALU.add)
            ts(dst, t1_, -PI, None, ALU.add)
        act(sbn, r1, AF.Sin)
        act(cbn, r2, AF.Sin)
        tt(Lre, ea, cbn, ALU.mult)
        tt(Lim, ea, sbn, ALU.mult)
        ts(nr, Lre, -1.0, None, ALU.add)
        tt(den, lr, lr, ALU.mult)
        tt(t0, lamim, lamim, ALU.mult)
        tt(den, den, t0, ALU.add)
        k.op("dve", lambda e: e.reciprocal(out=den, in_=den), reads=[wb], writes=[wb])
        tt(t0, nr, lr, ALU.mult)
        tt(t1_, Lim, lamim, ALU.mult)
        tt(t0, t0, t1_, ALU.add)
        tt(cr, t0, den, ALU.mult)
        tt(t0, Lim, lr, ALU.mult)
        tt(t1_, nr, lamim, ALU.mult)
        tt(t0, t0, t1_, ALU.subtract)
        tt(ci, t0, den, ALU.mult)
        Bb = sb("s5Bb" + tag, [128, 2, 32, 16], F32, ss)
        T3 = sb("s5T3" + tag, [128, 2, 32, 16], F32, ss)
        bc = lambda v: v.unsqueeze(2).broadcast_to([128, 32, 16])
        tt(T3[:, 0], Bre, bc(cr), ALU.mult)
        tt(T3[:, 1], Bim, bc(ci), ALU.mult)
        tt(Bb[:, 0], T3[:, 0], T3[:, 1], ALU.subtract)
        tt(T3[:, 0], Bim, bc(cr), ALU.mult)
        tt(T3[:, 1], Bre, bc(ci), ALU.mult)
        tt(Bb[:, 1], T3[:, 0], T3[:, 1], ALU.add)
        k.op("dve", lambda e: e.memset(Pre[:, 0, :], 1.0), reads=[wb], writes=[wb])
        k.op("dve", lambda e: e.memset(Pim[:, 0, :], 0.0), reads=[wb], writes=[wb])
        for kk in range(8):
            tt(t0, Pre[:, kk, :], Lre, ALU.mult)
            tt(t1_, Pim[:, kk, :], Lim, ALU.mult)
            tt(Pre[:, kk + 1, :], t0, t1_, ALU.subtract)
            tt(t0, Pre[:, kk, :], Lim, ALU.mult)
            tt(t1_, Pim[:, kk, :], Lre, ALU.mult)
            tt(Pim[:, kk + 1, :], t0, t1_, ALU.add)
        k.op("dve", lambda e: e.tensor_copy(out=Dre[:, 0, :], in_=Pre[:, 8, :]), reads=[wb], writes=[wb])
        k.op("dve", lambda e: e.tensor_copy(out=Dim[:, 0, :], in_=Pim[:, 8, :]), reads=[wb], writes=[wb])
        for i in range(8):
            tt(t0, Dre[:, i, :], Dre[:, i, :], ALU.mult)
            tt(t1_, Dim[:, i, :], Dim[:, i, :], ALU.mult)
            tt(Dre[:, i + 1, :], t0, t1_, ALU.subtract)
            tt(t0, Dre[:, i, :], Dim[:, i, :], ALU.mult)
            ts(Dim[:, i + 1, :], t0, 2.0, None, ALU.mult)
        ts(nDim[:, :, :], Dim[:, :, :], -1.0, None, ALU.mult)
        for sx in range(8):
            pr = bc(Pre[:, 7 - sx, :])
            pi_ = bc(Pim[:, 7 - sx, :])
            tt(T3[:, 0], Bb[:, 0], pr, ALU.mult)
            tt(T3[:, 1], Bb[:, 1], pi_, ALU.mult)
            tt(RBc[:, :, sx, 0, :], T3[:, 0], T3[:, 1], ALU.subtract)
            tt(T3[:, 0], Bb[:, 1], pr, ALU.mult)
            tt(T3[:, 1], Bb[:, 0], pi_, ALU.mult)
            tt(RBc[:, :, sx, 1, :], T3[:, 0], T3[:, 1], ALU.add)
        for kk in (range(9) if with_cp else ()):
            pr = bc(Pre[:, kk, :])
            pi_ = bc(Pim[:, kk, :])
            tt(T3[:, 0], Cre, pr, ALU.mult)
            tt(T3[:, 1], Cim, pi_, ALU.mult)
            tt(CPc[:, :, kk, 0, :], T3[:, 0], T3[:, 1], ALU.subtract)
            tt(T3[:, 0], Cre, pi_, ALU.mult)
            tt(T3[:, 1], Cim, pr, ALU.mult)
            tt(T3[:, 0], T3[:, 0], T3[:, 1], ALU.add)
            ts(CPc[:, :, kk, 1, :], T3[:, 0], -1.0, None, ALU.mult)
        return wb

    cs = ExitStack()
    S = sb("S", [128, H, 128], F32, cs)
    Sbf = sb("Sbf", [128, H, 128], BF16, cs)
    S_b = k.bufs(H, "S")
    Sbf_b = k.bufs(H, "Sbf")
    carryP = sb("carryP", [128, 32, 2], F32, cs)
    carryP_b = k.buf("carryP")
    tabs1 = sb("tabs1", [128, NCORES], F32, cs)
    tabs1_b = k.buf("tabs1")
    k.dma("sp", tabs1[:], tabs_d[:, TAB_ONEHOT:TAB_W], writes=[tabs1_b])

    def stage1_tile(src_rows, dst3, dst_b, nT, xt_, xt_b_, xs_, xs_b_, st_, st_b_, pbanks, part="both"):
        if part in ("both", "a", "dma"):
            k.dma("sp", xt_[:], src_rows, writes=[xt_b_])
        if part in ("both", "a", "a2"):
            k.op("act", lambda e: e.activation(out=xs_[:], in_=xt_[:], func=AF.Square, accum_out=st_[:, 0:1]), reads=[xt_b_], writes=[xs_b_, st_b_])
            k.op("act", lambda e: e.activation(out=st_[:, 1:2], in_=st_[:, 0:1], func=AF.Sqrt, scale=1.0 / D, bias=EPS), reads=[st_b_], writes=[st_b_])
            k.op("dve", lambda e: e.reciprocal(out=st_[:, 1:2], in_=st_[:, 1:2]), reads=[st_b_], writes=[st_b_])
            k.op("act", lambda e: e.activation(out=xs_[:], in_=xt_[:], func=AF.Copy, scale=st_[:, 1:2]), reads=[xt_b_, st_b_, xs_b_], writes=[xs_b_])
        if part in ("both", "b"):
            for half in range(2):
                pbank = ps[pbanks[half]].bitcast(BF16)
                for j in range(8):
                    kc = half * 8 + j
                    k.op("pe", lambda e: e.transpose(out=pbank[:, j * 128:(j + 1) * 128], in_=xs_[:, kc * 128:(kc + 1) * 128], identity=identb[:]),
                         reads=[xs_b_, identb_b], writes=[psb[pbanks[half]]])
                k.op("dve", lambda e: e.tensor_tensor(out=dst3[:, half * 8:(half + 1) * 8, :], in0=pbank[:, :].rearrange("p (j t) -> p j t", t=128),
                                                      in1=nT[:, half * 8:(half + 1) * 8].unsqueeze(2).broadcast_to([128, 8, 128]), op=ALU.mult),
                     reads=[psb[pbanks[half]], small_b], writes=[dst_b])

    if mode == "fused":
        uTp_d = dscr("uTp", [RW, NPT * 128], BF16)
        uTp_b = k.bufs(NPT, "uTp")
        with ExitStack() as pa:
            Wkv = sb("pWkv", [128, KC, 2048], BF16, pa)
            Wkv_b = k.bufs(4, "pWkv")
            for cb in range(4):
                c0 = RW + cb * 512
                k.dma("pool", Wkv[:, :, cb * 512:(cb + 1) * 512], w_in_d[:, c0:c0 + 512].rearrange("(kc p) c -> p kc c", p=128), writes=[Wkv_b[cb]])
            kdecP = sb("kdecP", [128, NPT, H], F32, pa)
            kdecP_b = k.buf("kdecP")
            k.dma("sp", kdecP[:], kdecp_d[:, :].rearrange("p (g h) -> p g h", h=H), writes=[kdecP_b])
            ropeP = [sb("ropeP%d" % i, [128, 2, 64], F32, pa) for i in range(2)]
            ropeP_b = k.bufs(2, "ropeP")
            xtA = [sb("pxt%d" % i, [128, D], F32, pa) for i in range(4)]
            xsA = [sb("pxs%d" % i, [128, D], BF16, pa) for i in range(3)]
            stA = [sb("pst%d" % i, [128, 2], F32, pa) for i in range(3)]
            xtA_b, xsA_b, stA_b = k.bufs(4, "pxt"), k.bufs(3, "pxs"), k.bufs(3, "pst")
            hTt = [sb("phTt%d" % i, [128, KC, 128], BF16, pa) for i in range(2)]
            hTt_b = k.bufs(2, "phTt")
            tt4 = [sb("ptt%d" % i, [128, 4, 64], F32, pa) for i in range(4)]
            tt4_b = k.bufs(4, "ptt")
            krr = [sb("pkr%d" % i, [128, 4, 2, 64], F32, pa) for i in range(2)]
            krr1_b, krr2_b = k.bufs(2, "pkr1"), k.bufs(2, "pkr2")
            ktdA = [sb("pktd%d" % i, [128, H, 128], BF16, pa) for i in range(2)]
            ktdA_b = [k.bufs(2, "pktd%d_" % i) for i in range(2)]
            vbA = [sb("pvb%d" % i, [128, 1024], BF16, pa) for i in range(2)]
            vbA_b = [k.bufs(2, "pvb%d_" % i) for i in range(2)]
            KV0, KV1 = 6, 7
            Wu = sb("pWu", [128, KC, RW], BF16, pa)
            Wu_b = k.bufs(2, "pWu")
            for cb in range(2):
                c0 = 4 * RW + cb * 512
                k.dma("pool", Wu[:, :, cb * 512:(cb + 1) * 512], w_in_d[:, c0:c0 + 512].rearrange("(kc p) c -> p kc c", p=128), writes=[Wu_b[cb]])
            uTt = [sb("puTt%d" % i, [128, 8, 128], BF16, pa) for i in range(2)]
            uTt_b = k.bufs(2, "puTt")
            utok = [sb("putok%d" % i, [128, RW], BF16, pa) for i in range(2)]
            utok_b = [k.bufs(2, "putok%d_" % i) for i in range(2)]
            for g in range(NPT):
                b = g % 2
                k.dma("sp", ropeP[b][:], ropep_d[g].rearrange("p (c f) -> p c f", c=2), writes=[ropeP_b[b]])

                def s1(gg, part):
                    b3 = gg % 3
                    b4 = gg % 4
                    stage1_tile(xfull_d[gg * 128:(gg + 1) * 128, :], hTt[gg % 2][:, :, :], hTt_b[gg % 2], nmixT, xtA[b4], xtA_b[b4], xsA[b3], xsA_b[b3],
                                stA[b3], stA_b[b3], (0, 1), part=part)
                if g == 0:
                    s1(0, "a")
                    s1(1, "a")
                    s1(2, "dma")
                    s1(0, "b")
                if g + 3 < NPT:
                    s1(g + 3, "dma")
                if g + 2 < NPT:
                    s1(g + 2, "a2")
                if g + 1 < NPT:
                    s1(g + 1, "b")
                for cb in range(4):
                    pb = 2 + cb % 2
                    for kc in range(KC):
                        k.op("pe", lambda e: e.matmul(ps[pb][:, :], hTt[b][:, kc, :], Wkv[:, kc, cb * 512:(cb + 1) * 512], start=(kc == 0), stop=(kc == KC - 1)),
                             reads=[hTt_b[b], Wkv_b[cb]], writes=[psb[pb]])
                    if cb < 2:
                        X4 = ps[pb][:, :].rearrange("p (h c f) -> p h c f", c=2, f=64)
                        A, B = X4[:, :, 0, :], X4[:, :, 1, :]
                        C = ropeP[b][:, 0, :].unsqueeze(1).broadcast_to([128, 4, 64])
                        Sn = ropeP[b][:, 1, :].unsqueeze(1).broadcast_to([128, 4, 64])
                        kb = cb % 2
                        k.op("dve", lambda e: e.tensor_tensor(out=tt4[0][:], in0=A, in1=C, op=ALU.mult), reads=[psb[pb], ropeP_b[b]], writes=[tt4_b[0]])
                        k.op("dve", lambda e: e.tensor_tensor(out=tt4[1][:], in0=B, in1=Sn, op=ALU.mult), reads=[psb[pb], ropeP_b[b]], writes=[tt4_b[1]])
                        k.op("dve", lambda e: e.tensor_tensor(out=tt4[2][:], in0=A, in1=Sn, op=ALU.mult), reads=[psb[pb], ropeP_b[b]], writes=[tt4_b[2]])
                        k.op("dve", lambda e: e.tensor_tensor(out=tt4[3][:], in0=B, in1=C, op=ALU.mult), reads=[psb[pb], ropeP_b[b]], writes=[tt4_b[3]])
                        k.op("pool", lambda e: e.tensor_tensor(out=krr[kb][:, :, 0, :], in0=tt4[0][:], in1=tt4[1][:], op=ALU.subtract),
                             reads=[tt4_b[0], tt4_b[1]], writes=[krr1_b[kb]])
                        k.op("pool", lambda e: e.tensor_tensor(out=krr[kb][:, :, 1, :], in0=tt4[2][:], in1=tt4[3][:], op=ALU.add),
                             reads=[tt4_b[2], tt4_b[3]], writes=[krr2_b[kb]])
                        k.op("pool", lambda e: e.tensor_tensor(
                            out=ktdA[b][:, cb * 4:(cb + 1) * 4, :], in0=krr[kb][:, :, :, :].rearrange("p h c f -> p h (c f)"),
                            in1=kdecP[:, g, cb * 4:(cb + 1) * 4].unsqueeze(2).broadcast_to([128, 4, 128]), op=ALU.mult),
                            reads=[krr1_b[kb], krr2_b[kb], kdecP_b], writes=[ktdA_b[b][cb]])
                    else:
                        k.op("act", lambda e: e.copy(out=vbA[b][:, (cb - 2) * 512:(cb - 1) * 512], in_=ps[pb][:, :]), reads=[psb[pb]], writes=[vbA_b[b][cb - 2]])
                for cb in range(2):
                    pbk = 4 + cb
                    for kc in range(KC):
                        k.op("pe", lambda e: e.matmul(ps[pbk][:, :], hTt[b][:, kc, :], Wu[:, kc, cb * 512:(cb + 1) * 512], start=(kc == 0), stop=(kc == KC - 1)),
                             reads=[hTt_b[b], Wu_b[cb]], writes=[psb[pbk]])
                    k.op("act", lambda e: e.copy(out=utok[b][:, cb * 512:(cb + 1) * 512], in_=ps[pbk][:, :]), reads=[psb[pbk]], writes=[utok_b[b][cb]])
                for cb in range(2):
                    pbk = 4 + cb
                    ubank = ps[pbk].bitcast(BF16)
                    for c4 in range(4):
                        ct = cb * 4 + c4
                        k.op("pe", lambda e: e.transpose(out=ubank[:, c4 * 128:(c4 + 1) * 128], in_=utok[b][:, ct * 128:(ct + 1) * 128], identity=identb[:]),
                             reads=[utok_b[b][cb], identb_b], writes=[psb[pbk]])
                    for c4 in range(4):
                        ct = cb * 4 + c4
                        eng = "act" if c4 % 2 == 0 else "dve"
                        fnc = (lambda e: e.copy(out=uTt[b][:, ct, :].rearrange("p (s m) -> p s m", s=8),
                                                in_=ubank[:, c4 * 128:(c4 + 1) * 128].rearrange("p (m s) -> p s m", s=8))) if eng == "act" else \
                              (lambda e: e.tensor_copy(out=uTt[b][:, ct, :].rearrange("p (s m) -> p s m", s=8),
                                                       in_=ubank[:, c4 * 128:(c4 + 1) * 128].rearrange("p (m s) -> p s m", s=8)))
                        k.op(eng, fnc, reads=[psb[pbk], uTt_b[b]], writes=[uTt_b[b]])
                k.dma("sp", uTp_d[:, g * 128:(g + 1) * 128].rearrange("(ct p) t -> p ct t", p=128), uTt[b][:], reads=[uTt_b[b]], writes=[uTp_b[g]])
                for h in range(H):
                    kvb = KV0 if h < 4 else KV1
                    k.op("pe", lambda e: e.matmul(ps[kvb][:, (h % 4) * 128:(h % 4 + 1) * 128], ktdA[b][:, h, :], vbA[b][:, h * 128:(h + 1) * 128],
                                                  start=(g == 0 and h % 4 == 0), stop=(g == NPT - 1 and h % 4 == 3)), reads=[ktdA_b[b][h // 4], vbA_b[b][h // 4]], writes=[psb[kvb]])
            for h in range(H):
                kvb = KV0 if h < 4 else KV1
                k.op("act", lambda e: e.copy(out=S[:, h, :], in_=ps[kvb][:, (h % 4) * 128:(h % 4 + 1) * 128]), reads=[psb[kvb]], writes=[S_b[h]])
                k.op("act", lambda e: e.copy(out=Sbf[:, h, :], in_=S[:, h, :]), reads=[S_b[h]], writes=[Sbf_b[h]])
        k.barrier()
        with ExitStack() as pb_:
            Pre = sb("qPre", [128, 9, 32], F32, pb_)
            Pim = sb("qPim", [128, 9, 32], F32, pb_)
            Dre = sb("qDre", [128, 9, 32], F32, pb_)
            Dim = sb("qDim", [128, 9, 32], F32, pb_)
            nDim = sb("qnDim", [128, 9, 32], F32, pb_)
            RBc = sb("qRBc", [128, 32, 8, 2, 16], BF16, pb_)
            CPc = None
            with ExitStack() as ss:
                mats_b = s5_setup(ss, with_cp=False, tag="q")
            k.barrier()
            NSEG = NCORES - 1
            NCH = NSEG * 256
            RBm = [sb("qRBm%d" % i, [128, 8, 2, 32], BF16, pb_) for i in range(2)]
            RBm_b = k.bufs(2, "qRBm")
            RBT = [sb("qRBT%d" % i, [32, 16, 128], BF16, pb_) for i in range(2)]
            RBT_b = k.bufs(2, "qRBT")
            uq = [sb("quq%d" % i, [32, NCH * 8], BF16, pb_) for i in range(2)]
            uq_b = k.bufs(2, "quq")
            wA = [sb("qwA%d" % i, [128, 2, NSEG, 256], F32, pb_) for i in range(2)]
            wA_b = k.bufs(2, "qwA")
            wB = [sb("qwB%d" % i, [128, 2, NSEG, 128], F32, pb_) for i in range(2)]
            wB_b = k.bufs(2, "qwB")
            Eall = sb("qEall", [128, 32, 2, NSEG], F32, pb_)
            Eall_b = k.bufs(32, "qEall")
            for i in range(2):
                k.op("pool", lambda e: e.memset(RBm[i][:], 0.0), writes=[RBm_b[i]])
            PTA, PTB = 0, 1
            NBK = 4
            CW = NCH // NBK
            def pfx_pair(q):
                b = q % 2
                for gi in range(2):
                    prt = slice(gi * 64, gi * 64 + 64)
                    csl = slice(gi * 16, gi * 16 + 16)
                    k.op("pool", lambda e: e.tensor_copy(out=RBm[b][prt, :, :, csl], in_=RBc[prt, q, :, :, :]), reads=[mats_b], writes=[RBm_b[b]])
                pTA = ps[PTA].bitcast(BF16)
                pTB = ps[PTB].bitcast(BF16)
                for sx in range(8):
                    for ri in range(2):
                        idx = sx * 2 + ri
                        pt, pbb = (pTA, psb[PTA]) if idx < 8 else (pTB, psb[PTB])
                        k.op("pe", lambda e: e.transpose(out=pt[0:32, (idx % 8) * 128:(idx % 8 + 1) * 128], in_=RBm[b][:, sx, ri, :], identity=identb[:]),
                             reads=[RBm_b[b], identb_b], writes=[pbb])
                k.op("act", lambda e: e.copy(out=RBT[b][:, 0:8, :], in_=pTA[0:32, :].rearrange("p (a c) -> p a c", c=128)), reads=[psb[PTA]], writes=[RBT_b[b]])
                k.op("act", lambda e: e.copy(out=RBT[b][:, 8:16, :], in_=pTB[0:32, :].rearrange("p (a c) -> p a c", c=128)), reads=[psb[PTB], RBT_b[b]], writes=[RBT_b[b]])
                yield
                k.dma("sp", uq[b][:], uTp_d[q * 32:(q + 1) * 32, :], reads=uTp_b, writes=[uq_b[b]])
                uq4 = uq[b][:, :].rearrange("p (g s m) -> p g s m", s=8, m=16)
                TPB = CW // 16
                wflat = wA[b][:, :, :, :].rearrange("p r g m -> p r (g m)")
                for ri in range(2):
                    for nbk in range(NBK):
                        pbk = 2 + (ri * NBK + nbk) % 6
                        for sx in range(8):
                            k.op("pe", lambda e: e.matmul(ps[pbk][:, 0:CW], RBT[b][:, sx * 2 + ri, :], uq4[:, nbk * TPB:(nbk + 1) * TPB, sx, :],
                                                          start=(sx == 0), stop=(sx == 7)), reads=[RBT_b[b], uq_b[b]], writes=[psb[pbk]])
                        k.op("act", lambda e: e.copy(out=wflat[:, ri, nbk * CW:(nbk + 1) * CW], in_=ps[pbk][:, 0:CW]), reads=[psb[pbk]], writes=[wA_b[b]])
                yield
                src, src_b2 = wA[b], wA_b[b]
                dst, dst_b2 = wB[b], wB_b[b]
                n = 256
                for lv in range(8):
                    hn = n // 2
                    v = src[:, :, :, 0:n].rearrange("p r g (m two) -> p r g m two", two=2)
                    dre, dim_, ndim = Dre[:, lv, q:q + 1], Dim[:, lv, q:q + 1], nDim[:, lv, q:q + 1]
                    o_re, o_im = dst[:, 0, :, 0:hn], dst[:, 1, :, 0:hn]
                    k.op("dve", lambda e: e.scalar_tensor_tensor(out=o_re, in0=v[:, 0, :, :, 0], scalar=dre, in1=v[:, 0, :, :, 1], op0=ALU.mult, op1=ALU.add),
                         reads=[src_b2, mats_b, dst_b2], writes=[dst_b2])
                    k.op("dve", lambda e: e.scalar_tensor_tensor(out=o_im, in0=v[:, 1, :, :, 0], scalar=dre, in1=v[:, 1, :, :, 1], op0=ALU.mult, op1=ALU.add),
                         reads=[src_b2, mats_b, dst_b2], writes=[dst_b2])
                    yield
                    k.op("dve", lambda e: e.scalar_tensor_tensor(out=o_re, in0=v[:, 1, :, :, 0], scalar=ndim, in1=o_re, op0=ALU.mult, op1=ALU.add),
                         reads=[src_b2, mats_b, dst_b2], writes=[dst_b2])
                    k.op("dve", lambda e: e.scalar_tensor_tensor(out=o_im, in0=v[:, 0, :, :, 0], scalar=dim_, in1=o_im, op0=ALU.mult, op1=ALU.add),
                         reads=[src_b2, mats_b, dst_b2], writes=[dst_b2])
                    yield
                    src, src_b2, dst, dst_b2 = dst, dst_b2, src, src_b2
                    n = hn
                k.op("act", lambda e: e.copy(out=Eall[:, q, :, :], in_=src[:, :, :, 0]), reads=[src_b2], writes=[Eall_b[q]])

            for q0 in range(0, NQ, 2):
                gens = [pfx_pair(q0), pfx_pair(q0 + 1)]
                while gens:
                    for gnr in list(gens):
                        try:
                            next(gnr)
                        except StopIteration:
                            gens.remove(gnr)
            Xc = sb("qXc", [128, 32, 2], F32, pb_)
            tq = sb("qtq", [128, 4, 32], F32, pb_)
            xb = k.buf("qXc")
            k.op("dve", lambda e: e.memset(Xc[:], 0.0), writes=[xb])
            k.op("dve", lambda e: e.memset(carryP[:], 0.0), writes=[carryP_b])
            d8r, d8i = Dre[:, 8, :], Dim[:, 8, :]
            for r in range(NSEG):
                tt_ = lambda o, a, b2, op: k.op("dve", lambda e: e.tensor_tensor(out=o, in0=a, in1=b2, op=op), reads=[xb, mats_b] + Eall_b, writes=[xb])
                tt_(tq[:, 0, :], Xc[:, :, 0], d8r, ALU.mult)
                tt_(tq[:, 1, :], Xc[:, :, 1], d8i, ALU.mult)
                tt_(tq[:, 2, :], Xc[:, :, 1], d8r, ALU.mult)
                tt_(tq[:, 3, :], Xc[:, :, 0], d8i, ALU.mult)
                tt_(tq[:, 0, :], tq[:, 0, :], tq[:, 1, :], ALU.subtract)
                tt_(tq[:, 2, :], tq[:, 2, :], tq[:, 3, :], ALU.add)
                tt_(Xc[:, :, 0], tq[:, 0, :], Eall[:, :, 0, r], ALU.add)
                tt_(Xc[:, :, 1], tq[:, 2, :], Eall[:, :, 1, r], ALU.add)
                k.op("dve", lambda e: e.scalar_tensor_tensor(out=carryP[:, :, :].rearrange("p q r -> p (q r)"), in0=Xc[:, :, :].rearrange("p q r -> p (q r)"),
                                                             scalar=tabs1[:, r + 1:r + 2], in1=carryP[:, :, :].rearrange("p q r -> p (q r)"),
                                                             op0=ALU.mult, op1=ALU.add), reads=[xb, tabs1_b, carryP_b], writes=[carryP_b])
        k.barrier()

    hs = ExitStack()
    hT = sb("hT", [128, KC, T], BF16, hs)
    hT_b = k.bufs(NT, "hT")
    ms = ExitStack()
    tabs = sb("tabs", [128, TAB_W], F32, ms)
    tabs_b = k.buf("tabs")
    k.dma("sp", tabs[:], tabs_d[:, :], writes=[tabs_b])
    cosT = tabs[:, TAB_COS:TAB_SIN].rearrange("p (i f) -> p i f", f=64)
    sinT = tabs[:, TAB_SIN:TAB_MASK].rearrange("p (i f) -> p i f", f=64)
    maskT = tabs[:, TAB_MASK:TAB_QDEC].rearrange("p (h f) -> p h f", f=128)
    qdec = tabs[:, TAB_QDEC:TAB_KDEC]
    kdec = tabs[:, TAB_KDEC:TAB_ONEHOT]
    onehot = tabs[:, TAB_ONEHOT:TAB_W]


    with ExitStack() as s1:
        xt = [sb("xt%d" % i, [128, D], F32, s1) for i in range(2)]
        xt_b = k.bufs(2, "xt")
        xs = [sb("xs%d" % i, [128, D], BF16, s1) for i in range(2)]
        xs_b = k.bufs(2, "xs")
        junk = sb("junk", [128, D], BF16, s1)
        junk_b = k.buf("junk")
        ss = sb("ss", [128, NT], F32, s1)
        rstd = sb("rstd", [128, NT], F32, s1)
        ss_b = k.bufs(NT, "ss")
        rstd_b = k.bufs(NT, "rstd")
        for i in range(NT):
            b = i % 2
            k.dma("sp", xt[b][:], x_d[i * 128:(i + 1) * 128, :], writes=[xt_b[b]])
            k.op("act", lambda e: e.activation(out=junk[:], in_=xt[b][:], func=AF.Square, accum_out=ss[:, i:i + 1]),
                 reads=[xt_b[b]], writes=[junk_b, ss_b[i]])
            k.op("act", lambda e: e.activation(out=rstd[:, i:i + 1], in_=ss[:, i:i + 1], func=AF.Sqrt, scale=1.0 / D, bias=EPS),
                 reads=[ss_b[i]], writes=[rstd_b[i]])
            k.op("dve", lambda e: e.reciprocal(out=rstd[:, i:i + 1], in_=rstd[:, i:i + 1]), reads=[rstd_b[i]], writes=[rstd_b[i]])
            k.op("act", lambda e: e.activation(out=xs[b][:], in_=xt[b][:], func=AF.Copy, scale=rstd[:, i:i + 1]),
                 reads=[xt_b[b], rstd_b[i]], writes=[xs_b[b]])
            for half in range(2):
                pbank = ps[half].bitcast(BF16)
                for j in range(8):
                    kc = half * 8 + j
                    k.op("pe", lambda e: e.transpose(out=pbank[:, j * 128:(j + 1) * 128],
                                                     in_=xs[b][:, kc * 128:(kc + 1) * 128], identity=identb[:]),
                         reads=[xs_b[b], identb_b], writes=[psb[half]])
                k.op("dve", lambda e: e.tensor_tensor(
                    out=hT[:, half * 8:(half + 1) * 8, i * 128:(i + 1) * 128],
                    in0=pbank[:, :].rearrange("p (j t) -> p j t", t=128),
                    in1=nmixT[:, half * 8:(half + 1) * 8].unsqueeze(2).broadcast_to([128, 8, 128]),
                    op=ALU.mult), reads=[psb[half], small_b], writes=[hT_b[i]])

    k.barrier()

    def retention(full, rs):
        Wh = [sb("Wh%d_%d" % (i, full), [128, KC, 512], BF16, rs) for i in range(2)]
        Wh_b = k.bufs(2, "Wh")
        xqk = [sb("xqk%d_%d" % (i, full), [128, 256], F32, rs) for i in range(4)]
        xqk_b = k.bufs(4, "xqk")
        t1 = [sb("t1_%d_%d" % (i, full), [128, 2, 64], F32, rs) for i in range(4)]
        t2 = [sb("t2_%d_%d" % (i, full), [128, 2, 64], F32, rs) for i in range(4)]
        t3 = [sb("t3_%d_%d" % (i, full), [128, 2, 64], F32, rs) for i in range(4)]
        t4 = [sb("t4_%d_%d" % (i, full), [128, 2, 64], F32, rs) for i in range(4)]
        t1_b, t2_b, t3_b, t4_b = k.bufs(4, "t1"), k.bufs(4, "t2"), k.bufs(4, "t3"), k.bufs(4, "t4")
        qkr = [sb("qkr%d_%d" % (i, full), [128, 2, 2, 64], BF16, rs) for i in range(4)]
        qkr1_b, qkr2_b = k.bufs(4, "qkr1"), k.bufs(4, "qkr2")
        vb = [sb("vb%d_%d" % (i, full), [128, 128], BF16, rs) for i in range(4)]
        vb_b = k.bufs(4, "vb")
        sg = [sb("sg%d_%d" % (i, full), [128, 128], F32, rs) for i in range(4)]
        sg_b = k.bufs(4, "sg")
        qd = [sb("qd%d_%d" % (i, full), [128, 128], BF16, rs) for i in range(4)]
        qd_b = k.bufs(4, "qd")
        ktd = [sb("ktd%d_%d" % (i, full), [128, 128], BF16, rs) for i in range(4)]
        ktd_b = k.bufs(4, "ktd")
        qkT = [sb("qkT%d_%d" % (i, full), [128, 384], BF16, rs) for i in range(4)]
        qkT_b = k.bufs(4, "qkT")
        Pm = [sb("Pm%d_%d" % (i, full), [128, 128], BF16, rs) for i in range(4)]
        Pm_b = k.bufs(4, "Pm")
        st6 = [sb("st6_%d_%d" % (i, full), [128, 6], F32, rs) for i in range(4)]
        mv = [sb("mv%d_%d" % (i, full), [128, 4], F32, rs) for i in range(4)]
        st6_b, mv_b = k.bufs(4, "st6"), k.bufs(4, "mv")
        yn = [sb("yn%d_%d" % (i, full), [128, 128], F32, rs) for i in range(4)]
        yn_b = k.bufs(4, "yn")
        yr = [sb("yr%d_%d" % (i, full), [128, 128], BF16, rs) for i in range(4)]
        yr_b = k.bufs(4, "yr")
        yst = [sb("yst%d_%d" % (i, full), [128, T], BF16, rs) for i in range(2)]
        yst_b = k.bufs(2, "yst")
        PJb, PTb, MB = (0, 3), (1, 4), (2, 5)
        t_sc, t_py, t_kv, t_t2 = k.bufs(2, "r_sc"), k.bufs(2, "r_py"), k.bufs(2, "r_kv"), k.bufs(2, "r_t2")

        def load_w(h):
            hb = h % 2
            for j in range(4):
                c0 = j * RW + h * DH
                k.dma("pool", Wh[hb][:, :, j * 128:(j + 1) * 128],
                      w_in_d[:, c0:c0 + 128].rearrange("(kc p) c -> p kc c", p=128), writes=[Wh_b[hb]] if j == 3 else [])
        def load_w_tracked(h):
            hb = h % 2
            for j in range(4):
                if not full and j in (0, 3):
                    continue
                c0 = j * RW + h * DH
                k.dma("pool", Wh[hb][:, :, j * 128:(j + 1) * 128],
                      w_in_d[:, c0:c0 + 128].rearrange("(kc p) c -> p kc c", p=128), writes=[Whj_b[hb][j]])

        Whj_b = [k.bufs(4, "Whj%d_" % i) for i in range(2)]
        def head_body(h):
            hb = h % 2
            wreads = [Whj_b[hb][j] for j in ((0, 1, 2, 3) if full else (1, 2))]
            for i in range(NT):
                b = (h % 2) * 2 + i % 2
                hp = h % 2
                pj = ps[PJb[hp]]
                pjb = psb[PJb[hp]]
                tok = slice(i * 128, (i + 1) * 128)
                c_lo, c_hi = (0, 512) if full else (128, 384)
                for kc in range(KC):
                    k.op("pe", lambda e: e.matmul(pj[:, c_lo:c_hi], hT[:, kc, tok], Wh[hb][:, kc, c_lo:c_hi],
                                                  start=(kc == 0), stop=(kc == KC - 1)),
                         reads=[hT_b[i]] + wreads, writes=[pjb])
                if h == 0 and i == 0 and full:
                    dump("Wh", Wh[hb][:, 3, :], wreads, 512)
                    dump("pj", pj[:, :], [pjb], 512)
                yield
                k.op("act", lambda e: e.copy(out=xqk[b][:, c_lo:256], in_=pj[:, c_lo:256]), reads=[pjb], writes=[xqk_b[b]])
                k.op("act", lambda e: e.copy(out=vb[b][:], in_=pj[:, 256:384]), reads=[pjb], writes=[vb_b[b]])
                if full:
                    k.op("act", lambda e: e.activation(out=sg[b][:], in_=pj[:, 384:512], func=AF.Silu), reads=[pjb], writes=[sg_b[b]])
                yield
                X = xqk[b][:, :].rearrange("p (a c f) -> p a c f", a=2, c=2)
                a0 = 0 if full else 1
                A = X[:, a0:2, 0, :]
                B = X[:, a0:2, 1, :]
                na = 2 - a0
                C = cosT[:, i, :].unsqueeze(1).broadcast_to([128, na, 64])
                Sn = sinT[:, i, :].unsqueeze(1).broadcast_to([128, na, 64])
                k.op("dve", lambda e: e.tensor_tensor(out=t1[b][:, a0:2, :], in0=A, in1=C, op=ALU.mult), reads=[xqk_b[b], tabs_b], writes=[t1_b[b]])
                k.op("dve", lambda e: e.tensor_tensor(out=t2[b][:, a0:2, :], in0=B, in1=Sn, op=ALU.mult), reads=[xqk_b[b], tabs_b], writes=[t2_b[b]])
                k.op("dve", lambda e: e.tensor_tensor(out=qkr[b][:, a0:2, 0, :], in0=t1[b][:, a0:2, :], in1=t2[b][:, a0:2, :], op=ALU.subtract),
                     reads=[t1_b[b], t2_b[b]], writes=[qkr1_b[b]])
                k.op("pool", lambda e: e.tensor_tensor(out=t3[b][:, a0:2, :], in0=A, in1=Sn, op=ALU.mult), reads=[xqk_b[b], tabs_b], writes=[t3_b[b]])
                k.op("pool", lambda e: e.tensor_tensor(out=t4[b][:, a0:2, :], in0=B, in1=C, op=ALU.mult), reads=[xqk_b[b], tabs_b], writes=[t4_b[b]])
                k.op("pool", lambda e: e.tensor_tensor(out=qkr[b][:, a0:2, 1, :], in0=t3[b][:, a0:2, :], in1=t4[b][:, a0:2, :], op=ALU.add),
                     reads=[t3_b[b], t4_b[b]], writes=[qkr2_b[b]])
                if h == 0 and i == 0 and full:
                    dump("xqk", xqk[b][:, :], [xqk_b[b]], 256)
                    dump("qkr", qkr[b][:, :, :, :].rearrange("p a c f -> p (a c f)"), [qkr1_b[b], qkr2_b[b]], 256)
                yield
                qr = qkr[b][:, 0, :, :].rearrange("p c f -> p (c f)")
                kr = qkr[b][:, 1, :, :].rearrange("p c f -> p (c f)")
                k.op("pool", lambda e: e.tensor_scalar(out=ktd[b][:], in0=kr, scalar1=kdec[:, h:h + 1], scalar2=None, op0=ALU.mult),
                     reads=[qkr1_b[b], qkr2_b[b], tabs_b], writes=[ktd_b[b]])
                if full:
                    k.op("act", lambda e: e.activation(out=qd[b][:], in_=qr, func=AF.Copy, scale=qdec[:, h:h + 1]),
                         reads=[qkr1_b[b], qkr2_b[b], tabs_b], writes=[qd_b[b]])
                    pT = ps[PTb[hp]].bitcast(BF16)
                    k.op("pe", lambda e: e.transpose(out=pT[:, 0:128], in_=qr, identity=identb[:]),
                         reads=[qkr1_b[b], qkr2_b[b], identb_b], writes=[psb[PTb[hp]]])
                    k.op("pe", lambda e: e.transpose(out=pT[:, 128:256], in_=qd[b][:], identity=identb[:]),
                         reads=[qd_b[b], identb_b], writes=[psb[PTb[hp]]])
                    k.op("pe", lambda e: e.transpose(out=pT[:, 256:384], in_=kr, identity=identb[:]),
                         reads=[qkr1_b[b], qkr2_b[b], identb_b], writes=[psb[PTb[hp]]])
                    k.op("act", lambda e: e.copy(out=qkT[b][:], in_=pT[:, 0:384]), reads=[psb[PTb[hp]]], writes=[qkT_b[b]])
                    yield
                    k.op("pe", lambda e: e.matmul(ps[MB[hp]][:, 0:128], qkT[b][:, 256:384], qkT[b][:, 0:128], start=True, stop=True),
                         reads=[qkT_b[b]], writes=[t_sc[hp]])
                    k.op("dve", lambda e: e.tensor_tensor(out=Pm[b][:], in0=ps[MB[hp]][:, 0:128], in1=maskT[:, h, :], op=ALU.mult),
                         reads=[t_sc[hp], tabs_b], writes=[Pm_b[b]])
                    if h == 0 and i == 0:
                        dump("qkT", qkT[b][:, :], [qkT_b[b]], 384)
                        dump("Pm", Pm[b][:, :], [Pm_b[b]], 128)
                    k.op("pe", lambda e: e.matmul(ps[MB[hp]][:, 128:256], Pm[b][:], vb[b][:], start=True, stop=False),
                         reads=[Pm_b[b], vb_b[b]], writes=[t_py[hp]])
                    k.op("pe", lambda e: e.matmul(ps[MB[hp]][:, 128:256], qkT[b][:, 128:256], Sbf[:, h, :], start=False, stop=True),
                         reads=[qkT_b[b], Sbf_b[h]], writes=[t_py[hp]])
                yield
                k.op("pe", lambda e: e.matmul(ps[MB[hp]][:, 256:384], ktd[b][:], vb[b][:], start=True, stop=True),
                     reads=[ktd_b[b], vb_b[b]], writes=[t_kv[hp]])
                k.op("dve", lambda e: e.scalar_tensor_tensor(out=S[:, h, :], in0=S[:, h, :], scalar=G128[h], in1=ps[MB[hp]][:, 256:384],
                                                             op0=ALU.mult, op1=ALU.add), reads=[S_b[h], t_kv[hp]], writes=[S_b[h]])
                if full:
                    k.op("act", lambda e: e.copy(out=Sbf[:, h, :], in_=S[:, h, :]), reads=[S_b[h]], writes=[Sbf_b[h]])
                    yield
                    k.op("dve", lambda e: e.bn_stats(out=st6[b][:], in_=ps[MB[hp]][:, 128:256]), reads=[t_py[hp]], writes=[st6_b[b]])
                    k.op("dve", lambda e: e.bn_aggr(out=mv[b][:, 0:2], in_=st6[b][:]), reads=[st6_b[b]], writes=[mv_b[b]])
                    k.op("act", lambda e: e.activation(out=mv[b][:, 2:3], in_=mv[b][:, 1:2], func=AF.Sqrt, bias=EPS, scale=1.0),
                         reads=[mv_b[b]], writes=[mv_b[b]])
                    k.op("dve", lambda e: e.reciprocal(out=mv[b][:, 2:3], in_=mv[b][:, 2:3]), reads=[mv_b[b]], writes=[mv_b[b]])
                    k.op("dve", lambda e: e.tensor_scalar(out=mv[b][:, 3:4], in0=mv[b][:, 0:1], scalar1=mv[b][:, 2:3], scalar2=-1.0,
                                                          op0=ALU.mult, op1=ALU.mult), reads=[mv_b[b]], writes=[mv_b[b]])
                    k.op("act", lambda e: e.activation(out=yn[b][:], in_=ps[MB[hp]][:, 128:256], func=AF.Identity,
                                                       scale=mv[b][:, 2:3], bias=mv[b][:, 3:4]), reads=[t_py[hp], mv_b[b]], writes=[yn_b[b]])
                    if h == 0 and i == 0:
                        dump("mv", mv[b][:, :], [mv_b[b]], 4)
                        dump("yn", yn[b][:, :], [yn_b[b]], 128)
                    k.op("pool", lambda e: e.tensor_tensor(out=yn[b][:], in0=yn[b][:], in1=gnw[:, h * 128:(h + 1) * 128], op=ALU.mult),
                         reads=[yn_b[b], small_b], writes=[yn_b[b]])
                    k.op("pool", lambda e: e.tensor_tensor(out=yr[b][:], in0=yn[b][:], in1=sg[b][:], op=ALU.mult),
                         reads=[yn_b[b], sg_b[b]], writes=[yr_b[b]])
                    yield
                    pT2 = ps[MB[hp]].bitcast(BF16)
                    if h == 0 and i == 0:
                        dump("yr", yr[b][:, :], [yr_b[b]], 128)
                    k.op("pe", lambda e: e.transpose(out=pT2[:, 768:896], in_=yr[b][:], identity=identb[:]),
                         reads=[yr_b[b], identb_b], writes=[t_t2[hp]])
                    k.op("act", lambda e: e.copy(out=yst[hb][:, tok], in_=pT2[:, 768:896]), reads=[t_t2[hp]], writes=[yst_b[hb]])
            if full:
                k.dma("sp", yretT_d[h * 128:(h + 1) * 128, :], yst[hb][:], reads=[yst_b[hb]], writes=[yretT_b[h]])

        load_w_tracked(0)
        load_w_tracked(1)
        for h0 in range(0, H, 2):
            gens = [head_body(h0), head_body(h0 + 1)]
            while gens:
                for gnr in list(gens):
                    try:
                        next(gnr)
                    except StopIteration:
                        gens.remove(gnr)
            if h0 + 2 < H:
                load_w_tracked(h0 + 2)
                load_w_tracked(h0 + 3)

    yretT_b = k.bufs(H, "yretT_d")
    for h in (range(H) if mode != "fused" else ()):
        k.op("dve", lambda e: e.memset(S[:, h, :], 0.0), writes=[S_b[h]])
        k.op("pool", lambda e: e.memset(Sbf[:, h, :], 0.0), writes=[Sbf_b[h]])
    if mode == "states":
        with ExitStack() as rs:
            retention(False, rs)
        k.barrier()


    k.barrier()

    def s5_uproj(us):
        Wu = [sb("Wu%d" % i, [128, KC, 128], BF16, us) for i in range(2)]
        Wu_b = k.bufs(2, "Wu")
        for ct in range(8):
            b = ct % 2
            c0 = 4 * RW + ct * 128
            k.dma("pool", Wu[b][:], w_in_d[:, c0:c0 + 128].rearrange("(kc p) c -> p kc c", p=128), writes=[Wu_b[b]])
            for tb in range(4):
                pb = tb % 2
                for kc in range(KC):
                    k.op("pe", lambda e: e.matmul(ps[pb][:, :], Wu[b][:, kc, :], hT[:, kc, tb * 512:(tb + 1) * 512],
                                                  start=(kc == 0), stop=(kc == KC - 1)),
                         reads=[Wu_b[b]] + hT_b[tb * 4:(tb + 1) * 4], writes=[psb[pb]])
                k.op("act", lambda e: e.copy(out=uT[:, ct, :].rearrange("p (hf s m) -> p hf s m", hf=2, s=8)[:, tb // 2, :, (tb % 2) * 64:(tb % 2) * 64 + 64],
                                             in_=ps[pb][:, :].rearrange("p (m s) -> p s m", s=8)), reads=[psb[pb]],
                     writes=[uT_b[ct * 4 + j] for j in range(4)])

    def s5_pairs(full, ps_):
        RBm = [sb("RBm%d_%d" % (i, full), [128, 8, 2, 32], BF16, ps_) for i in range(2)]
        CPm = [sb("CPm%d_%d" % (i, full), [128, 9, 2, 32], BF16, ps_) for i in range(2)]
        RBm_b, CPm_b = k.bufs(2, "RBm"), k.bufs(2, "CPm")
        RBT = [sb("RBT%d_%d" % (i, full), [32, 16, 128], BF16, ps_) for i in range(2)]
        RBT_b = k.bufs(2, "RBT")
        KT = [sb("KT%d_%d" % (i, full), [32, 8, 32], BF16, ps_) for i in range(2)]
        KT_b = k.bufs(2, "KT")
        uq = [sb("uq%d_%d" % (i, full), [32, T], BF16, ps_) for i in range(2)]
        uq_b = k.bufs(2, "uq")
        XA = [sb("XA%d_%d" % (i, full), [128, 2, 129], F32, ps_) for i in range(2)]
        XB = [sb("XB%d_%d" % (i, full), [128, 2, 129], F32, ps_) for i in range(2)]
        XA_b, XB_b = k.bufs(2, "XA"), k.bufs(2, "XB")
        xp = [sb("xp%d_%d" % (i, full), [128, 2, 128], BF16, ps_) for i in range(2)]
        xp_b = k.bufs(2, "xp")
        yq = [sb("yq%d_%d" % (i, full), [32, 1024], BF16, ps_) for i in range(2)]
        yq_b = k.bufs(2, "yq")
        for i in range(2):
            k.op("pool", lambda e: e.memset(RBm[i][:], 0.0), writes=[RBm_b[i]])
            k.op("pool", lambda e: e.memset(CPm[i][:], 0.0), writes=[CPm_b[i]])
        PTA, PTB, PK, PYA, PYB = 0, 1, 2, 4, 5

        def pair_body(q):
            b = q % 2
            PW = 3 if b == 0 else 6
            ct, ql = q // 4, q % 4
            for gi in range(2):
                prt = slice(gi * 64, gi * 64 + 64)
                csl = slice(gi * 16, gi * 16 + 16)
                k.op("pool", lambda e: e.tensor_copy(out=RBm[b][prt, :, :, csl], in_=RBc[prt, q, :, :, :]), reads=[mats_b], writes=[RBm_b[b]])
                if full:
                    k.op("pool", lambda e: e.tensor_copy(out=CPm[b][prt, :, :, csl], in_=CPc[prt, q, :, :, :]), reads=[mats_b], writes=[CPm_b[b]])
            pTA = ps[PTA].bitcast(BF16)
            pTB = ps[PTB].bitcast(BF16)
            for sx in range(8):
                for ri in range(2):
                    idx = sx * 2 + ri
                    pt, pbb = (pTA, psb[PTA]) if idx < 8 else (pTB, psb[PTB])
                    k.op("pe", lambda e: e.transpose(out=pt[0:32, (idx % 8) * 128:(idx % 8 + 1) * 128], in_=RBm[b][:, sx, ri, :], identity=identb[:]),
                         reads=[RBm_b[b], identb_b], writes=[pbb])
            k.op("act", lambda e: e.copy(out=RBT[b][:, 0:8, :], in_=pTA[0:32, :].rearrange("p (a c) -> p a c", c=128)), reads=[psb[PTA]], writes=[RBT_b[b]])
            k.op("act", lambda e: e.copy(out=RBT[b][:, 8:16, :], in_=pTB[0:32, :].rearrange("p (a c) -> p a c", c=128)), reads=[psb[PTB], RBT_b[b]], writes=[RBT_b[b]])
            if full:
                for kk in range(8):
                    k.op("pe", lambda e: e.matmul(ps[PK][0:32, kk * 32:(kk + 1) * 32], RBm[b][:, 7, 0, :], CPm[b][:, kk, 0, :], start=True, stop=False),
                         reads=[RBm_b[b], CPm_b[b]], writes=[psb[PK]])
                    k.op("pe", lambda e: e.matmul(ps[PK][0:32, kk * 32:(kk + 1) * 32], RBm[b][:, 7, 1, :], CPm[b][:, kk, 1, :], start=False, stop=True),
                         reads=[RBm_b[b], CPm_b[b]], writes=[psb[PK]])
                k.op("act", lambda e: e.copy(out=KT[b][:], in_=ps[PK][0:32, 0:256].rearrange("p (a c) -> p a c", c=32)), reads=[psb[PK]], writes=[KT_b[b]])
            yield
            k.dma("sp", uq[b][:], uT[32 * ql:32 * ql + 32, ct, :], reads=[uT_b[ct * 4 + j] for j in range(4)], writes=[uq_b[b]])
            uqs = uq[b][:, :].rearrange("p (hf s m) -> p hf s m", hf=2, s=8)
            for hf in range(2):
                for ri in range(2):
                    for sx in range(8):
                        k.op("pe", lambda e: e.matmul(ps[PW][:, ri * 128:(ri + 1) * 128], RBT[b][:, sx * 2 + ri, :], uqs[:, hf, sx, :],
                                                      start=(sx == 0), stop=(sx == 7)), reads=[RBT_b[b], uq_b[b]], writes=[psb[PW]])
                yield
                k.op("act", lambda e: e.copy(out=XA[b][:, :, 1:129], in_=ps[PW][:, 0:256].rearrange("p (r m) -> p r m", r=2)),
                     reads=[psb[PW]], writes=[XA_b[b]])
                k.op("dve", lambda e: e.tensor_copy(out=XA[b][:, :, 0], in_=carry[:, q, :]), reads=[carry_b[q], XA_b[b]], writes=[XA_b[b]])
                src, dst, src_b, dst_b = XA[b], XB[b], XA_b[b], XB_b[b]
                N = 129
                for st in range(8):
                    d = 1 << st
                    k.op("pool", lambda e: e.tensor_copy(out=dst[:, :, 0:d], in_=src[:, :, 0:d]), reads=[src_b], writes=[dst_b])
                    dre, dim_, ndim = Dre[:, st, q:q + 1], Dim[:, st, q:q + 1], nDim[:, st, q:q + 1]
                    k.op("dve", lambda e: e.scalar_tensor_tensor(out=dst[:, 0, d:N], in0=src[:, 0, 0:N - d], scalar=dre, in1=src[:, 0, d:N],
                                                                 op0=ALU.mult, op1=ALU.add), reads=[src_b, mats_b, dst_b], writes=[dst_b])
                    k.op("dve", lambda e: e.scalar_tensor_tensor(out=dst[:, 1, d:N], in0=src[:, 1, 0:N - d], scalar=dre, in1=src[:, 1, d:N],
                                                                 op0=ALU.mult, op1=ALU.add), reads=[src_b, mats_b, dst_b], writes=[dst_b])
                    yield
                    k.op("dve", lambda e: e.scalar_tensor_tensor(out=dst[:, 0, d:N], in0=src[:, 1, 0:N - d], scalar=ndim, in1=dst[:, 0, d:N],
                                                                 op0=ALU.mult, op1=ALU.add), reads=[src_b, mats_b, dst_b], writes=[dst_b])
                    k.op("dve", lambda e: e.scalar_tensor_tensor(out=dst[:, 1, d:N], in0=src[:, 0, 0:N - d], scalar=dim_, in1=dst[:, 1, d:N],
                                                                 op0=ALU.mult, op1=ALU.add), reads=[src_b, mats_b, dst_b], writes=[dst_b])
                    yield
                    src, dst, src_b, dst_b = dst, src, dst_b, src_b
                X, X_b = src, src_b
                k.op("dve", lambda e: e.tensor_copy(out=carry[:, q, :], in_=X[:, :, 128]), reads=[X_b], writes=[carry_b[q]])
                yield
                if full:
                    k.op("act", lambda e: e.copy(out=xp[b][:], in_=X[:, :, 0:128]), reads=[X_b], writes=[xp_b[b]])
                    for j in range(8):
                        pyi = PYA if j < 4 else PYB
                        o = ps[pyi][0:32, (j % 4) * 128:(j % 4 + 1) * 128]
                        k.op("pe", lambda e: e.matmul(o, CPm[b][:, j + 1, 0, :], xp[b][:, 0, :], start=True, stop=False),
                             reads=[CPm_b[b], xp_b[b]], writes=[psb[pyi]])
                        k.op("pe", lambda e: e.matmul(o, CPm[b][:, j + 1, 1, :], xp[b][:, 1, :], start=False, stop=False),
                             reads=[CPm_b[b], xp_b[b]], writes=[psb[pyi]])
                        for sx in range(j + 1):
                            k.op("pe", lambda e: e.matmul(o, KT[b][:, j - sx, :], uqs[:, hf, sx, :], start=False, stop=(sx == j)),
                                 reads=[KT_b[b], uq_b[b]], writes=[psb[pyi]])
                    yb = b
                    yq3 = yq[yb][:, :].rearrange("p (m s) -> p m s", s=8)
                    for jj, pyi in ((0, PYA), (1, PYB)):
                        k.op("dve", lambda e: e.scalar_tensor_tensor(
                            out=yq3[:, :, jj * 4:(jj + 1) * 4], in0=uqs[:, hf, jj * 4:(jj + 1) * 4, :].rearrange("p j m -> p m j"), scalar=s5d[0:32, 8 + q:9 + q],
                            in1=ps[pyi][0:32, :].rearrange("p (j m) -> p m j", j=4), op0=ALU.mult, op1=ALU.add),
                            reads=[uq_b[b], psb[pyi], s5d_b, yq_b[yb]], writes=[yq_b[yb]])
                    k.dma("sp", uT[32 * ql:32 * ql + 32, ct, hf * 1024:(hf + 1) * 1024], yq[yb][:], reads=[yq_b[yb]],
                          writes=[uT_b[ct * 4 + hf * 2], uT_b[ct * 4 + hf * 2 + 1]])

        for q0 in range(0, NQ, 2):
            gens = [pair_body(q0), pair_body(q0 + 1)]
            while gens:
                for gnr in list(gens):
                    try:
                        next(gnr)
                    except StopIteration:
                        gens.remove(gnr)

    def s5_post(gs):
        f1 = [sb("g1_%d" % i, [128, 512], F32, gs) for i in range(2)]
        f2 = [sb("g2_%d" % i, [128, 512], F32, gs) for i in range(2)]
        f1_b, f2_b = k.bufs(2, "f1"), k.bufs(2, "f2")
        for ct in range(8):
            for tb in range(4):
                b = (ct * 4 + tb) % 2
                y = uT[:, ct, tb * 512:(tb + 1) * 512]
                yb_ = uT_b[ct * 4 + tb]
                k.op("dve", lambda e: e.tensor_tensor(out=f1[b][:], in0=y, in1=y, op=ALU.mult), reads=[yb_], writes=[f1_b[b]])
                k.op("dve", lambda e: e.tensor_scalar(out=f1[b][:], in0=f1[b][:], scalar1=0.044715, scalar2=1.0, op0=ALU.mult, op1=ALU.add),
                     reads=[f1_b[b]], writes=[f1_b[b]])
                k.op("pool", lambda e: e.tensor_tensor(out=f1[b][:], in0=f1[b][:], in1=y, op=ALU.mult), reads=[f1_b[b], yb_], writes=[f1_b[b]])
                k.op("act", lambda e: e.activation(out=f2[b][:], in_=f1[b][:], func=AF.Sigmoid, scale=1.5957691216057308),
                     reads=[f1_b[b]], writes=[f2_b[b]])
                k.op("pool", lambda e: e.tensor_tensor(out=y, in0=f2[b][:], in1=y, op=ALU.mult), reads=[f2_b[b], yb_], writes=[yb_])
        Wg = [sb("Wg%d" % i, [128, 8, 128], BF16, gs) for i in range(2)]
        Wg_b = k.bufs(2, "Wg")
        og = [sb("og%d" % i, [128, 512], BF16, gs) for i in range(2)]
        og_b = k.bufs(2, "og")
        for co in range(8):
            wbi = co % 2
            k.dma("pool", Wg[wbi][:], w_glu_d[:, co * 128:(co + 1) * 128].rearrange("(kc p) c -> p kc c", p=128), writes=[Wg_b[wbi]])
            for tb in range(4):
                b = (co * 4 + tb) % 2
                for ct in range(8):
                    k.op("pe", lambda e: e.matmul(ps[b][:, :], Wg[wbi][:, ct, :], uT[:, ct, tb * 512:(tb + 1) * 512], start=(ct == 0), stop=(ct == 7)),
                         reads=[Wg_b[wbi], uT_b[ct * 4 + tb]], writes=[psb[b]])
                k.op("act", lambda e: e.activation(out=f2[b][:], in_=ps[b][:, :], func=AF.Sigmoid), reads=[psb[b]], writes=[f2_b[b]])
                k.op("dve", lambda e: e.tensor_tensor(out=og[b][:], in0=f2[b][:], in1=uT[:, co, tb * 512:(tb + 1) * 512], op=ALU.mult),
                     reads=[f2_b[b], uT_b[co * 4 + tb]], writes=[og_b[b]])
                k.dma("sp", yssmT_d[co * 128:(co + 1) * 128, tb * 512:(tb + 1) * 512], og[b][:], reads=[og_b[b]], writes=[yssmT_b[co * 4 + tb]])

    yssmT_b = k.bufs(32, "yssmT")
    k.barrier()
    with ExitStack() as s5s:
        uT = sb("uT", [128, 8, T], BF16, s5s)
        uT_b = k.bufs(32, "uT")
        s5d = sb("s5d", [128, 40], F32, s5s)
        s5d_b = k.buf("s5d")
        k.dma("sp", s5d[:], s5d_d[:, :], writes=[s5d_b])
        Pre = sb("s5Pre", [128, 9, 32], F32, s5s)
        Pim = sb("s5Pim", [128, 9, 32], F32, s5s)
        Dre = sb("s5Dre", [128, 9, 32], F32, s5s)
        Dim = sb("s5Dim", [128, 9, 32], F32, s5s)
        nDim = sb("s5nDim", [128, 9, 32], F32, s5s)
        RBc = sb("s5RBc", [128, 32, 8, 2, 16], BF16, s5s)
        CPc = sb("s5CPc", [128, 32, 9, 2, 16], BF16, s5s)
        carry = sb("s5carry", [128, 32, 2], F32, s5s)
        carry_b = k.bufs(NQ, "carry")
        with ExitStack() as ss:
            mats_b = s5_setup(ss)
        k.barrier()
        if "s5mats" in dbg:
            dump("Pre", Pre[:, :, :].rearrange("p a b -> p (a b)"), [mats_b], 288)
            dump("Pim", Pim[:, :, :].rearrange("p a b -> p (a b)"), [mats_b], 288)
        with ExitStack() as us:
            s5_uproj(us)
        k.barrier()
        if "uT" in dbg:
            dump("uT", uT[:, 0, 0:512], [uT_b[0]], 512)
        for q in range(NQ):
            k.op("dve", lambda e: e.memset(carry[:, q, :], 0.0), writes=[carry_b[q]])
        if mode == "states":
            with ExitStack() as p1:
                s5_pairs(False, p1)
            k.barrier()
            bounce_o = dout("bounce", [128, AGW])
            k.dma("sp", bounce_o[:, 0:1024], S[:, :, :].rearrange("p h e -> p (h e)"), reads=S_b)
            k.dma("sp", bounce_o[:, 1024:AGW], carry[:, :, :].rearrange("p q r -> p (q r)"), reads=carry_b)
        if mode != "states":
            if mode == "fused":
                for q in range(NQ):
                    k.op("dve", lambda e: e.tensor_copy(out=carry[:, q, :], in_=carryP[:, q, :]), reads=[carryP_b], writes=[carry_b[q]])
            elif with_carry:
                with ExitStack() as gsx:
                    gath_d = din("gath", [NCORES * 128, AGW])
                    gb_ = k.buf("gath")
                    G = sb("G", [128, NCORES, AGW], F32, gsx)
                    G_b = k.buf("G")
                    k.dma("sp", G[:], gath_d[:, :].rearrange("(r p) w -> p r w", p=128), reads=[gb_], writes=[G_b])
                    X = sb("Xpre", [128, 1024], F32, gsx)
                    Xc = sb("Xcpre", [128, 32, 2], F32, gsx)
                    tq = sb("tqpre", [128, 4, 32], F32, gsx)
                    xb = k.buf("Xpre")
                    k.op("dve", lambda e: e.memset(X[:], 0.0), writes=[xb])
                    k.op("dve", lambda e: e.memset(Xc[:], 0.0), reads=[xb], writes=[xb])
                    for h in range(H):
                        k.op("dve", lambda e: e.memset(S[:, h, :], 0.0), writes=[S_b[h]])
                    for q in range(NQ):
                        k.op("dve", lambda e: e.memset(carry[:, q, :], 0.0), writes=[carry_b[q]])
                    d8r, d8i = Dre[:, 8, :], Dim[:, 8, :]
                    for r in range(NCORES - 1):
                        for h in range(H):
                            k.op("dve", lambda e: e.scalar_tensor_tensor(out=X[:, h * 128:(h + 1) * 128], in0=X[:, h * 128:(h + 1) * 128], scalar=G2048[h],
                                                                         in1=G[:, r, h * 128:(h + 1) * 128], op0=ALU.mult, op1=ALU.add),
                                 reads=[xb, G_b], writes=[xb])
                        k.op("dve", lambda e: e.scalar_tensor_tensor(out=S[:, :, :].rearrange("p h e -> p (h e)"), in0=X[:], scalar=onehot[:, r + 1:r + 2],
                                                                     in1=S[:, :, :].rearrange("p h e -> p (h e)"), op0=ALU.mult, op1=ALU.add),
                             reads=[xb, tabs_b] + S_b, writes=S_b)
                        Er = G[:, r, 1024:AGW].rearrange("p (q r) -> p q r", r=2)
                        tt_ = lambda o, a, b2, op: k.op("dve", lambda e: e.tensor_tensor(out=o, in0=a, in1=b2, op=op), reads=[xb, G_b, mats_b], writes=[xb])
                        tt_(tq[:, 0, :], Xc[:, :, 0], d8r, ALU.mult)
                        tt_(tq[:, 1, :], Xc[:, :, 1], d8i, ALU.mult)
                        tt_(tq[:, 2, :], Xc[:, :, 1], d8r, ALU.mult)
                        tt_(tq[:, 3, :], Xc[:, :, 0], d8i, ALU.mult)
                        tt_(tq[:, 0, :], tq[:, 0, :], tq[:, 1, :], ALU.subtract)
                        tt_(tq[:, 2, :], tq[:, 2, :], tq[:, 3, :], ALU.add)
                        tt_(Xc[:, :, 0], tq[:, 0, :], Er[:, :, 0], ALU.add)
                        tt_(Xc[:, :, 1], tq[:, 2, :], Er[:, :, 1], ALU.add)
                        k.op("dve", lambda e: e.scalar_tensor_tensor(out=carry[:, :, :].rearrange("p q r -> p (q r)"), in0=Xc[:, :, :].rearrange("p q r -> p (q r)"),
                                                                     scalar=onehot[:, r + 1:r + 2], in1=carry[:, :, :].rearrange("p q r -> p (q r)"),
                                                                     op0=ALU.mult, op1=ALU.add), reads=[xb, tabs_b] + carry_b, writes=carry_b)
                    for h in range(H):
                        k.op("act", lambda e: e.copy(out=Sbf[:, h, :], in_=S[:, h, :]), reads=[S_b[h]], writes=[Sbf_b[h]])
                k.barrier()
            with ExitStack() as p2:
                s5_pairs(True, p2)
            k.barrier()
            if "ypre" in dbg:
                dump("ypre", uT[:, 0, 0:512], [uT_b[0]], 512)
            with ExitStack() as gs:
                s5_post(gs)
            k.barrier()

    if mode != "states":
        with ExitStack() as rs:
            retention(True, rs)
        k.barrier()

        if "yret" in dbg:
            with ExitStack() as sd:
                for h in range(H):
                    tmpb = sb("dbgy_b%d" % h, [128, T], BF16, sd)
                    tmpf = sb("dbgy_f%d" % h, [128, T], F32, sd)
                    tb, tb2 = k.buf(), k.buf()
                    k.dma("sp", tmpb[:], yretT_d[h * 128:(h + 1) * 128, :], reads=[yretT_b[h]], writes=[tb])
                    k.op("act", lambda e: e.copy(out=tmpf[:], in_=tmpb[:]), reads=[tb], writes=[tb2])
                    k.dma("sp", dbg["yret"][h * 128:(h + 1) * 128, :], tmpf[:], reads=[tb2])
            k.barrier()

        if "yssm" in dbg:
            with ExitStack() as sd:
                for co in range(8):
                    tmpb = sb("dbgs_b%d" % co, [128, T], BF16, sd)
                    tmpf = sb("dbgs_f%d" % co, [128, T], F32, sd)
                    tb_, tb2 = k.buf(), k.buf()
                    k.dma("sp", tmpb[:], yssmT_d[co * 128:(co + 1) * 128, :], reads=yssmT_b[co * 4:(co + 1) * 4], writes=[tb_])
                    k.op("act", lambda e: e.copy(out=tmpf[:], in_=tmpb[:]), reads=[tb_], writes=[tb2])
                    k.dma("sp", dbg["yssm"][co * 128:(co + 1) * 128, :], tmpf[:], reads=[tb2])
            k.barrier()


        ms.close()
        k.barrier()
        x1_b = k.bufs(NT, "x1")
        with ExitStack() as gs4:
            yr = sb("m_yr", [128, 8, 1024], BF16, gs4)
            ys = sb("m_ys", [128, 8, 1024], BF16, gs4)
            yr_b4, ys_b4 = k.buf("m_yr"), k.buf("m_ys")
            mT = sb("m_mT", [128, KC, 1024], BF16, gs4)
            mT_b = k.bufs(2, "m_mT")
            wma = [sb("m_wma%d" % i, [128, KC, 128], BF16, gs4) for i in range(2)]
            wmb = [sb("m_wmb%d" % i, [128, KC, 128], BF16, gs4) for i in range(2)]
            wa = [sb("m_wa%d" % i, [128, 8, 128], BF16, gs4) for i in range(2)]
            wb_ = [sb("m_wb%d" % i, [128, 8, 128], BF16, gs4) for i in range(2)]
            wma_b, wmb_b, wa_b, wbb_b = k.bufs(2, "wma"), k.bufs(2, "wmb"), k.bufs(2, "wa"), k.bufs(2, "wb")
            wo = [sb("m_wo%d" % i, [128, KC, 256], BF16, gs4) for i in range(2)]
            wo_b = k.bufs(2, "wo")
            sga = [sb("m_sga%d" % i, [128, 512], F32, gs4) for i in range(2)]
            sgb = [sb("m_sgb%d" % i, [128, 512], F32, gs4) for i in range(2)]
            sga_b, sgb_b = k.bufs(2, "sga"), k.bufs(2, "sgb")
            xo = [sb("m_xo%d" % i, [128, 256], F32, gs4) for i in range(2)]
            oo = [sb("m_oo%d" % i, [128, 256], F32, gs4) for i in range(2)]
            xo_b, oo_b = k.bufs(2, "xo"), k.bufs(2, "oo")

            def load_mw(j):
                b = j % 2
                k.dma("pool", wma[b][:], w_merge_d[:, j * 128:(j + 1) * 128].rearrange("(kc p) c -> p kc c", p=128), writes=[wma_b[b]])
                k.dma("pool", wmb[b][:], w_merge_d[:, D + j * 128:D + (j + 1) * 128].rearrange("(kc p) c -> p kc c", p=128), writes=[wmb_b[b]])
                k.dma("pool", wa[b][:], w_a_d[:, j * 128:(j + 1) * 128].rearrange("(kc p) c -> p kc c", p=128), writes=[wa_b[b]])
                k.dma("pool", wb_[b][:], w_b_d[:, j * 128:(j + 1) * 128].rearrange("(kc p) c -> p kc c", p=128), writes=[wbb_b[b]])

            it = 0
            for hf in range(2):
                t0 = hf * 1024
                k.dma("sp", yr[:], yretT_d[:, t0:t0 + 1024].rearrange("(hc p) t -> p hc t", p=128), reads=yretT_b, writes=[yr_b4])
                k.dma("sp", ys[:], yssmT_d[:, t0:t0 + 1024].rearrange("(hc p) t -> p hc t", p=128), reads=yssmT_b, writes=[ys_b4])
                load_mw(0)
                for j in range(KC):
                    b = j % 2
                    if j + 1 < KC:
                        load_mw(j + 1)
                    for tb in range(2):
                        pbase = 4 * (it % 2)
                        it += 1
                        tsl = slice(t0 + tb * 512, t0 + (tb + 1) * 512)
                        lsl = slice(tb * 512, (tb + 1) * 512)
                        hb_ = hT_b[(t0 + tb * 512) // 128:(t0 + tb * 512) // 128 + 4]
                        for kc in range(KC):
                            k.op("pe", lambda e: e.matmul(ps[pbase][:, :], wma[b][:, kc, :], hT[:, kc, tsl], start=(kc == 0), stop=(kc == KC - 1)),
                                 reads=[wma_b[b]] + hb_, writes=[psb[pbase]])
                        for kc in range(KC):
                            k.op("pe", lambda e: e.matmul(ps[pbase + 1][:, :], wmb[b][:, kc, :], hT[:, kc, tsl], start=(kc == 0), stop=(kc == KC - 1)),
                                 reads=[wmb_b[b]] + hb_, writes=[psb[pbase + 1]])
                        for hc in range(8):
                            k.op("pe", lambda e: e.matmul(ps[pbase + 2][:, :], wa[b][:, hc, :], yr[:, hc, lsl], start=(hc == 0), stop=(hc == 7)),
                                 reads=[wa_b[b], yr_b4], writes=[psb[pbase + 2]])
                        for hc in range(8):
                            k.op("pe", lambda e: e.matmul(ps[pbase + 3][:, :], wb_[b][:, hc, :], ys[:, hc, lsl], start=(hc == 0), stop=(hc == 7)),
                                 reads=[wbb_b[b], ys_b4], writes=[psb[pbase + 3]])
                        sb_i = it % 2
                        k.op("act", lambda e: e.activation(out=sga[sb_i][:], in_=ps[pbase][:, :], func=AF.Sigmoid), reads=[psb[pbase]], writes=[sga_b[sb_i]])
                        k.op("act", lambda e: e.activation(out=sgb[sb_i][:], in_=ps[pbase + 1][:, :], func=AF.Sigmoid), reads=[psb[pbase + 1]], writes=[sgb_b[sb_i]])
                        k.op("dve", lambda e: e.tensor_tensor(out=sga[sb_i][:], in0=sga[sb_i][:], in1=ps[pbase + 2][:, :], op=ALU.mult),
                             reads=[sga_b[sb_i], psb[pbase + 2]], writes=[sga_b[sb_i]])
                        k.op("dve", lambda e: e.tensor_tensor(out=sgb[sb_i][:], in0=sgb[sb_i][:], in1=ps[pbase + 3][:, :], op=ALU.mult),
                             reads=[sgb_b[sb_i], psb[pbase + 3]], writes=[sgb_b[sb_i]])
                        k.op("pool", lambda e: e.tensor_tensor(out=mT[:, j, lsl], in0=sga[sb_i][:], in1=sgb[sb_i][:], op=ALU.add),
                             reads=[sga_b[sb_i], sgb_b[sb_i]], writes=[mT_b[tb]])
                for nb in range(8):
                    wbi = nb % 2
                    k.dma("pool", wo[wbi][:], w_out_d[:, nb * 256:(nb + 1) * 256].rearrange("(kc p) c -> p kc c", p=128), writes=[wo_b[wbi]])
                    for tl in range(8):
                        pb = 2 * ((nb * 8 + tl) % 2)
                        xb_ = (nb * 8 + tl) % 2
                        row0 = t0 + tl * 128
                        k.dma("sp", xo[xb_][:], x_d[row0:row0 + 128, nb * 256:(nb + 1) * 256], writes=[xo_b[xb_]])
                        for j in range(KC):
                            k.op("pe", lambda e: e.matmul(ps[pb][:, 0:256], mT[:, j, tl * 128:(tl + 1) * 128], wo[wbi][:, j, :], start=(j == 0), stop=(j == KC - 1)),
                                 reads=[mT_b[tl // 4], wo_b[wbi]], writes=[psb[pb]])
                        k.op("dve", lambda e: e.tensor_tensor(out=oo[xb_][:], in0=xo[xb_][:], in1=ps[pb][:, 0:256], op=ALU.add),
                             reads=[xo_b[xb_], psb[pb]], writes=[oo_b[xb_]])
                        k.dma("sp", x1_d[row0:row0 + 128, nb * 256:(nb + 1) * 256], oo[xb_][:], reads=[oo_b[xb_]], writes=[x1_w[row0 // 128][nb]])
        hs.close()
        k.barrier()
        if "x1" in dbg:
            with ExitStack() as sd:
                for i in range(NT):
                    tmpf = sb("dbgx1_%d" % i, [128, D], F32, sd)
                    tb_ = k.buf()
                    k.dma("sp", tmpf[:], x1_d[i * 128:(i + 1) * 128, :], reads=x1_w[i], writes=[tb_])
                    k.dma("sp", dbg["x1"][i * 128:(i + 1) * 128, :], tmpf[:], reads=[tb_])
            k.barrier()


        out_d = dout("out", [T, D])
        with ExitStack() as g5:
            st5 = sb("st5", [128, 8, 4], F32, g5)
            st5_b = k.bufs(8, "st5")
            wr = sb("wr", [128, KC, 36], BF16, g5)
            wr_b = k.buf("wr")
            k.dma("pool", wr[:], w_rt_d[:, :].rearrange("(kc p) c -> p kc c", p=128), writes=[wr_b])
            wts = sb("wts", [128, 8, NE], F32, g5)
            wts_b = k.bufs(8, "wts")
            wtsb = sb("wtsb", [128, 8, NE], BF16, g5)
            wtsb_b = k.bufs(8, "wtsb")
            Ab = sb("Ab", [128, 8, 4], BF16, g5)
            Ab_b = k.bufs(8, "Ab")
            rank = sb("rank", [128, 8, 4], F32, g5)
            rank_b = k.bufs(8, "rank")
            rt = sb("rt", [128, 8, 40], F32, g5)
            rl = sb("rl", [128, 4, 36], F32, g5)
            rt_b = k.buf("rt")
            ltso = sb("ltso", [128, 256], BF16, g5)
            iota = sb("iota", [128, CG], F32, g5)
            cst_b = k.buf("moec")
            k.dma("pool", ltso[:], moec_d[:, 0:256], writes=[cst_b])
            k.dma("sp", iota[:], moec_d[:, 256:256 + CG], reads=[cst_b], writes=[cst_b])

            def norm_tile(src, src_b, xs_ap, xs_b, junk_ap, junk_b, stc, stc_b, dst3, dst_b, nT, pbanks):
                k.op("act", lambda e: e.activation(out=junk_ap, in_=src, func=AF.Square, accum_out=stc[:, 0:1]), reads=[src_b], writes=[junk_b, stc_b])
                k.op("act", lambda e: e.activation(out=stc[:, 1:2], in_=stc[:, 0:1], func=AF.Sqrt, scale=1.0 / D, bias=EPS), reads=[stc_b], writes=[stc_b])
                k.op("dve", lambda e: e.reciprocal(out=stc[:, 1:2], in_=stc[:, 1:2]), reads=[stc_b], writes=[stc_b])
                k.op("act", lambda e: e.activation(out=xs_ap, in_=src, func=AF.Copy, scale=stc[:, 1:2]), reads=[src_b, stc_b], writes=[xs_b])
                for half in range(2):
                    pbank = ps[pbanks[half]].bitcast(BF16)
                    for j in range(8):
                        kc = half * 8 + j
                        k.op("pe", lambda e: e.transpose(out=pbank[:, j * 128:(j + 1) * 128], in_=xs_ap[:, kc * 128:(kc + 1) * 128], identity=identb[:]),
                             reads=[xs_b, identb_b], writes=[psb[pbanks[half]]])
                    k.op("dve", lambda e: e.tensor_tensor(out=dst3[:, half * 8:(half + 1) * 8, :], in0=pbank[:, :].rearrange("p (j t) -> p j t", t=128),
                                                          in1=nT[:, half * 8:(half + 1) * 8].unsqueeze(2).broadcast_to([128, 8, 128]), op=ALU.mult),
                         reads=[psb[pbanks[half]], small_b], writes=[dst_b])

            for hf in range(2):
                t0 = hf * 1024
                with ExitStack() as gm:
                    hn = sb("hn%d" % hf, [128, 8, D], BF16, gm)
                    hn_b = k.bufs(8, "hn")
                    hrt = [sb("hrt%d_%d" % (hf, i), [128, KC, 128], BF16, gm) for i in range(2)]
                    hrt_b = k.bufs(2, "hrt")
                    xt5 = [sb("xt5_%d_%d" % (hf, i), [128, D], F32, gm) for i in range(2)]
                    xt5_b = k.bufs(2, "xt5")
                    Gw = sb("Gw%d" % hf, [128, KC, FE], BF16, gm)
                    Uw = sb("Uw%d" % hf, [128, KC, FE], BF16, gm)
                    Dw = sb("Dw%d" % hf, [128, 4, D], BF16, gm)
                    Gw_b, Uw_b, Dw_b = k.buf("Gw"), k.buf("Uw"), k.buf("Dw")
                    sgT = sb("sgT%d" % hf, [128, 4, CG], BF16, gm)
                    aT = sb("aT%d" % hf, [128, 4, CG], BF16, gm)
                    sgT_b, aT_b = k.bufs(4, "sgT"), k.bufs(4, "aT")
                    xg = sb("xg%d" % hf, [128, KC * CG], BF16, gm)
                    xg3 = xg[:, :].rearrange("p (kc c) -> p kc c", c=CG)
                    accgb = xg[:, :].rearrange("p (b d) -> p b d", d=D)
                    xg_b = k.buf("xg")
                    Selg = sb("Selg%d" % hf, [128, 8, CG], BF16, gm)
                    Selg_b = k.bufs(8, "Selg")
                    SelgT = sb("SelgT%d" % hf, [128, 3, 8, 128], BF16, gm)
                    SelgT_b = k.bufs(3, "SelgT")
                    accg = sb("accg%d" % hf, [128, 3, D], F32, gm)
                    accg_b = k.bufs(3, "accg")
                    wtsg = sb("wtsg%d" % hf, [128, 3, 8], F32, gm)
                    wtsg_b = k.buf("wtsg")
                    for tl in range(8):
                        b = tl % 2
                        gt = t0 // 128 + tl
                        k.dma("sp", xt5[b][:], x1_d[gt * 128:(gt + 1) * 128, :], reads=x1_w[gt], writes=[xt5_b[b]])
                        norm_tile(xt5[b][:], xt5_b[b], hn[:, tl, :], hn_b[tl], xg[:, 0:D], xg_b, st5[:, tl, :], st5_b[tl], hrt[b][:, :, :], hrt_b[b], nffnT, (6, 7))
                        R = rt[:, tl, :]
                        for kc in range(KC):
                            k.op("pe", lambda e: e.matmul(ps[5][:, 0:36], hrt[b][:, kc, :], wr[:, kc, :], start=(kc == 0), stop=(kc == KC - 1)),
                                 reads=[hrt_b[b], wr_b], writes=[psb[5]])
                        L = rl[:, tl % 4, :]
                        rb_ = [rt_b]
                        k.op("dve", lambda e: e.tensor_tensor(out=L, in0=ps[5][:, 0:36], in1=rbias, op=ALU.add), reads=[psb[5], small_b] + rb_, writes=rb_)
                        lg, le = L[:, 0:4], L[:, 4:36]
                        gmax, ngmax, gsum, gw, m1, m2, d12, w1, w2 = [R[:, i:i + 1] for i in range(9)]
                        ohg, pen = R[:, 12:16], R[:, 16:20]
                        k.op("dve", lambda e: e.reduce_max(out=gmax, in_=lg, axis=mybir.AxisListType.X), reads=rb_, writes=rb_)
                        k.op("dve", lambda e: e.tensor_scalar(out=ngmax, in0=gmax, scalar1=-1.0, scalar2=None, op0=ALU.mult), reads=rb_, writes=rb_)
                        k.op("act", lambda e: e.activation(out=R[:, 20:24], in_=lg, func=AF.Exp, bias=ngmax, scale=1.0, accum_out=gsum), reads=rb_, writes=rb_)
                        k.op("dve", lambda e: e.reciprocal(out=gw, in_=gsum), reads=rb_, writes=rb_)
                        k.op("dve", lambda e: e.tensor_scalar(out=ohg, in0=lg, scalar1=gmax, scalar2=None, op0=ALU.is_equal), reads=rb_, writes=rb_)
                        k.op("dve", lambda e: e.tensor_scalar(out=pen, in0=ohg, scalar1=-1.0, scalar2=1e30, op0=ALU.add, op1=ALU.mult), reads=rb_, writes=rb_)
                        le3 = le.rearrange("p (g x) -> p g x", x=8)
                        k.op("dve", lambda e: e.tensor_tensor(out=le3, in0=le3, in1=pen.unsqueeze(2).broadcast_to([128, 4, 8]), op=ALU.add), reads=rb_, writes=rb_)
                        k.op("dve", lambda e: e.reduce_max(out=m1, in_=le, axis=mybir.AxisListType.X), reads=rb_, writes=rb_)
                        mk1, mk2 = wts[:, tl, :], L[:, 4:36]
                        k.op("dve", lambda e: e.tensor_scalar(out=mk1, in0=le, scalar1=m1, scalar2=None, op0=ALU.is_equal), reads=rb_, writes=rb_ + [wts_b[tl]])
                        k.op("dve", lambda e: e.scalar_tensor_tensor(out=le, in0=mk1, scalar=-1e30, in1=le, op0=ALU.mult, op1=ALU.add), reads=rb_ + [wts_b[tl]], writes=rb_)
                        k.op("dve", lambda e: e.reduce_max(out=m2, in_=le, axis=mybir.AxisListType.X), reads=rb_, writes=rb_)
                        k.op("dve", lambda e: e.tensor_scalar(out=mk2, in0=le, scalar1=m2, scalar2=None, op0=ALU.is_equal), reads=rb_, writes=rb_)
                        k.op("dve", lambda e: e.tensor_tensor(out=d12, in0=m1, in1=m2, op=ALU.subtract), reads=rb_, writes=rb_)
                        k.op("act", lambda e: e.activation(out=w1, in_=d12, func=AF.Sigmoid), reads=rb_, writes=rb_)
                        k.op("act", lambda e: e.activation(out=w2, in_=d12, func=AF.Sigmoid, scale=-1.0), reads=rb_, writes=rb_)
                        k.op("dve", lambda e: e.tensor_tensor(out=w1, in0=w1, in1=gw, op=ALU.mult), reads=rb_, writes=rb_)
                        k.op("dve", lambda e: e.tensor_tensor(out=w2, in0=w2, in1=gw, op=ALU.mult), reads=rb_, writes=rb_)
                        k.op("dve", lambda e: e.tensor_scalar(out=mk1, in0=mk1, scalar1=w1, scalar2=None, op0=ALU.mult), reads=rb_ + [wts_b[tl]], writes=[wts_b[tl]])
                        k.op("dve", lambda e: e.scalar_tensor_tensor(out=mk1, in0=mk2, scalar=w2, in1=mk1, op0=ALU.mult, op1=ALU.add), reads=rb_ + [wts_b[tl]], writes=[wts_b[tl]])
                        k.op("act", lambda e: e.copy(out=wtsb[:, tl, :], in_=wts[:, tl, :]), reads=[wts_b[tl]], writes=[wtsb_b[tl]])
                        k.op("act", lambda e: e.copy(out=Ab[:, tl, :], in_=ohg), reads=rb_, writes=[Ab_b[tl]])
                    if hf == 0 and "wts" in dbg:
                        dump("wts", wts[:, :, :].rearrange("p a b -> p (a b)"), wts_b, 256)
                    for tl in range(8):
                        k.op("pe", lambda e: e.matmul(ps[5][:, 0:4], ltso[:, 0:128], Ab[:, tl, :], start=True, stop=(tl == 0)), reads=[Ab_b[tl], cst_b], writes=[psb[5]])
                        for tp in range(tl):
                            k.op("pe", lambda e: e.matmul(ps[5][:, 0:4], ltso[:, 128:256], Ab[:, tp, :], start=False, stop=(tp == tl - 1)), reads=[Ab_b[tp], cst_b], writes=[psb[5]])
                        k.op("act", lambda e: e.copy(out=rank[:, tl, :], in_=ps[5][:, 0:4]), reads=[psb[5]], writes=[rank_b[tl]])
                    for g in range(4):
                        for tl in range(8):
                            eng = "dve" if tl % 2 == 0 else "pool"
                            k.op(eng, lambda e: e.tensor_scalar(out=Selg[:, tl, :], in0=iota[:], scalar1=rank[:, tl, g:g + 1], scalar2=rt[:, tl, 12 + g:13 + g],
                                                                op0=ALU.is_equal, op1=ALU.mult), reads=[rank_b[tl], rt_b, cst_b], writes=[Selg_b[tl]])
                        for blk in range(3):
                            bank = ps[blk].bitcast(BF16)
                            for tl in range(8):
                                k.op("pe", lambda e: e.transpose(out=bank[:, tl * 128:(tl + 1) * 128], in_=Selg[:, tl, blk * 128:(blk + 1) * 128], identity=identb[:]),
                                     reads=[Selg_b[tl], identb_b], writes=[psb[blk]])
                            k.op("act", lambda e: e.copy(out=SelgT[:, blk, :, :], in_=bank[:, :].rearrange("p (t c) -> p t c", c=128)), reads=[psb[blk]], writes=[SelgT_b[blk]])
                        for blk in range(3):
                            for tl in range(8):
                                k.op("pe", lambda e: e.matmul(ps[3][:, blk * 8:(blk + 1) * 8], Selg[:, tl, blk * 128:(blk + 1) * 128], wtsb[:, tl, g * 8:(g + 1) * 8],
                                                              start=(tl == 0), stop=(tl == 7)), reads=[Selg_b[tl], wtsb_b[tl]], writes=[psb[3]])
                        k.op("act", lambda e: e.copy(out=wtsg[:, :, :], in_=ps[3][:, 0:24].rearrange("p (b x) -> p b x", x=8)), reads=[psb[3]], writes=[wtsg_b])
                        for r4 in range(4):
                            for kcl in range(4):
                                kc = r4 * 4 + kcl
                                bank = 4 + kcl
                                for tl in range(8):
                                    k.op("pe", lambda e: e.matmul(ps[bank][:, 0:CG], hn[:, tl, kc * 128:(kc + 1) * 128], Selg[:, tl, :], start=(tl == 0), stop=(tl == 7)),
                                         reads=[hn_b[tl], Selg_b[tl]], writes=[psb[bank]])
                                if kcl % 2 == 0:
                                    k.op("act", lambda e: e.activation(out=xg3[:, kc, :], in_=ps[bank][:, 0:CG], func=AF.Copy, scale=nffnT[:, kc:kc + 1]),
                                         reads=[psb[bank], small_b, xg_b], writes=[xg_b])
                                else:
                                    k.op("dve", lambda e: e.tensor_scalar(out=xg3[:, kc, :], in0=ps[bank][:, 0:CG], scalar1=nffnT[:, kc:kc + 1], scalar2=None, op0=ALU.mult),
                                         reads=[psb[bank], small_b, xg_b], writes=[xg_b])
                        for blk in range(3):
                            k.op("pool", lambda e: e.memset(accg[:, blk, :], 0.0), writes=[accg_b[blk]])
                        for el in range(8):
                            ex = g * 8 + el
                            k.dma("pool", Gw[:], w_eg_d[ex].rearrange("(kc p) f -> p kc f", p=128), writes=[Gw_b])
                            k.dma("pool", Uw[:], w_eu_d[ex].rearrange("(kc p) f -> p kc f", p=128), writes=[Uw_b])
                            k.dma("pool", Dw[:], w_ed_d[ex].rearrange("(fc p) d -> p fc d", p=128), writes=[Dw_b])
                            for ft in range(4):
                                for kc in range(KC):
                                    k.op("pe", lambda e: e.matmul(ps[ft][:, 0:CG], Gw[:, kc, ft * 128:(ft + 1) * 128], xg3[:, kc, :], start=(kc == 0), stop=(kc == KC - 1)),
                                         reads=[Gw_b, xg_b], writes=[psb[ft]])
                                k.op("act", lambda e: e.activation(out=sgT[:, ft, :], in_=ps[ft][:, 0:CG], func=AF.Silu), reads=[psb[ft]], writes=[sgT_b[ft]])
                            for ft in range(4):
                                for kc in range(KC):
                                    k.op("pe", lambda e: e.matmul(ps[4 + ft][:, 0:CG], Uw[:, kc, ft * 128:(ft + 1) * 128], xg3[:, kc, :], start=(kc == 0), stop=(kc == KC - 1)),
                                         reads=[Uw_b, xg_b], writes=[psb[4 + ft]])
                                k.op("dve", lambda e: e.tensor_tensor(out=aT[:, ft, :], in0=sgT[:, ft, :], in1=ps[4 + ft][:, 0:CG], op=ALU.mult),
                                     reads=[psb[4 + ft], sgT_b[ft]], writes=[aT_b[ft]])
                            cnt = 0
                            for blk in range(3):
                                for nb in range(4):
                                    pb = cnt % 4
                                    cnt += 1
                                    for fc in range(4):
                                        k.op("pe", lambda e: e.matmul(ps[pb][:, :], aT[:, fc, blk * 128:(blk + 1) * 128], Dw[:, fc, nb * 512:(nb + 1) * 512],
                                                                      start=(fc == 0), stop=(fc == 3)), reads=[Dw_b] + aT_b, writes=[psb[pb]])
                                    k.op("dve", lambda e: e.scalar_tensor_tensor(out=accg[:, blk, nb * 512:(nb + 1) * 512], in0=ps[pb][:, :], scalar=wtsg[:, blk, el:el + 1],
                                                                                 in1=accg[:, blk, nb * 512:(nb + 1) * 512], op0=ALU.mult, op1=ALU.add),
                                         reads=[psb[pb], wtsg_b, accg_b[blk]], writes=[accg_b[blk]])
                        for blk in range(3):
                            k.op("act", lambda e: e.copy(out=accgb[:, blk, :], in_=accg[:, blk, :]), reads=[accg_b[blk], xg_b], writes=[xg_b])
                        cnt = 0
                        for tl in range(8):
                            b = tl % 2
                            gt = t0 // 128 + tl
                            k.dma("sp", xt5[b][:], x1_d[gt * 128:(gt + 1) * 128, :], reads=x1_w[gt], writes=[xt5_b[b]])
                            for nb in range(4):
                                pb = 4 + cnt % 4
                                cnt += 1
                                for blk in range(3):
                                    k.op("pe", lambda e: e.matmul(ps[pb][:, :], SelgT[:, blk, tl, :], accgb[:, blk, nb * 512:(nb + 1) * 512], start=(blk == 0), stop=(blk == 2)),
                                         reads=[SelgT_b[blk], xg_b], writes=[psb[pb]])
                                k.op("dve", lambda e: e.tensor_tensor(out=xt5[b][:, nb * 512:(nb + 1) * 512], in0=xt5[b][:, nb * 512:(nb + 1) * 512], in1=ps[pb][:, :], op=ALU.add),
                                     reads=[psb[pb], xt5_b[b]], writes=[xt5_b[b]])
                            k.dma("sp", x1_d[gt * 128:(gt + 1) * 128, :], xt5[b][:], reads=[xt5_b[b]], writes=x1_w[gt])
                k.barrier()
                gpx = ExitStack()
                acc = sb("acc%d" % hf, [128, 8, D], F32, gpx)
                acc_b = k.bufs(8, "acc")
                hh = sb("hh%d" % hf, [128, KC, 1024], BF16, gpx)
                hh_b = k.bufs(8, "hh")
                xs5 = sb("xs5_%d" % hf, [128, D], BF16, gpx)
                junk5 = sb("junk5_%d" % hf, [128, D], BF16, gpx)
                xs5_b, junk5_b = k.buf("xs5"), k.buf("junk5")
                for tl in range(8):
                    k.dma("sp", acc[:, tl, :], x1_d[t0 + tl * 128:t0 + (tl + 1) * 128, :], reads=x1_w[(t0 // 128) + tl], writes=[acc_b[tl]])
                if hf == 0 and "x2" in dbg:
                    for tl in range(8):
                        k.dma("sp", dbg["x2"][tl * 128:(tl + 1) * 128, :], acc[:, tl, :], reads=[acc_b[tl]])

                def norm_to_hh(tl, nT):
                    norm_tile(acc[:, tl, :], acc_b[tl], xs5[:], xs5_b, junk5[:], junk5_b, st5[:, tl, :], st5_b[tl],
                              hh[:, :, tl * 128:(tl + 1) * 128], hh_b[tl], nT, (6, 7))
                with ExitStack() as gp:
                    pT = sb("pT%d" % hf, [128, 2, 1024], BF16, gp)
                    pT_b = k.bufs(8, "pT")
                    pld = [sb("pld%d_%d" % (hf, i), [128, 256], F32, gp) for i in range(2)]
                    plb = [sb("plb%d_%d" % (hf, i), [128, 256], BF16, gp) for i in range(2)]
                    pld_b, plb_b = k.bufs(2, "pld"), k.bufs(2, "plb")
                    wpg = [sb("wpg%d_%d" % (hf, i), [128, KC, 256], BF16, gp) for i in range(2)]
                    wpl = [sb("wpl%d_%d" % (hf, i), [128, 2, 256], BF16, gp) for i in range(2)]
                    wpg_b, wpl_b = k.bufs(2, "wpg"), k.bufs(2, "wpl")
                    sg5 = [sb("sg5_%d_%d" % (hf, i), [128, 256], F32, gp) for i in range(2)]
                    sg5_b = k.bufs(2, "sg5")
                    nfb = sb("nfb%d" % hf, [128, D], F32, gp)
                    nfb_b = k.buf("nfb")
                    k.dma("sp", nfb[:], nfb_d[:, :], writes=[nfb_b])
                    ot = [sb("ot%d_%d" % (hf, i), [128, D], F32, gp) for i in range(2)]
                    ot_b = k.bufs(2, "ot")
                    for tl in range(8):
                        b = tl % 2
                        norm_to_hh(tl, npleT)
                        k.dma("sp", pld[b][:], p_d[t0 + tl * 128:t0 + (tl + 1) * 128, :], writes=[pld_b[b]])
                        k.op("act", lambda e: e.copy(out=plb[b][:], in_=pld[b][:]), reads=[pld_b[b]], writes=[plb_b[b]])
                        pbank = ps[5].bitcast(BF16)
                        for c2 in range(2):
                            k.op("pe", lambda e: e.transpose(out=pbank[:, c2 * 128:(c2 + 1) * 128], in_=plb[b][:, c2 * 128:(c2 + 1) * 128], identity=identb[:]),
                                 reads=[plb_b[b], identb_b], writes=[psb[5]])
                        k.op("act", lambda e: e.copy(out=pT[:, :, tl * 128:(tl + 1) * 128], in_=pbank[:, 0:256].rearrange("p (c t) -> p c t", t=128)),
                             reads=[psb[5]], writes=[pT_b[tl]])
                    cnt = 0
                    for nb in range(8):
                        wbi = nb % 2
                        k.dma("pool", wpg[wbi][:], w_pg_d[:, nb * 256:(nb + 1) * 256].rearrange("(kc p) c -> p kc c", p=128), writes=[wpg_b[wbi]])
                        k.dma("pool", wpl[wbi][:], w_ple_d[:, nb * 256:(nb + 1) * 256].rearrange("(kc p) c -> p kc c", p=128), writes=[wpl_b[wbi]])
                        for tl in range(8):
                            pb = 2 * (cnt % 2)
                            sb_i = cnt % 2
                            cnt += 1
                            for kc in range(KC):
                                k.op("pe", lambda e: e.matmul(ps[pb][:, 0:256], hh[:, kc, tl * 128:(tl + 1) * 128], wpg[wbi][:, kc, :], start=(kc == 0), stop=(kc == KC - 1)),
                                     reads=[hh_b[tl], wpg_b[wbi]], writes=[psb[pb]])
                            for c2 in range(2):
                                k.op("pe", lambda e: e.matmul(ps[pb + 1][:, 0:256], pT[:, c2, tl * 128:(tl + 1) * 128], wpl[wbi][:, c2, :], start=(c2 == 0), stop=(c2 == 1)),
                                     reads=[pT_b[tl], wpl_b[wbi]], writes=[psb[pb + 1]])
                            k.op("act", lambda e: e.activation(out=sg5[sb_i][:], in_=ps[pb][:, 0:256], func=AF.Sigmoid), reads=[psb[pb]], writes=[sg5_b[sb_i]])
                            k.op("dve", lambda e: e.tensor_tensor(out=sg5[sb_i][:], in0=sg5[sb_i][:], in1=ps[pb + 1][:, 0:256], op=ALU.mult),
                                 reads=[sg5_b[sb_i], psb[pb + 1]], writes=[sg5_b[sb_i]])
                            k.op("pool", lambda e: e.tensor_tensor(out=acc[:, tl, nb * 256:(nb + 1) * 256], in0=acc[:, tl, nb * 256:(nb + 1) * 256], in1=sg5[sb_i][:], op=ALU.add),
                                 reads=[sg5_b[sb_i], acc_b[tl]], writes=[acc_b[tl]])
                    for tl in range(8):
                        b = tl % 2
                        k.op("act", lambda e: e.activation(out=junk5[:], in_=acc[:, tl, :], func=AF.Square, accum_out=st5[:, tl, 2:3]),
                             reads=[acc_b[tl]], writes=[junk5_b, st5_b[tl]])
                        k.op("act", lambda e: e.activation(out=st5[:, tl, 3:4], in_=st5[:, tl, 2:3], func=AF.Sqrt, scale=1.0 / D, bias=EPS),
                             reads=[st5_b[tl]], writes=[st5_b[tl]])
                        k.op("dve", lambda e: e.reciprocal(out=st5[:, tl, 3:4], in_=st5[:, tl, 3:4]), reads=[st5_b[tl]], writes=[st5_b[tl]])
                        k.op("dve", lambda e: e.scalar_tensor_tensor(out=ot[b][:], in0=acc[:, tl, :], scalar=st5[:, tl, 3:4], in1=nfb[:], op0=ALU.mult, op1=ALU.mult),
                             reads=[acc_b[tl], st5_b[tl], nfb_b], writes=[ot_b[b]])
                        k.dma("sp", out_d[t0 + tl * 128:t0 + (tl + 1) * 128, :], ot[b][:], reads=[ot_b[b]])
                k.barrier()
                gpx.close()

    else:
        ms.close()
        hs.close()
    k.finish()
    cs.close()
    es.close()
    return nc


def prefix_tables(core):
    half = DH // 2
    inv_freq = (np.float32(10000.0) ** (-np.arange(half, dtype=np.float32) / np.float32(half))).astype(np.float32)
    npos = NPT * 128
    pos = np.arange(npos).astype(np.float32)
    ang = (pos[:, None] * inv_freq[None, :]).astype(np.float32).astype(np.float64)
    ropep = np.concatenate([np.cos(ang), np.sin(ang)], axis=1).astype(np.float32).reshape(NPT, 128, 128)
    g = _gammas()
    t0 = core * T
    t = np.arange(npos)
    dist = (t0 - 1 - t).astype(np.float64)
    kd = np.zeros((npos, H), np.float64)
    valid = t < t0
    for h in range(H):
        kd[valid, h] = g[h] ** dist[valid] * DH ** -0.5
    kdecp = kd.reshape(NPT, 128, H).transpose(1, 0, 2).reshape(128, NPT * H).astype(np.float32)
    return np.ascontiguousarray(ropep), np.ascontiguousarray(kdecp)


def s5_host_layout(inp):
    def st(a):
        return a.reshape(32, 2, 64).transpose(1, 2, 0).reshape(128, 32)
    lamre = st(inp["ssm_lam_re"][0])
    lamim = st(inp["ssm_lam_im"][0])
    logdt = st(np.broadcast_to(inp["ssm_log_dt"][0][:, None], (64, 64)))
    def stb(a):
        return a.reshape(32, 2, 64, 16).transpose(1, 2, 0, 3).reshape(128, 32 * 16)
    bre = stb(inp["ssm_b_re"][0])
    bim = stb(inp["ssm_b_im"][0])
    cre = stb(inp["ssm_c_re"][0].transpose(0, 2, 1))
    cim = stb(inp["ssm_c_im"][0].transpose(0, 2, 1))
    s5p = np.ascontiguousarray(np.concatenate([lamre, lamim, logdt, bre, bim, cre, cim], axis=1).astype(np.float32))
    d = inp["ssm_d"][0]
    dfull = d.reshape(8, 128).T
    dpair = np.zeros((128, 32), np.float32)
    dpair[0:32, :] = d.reshape(32, 32).T
    s5d = np.ascontiguousarray(np.concatenate([dfull, dpair], axis=1).astype(np.float32))
    return s5p, s5d


def _bf16(a):
    return np.ascontiguousarray(a).astype(ml_dtypes.bfloat16)


def make_in_maps(inp, fused=False):
    x = np.ascontiguousarray(inp["x"][0])
    nmixT = np.ascontiguousarray(inp["norm_mix"][0].reshape(KC, 128).T)
    gnw_b = np.broadcast_to(inp["ret_gn_w"][0][None, :], (128, RW))
    nffnT = inp["norm_ffn"][0].reshape(KC, 128).T
    npleT = inp["norm_ple"][0].reshape(KC, 128).T
    rb = np.broadcast_to(np.concatenate([inp["b_router_group"][0], inp["b_router_expert"][0]])[None, :], (128, 36))
    small = np.ascontiguousarray(np.concatenate([nmixT, gnw_b, nffnT, npleT, rb], axis=1).astype(np.float32))
    ident = np.eye(128, dtype=np.float32)
    s5p, s5d = s5_host_layout(inp)
    ar = np.arange(128)
    lts = (ar[:, None] < ar[None, :]).astype(np.float32)
    moec = np.ascontiguousarray(np.concatenate([lts, np.ones((128, 128), np.float32),
                                                np.broadcast_to(np.arange(CG, dtype=np.float32)[None, :], (128, CG))], axis=1))
    maps = []
    for c in range(NCORES):
        maps.append({
            "x": np.ascontiguousarray(x[c * T:(c + 1) * T]),
            "w_in": np.ascontiguousarray(inp["w_in"][0]),
            "tabs": host_tables(c),
            "small": small,
            "identb": ident,
            "s5p": s5p,
            "s5d": s5d,
            "w_glu": np.ascontiguousarray(inp["w_glu"][0]),
            "w_merge": np.ascontiguousarray(inp["w_merge"][0]),
            "w_a": np.ascontiguousarray(inp["w_branch_a"][0]),
            "w_b": np.ascontiguousarray(inp["w_branch_b"][0]),
            "w_out": np.ascontiguousarray(inp["w_out"][0]),
            "w_rt": np.ascontiguousarray(np.concatenate([inp["w_router_group"][0], inp["w_router_expert"][0]], axis=1)),
            "w_eg": np.ascontiguousarray(inp["w_exp_gate"][0]),
            "w_eu": np.ascontiguousarray(inp["w_exp_up"][0]),
            "w_ed": np.ascontiguousarray(inp["w_exp_down"][0]),
            "w_pg": np.ascontiguousarray(inp["w_ple_gate"][0]),
            "w_ple": np.ascontiguousarray(inp["w_ple"][0]),
            "p": np.ascontiguousarray(inp["p"][0, 0][c * T:(c + 1) * T]),
            "nfb": np.ascontiguousarray(np.broadcast_to(inp["norm_f"][None, :], (128, D))),
            "moec": moec,
        })
        if fused:
            ropep, kdecp = prefix_tables(c)
            maps[-1]["xfull"] = x
            maps[-1]["ropep"] = ropep
            maps[-1]["kdecp"] = kdecp
    return maps


def kernel(**inp):
    maps = make_in_maps(inp, fused=True)
    nc = build(mode="fused")
    res = run_bass_kernel_spmd(nc, maps, core_ids=list(range(NCORES)))
    return np.concatenate([r["out"] for r in res.results], axis=0)[None]
```

```python
import numpy as np
import ml_dtypes
from contextlib import ExitStack
import concourse.bass as bass
import concourse.mybir as mybir
from concourse.bass_utils import run_bass_kernel_spmd

F32 = mybir.dt.float32
BF16 = mybir.dt.bfloat16
AF = mybir.ActivationFunctionType
ALU = mybir.AluOpType

NCORES = 8
SEQ = 16384
T = SEQ // NCORES
NT = T // 128
D = 2048
KC = D // 128
H = 8
DH = 128
RW = 1024
EPS = 1e-6
PI = float(np.pi)


class Buf:
    __slots__ = ("name", "w", "r")

    def __init__(self, name):
        self.name = name
        self.w = None
        self.r = {}


class K:
    NDSEM = 24

    def __init__(self, nc, es):
        self.nc = nc
        self.es = es
        self.eng = {"pe": nc.tensor, "act": nc.scalar, "dve": nc.vector, "pool": nc.gpsimd, "sp": nc.sync}
        self.sem = {n: es.enter_context(nc.semaphore("s_" + n)) for n in self.eng}
        self.cnt = {n: 0 for n in self.eng}
        self.known = {n: {} for n in self.eng}
        self.dsem = {q: [es.enter_context(nc.semaphore("d_%s%d" % (q, i))) for i in range(self.NDSEM)]
                     for q in ("sp", "pool", "act")}
        self.duse = {q: [0] * self.NDSEM for q in self.dsem}
        self.dnext = {q: 0 for q in self.dsem}
        self.nbuf = 0

    def buf(self, name=None):
        self.nbuf += 1
        return Buf(name or ("b%d" % self.nbuf))

    def bufs(self, n, name="b"):
        return [self.buf("%s%d" % (name, i)) for i in range(n)]

    def _semof(self, key):
        return self.sem[key] if isinstance(key, str) else self.dsem[key[1]][key[2]]

    def _wait(self, e, deps):
        need = {}
        for t in deps:
            if t is None:
                continue
            k, v = t
            if need.get(k, 0) < v:
                need[k] = v
        kn = self.known[e]
        for k, v in need.items():
            if kn.get(k, 0) >= v:
                continue
            self.eng[e].wait_ge(self._semof(k), v)
            kn[k] = v

    SAME_ENGINE_WAIT = True

    def _deps(self, e, reads, writes):
        deps = []
        skip_same = (e == "pe") or (not self.SAME_ENGINE_WAIT)
        for b in reads:
            if b.w is not None and not (skip_same and b.w[0] == e):
                deps.append(b.w)
        for b in writes:
            if b.w is not None and not (skip_same and b.w[0] == e):
                deps.append(b.w)
            for k, v in b.r.items():
                if k == e:
                    continue
                deps.append((k, v))
        return deps

    def op(self, e, fn, reads=(), writes=()):
        self._wait(e, self._deps(e, reads, writes))
        ins = fn(self.eng[e])
        self.cnt[e] += 1
        c = self.cnt[e]
        ins.then_inc(self.sem[e], 1)
        for b in reads:
            if b.r.get(e, 0) < c:
                b.r[e] = c
        for b in writes:
            b.w = (e, c)
            b.r = {}
        return ins

    def dma(self, q, out, in_, reads=(), writes=()):
        deps = self._deps(q, reads, writes)
        idx = self.dnext[q]
        self.dnext[q] = (idx + 1) % self.NDSEM
        key = ("d", q, idx)
        if self.duse[q][idx] > 0:
            deps.append((key, 16 * self.duse[q][idx]))
        self._wait(q, deps)
        ins = self.eng[q].dma_start(out=out, in_=in_)
        self.duse[q][idx] += 1
        v = 16 * self.duse[q][idx]
        ins.then_inc(self.dsem[q][idx], 16)
        for b in reads:
            if b.r.get(key, 0) < v:
                b.r[key] = v
        for b in writes:
            b.w = (key, v)
            b.r = {}
        return ins

    def qop(self, q, fn, reads=(), writes=()):
        deps = self._deps(q, reads, writes)
        idx = self.dnext[q]
        self.dnext[q] = (idx + 1) % self.NDSEM
        key = ("d", q, idx)
        if self.duse[q][idx] > 0:
            deps.append((key, 16 * self.duse[q][idx]))
        self._wait(q, deps)
        ins = fn(self.eng[q])
        self.duse[q][idx] += 1
        v = 16 * self.duse[q][idx]
        ins.then_inc(self.dsem[q][idx], 16)
        for b in reads:
            if b.r.get(key, 0) < v:
                b.r[key] = v
        for b in writes:
            b.w = (key, v)
            b.r = {}
        return ins

    def barrier(self):
        deps = [(n, c) for n, c in self.cnt.items() if c > 0]
        for q in self.dsem:
            for i, u in enumerate(self.duse[q]):
                if u > 0:
                    deps.append((("d", q, i), 16 * u))
        for e in self.eng:
            self._wait(e, [d for d in deps if d[0] != e])

    def finish(self):
        deps = [(n, c) for n, c in self.cnt.items() if c > 0 and n != "sp"]
        for q in self.dsem:
            for i, u in enumerate(self.duse[q]):
                if u > 0:
                    deps.append((("d", q, i), 16 * u))
        self._wait("sp", deps)


def _gammas():
    return 1.0 - np.exp2(-5.0 - np.arange(H, dtype=np.float64))


def host_tables(core):
    half = DH // 2
    inv_freq = (np.float32(10000.0) ** (-np.arange(half, dtype=np.float32) / np.float32(half))).astype(np.float32)
    pos = (core * T + np.arange(T)).astype(np.float32)
    ang = (pos[:, None] * inv_freq[None, :]).astype(np.float32).astype(np.float64)
    cos = np.cos(ang).astype(np.float32).reshape(NT, 128, half).transpose(1, 0, 2)
    sin = np.sin(ang).astype(np.float32).reshape(NT, 128, half).transpose(1, 0, 2)
    g = _gammas()
    a = np.arange(128)
    ka, qb = a[:, None], a[None, :]
    same = (ka // 64) == (qb // 64)
    earlier = (ka // 64) < (qb // 64)
    mask = np.zeros((128, H, 128), np.float64)
    for h in range(H):
        m = np.where(same, g[h] ** np.abs(qb - ka), np.where(earlier, g[h] ** (qb - ka).clip(0), 0.0))
        mask[:, h, :] = m * DH ** -0.5
    qdec = np.stack([g[h] ** (a + 1.0) for h in range(H)], axis=1)
    kdec = np.stack([g[h] ** (127.0 - a) * DH ** -0.5 for h in range(H)], axis=1)
    onehot = np.zeros((128, NCORES), np.float64)
    onehot[:, core] = 1.0
    tabs = np.concatenate([cos.reshape(128, -1), sin.reshape(128, -1), mask.reshape(128, -1), qdec, kdec, onehot],
                          axis=1).astype(np.float32)
    return np.ascontiguousarray(tabs)


TAB_COS = 0
TAB_SIN = TAB_COS + NT * 64
TAB_MASK = TAB_SIN + NT * 64
TAB_QDEC = TAB_MASK + H * 128
TAB_KDEC = TAB_QDEC + H
TAB_ONEHOT = TAB_KDEC + H
TAB_W = TAB_ONEHOT + NCORES
S5P_W = 96 + 4 * 512
NQ = 32
AGW = 1024 + 64
SMALL_W = 16 + 1024 + 16 + 16 + 36
NE = 32
NPT = (NCORES - 1) * NT
CG = 384
FE = 512


class _Done(Exception):
    pass


def build(debug=(), mode="single"):
    with_carry = mode in ("states", "main", "fused")
    nc = bass.Bass("TRN2", target_bir_lowering=False)
    es = ExitStack()
    k = K(nc, es)
    G128 = [float(x) for x in _gammas() ** 128]
    G2048 = [float(x) for x in _gammas() ** 2048]

    def din(name, shape, dt=F32):
        return nc.dram_tensor(name, list(shape), dt, kind="ExternalInput").ap()

    def dout(name, shape, dt=F32):
        return nc.dram_tensor(name, list(shape), dt, kind="ExternalOutput").ap()

    def dscr(name, shape, dt=F32):
        return nc.dram_tensor(name, list(shape), dt, kind="Internal").ap()

    def sb(name, shape, dt=F32, stack=None):
        return (stack or es).enter_context(nc.sbuf_tensor("sb_" + name, list(shape), dt))

    x_d = din("x", [T, D])
    if mode == "fused":
        xfull_d = din("xfull", [SEQ, D])
        ropep_d = din("ropep", [NPT, 128, 128])
        kdecp_d = din("kdecp", [128, NPT * H])
    w_in_d = din("w_in", [D, 5120])
    tabs_d = din("tabs", [128, TAB_W])
    small_d = din("small", [128, SMALL_W])
    identb_d = din("identb", [128, 128])
    s5p_d = din("s5p", [128, S5P_W])
    s5d_d = din("s5d", [128, 8 + 32])
    w_glu_d = din("w_glu", [RW, RW])
    w_merge_d = din("w_merge", [D, 2 * D])
    w_a_d = din("w_a", [RW, D])
    w_b_d = din("w_b", [RW, D])
    w_out_d = din("w_out", [D, D])
    w_rt_d = din("w_rt", [D, 36])
    w_eg_d = din("w_eg", [NE, D, FE])
    w_eu_d = din("w_eu", [NE, D, FE])
    w_ed_d = din("w_ed", [NE, FE, D])
    w_pg_d = din("w_pg", [D, D])
    w_ple_d = din("w_ple", [256, D])
    p_d = din("p", [T, 256])
    nfb_d = din("nfb", [128, D])
    moec_d = din("moec", [128, 256 + CG])
    dbg = {}
    for nm, shp in debug:
        dbg[nm] = dout("dbg_" + nm, shp)

    yretT_d = dscr("yretT", [RW, T], BF16)
    yssmT_d = dscr("yssmT", [RW, T], BF16)
    x1_d = dscr("x1", [T, D])
    x1_w = [k.bufs(8, "x1w%d_" % i) for i in range(NT)]
    dcount = [0]
    dtmps = [sb("dump%d" % i, [128, 512], F32) for i in range(len(debug))]

    def dump(name, src, src_bufs, cols):
        if name not in dbg or dbg[name] is None:
            return
        tmp = dtmps[dcount[0]][:, 0:cols]
        dcount[0] += 1
        tb = k.buf()
        k.op("act", lambda e: e.copy(out=tmp, in_=src), reads=src_bufs, writes=[tb])
        k.dma("sp", dbg[name][:, :], tmp, reads=[tb])
        dbg[name] = None

    ps = [es.enter_context(nc.psum_tensor("ps%d" % i, [128, 512], F32)) for i in range(8)]
    psb = k.bufs(8, "ps")

    identb = sb("identb", [128, 128], BF16)
    identb_b = k.buf("identb")
    small = sb("small", [128, SMALL_W])
    small_b = k.buf("small")
    k.dma("pool", identb[:], identb_d[:, :], writes=[identb_b])
    k.dma("sp", small[:], small_d[:, :], writes=[small_b])
    nmixT = small[:, 0:16]
    gnw = small[:, 16:16 + 1024]
    nffnT = small[:, 1040:1056]
    npleT = small[:, 1056:1072]
    rbias = small[:, 1072:1108]
    def s5_setup(ss, with_cp=True, tag=""):
        s5p = sb("s5p" + tag, [128, S5P_W], F32, ss)
        s5p_b = k.buf("s5p")
        k.dma("sp", s5p[:], s5p_d[:, :], writes=[s5p_b])
        lamre, lamim, logdt = s5p[:, 0:32], s5p[:, 32:64], s5p[:, 64:96]
        Bre = s5p[:, 96:608].rearrange("p (q c) -> p q c", c=16)
        Bim = s5p[:, 608:1120].rearrange("p (q c) -> p q c", c=16)
        Cre = s5p[:, 1120:1632].rearrange("p (q c) -> p q c", c=16)
        Cim = s5p[:, 1632:2144].rearrange("p (q c) -> p q c", c=16)
        W = sb("s5w" + tag, [128, 20, 32], F32, ss)
        wb = k.buf("s5w")

        def tt(o, a, b_, op, eng="dve"):
            k.op(eng, lambda e: e.tensor_tensor(out=o, in0=a, in1=b_, op=op), reads=[wb, s5p_b], writes=[wb])

        def ts(o, a, s1, s2, op0, op1=None, eng="dve"):
            if op1 is None:
                k.op(eng, lambda e: e.tensor_scalar(out=o, in0=a, scalar1=s1, scalar2=None, op0=op0), reads=[wb, s5p_b], writes=[wb])
            else:
                k.op(eng, lambda e: e.tensor_scalar(out=o, in0=a, scalar1=s1, scalar2=s2, op0=op0, op1=op1), reads=[wb, s5p_b], writes=[wb])

        def act(o, a, f):
            k.op("act", lambda e: e.activation(out=o, in_=a, func=f), reads=[wb, s5p_b], writes=[wb])

        lr, dt, a_, b_, ea, r1, r2, sbn, cbn, Lre, Lim, nr, den, t0, t1_, cr, ci, t2_, t3_ = [W[:, i, :] for i in range(19)]
        ts(lr, lamre, -1e-4, None, ALU.min)
        act(dt, logdt, AF.Exp)
        tt(a_, lr, dt, ALU.mult)
        tt(b_, lamim, dt, ALU.mult)
        act(ea, a_, AF.Exp)
        for (dst, off) in ((r1, PI), (r2, 1.5 * PI)):
            ts(dst, b_, off, None, ALU.add)
            ts(t0, dst, 2 * PI, -2 * PI, ALU.is_ge, ALU.mult)
            tt(t1_, dst, t0, ALU.add)
            ts(t0, dst, 4 * PI, -2 * PI, ALU.is_ge, ALU.mult)
            tt(t1_, t1_, t0, ALU.add)
            ts(t0, dst, 6 * PI, -2 * PI, ALU.is_ge, ALU.mult)
            tt(t1_, t1_, t0, ALU.add)
            ts(dst, t1_, -PI, None, ALU.add)
        act(sbn, r1, AF.Sin)
        act(cbn, r2, AF.Sin)
        tt(Lre, ea, cbn, ALU.mult)
        tt(Lim, ea, sbn, ALU.mult)
        ts(nr, Lre, -1.0, None, ALU.add)
        tt(den, lr, lr, ALU.mult)
        tt(t0, lamim, lamim, ALU.mult)
        tt(den, den, t0, ALU.add)
        k.op("dve", lambda e: e.reciprocal(out=den, in_=den), reads=[wb], writes=[wb])
        tt(t0, nr, lr, ALU.mult)
        tt(t1_, Lim, lamim, ALU.mult)
        tt(t0, t0, t1_, ALU.add)
        tt(cr, t0, den, ALU.mult)
        tt(t0, Lim, lr, ALU.mult)
        tt(t1_, nr, lamim, ALU.mult)
        tt(t0, t0, t1_, ALU.subtract)
        tt(ci, t0, den, ALU.mult)
        Bb = sb("s5Bb" + tag, [128, 2, 32, 16], F32, ss)
        T3 = sb("s5T3" + tag, [128, 2, 32, 16], F32, ss)
        bc = lambda v: v.unsqueeze(2).broadcast_to([128, 32, 16])
        tt(T3[:, 0], Bre, bc(cr), ALU.mult)
        tt(T3[:, 1], Bim, bc(ci), ALU.mult)
        tt(Bb[:, 0], T3[:, 0], T3[:, 1], ALU.subtract)
        tt(T3[:, 0], Bim, bc(cr), ALU.mult)
        tt(T3[:, 1], Bre, bc(ci), ALU.mult)
        tt(Bb[:, 1], T3[:, 0], T3[:, 1], ALU.add)
        k.op("dve", lambda e: e.memset(Pre[:, 0, :], 1.0), reads=[wb], writes=[wb])
        k.op("dve", lambda e: e.memset(Pim[:, 0, :], 0.0), reads=[wb], writes=[wb])
        for kk in range(8):
            tt(t0, Pre[:, kk, :], Lre, ALU.mult)
            tt(t1_, Pim[:, kk, :], Lim, ALU.mult)
            tt(Pre[:, kk + 1, :], t0, t1_, ALU.subtract)
            tt(t0, Pre[:, kk, :], Lim, ALU.mult)
            tt(t1_, Pim[:, kk, :], Lre, ALU.mult)
            tt(Pim[:, kk + 1, :], t0, t1_, ALU.add)
        k.op("dve", lambda e: e.tensor_copy(out=Dre[:, 0, :], in_=Pre[:, 8, :]), reads=[wb], writes=[wb])
        k.op("dve", lambda e: e.tensor_copy(out=Dim[:, 0, :], in_=Pim[:, 8, :]), reads=[wb], writes=[wb])
        for i in range(8):
            tt(t0, Dre[:, i, :], Dre[:, i, :], ALU.mult)
            tt(t1_, Dim[:, i, :], Dim[:, i, :], ALU.mult)
            tt(Dre[:, i + 1, :], t0, t1_, ALU.subtract)
            tt(t0, Dre[:, i, :], Dim[:, i, :], ALU.mult)
            ts(Dim[:, i + 1, :], t0, 2.0, None, ALU.mult)
        ts(nDim[:, :, :], Dim[:, :, :], -1.0, None, ALU.mult)
        for sx in range(8):
            pr = bc(Pre[:, 7 - sx, :])
            pi_ = bc(Pim[:, 7 - sx, :])
            tt(T3[:, 0], Bb[:, 0], pr, ALU.mult)
            tt(T3[:, 1], Bb[:, 1], pi_, ALU.mult)
            tt(RBc[:, :, sx, 0, :], T3[:, 0], T3[:, 1], ALU.subtract)
            tt(T3[:, 0], Bb[:, 1], pr, ALU.mult)
            tt(T3[:, 1], Bb[:, 0], pi_, ALU.mult)
            tt(RBc[:, :, sx, 1, :], T3[:, 0], T3[:, 1], ALU.add)
        for kk in (range(9) if with_cp else ()):
            pr = bc(Pre[:, kk, :])
            pi_ = bc(Pim[:, kk, :])
            tt(T3[:, 0], Cre, pr, ALU.mult)
            tt(T3[:, 1], Cim, pi_, ALU.mult)
            tt(CPc[:, :, kk, 0, :], T3[:, 0], T3[:, 1], ALU.subtract)
            tt(T3[:, 0], Cre, pi_, ALU.mult)
            tt(T3[:, 1], Cim, pr, ALU.mult)
            tt(T3[:, 0], T3[:, 0], T3[:, 1], ALU.add)
            ts(CPc[:, :, kk, 1, :], T3[:, 0], -1.0, None, ALU.mult)
        return wb

    cs = ExitStack()
    S = sb("S", [128, H, 128], F32, cs)
    Sbf = sb("Sbf", [128, H, 128], BF16, cs)
    S_b = k.bufs(H, "S")
    Sbf_b = k.bufs(H, "Sbf")
    carryP = sb("carryP", [128, 32, 2], F32, cs)
    carryP_b = k.buf("carryP")
    tabs1 = sb("tabs1", [128, NCORES], F32, cs)
    tabs1_b = k.buf("tabs1")
    k.dma("sp", tabs1[:], tabs_d[:, TAB_ONEHOT:TAB_W], writes=[tabs1_b])

    def stage1_tile(src_rows, dst3, dst_b, nT, xt_, xt_b_, xs_, xs_b_, st_, st_b_, pbanks, part="both"):
        if part in ("both", "a", "dma"):
            k.dma("sp", xt_[:], src_rows, writes=[xt_b_])
        if part in ("both", "a", "a2"):
            k.op("act", lambda e: e.activation(out=xs_[:], in_=xt_[:], func=AF.Square, accum_out=st_[:, 0:1]), reads=[xt_b_], writes=[xs_b_, st_b_])
            k.op("act", lambda e: e.activation(out=st_[:, 1:2], in_=st_[:, 0:1], func=AF.Sqrt, scale=1.0 / D, bias=EPS), reads=[st_b_], writes=[st_b_])
            k.op("dve", lambda e: e.reciprocal(out=st_[:, 1:2], in_=st_[:, 1:2]), reads=[st_b_], writes=[st_b_])
            k.op("act", lambda e: e.activation(out=xs_[:], in_=xt_[:], func=AF.Copy, scale=st_[:, 1:2]), reads=[xt_b_, st_b_, xs_b_], writes=[xs_b_])
        if part in ("both", "b"):
            for half in range(2):
                pbank = ps[pbanks[half]].bitcast(BF16)
                for j in range(8):
                    kc = half * 8 + j
                    k.op("pe", lambda e: e.transpose(out=pbank[:, j * 128:(j + 1) * 128], in_=xs_[:, kc * 128:(kc + 1) * 128], identity=identb[:]),
                         reads=[xs_b_, identb_b], writes=[psb[pbanks[half]]])
                k.op("dve", lambda e: e.tensor_tensor(out=dst3[:, half * 8:(half + 1) * 8, :], in0=pbank[:, :].rearrange("p (j t) -> p j t", t=128),
                                                      in1=nT[:, half * 8:(half + 1) * 8].unsqueeze(2).broadcast_to([128, 8, 128]), op=ALU.mult),
                     reads=[psb[pbanks[half]], small_b], writes=[dst_b])

    if mode == "fused":
        uTp_d = dscr("uTp", [RW, NPT * 128], BF16)
        uTp_b = k.bufs(NPT, "uTp")
        with ExitStack() as pa:
            Wkv = sb("pWkv", [128, KC, 2048], BF16, pa)
            Wkv_b = k.bufs(4, "pWkv")
            for cb in range(4):
                c0 = RW + cb * 512
                k.dma("pool", Wkv[:, :, cb * 512:(cb + 1) * 512], w_in_d[:, c0:c0 + 512].rearrange("(kc p) c -> p kc c", p=128), writes=[Wkv_b[cb]])
            kdecP = sb("kdecP", [128, NPT, H], F32, pa)
            kdecP_b = k.buf("kdecP")
            k.dma("sp", kdecP[:], kdecp_d[:, :].rearrange("p (g h) -> p g h", h=H), writes=[kdecP_b])
            ropeP = [sb("ropeP%d" % i, [128, 2, 64], F32, pa) for i in range(2)]
            ropeP_b = k.bufs(2, "ropeP")
            xtA = [sb("pxt%d" % i, [128, D], F32, pa) for i in range(4)]
            xsA = [sb("pxs%d" % i, [128, D], BF16, pa) for i in range(3)]
            stA = [sb("pst%d" % i, [128, 2], F32, pa) for i in range(3)]
            xtA_b, xsA_b, stA_b = k.bufs(4, "pxt"), k.bufs(3, "pxs"), k.bufs(3, "pst")
            hTt = [sb("phTt%d" % i, [128, KC, 128], BF16, pa) for i in range(2)]
            hTt_b = k.bufs(2, "phTt")
            tt4 = [sb("ptt%d" % i, [128, 4, 64], F32, pa) for i in range(4)]
            tt4_b = k.bufs(4, "ptt")
            krr = [sb("pkr%d" % i, [128, 4, 2, 64], F32, pa) for i in range(2)]
            krr1_b, krr2_b = k.bufs(2, "pkr1"), k.bufs(2, "pkr2")
            ktdA = [sb("pktd%d" % i, [128, H, 128], BF16, pa) for i in range(2)]
            ktdA_b = [k.bufs(2, "pktd%d_" % i) for i in range(2)]
            vbA = [sb("pvb%d" % i, [128, 1024], BF16, pa) for i in range(2)]
            vbA_b = [k.bufs(2, "pvb%d_" % i) for i in range(2)]
            KV0, KV1 = 6, 7
            Wu = sb("pWu", [128, KC, RW], BF16, pa)
            Wu_b = k.bufs(2, "pWu")
            for cb in range(2):
                c0 = 4 * RW + cb * 512
                k.dma("pool", Wu[:, :, cb * 512:(cb + 1) * 512], w_in_d[:, c0:c0 + 512].rearrange("(kc p) c -> p kc c", p=128), writes=[Wu_b[cb]])
            uTt = [sb("puTt%d" % i, [128, 8, 128], BF16, pa) for i in range(2)]
            uTt_b = k.bufs(2, "puTt")
            utok = [sb("putok%d" % i, [128, RW], BF16, pa) for i in range(2)]
            utok_b = [k.bufs(2, "putok%d_" % i) for i in range(2)]
            for g in range(NPT):
                b = g % 2
                k.dma("sp", ropeP[b][:], ropep_d[g].rearrange("p (c f) -> p c f", c=2), writes=[ropeP_b[b]])

                def s1(gg, part):
                    b3 = gg % 3
                    b4 = gg % 4
                    stage1_tile(xfull_d[gg * 128:(gg + 1) * 128, :], hTt[gg % 2][:, :, :], hTt_b[gg % 2], nmixT, xtA[b4], xtA_b[b4], xsA[b3], xsA_b[b3],
                                stA[b3], stA_b[b3], (0, 1), part=part)
                if g == 0:
                    s1(0, "a")
                    s1(1, "a")
                    s1(2, "dma")
                    s1(0, "b")
                if g + 3 < NPT:
                    s1(g + 3, "dma")
                if g + 2 < NPT:
                    s1(g + 2, "a2")
                if g + 1 < NPT:
                    s1(g + 1, "b")
                for cb in range(4):
                    pb = 2 + cb % 2
                    for kc in range(KC):
                        k.op("pe", lambda e: e.matmul(ps[pb][:, :], hTt[b][:, kc, :], Wkv[:, kc, cb * 512:(cb + 1) * 512], start=(kc == 0), stop=(kc == KC - 1)),
                             reads=[hTt_b[b], Wkv_b[cb]], writes=[psb[pb]])
                    if cb < 2:
                        X4 = ps[pb][:, :].rearrange("p (h c f) -> p h c f", c=2, f=64)
                        A, B = X4[:, :, 0, :], X4[:, :, 1, :]
                        C = ropeP[b][:, 0, :].unsqueeze(1).broadcast_to([128, 4, 64])
                        Sn = ropeP[b][:, 1, :].unsqueeze(1).broadcast_to([128, 4, 64])
                        kb = cb % 2
                        k.op("dve", lambda e: e.tensor_tensor(out=tt4[0][:], in0=A, in1=C, op=ALU.mult), reads=[psb[pb], ropeP_b[b]], writes=[tt4_b[0]])
                        k.op("dve", lambda e: e.tensor_tensor(out=tt4[1][:], in0=B, in1=Sn, op=ALU.mult), reads=[psb[pb], ropeP_b[b]], writes=[tt4_b[1]])
                        k.op("dve", lambda e: e.tensor_tensor(out=tt4[2][:], in0=A, in1=Sn, op=ALU.mult), reads=[psb[pb], ropeP_b[b]], writes=[tt4_b[2]])
                        k.op("dve", lambda e: e.tensor_tensor(out=tt4[3][:], in0=B, in1=C, op=ALU.mult), reads=[psb[pb], ropeP_b[b]], writes=[tt4_b[3]])
                        k.op("pool", lambda e: e.tensor_tensor(out=krr[kb][:, :, 0, :], in0=tt4[0][:], in1=tt4[1][:], op=ALU.subtract),
                             reads=[tt4_b[0], tt4_b[1]], writes=[krr1_b[kb]])
                        k.op("pool", lambda e: e.tensor_tensor(out=krr[kb][:, :, 1, :], in0=tt4[2][:], in1=tt4[3][:], op=ALU.add),
                             reads=[tt4_b[2], tt4_b[3]], writes=[krr2_b[kb]])
                        k.op("pool", lambda e: e.tensor_tensor(
                            out=ktdA[b][:, cb * 4:(cb + 1) * 4, :], in0=krr[kb][:, :, :, :].rearrange("p h c f -> p h (c f)"),
                            in1=kdecP[:, g, cb * 4:(cb + 1) * 4].unsqueeze(2).broadcast_to([128, 4, 128]), op=ALU.mult),
                            reads=[krr1_b[kb], krr2_b[kb], kdecP_b], writes=[ktdA_b[b][cb]])
                    else:
                        k.op("act", lambda e: e.copy(out=vbA[b][:, (cb - 2) * 512:(cb - 1) * 512], in_=ps[pb][:, :]), reads=[psb[pb]], writes=[vbA_b[b][cb - 2]])
                for cb in range(2):
                    pbk = 4 + cb
                    for kc in range(KC):
                        k.op("pe", lambda e: e.matmul(ps[pbk][:, :], hTt[b][:, kc, :], Wu[:, kc, cb * 512:(cb + 1) * 512], start=(kc == 0), stop=(kc == KC - 1)),
                             reads=[hTt_b[b], Wu_b[cb]], writes=[psb[pbk]])
                    k.op("act", lambda e: e.copy(out=utok[b][:, cb * 512:(cb + 1) * 512], in_=ps[pbk][:, :]), reads=[psb[pbk]], writes=[utok_b[b][cb]])
                for cb in range(2):
                    pbk = 4 + cb
                    ubank = ps[pbk].bitcast(BF16)
                    for c4 in range(4):
                        ct = cb * 4 + c4
                        k.op("pe", lambda e: e.transpose(out=ubank[:, c4 * 128:(c4 + 1) * 128], in_=utok[b][:, ct * 128:(ct + 1) * 128], identity=identb[:]),
                             reads=[utok_b[b][cb], identb_b], writes=[psb[pbk]])
                    for c4 in range(4):
                        ct = cb * 4 + c4
                        eng = "act" if c4 % 2 == 0 else "dve"
                        fnc = (lambda e: e.copy(out=uTt[b][:, ct, :].rearrange("p (s m) -> p s m", s=8),
                                                in_=ubank[:, c4 * 128:(c4 + 1) * 128].rearrange("p (m s) -> p s m", s=8))) if eng == "act" else \
                              (lambda e: e.tensor_copy(out=uTt[b][:, ct, :].rearrange("p (s m) -> p s m", s=8),
                                                       in_=ubank[:, c4 * 128:(c4 + 1) * 128].rearrange("p (m s) -> p s m", s=8)))
                        k.op(eng, fnc, reads=[psb[pbk], uTt_b[b]], writes=[uTt_b[b]])
                k.dma("sp", uTp_d[:, g * 128:(g + 1) * 128].rearrange("(ct p) t -> p ct t", p=128), uTt[b][:], reads=[uTt_b[b]], writes=[uTp_b[g]])
                for h in range(H):
                    kvb = KV0 if h < 4 else KV1
                    k.op("pe", lambda e: e.matmul(ps[kvb][:, (h % 4) * 128:(h % 4 + 1) * 128], ktdA[b][:, h, :], vbA[b][:, h * 128:(h + 1) * 128],
                                                  start=(g == 0 and h % 4 == 0), stop=(g == NPT - 1 and h % 4 == 3)), reads=[ktdA_b[b][h // 4], vbA_b[b][h // 4]], writes=[psb[kvb]])
            for h in range(H):
                kvb = KV0 if h < 4 else KV1
                k.op("act", lambda e: e.copy(out=S[:, h, :], in_=ps[kvb][:, (h % 4) * 128:(h % 4 + 1) * 128]), reads=[psb[kvb]], writes=[S_b[h]])
                k.op("act", lambda e: e.copy(out=Sbf[:, h, :], in_=S[:, h, :]), reads=[S_b[h]], writes=[Sbf_b[h]])
        k.barrier()
        with ExitStack() as pb_:
            Pre = sb("qPre", [128, 9, 32], F32, pb_)
            Pim = sb("qPim", [128, 9, 32], F32, pb_)
            Dre = sb("qDre", [128, 9, 32], F32, pb_)
            Dim = sb("qDim", [128, 9, 32], F32, pb_)
            nDim = sb("qnDim", [128, 9, 32], F32, pb_)
            RBc = sb("qRBc", [128, 32, 8, 2, 16], BF16, pb_)
            CPc = None
            with ExitStack() as ss:
                mats_b = s5_setup(ss, with_cp=False, tag="q")
            k.barrier()
            NSEG = NCORES - 1
            NCH = NSEG * 256
            RBm = [sb("qRBm%d" % i, [128, 8, 2, 32], BF16, pb_) for i in range(2)]
            RBm_b = k.bufs(2, "qRBm")
            RBT = [sb("qRBT%d" % i, [32, 16, 128], BF16, pb_) for i in range(2)]
            RBT_b = k.bufs(2, "qRBT")
            uq = [sb("quq%d" % i, [32, NCH * 8], BF16, pb_) for i in range(2)]
            uq_b = k.bufs(2, "quq")
            wA = [sb("qwA%d" % i, [128, 2, NSEG, 256], F32, pb_) for i in range(2)]
            wA_b = k.bufs(2, "qwA")
            wB = [sb("qwB%d" % i, [128, 2, NSEG, 128], F32, pb_) for i in range(2)]
            wB_b = k.bufs(2, "qwB")
            Eall = sb("qEall", [128, 32, 2, NSEG], F32, pb_)
            Eall_b = k.bufs(32, "qEall")
            for i in range(2):
                k.op("pool", lambda e: e.memset(RBm[i][:], 0.0), writes=[RBm_b[i]])
            PTA, PTB = 0, 1
            NBK = 4
            CW = NCH // NBK
            def pfx_pair(q):
                b = q % 2
                for gi in range(2):
                    prt = slice(gi * 64, gi * 64 + 64)
                    csl = slice(gi * 16, gi * 16 + 16)
                    k.op("pool", lambda e: e.tensor_copy(out=RBm[b][prt, :, :, csl], in_=RBc[prt, q, :, :, :]), reads=[mats_b], writes=[RBm_b[b]])
                pTA = ps[PTA].bitcast(BF16)
                pTB = ps[PTB].bitcast(BF16)
                for sx in range(8):
                    for ri in range(2):
                        idx = sx * 2 + ri
                        pt, pbb = (pTA, psb[PTA]) if idx < 8 else (pTB, psb[PTB])
                        k.op("pe", lambda e: e.transpose(out=pt[0:32, (idx % 8) * 128:(idx % 8 + 1) * 128], in_=RBm[b][:, sx, ri, :], identity=identb[:]),
                             reads=[RBm_b[b], identb_b], writes=[pbb])
                k.op("act", lambda e: e.copy(out=RBT[b][:, 0:8, :], in_=pTA[0:32, :].rearrange("p (a c) -> p a c", c=128)), reads=[psb[PTA]], writes=[RBT_b[b]])
                k.op("act", lambda e: e.copy(out=RBT[b][:, 8:16, :], in_=pTB[0:32, :].rearrange("p (a c) -> p a c", c=128)), reads=[psb[PTB], RBT_b[b]], writes=[RBT_b[b]])
                yield
                k.dma("sp", uq[b][:], uTp_d[q * 32:(q + 1) * 32, :], reads=uTp_b, writes=[uq_b[b]])
                uq4 = uq[b][:, :].rearrange("p (g s m) -> p g s m", s=8, m=16)
                TPB = CW // 16
                wflat = wA[b][:, :, :, :].rearrange("p r g m -> p r (g m)")
                for ri in range(2):
                    for nbk in range(NBK):
                        pbk = 2 + (ri * NBK + nbk) % 6
                        for sx in range(8):
                            k.op("pe", lambda e: e.matmul(ps[pbk][:, 0:CW], RBT[b][:, sx * 2 + ri, :], uq4[:, nbk * TPB:(nbk + 1) * TPB, sx, :],
                                                          start=(sx == 0), stop=(sx == 7)), reads=[RBT_b[b], uq_b[b]], writes=[psb[pbk]])
                        k.op("act", lambda e: e.copy(out=wflat[:, ri, nbk * CW:(nbk + 1) * CW], in_=ps[pbk][:, 0:CW]), reads=[psb[pbk]], writes=[wA_b[b]])
                yield
                src, src_b2 = wA[b], wA_b[b]
                dst, dst_b2 = wB[b], wB_b[b]
                n = 256
                for lv in range(8):
                    hn = n // 2
                    v = src[:, :, :, 0:n].rearrange("p r g (m two) -> p r g m two", two=2)
                    dre, dim_, ndim = Dre[:, lv, q:q + 1], Dim[:, lv, q:q + 1], nDim[:, lv, q:q + 1]
                    o_re, o_im = dst[:, 0, :, 0:hn], dst[:, 1, :, 0:hn]
                    k.op("dve", lambda e: e.scalar_tensor_tensor(out=o_re, in0=v[:, 0, :, :, 0], scalar=dre, in1=v[:, 0, :, :, 1], op0=ALU.mult, op1=ALU.add),
                         reads=[src_b2, mats_b, dst_b2], writes=[dst_b2])
                    k.op("dve", lambda e: e.scalar_tensor_tensor(out=o_im, in0=v[:, 1, :, :, 0], scalar=dre, in1=v[:, 1, :, :, 1], op0=ALU.mult, op1=ALU.add),
                         reads=[src_b2, mats_b, dst_b2], writes=[dst_b2])
                    yield
                    k.op("dve", lambda e: e.scalar_tensor_tensor(out=o_re, in0=v[:, 1, :, :, 0], scalar=ndim, in1=o_re, op0=ALU.mult, op1=ALU.add),
                         reads=[src_b2, mats_b, dst_b2], writes=[dst_b2])
                    k.op("dve", lambda e: e.scalar_tensor_tensor(out=o_im, in0=v[:, 0, :, :, 0], scalar=dim_, in1=o_im, op0=ALU.mult, op1=ALU.add),
                         reads=[src_b2, mats_b, dst_b2], writes=[dst_b2])
                    yield
                    src, src_b2, dst, dst_b2 = dst, dst_b2, src, src_b2
                    n = hn
                k.op("act", lambda e: e.copy(out=Eall[:, q, :, :], in_=src[:, :, :, 0]), reads=[src_b2], writes=[Eall_b[q]])

            for q0 in range(0, NQ, 2):
                gens = [pfx_pair(q0), pfx_pair(q0 + 1)]
                while gens:
                    for gnr in list(gens):
                        try:
                            next(gnr)
                        except StopIteration:
                            gens.remove(gnr)
            Xc = sb("qXc", [128, 32, 2], F32, pb_)
            tq = sb("qtq", [128, 4, 32], F32, pb_)
            xb = k.buf("qXc")
            k.op("dve", lambda e: e.memset(Xc[:], 0.0), writes=[xb])
            k.op("dve", lambda e: e.memset(carryP[:], 0.0), writes=[carryP_b])
            d8r, d8i = Dre[:, 8, :], Dim[:, 8, :]
            for r in range(NSEG):
                tt_ = lambda o, a, b2, op: k.op("dve", lambda e: e.tensor_tensor(out=o, in0=a, in1=b2, op=op), reads=[xb, mats_b] + Eall_b, writes=[xb])
                tt_(tq[:, 0, :], Xc[:, :, 0], d8r, ALU.mult)
                tt_(tq[:, 1, :], Xc[:, :, 1], d8i, ALU.mult)
                tt_(tq[:, 2, :], Xc[:, :, 1], d8r, ALU.mult)
                tt_(tq[:, 3, :], Xc[:, :, 0], d8i, ALU.mult)
                tt_(tq[:, 0, :], tq[:, 0, :], tq[:, 1, :], ALU.subtract)
                tt_(tq[:, 2, :], tq[:, 2, :], tq[:, 3, :], ALU.add)
                tt_(Xc[:, :, 0], tq[:, 0, :], Eall[:, :, 0, r], ALU.add)
                tt_(Xc[:, :, 1], tq[:, 2, :], Eall[:, :, 1, r], ALU.add)
                k.op("dve", lambda e: e.scalar_tensor_tensor(out=carryP[:, :, :].rearrange("p q r -> p (q r)"), in0=Xc[:, :, :].rearrange("p q r -> p (q r)"),
                                                             scalar=tabs1[:, r + 1:r + 2], in1=carryP[:, :, :].rearrange("p q r -> p (q r)"),
                                                             op0=ALU.mult, op1=ALU.add), reads=[xb, tabs1_b, carryP_b], writes=[carryP_b])
        k.barrier()

    hs = ExitStack()
    hT = sb("hT", [128, KC, T], BF16, hs)
    hT_b = k.bufs(NT, "hT")
    ms = ExitStack()
    tabs = sb("tabs", [128, TAB_W], F32, ms)
    tabs_b = k.buf("tabs")
    k.dma("sp", tabs[:], tabs_d[:, :], writes=[tabs_b])
    cosT = tabs[:, TAB_COS:TAB_SIN].rearrange("p (i f) -> p i f", f=64)
    sinT = tabs[:, TAB_SIN:TAB_MASK].rearrange("p (i f) -> p i f", f=64)
    maskT = tabs[:, TAB_MASK:TAB_QDEC].rearrange("p (h f) -> p h f", f=128)
    qdec = tabs[:, TAB_QDEC:TAB_KDEC]
    kdec = tabs[:, TAB_KDEC:TAB_ONEHOT]
    onehot = tabs[:, TAB_ONEHOT:TAB_W]


    with ExitStack() as s1:
        xt = [sb("xt%d" % i, [128, D], F32, s1) for i in range(2)]
        xt_b = k.bufs(2, "xt")
        xs = [sb("xs%d" % i, [128, D], BF16, s1) for i in range(2)]
        xs_b = k.bufs(2, "xs")
        junk = sb("junk", [128, D], BF16, s1)
        junk_b = k.buf("junk")
        ss = sb("ss", [128, NT], F32, s1)
        rstd = sb("rstd", [128, NT], F32, s1)
        ss_b = k.bufs(NT, "ss")
        rstd_b = k.bufs(NT, "rstd")
        for i in range(NT):
            b = i % 2
            k.dma("sp", xt[b][:], x_d[i * 128:(i + 1) * 128, :], writes=[xt_b[b]])
            k.op("act", lambda e: e.activation(out=junk[:], in_=xt[b][:], func=AF.Square, accum_out=ss[:, i:i + 1]),
                 reads=[xt_b[b]], writes=[junk_b, ss_b[i]])
            k.op("act", lambda e: e.activation(out=rstd[:, i:i + 1], in_=ss[:, i:i + 1], func=AF.Sqrt, scale=1.0 / D, bias=EPS),
                 reads=[ss_b[i]], writes=[rstd_b[i]])
            k.op("dve", lambda e: e.reciprocal(out=rstd[:, i:i + 1], in_=rstd[:, i:i + 1]), reads=[rstd_b[i]], writes=[rstd_b[i]])
            k.op("act", lambda e: e.activation(out=xs[b][:], in_=xt[b][:], func=AF.Copy, scale=rstd[:, i:i + 1]),
                 reads=[xt_b[b], rstd_b[i]], writes=[xs_b[b]])
            for half in range(2):
                pbank = ps[half].bitcast(BF16)
                for j in range(8):
                    kc = half * 8 + j
                    k.op("pe", lambda e: e.transpose(out=pbank[:, j * 128:(j + 1) * 128],
                                                     in_=xs[b][:, kc * 128:(kc + 1) * 128], identity=identb[:]),
                         reads=[xs_b[b], identb_b], writes=[psb[half]])
                k.op("dve", lambda e: e.tensor_tensor(
                    out=hT[:, half * 8:(half + 1) * 8, i * 128:(i + 1) * 128],
                    in0=pbank[:, :].rearrange("p (j t) -> p j t", t=128),
                    in1=nmixT[:, half * 8:(half + 1) * 8].unsqueeze(2).broadcast_to([128, 8, 128]),
                    op=ALU.mult), reads=[psb[half], small_b], writes=[hT_b[i]])

    k.barrier()

    def retention(full, rs):
        Wh = [sb("Wh%d_%d" % (i, full), [128, KC, 512], BF16, rs) for i in range(2)]
        Wh_b = k.bufs(2, "Wh")
        xqk = [sb("xqk%d_%d" % (i, full), [128, 256], F32, rs) for i in range(4)]
        xqk_b = k.bufs(4, "xqk")
        t1 = [sb("t1_%d_%d" % (i, full), [128, 2, 64], F32, rs) for i in range(4)]
        t2 = [sb("t2_%d_%d" % (i, full), [128, 2, 64], F32, rs) for i in range(4)]
        t3 = [sb("t3_%d_%d" % (i, full), [128, 2, 64], F32, rs) for i in range(4)]
        t4 = [sb("t4_%d_%d" % (i, full), [128, 2, 64], F32, rs) for i in range(4)]
        t1_b, t2_b, t3_b, t4_b = k.bufs(4, "t1"), k.bufs(4, "t2"), k.bufs(4, "t3"), k.bufs(4, "t4")
        qkr = [sb("qkr%d_%d" % (i, full), [128, 2, 2, 64], BF16, rs) for i in range(4)]
        qkr1_b, qkr2_b = k.bufs(4, "qkr1"), k.bufs(4, "qkr2")
        vb = [sb("vb%d_%d" % (i, full), [128, 128], BF16, rs) for i in range(4)]
        vb_b = k.bufs(4, "vb")
        sg = [sb("sg%d_%d" % (i, full), [128, 128], F32, rs) for i in range(4)]
        sg_b = k.bufs(4, "sg")
        qd = [sb("qd%d_%d" % (i, full), [128, 128], BF16, rs) for i in range(4)]
        qd_b = k.bufs(4, "qd")
        ktd = [sb("ktd%d_%d" % (i, full), [128, 128], BF16, rs) for i in range(4)]
        ktd_b = k.bufs(4, "ktd")
        qkT = [sb("qkT%d_%d" % (i, full), [128, 384], BF16, rs) for i in range(4)]
        qkT_b = k.bufs(4, "qkT")
        Pm = [sb("Pm%d_%d" % (i, full), [128, 128], BF16, rs) for i in range(4)]
        Pm_b = k.bufs(4, "Pm")
        st6 = [sb("st6_%d_%d" % (i, full), [128, 6], F32, rs) for i in range(4)]
        mv = [sb("mv%d_%d" % (i, full), [128, 4], F32, rs) for i in range(4)]
        st6_b, mv_b = k.bufs(4, "st6"), k.bufs(4, "mv")
        yn = [sb("yn%d_%d" % (i, full), [128, 128], F32, rs) for i in range(4)]
        yn_b = k.bufs(4, "yn")
        yr = [sb("yr%d_%d" % (i, full), [128, 128], BF16, rs) for i in range(4)]
        yr_b = k.bufs(4, "yr")
        yst = [sb("yst%d_%d" % (i, full), [128, T], BF16, rs) for i in range(2)]
        yst_b = k.bufs(2, "yst")
        PJb, PTb, MB = (0, 3), (1, 4), (2, 5)
        t_sc, t_py, t_kv, t_t2 = k.bufs(2, "r_sc"), k.bufs(2, "r_py"), k.bufs(2, "r_kv"), k.bufs(2, "r_t2")

        def load_w(h):
            hb = h % 2
            for j in range(4):
                c0 = j * RW + h * DH
                k.dma("pool", Wh[hb][:, :, j * 128:(j + 1) * 128],
                      w_in_d[:, c0:c0 + 128].rearrange("(kc p) c -> p kc c", p=128), writes=[Wh_b[hb]] if j == 3 else [])
        def load_w_tracked(h):
            hb = h % 2
            for j in range(4):
                if not full and j in (0, 3):
                    continue
                c0 = j * RW + h * DH
                k.dma("pool", Wh[hb][:, :, j * 128:(j + 1) * 128],
                      w_in_d[:, c0:c0 + 128].rearrange("(kc p) c -> p kc c", p=128), writes=[Whj_b[hb][j]])

        Whj_b = [k.bufs(4, "Whj%d_" % i) for i in range(2)]
        def head_body(h):
            hb = h % 2
            wreads = [Whj_b[hb][j] for j in ((0, 1, 2, 3) if full else (1, 2))]
            for i in range(NT):
                b = (h % 2) * 2 + i % 2
                hp = h % 2
                pj = ps[PJb[hp]]
                pjb = psb[PJb[hp]]
                tok = slice(i * 128, (i + 1) * 128)
                c_lo, c_hi = (0, 512) if full else (128, 384)
                for kc in range(KC):
                    k.op("pe", lambda e: e.matmul(pj[:, c_lo:c_hi], hT[:, kc, tok], Wh[hb][:, kc, c_lo:c_hi],
                                                  start=(kc == 0), stop=(kc == KC - 1)),
                         reads=[hT_b[i]] + wreads, writes=[pjb])
                if h == 0 and i == 0 and full:
                    dump("Wh", Wh[hb][:, 3, :], wreads, 512)
                    dump("pj", pj[:, :], [pjb], 512)
                yield
                k.op("act", lambda e: e.copy(out=xqk[b][:, c_lo:256], in_=pj[:, c_lo:256]), reads=[pjb], writes=[xqk_b[b]])
                k.op("act", lambda e: e.copy(out=vb[b][:], in_=pj[:, 256:384]), reads=[pjb], writes=[vb_b[b]])
                if full:
                    k.op("act", lambda e: e.activation(out=sg[b][:], in_=pj[:, 384:512], func=AF.Silu), reads=[pjb], writes=[sg_b[b]])
                yield
                X = xqk[b][:, :].rearrange("p (a c f) -> p a c f", a=2, c=2)
                a0 = 0 if full else 1
                A = X[:, a0:2, 0, :]
                B = X[:, a0:2, 1, :]
                na = 2 - a0
                C = cosT[:, i, :].unsqueeze(1).broadcast_to([128, na, 64])
                Sn = sinT[:, i, :].unsqueeze(1).broadcast_to([128, na, 64])
                k.op("dve", lambda e: e.tensor_tensor(out=t1[b][:, a0:2, :], in0=A, in1=C, op=ALU.mult), reads=[xqk_b[b], tabs_b], writes=[t1_b[b]])
                k.op("dve", lambda e: e.tensor_tensor(out=t2[b][:, a0:2, :], in0=B, in1=Sn, op=ALU.mult), reads=[xqk_b[b], tabs_b], writes=[t2_b[b]])
                k.op("dve", lambda e: e.tensor_tensor(out=qkr[b][:, a0:2, 0, :], in0=t1[b][:, a0:2, :], in1=t2[b][:, a0:2, :], op=ALU.subtract),
                     reads=[t1_b[b], t2_b[b]], writes=[qkr1_b[b]])
                k.op("pool", lambda e: e.tensor_tensor(out=t3[b][:, a0:2, :], in0=A, in1=Sn, op=ALU.mult), reads=[xqk_b[b], tabs_b], writes=[t3_b[b]])
                k.op("pool", lambda e: e.tensor_tensor(out=t4[b][:, a0:2, :], in0=B, in1=C, op=ALU.mult), reads=[xqk_b[b], tabs_b], writes=[t4_b[b]])
                k.op("pool", lambda e: e.tensor_tensor(out=qkr[b][:, a0:2, 1, :], in0=t3[b][:, a0:2, :], in1=t4[b][:, a0:2, :], op=ALU.add),
                     reads=[t3_b[b], t4_b[b]], writes=[qkr2_b[b]])
                if h == 0 and i == 0 and full:
                    dump("xqk", xqk[b][:, :], [xqk_b[b]], 256)
                    dump("qkr", qkr[b][:, :, :, :].rearrange("p a c f -> p (a c f)"), [qkr1_b[b], qkr2_b[b]], 256)
                yield
                qr = qkr[b][:, 0, :, :].rearrange("p c f -> p (c f)")
                kr = qkr[b][:, 1, :, :].rearrange("p c f -> p (c f)")
                k.op("pool", lambda e: e.tensor_scalar(out=ktd[b][:], in0=kr, scalar1=kdec[:, h:h + 1], scalar2=None, op0=ALU.mult),
                     reads=[qkr1_b[b], qkr2_b[b], tabs_b], writes=[ktd_b[b]])
                if full:
                    k.op("act", lambda e: e.activation(out=qd[b][:], in_=qr, func=AF.Copy, scale=qdec[:, h:h + 1]),
                         reads=[qkr1_b[b], qkr2_b[b], tabs_b], writes=[qd_b[b]])
                    pT = ps[PTb[hp]].bitcast(BF16)
                    k.op("pe", lambda e: e.transpose(out=pT[:, 0:128], in_=qr, identity=identb[:]),
                         reads=[qkr1_b[b], qkr2_b[b], identb_b], writes=[psb[PTb[hp]]])
                    k.op("pe", lambda e: e.transpose(out=pT[:, 128:256], in_=qd[b][:], identity=identb[:]),
                         reads=[qd_b[b], identb_b], writes=[psb[PTb[hp]]])
                    k.op("pe", lambda e: e.transpose(out=pT[:, 256:384], in_=kr, identity=identb[:]),
                         reads=[qkr1_b[b], qkr2_b[b], identb_b], writes=[psb[PTb[hp]]])
                    k.op("act", lambda e: e.copy(out=qkT[b][:], in_=pT[:, 0:384]), reads=[psb[PTb[hp]]], writes=[qkT_b[b]])
                    yield
                    k.op("pe", lambda e: e.matmul(ps[MB[hp]][:, 0:128], qkT[b][:, 256:384], qkT[b][:, 0:128], start=True, stop=True),
                         reads=[qkT_b[b]], writes=[t_sc[hp]])
                    k.op("dve", lambda e: e.tensor_tensor(out=Pm[b][:], in0=ps[MB[hp]][:, 0:128], in1=maskT[:, h, :], op=ALU.mult),
                         reads=[t_sc[hp], tabs_b], writes=[Pm_b[b]])
                    if h == 0 and i == 0:
                        dump("qkT", qkT[b][:, :], [qkT_b[b]], 384)
                        dump("Pm", Pm[b][:, :], [Pm_b[b]], 128)
                    k.op("pe", lambda e: e.matmul(ps[MB[hp]][:, 128:256], Pm[b][:], vb[b][:], start=True, stop=False),
                         reads=[Pm_b[b], vb_b[b]], writes=[t_py[hp]])
                    k.op("pe", lambda e: e.matmul(ps[MB[hp]][:, 128:256], qkT[b][:, 128:256], Sbf[:, h, :], start=False, stop=True),
                         reads=[qkT_b[b], Sbf_b[h]], writes=[t_py[hp]])
                yield
                k.op("pe", lambda e: e.matmul(ps[MB[hp]][:, 256:384], ktd[b][:], vb[b][:], start=True, stop=True),
                     reads=[ktd_b[b], vb_b[b]], writes=[t_kv[hp]])
                k.op("dve", lambda e: e.scalar_tensor_tensor(out=S[:, h, :], in0=S[:, h, :], scalar=G128[h], in1=ps[MB[hp]][:, 256:384],
                                                             op0=ALU.mult, op1=ALU.add), reads=[S_b[h], t_kv[hp]], writes=[S_b[h]])
                if full:
                    k.op("act", lambda e: e.copy(out=Sbf[:, h, :], in_=S[:, h, :]), reads=[S_b[h]], writes=[Sbf_b[h]])
                    yield
                    k.op("dve", lambda e: e.bn_stats(out=st6[b][:], in_=ps[MB[hp]][:, 128:256]), reads=[t_py[hp]], writes=[st6_b[b]])
                    k.op("dve", lambda e: e.bn_aggr(out=mv[b][:, 0:2], in_=st6[b][:]), reads=[st6_b[b]], writes=[mv_b[b]])
                    k.op("act", lambda e: e.activation(out=mv[b][:, 2:3], in_=mv[b][:, 1:2], func=AF.Sqrt, bias=EPS, scale=1.0),
                         reads=[mv_b[b]], writes=[mv_b[b]])
                    k.op("dve", lambda e: e.reciprocal(out=mv[b][:, 2:3], in_=mv[b][:, 2:3]), reads=[mv_b[b]], writes=[mv_b[b]])
                    k.op("dve", lambda e: e.tensor_scalar(out=mv[b][:, 3:4], in0=mv[b][:, 0:1], scalar1=mv[b][:, 2:3], scalar2=-1.0,
                                                          op0=ALU.mult, op1=ALU.mult), reads=[mv_b[b]], writes=[mv_b[b]])
                    k.op("act", lambda e: e.activation(out=yn[b][:], in_=ps[MB[hp]][:, 128:256], func=AF.Identity,
                                                       scale=mv[b][:, 2:3], bias=mv[b][:, 3:4]), reads=[t_py[hp], mv_b[b]], writes=[yn_b[b]])
                    if h == 0 and i == 0:
                        dump("mv", mv[b][:, :], [mv_b[b]], 4)
                        dump("yn", yn[b][:, :], [yn_b[b]], 128)
                    k.op("pool", lambda e: e.tensor_tensor(out=yn[b][:], in0=yn[b][:], in1=gnw[:, h * 128:(h + 1) * 128], op=ALU.mult),
                         reads=[yn_b[b], small_b], writes=[yn_b[b]])
                    k.op("pool", lambda e: e.tensor_tensor(out=yr[b][:], in0=yn[b][:], in1=sg[b][:], op=ALU.mult),
                         reads=[yn_b[b], sg_b[b]], writes=[yr_b[b]])
                    yield
                    pT2 = ps[MB[hp]].bitcast(BF16)
                    if h == 0 and i == 0:
                        dump("yr", yr[b][:, :], [yr_b[b]], 128)
                    k.op("pe", lambda e: e.transpose(out=pT2[:, 768:896], in_=yr[b][:], identity=identb[:]),
                         reads=[yr_b[b], identb_b], writes=[t_t2[hp]])
                    k.op("act", lambda e: e.copy(out=yst[hb][:, tok], in_=pT2[:, 768:896]), reads=[t_t2[hp]], writes=[yst_b[hb]])
            if full:
                k.dma("sp", yretT_d[h * 128:(h + 1) * 128, :], yst[hb][:], reads=[yst_b[hb]], writes=[yretT_b[h]])

        load_w_tracked(0)
        load_w_tracked(1)
        for h0 in range(0, H, 2):
            gens = [head_body(h0), head_body(h0 + 1)]
            while gens:
                for gnr in list(gens):
                    try:
                        next(gnr)
                    except StopIteration:
                        gens.remove(gnr)
            if h0 + 2 < H:
                load_w_tracked(h0 + 2)
                load_w_tracked(h0 + 3)

    yretT_b = k.bufs(H, "yretT_d")
    for h in (range(H) if mode != "fused" else ()):
        k.op("dve", lambda e: e.memset(S[:, h, :], 0.0), writes=[S_b[h]])
        k.op("pool", lambda e: e.memset(Sbf[:, h, :], 0.0), writes=[Sbf_b[h]])
    if mode == "states":
        with ExitStack() as rs:
            retention(False, rs)
        k.barrier()


    k.barrier()

    def s5_uproj(us):
        Wu = [sb("Wu%d" % i, [128, KC, 128], BF16, us) for i in range(2)]
        Wu_b = k.bufs(2, "Wu")
        for ct in range(8):
            b = ct % 2
            c0 = 4 * RW + ct * 128
            k.dma("pool", Wu[b][:], w_in_d[:, c0:c0 + 128].rearrange("(kc p) c -> p kc c", p=128), writes=[Wu_b[b]])
            for tb in range(4):
                pb = tb % 2
                for kc in range(KC):
                    k.op("pe", lambda e: e.matmul(ps[pb][:, :], Wu[b][:, kc, :], hT[:, kc, tb * 512:(tb + 1) * 512],
                                                  start=(kc == 0), stop=(kc == KC - 1)),
                         reads=[Wu_b[b]] + hT_b[tb * 4:(tb + 1) * 4], writes=[psb[pb]])
                k.op("act", lambda e: e.copy(out=uT[:, ct, :].rearrange("p (hf s m) -> p hf s m", hf=2, s=8)[:, tb // 2, :, (tb % 2) * 64:(tb % 2) * 64 + 64],
                                             in_=ps[pb][:, :].rearrange("p (m s) -> p s m", s=8)), reads=[psb[pb]],
                     writes=[uT_b[ct * 4 + j] for j in range(4)])

    def s5_pairs(full, ps_):
        RBm = [sb("RBm%d_%d" % (i, full), [128, 8, 2, 32], BF16, ps_) for i in range(2)]
        CPm = [sb("CPm%d_%d" % (i, full), [128, 9, 2, 32], BF16, ps_) for i in range(2)]
        RBm_b, CPm_b = k.bufs(2, "RBm"), k.bufs(2, "CPm")
        RBT = [sb("RBT%d_%d" % (i, full), [32, 16, 128], BF16, ps_) for i in range(2)]
        RBT_b = k.bufs(2, "RBT")
        KT = [sb("KT%d_%d" % (i, full), [32, 8, 32], BF16, ps_) for i in range(2)]
        KT_b = k.bufs(2, "KT")
        uq = [sb("uq%d_%d" % (i, full), [32, T], BF16, ps_) for i in range(2)]
        uq_b = k.bufs(2, "uq")
        XA = [sb("XA%d_%d" % (i, full), [128, 2, 129], F32, ps_) for i in range(2)]
        XB = [sb("XB%d_%d" % (i, full), [128, 2, 129], F32, ps_) for i in range(2)]
        XA_b, XB_b = k.bufs(2, "XA"), k.bufs(2, "XB")
        xp = [sb("xp%d_%d" % (i, full), [128, 2, 128], BF16, ps_) for i in range(2)]
        xp_b = k.bufs(2, "xp")
        yq = [sb("yq%d_%d" % (i, full), [32, 1024], BF16, ps_) for i in range(2)]
        yq_b = k.bufs(2, "yq")
        for i in range(2):
            k.op("pool", lambda e: e.memset(RBm[i][:], 0.0), writes=[RBm_b[i]])
            k.op("pool", lambda e: e.memset(CPm[i][:], 0.0), writes=[CPm_b[i]])
        PTA, PTB, PK, PYA, PYB = 0, 1, 2, 4, 5

        def pair_body(q):
            b = q % 2
            PW = 3 if b == 0 else 6
            ct, ql = q // 4, q % 4
            for gi in range(2):
                prt = slice(gi * 64, gi * 64 + 64)
                csl = slice(gi * 16, gi * 16 + 16)
                k.op("pool", lambda e: e.tensor_copy(out=RBm[b][prt, :, :, csl], in_=RBc[prt, q, :, :, :]), reads=[mats_b], writes=[RBm_b[b]])
                if full:
                    k.op("pool", lambda e: e.tensor_copy(out=CPm[b][prt, :, :, csl], in_=CPc[prt, q, :, :, :]), reads=[mats_b], writes=[CPm_b[b]])
            pTA = ps[PTA].bitcast(BF16)
            pTB = ps[PTB].bitcast(BF16)
            for sx in range(8):
                for ri in range(2):
                    idx = sx * 2 + ri
                    pt, pbb = (pTA, psb[PTA]) if idx < 8 else (pTB, psb[PTB])
                    k.op("pe", lambda e: e.transpose(out=pt[0:32, (idx % 8) * 128:(idx % 8 + 1) * 128], in_=RBm[b][:, sx, ri, :], identity=identb[:]),
                         reads=[RBm_b[b], identb_b], writes=[pbb])
            k.op("act", lambda e: e.copy(out=RBT[b][:, 0:8, :], in_=pTA[0:32, :].rearrange("p (a c) -> p a c", c=128)), reads=[psb[PTA]], writes=[RBT_b[b]])
            k.op("act", lambda e: e.copy(out=RBT[b][:, 8:16, :], in_=pTB[0:32, :].rearrange("p (a c) -> p a c", c=128)), reads=[psb[PTB], RBT_b[b]], writes=[RBT_b[b]])
            if full:
                for kk in range(8):
                    k.op("pe", lambda e: e.matmul(ps[PK][0:32, kk * 32:(kk + 1) * 32], RBm[b][:, 7, 0, :], CPm[b][:, kk, 0, :], start=True, stop=False),
                         reads=[RBm_b[b], CPm_b[b]], writes=[psb[PK]])
                    k.op("pe", lambda e: e.matmul(ps[PK][0:32, kk * 32:(kk + 1) * 32], RBm[b][:, 7, 1, :], CPm[b][:, kk, 1, :], start=False, stop=True),
                         reads=[RBm_b[b], CPm_b[b]], writes=[psb[PK]])
                k.op("act", lambda e: e.copy(out=KT[b][:], in_=ps[PK][0:32, 0:256].rearrange("p (a c) -> p a c", c=32)), reads=[psb[PK]], writes=[KT_b[b]])
            yield
            k.dma("sp", uq[b][:], uT[32 * ql:32 * ql + 32, ct, :], reads=[uT_b[ct * 4 + j] for j in range(4)], writes=[uq_b[b]])
            uqs = uq[b][:, :].rearrange("p (hf s m) -> p hf s m", hf=2, s=8)
            for hf in range(2):
                for ri in range(2):
                    for sx in range(8):
                        k.op("pe", lambda e: e.matmul(ps[PW][:, ri * 128:(ri + 1) * 128], RBT[b][:, sx * 2 + ri, :], uqs[:, hf, sx, :],
                                                      start=(sx == 0), stop=(sx == 7)), reads=[RBT_b[b], uq_b[b]], writes=[psb[PW]])
                yield
                k.op("act", lambda e: e.copy(out=XA[b][:, :, 1:129], in_=ps[PW][:, 0:256].rearrange("p (r m) -> p r m", r=2)),
                     reads=[psb[PW]], writes=[XA_b[b]])
                k.op("dve", lambda e: e.tensor_copy(out=XA[b][:, :, 0], in_=carry[:, q, :]), reads=[carry_b[q], XA_b[b]], writes=[XA_b[b]])
                src, dst, src_b, dst_b = XA[b], XB[b], XA_b[b], XB_b[b]
                N = 129
                for st in range(8):
                    d = 1 << st
                    k.op("pool", lambda e: e.tensor_copy(out=dst[:, :, 0:d], in_=src[:, :, 0:d]), reads=[src_b], writes=[dst_b])
                    dre, dim_, ndim = Dre[:, st, q:q + 1], Dim[:, st, q:q + 1], nDim[:, st, q:q + 1]
                    k.op("dve", lambda e: e.scalar_tensor_tensor(out=dst[:, 0, d:N], in0=src[:, 0, 0:N - d], scalar=dre, in1=src[:, 0, d:N],
                                                                 op0=ALU.mult, op1=ALU.add), reads=[src_b, mats_b, dst_b], writes=[dst_b])
                    k.op("dve", lambda e: e.scalar_tensor_tensor(out=dst[:, 1, d:N], in0=src[:, 1, 0:N - d], scalar=dre, in1=src[:, 1, d:N],
                                                                 op0=ALU.mult, op1=ALU.add), reads=[src_b, mats_b, dst_b], writes=[dst_b])
                    yield
                    k.op("dve", lambda e: e.scalar_tensor_tensor(out=dst[:, 0, d:N], in0=src[:, 1, 0:N - d], scalar=ndim, in1=dst[:, 0, d:N],
                                                                 op0=ALU.mult, op1=ALU.add), reads=[src_b, mats_b, dst_b], writes=[dst_b])
                    k.op("dve", lambda e: e.scalar_tensor_tensor(out=dst[:, 1, d:N], in0=src[:, 0, 0:N - d], scalar=dim_, in1=dst[:, 1, d:N],
                                                                 op0=ALU.mult, op1=ALU.add), reads=[src_b, mats_b, dst_b], writes=[dst_b])
                    yield
                    src, dst, src_b, dst_b = dst, src, dst_b, src_b
                X, X_b = src, src_b
                k.op("dve", lambda e: e.tensor_copy(out=carry[:, q, :], in_=X[:, :, 128]), reads=[X_b], writes=[carry_b[q]])
                yield
                if full:
                    k.op("act", lambda e: e.copy(out=xp[b][:], in_=X[:, :, 0:128]), reads=[X_b], writes=[xp_b[b]])
                    for j in range(8):
                        pyi = PYA if j < 4 else PYB
                        o = ps[pyi][0:32, (j % 4) * 128:(j % 4 + 1) * 128]
                        k.op("pe", lambda e: e.matmul(o, CPm[b][:, j + 1, 0, :], xp[b][:, 0, :], start=True, stop=False),
                             reads=[CPm_b[b], xp_b[b]], writes=[psb[pyi]])
                        k.op("pe", lambda e: e.matmul(o, CPm[b][:, j + 1, 1, :], xp[b][:, 1, :], start=False, stop=False),
                             reads=[CPm_b[b], xp_b[b]], writes=[psb[pyi]])
                        for sx in range(j + 1):
                            k.op("pe", lambda e: e.matmul(o, KT[b][:, j - sx, :], uqs[:, hf, sx, :], start=False, stop=(sx == j)),
                                 reads=[KT_b[b], uq_b[b]], writes=[psb[pyi]])
                    yb = b
                    yq3 = yq[yb][:, :].rearrange("p (m s) -> p m s", s=8)
                    for jj, pyi in ((0, PYA), (1, PYB)):
                        k.op("dve", lambda e: e.scalar_tensor_tensor(
                            out=yq3[:, :, jj * 4:(jj + 1) * 4], in0=uqs[:, hf, jj * 4:(jj + 1) * 4, :].rearrange("p j m -> p m j"), scalar=s5d[0:32, 8 + q:9 + q],
                            in1=ps[pyi][0:32, :].rearrange("p (j m) -> p m j", j=4), op0=ALU.mult, op1=ALU.add),
                            reads=[uq_b[b], psb[pyi], s5d_b, yq_b[yb]], writes=[yq_b[yb]])
                    k.dma("sp", uT[32 * ql:32 * ql + 32, ct, hf * 1024:(hf + 1) * 1024], yq[yb][:], reads=[yq_b[yb]],
                          writes=[uT_b[ct * 4 + hf * 2], uT_b[ct * 4 + hf * 2 + 1]])

        for q0 in range(0, NQ, 2):
            gens = [pair_body(q0), pair_body(q0 + 1)]
            while gens:
                for gnr in list(gens):
                    try:
                        next(gnr)
                    except StopIteration:
                        gens.remove(gnr)

    def s5_post(gs):
        f1 = [sb("g1_%d" % i, [128, 512], F32, gs) for i in range(2)]
        f2 = [sb("g2_%d" % i, [128, 512], F32, gs) for i in range(2)]
        f1_b, f2_b = k.bufs(2, "f1"), k.bufs(2, "f2")
        for ct in range(8):
            for tb in range(4):
                b = (ct * 4 + tb) % 2
                y = uT[:, ct, tb * 512:(tb + 1) * 512]
                yb_ = uT_b[ct * 4 + tb]
                k.op("dve", lambda e: e.tensor_tensor(out=f1[b][:], in0=y, in1=y, op=ALU.mult), reads=[yb_], writes=[f1_b[b]])
                k.op("dve", lambda e: e.tensor_scalar(out=f1[b][:], in0=f1[b][:], scalar1=0.044715, scalar2=1.0, op0=ALU.mult, op1=ALU.add),
                     reads=[f1_b[b]], writes=[f1_b[b]])
                k.op("pool", lambda e: e.tensor_tensor(out=f1[b][:], in0=f1[b][:], in1=y, op=ALU.mult), reads=[f1_b[b], yb_], writes=[f1_b[b]])
                k.op("act", lambda e: e.activation(out=f2[b][:], in_=f1[b][:], func=AF.Sigmoid, scale=1.5957691216057308),
                     reads=[f1_b[b]], writes=[f2_b[b]])
                k.op("pool", lambda e: e.tensor_tensor(out=y, in0=f2[b][:], in1=y, op=ALU.mult), reads=[f2_b[b], yb_], writes=[yb_])
        Wg = [sb("Wg%d" % i, [128, 8, 128], BF16, gs) for i in range(2)]
        Wg_b = k.bufs(2, "Wg")
        og = [sb("og%d" % i, [128, 512], BF16, gs) for i in range(2)]
        og_b = k.bufs(2, "og")
        for co in range(8):
            wbi = co % 2
            k.dma("pool", Wg[wbi][:], w_glu_d[:, co * 128:(co + 1) * 128].rearrange("(kc p) c -> p kc c", p=128), writes=[Wg_b[wbi]])
            for tb in range(4):
                b = (co * 4 + tb) % 2
                for ct in range(8):
                    k.op("pe", lambda e: e.matmul(ps[b][:, :], Wg[wbi][:, ct, :], uT[:, ct, tb * 512:(tb + 1) * 512], start=(ct == 0), stop=(ct == 7)),
                         reads=[Wg_b[wbi], uT_b[ct * 4 + tb]], writes=[psb[b]])
                k.op("act", lambda e: e.activation(out=f2[b][:], in_=ps[b][:, :], func=AF.Sigmoid), reads=[psb[b]], writes=[f2_b[b]])
                k.op("dve", lambda e: e.tensor_tensor(out=og[b][:], in0=f2[b][:], in1=uT[:, co, tb * 512:(tb + 1) * 512], op=ALU.mult),
                     reads=[f2_b[b], uT_b[co * 4 + tb]], writes=[og_b[b]])
                k.dma("sp", yssmT_d[co * 128:(co + 1) * 128, tb * 512:(tb + 1) * 512], og[b][:], reads=[og_b[b]], writes=[yssmT_b[co * 4 + tb]])

    yssmT_b = k.bufs(32, "yssmT")
    k.barrier()
    with ExitStack() as s5s:
        uT = sb("uT", [128, 8, T], BF16, s5s)
        uT_b = k.bufs(32, "uT")
        s5d = sb("s5d", [128, 40], F32, s5s)
        s5d_b = k.buf("s5d")
        k.dma("sp", s5d[:], s5d_d[:, :], writes=[s5d_b])
        Pre = sb("s5Pre", [128, 9, 32], F32, s5s)
        Pim = sb("s5Pim", [128, 9, 32], F32, s5s)
        Dre = sb("s5Dre", [128, 9, 32], F32, s5s)
        Dim = sb("s5Dim", [128, 9, 32], F32, s5s)
        nDim = sb("s5nDim", [128, 9, 32], F32, s5s)
        RBc = sb("s5RBc", [128, 32, 8, 2, 16], BF16, s5s)
        CPc = sb("s5CPc", [128, 32, 9, 2, 16], BF16, s5s)
        carry = sb("s5carry", [128, 32, 2], F32, s5s)
        carry_b = k.bufs(NQ, "carry")
        with ExitStack() as ss:
            mats_b = s5_setup(ss)
        k.barrier()
        if "s5mats" in dbg:
            dump("Pre", Pre[:, :, :].rearrange("p a b -> p (a b)"), [mats_b], 288)
            dump("Pim", Pim[:, :, :].rearrange("p a b -> p (a b)"), [mats_b], 288)
        with ExitStack() as us:
            s5_uproj(us)
        k.barrier()
        if "uT" in dbg:
            dump("uT", uT[:, 0, 0:512], [uT_b[0]], 512)
        for q in range(NQ):
            k.op("dve", lambda e: e.memset(carry[:, q, :], 0.0), writes=[carry_b[q]])
        if mode == "states":
            with ExitStack() as p1:
                s5_pairs(False, p1)
            k.barrier()
            bounce_o = dout("bounce", [128, AGW])
            k.dma("sp", bounce_o[:, 0:1024], S[:, :, :].rearrange("p h e -> p (h e)"), reads=S_b)
            k.dma("sp", bounce_o[:, 1024:AGW], carry[:, :, :].rearrange("p q r -> p (q r)"), reads=carry_b)
        if mode != "states":
            if mode == "fused":
                for q in range(NQ):
                    k.op("dve", lambda e: e.tensor_copy(out=carry[:, q, :], in_=carryP[:, q, :]), reads=[carryP_b], writes=[carry_b[q]])
            elif with_carry:
                with ExitStack() as gsx:
                    gath_d = din("gath", [NCORES * 128, AGW])
                    gb_ = k.buf("gath")
                    G = sb("G", [128, NCORES, AGW], F32, gsx)
                    G_b = k.buf("G")
                    k.dma("sp", G[:], gath_d[:, :].rearrange("(r p) w -> p r w", p=128), reads=[gb_], writes=[G_b])
                    X = sb("Xpre", [128, 1024], F32, gsx)
                    Xc = sb("Xcpre", [128, 32, 2], F32, gsx)
                    tq = sb("tqpre", [128, 4, 32], F32, gsx)
                    xb = k.buf("Xpre")
                    k.op("dve", lambda e: e.memset(X[:], 0.0), writes=[xb])
                    k.op("dve", lambda e: e.memset(Xc[:], 0.0), reads=[xb], writes=[xb])
                    for h in range(H):
                        k.op("dve", lambda e: e.memset(S[:, h, :], 0.0), writes=[S_b[h]])
                    for q in range(NQ):
                        k.op("dve", lambda e: e.memset(carry[:, q, :], 0.0), writes=[carry_b[q]])
                    d8r, d8i = Dre[:, 8, :], Dim[:, 8, :]
                    for r in range(NCORES - 1):
                        for h in range(H):
                            k.op("dve", lambda e: e.scalar_tensor_tensor(out=X[:, h * 128:(h + 1) * 128], in0=X[:, h * 128:(h + 1) * 128], scalar=G2048[h],
                                                                         in1=G[:, r, h * 128:(h + 1) * 128], op0=ALU.mult, op1=ALU.add),
                                 reads=[xb, G_b], writes=[xb])
                        k.op("dve", lambda e: e.scalar_tensor_tensor(out=S[:, :, :].rearrange("p h e -> p (h e)"), in0=X[:], scalar=onehot[:, r + 1:r + 2],
                                                                     in1=S[:, :, :].rearrange("p h e -> p (h e)"), op0=ALU.mult, op1=ALU.add),
                             reads=[xb, tabs_b] + S_b, writes=S_b)
                        Er = G[:, r, 1024:AGW].rearrange("p (q r) -> p q r", r=2)
                        tt_ = lambda o, a, b2, op: k.op("dve", lambda e: e.tensor_tensor(out=o, in0=a, in1=b2, op=op), reads=[xb, G_b, mats_b], writes=[xb])
                        tt_(tq[:, 0, :], Xc[:, :, 0], d8r, ALU.mult)
                        tt_(tq[:, 1, :], Xc[:, :, 1], d8i, ALU.mult)
                        tt_(tq[:, 2, :], Xc[:, :, 1], d8r, ALU.mult)
                        tt_(tq[:, 3, :], Xc[:, :, 0], d8i, ALU.mult)
                        tt_(tq[:, 0, :], tq[:, 0, :], tq[:, 1, :], ALU.subtract)
                        tt_(tq[:, 2, :], tq[:, 2, :], tq[:, 3, :], ALU.add)
                        tt_(Xc[:, :, 0], tq[:, 0, :], Er[:, :, 0], ALU.add)
                        tt_(Xc[:, :, 1], tq[:, 2, :], Er[:, :, 1], ALU.add)
                        k.op("dve", lambda e: e.scalar_tensor_tensor(out=carry[:, :, :].rearrange("p q r -> p (q r)"), in0=Xc[:, :, :].rearrange("p q r -> p (q r)"),
                                                                     scalar=onehot[:, r + 1:r + 2], in1=carry[:, :, :].rearrange("p q r -> p (q r)"),
                                                                     op0=ALU.mult, op1=ALU.add), reads=[xb, tabs_b] + carry_b, writes=carry_b)
                    for h in range(H):
                        k.op("act", lambda e: e.copy(out=Sbf[:, h, :], in_=S[:, h, :]), reads=[S_b[h]], writes=[Sbf_b[h]])
                k.barrier()
            with ExitStack() as p2:
                s5_pairs(True, p2)
            k.barrier()
            if "ypre" in dbg:
                dump("ypre", uT[:, 0, 0:512], [uT_b[0]], 512)
            with ExitStack() as gs:
                s5_post(gs)
            k.barrier()

    if mode != "states":
        with ExitStack() as rs:
            retention(True, rs)
        k.barrier()

        if "yret" in dbg:
            with ExitStack() as sd:
                for h in range(H):
                    tmpb = sb("dbgy_b%d" % h, [128, T], BF16, sd)
                    tmpf = sb("dbgy_f%d" % h, [128, T], F32, sd)
                    tb, tb2 = k.buf(), k.buf()
                    k.dma("sp", tmpb[:], yretT_d[h * 128:(h + 1) * 128, :], reads=[yretT_b[h]], writes=[tb])
                    k.op("act", lambda e: e.copy(out=tmpf[:], in_=tmpb[:]), reads=[tb], writes=[tb2])
                    k.dma("sp", dbg["yret"][h * 128:(h + 1) * 128, :], tmpf[:], reads=[tb2])
            k.barrier()

        if "yssm" in dbg:
            with ExitStack() as sd:
                for co in range(8):
                    tmpb = sb("dbgs_b%d" % co, [128, T], BF16, sd)
                    tmpf = sb("dbgs_f%d" % co, [128, T], F32, sd)
                    tb_, tb2 = k.buf(), k.buf()
                    k.dma("sp", tmpb[:], yssmT_d[co * 128:(co + 1) * 128, :], reads=yssmT_b[co * 4:(co + 1) * 4], writes=[tb_])
                    k.op("act", lambda e: e.copy(out=tmpf[:], in_=tmpb[:]), reads=[tb_], writes=[tb2])
                    k.dma("sp", dbg["yssm"][co * 128:(co + 1) * 128, :], tmpf[:], reads=[tb2])
            k.barrier()


        ms.close()
        k.barrier()
        x1_b = k.bufs(NT, "x1")
        with ExitStack() as gs4:
            yr = sb("m_yr", [128, 8, 1024], BF16, gs4)
            ys = sb("m_ys", [128, 8, 1024], BF16, gs4)
            yr_b4, ys_b4 = k.buf("m_yr"), k.buf("m_ys")
            mT = sb("m_mT", [128, KC, 1024], BF16, gs4)
            mT_b = k.bufs(2, "m_mT")
            wma = [sb("m_wma%d" % i, [128, KC, 128], BF16, gs4) for i in range(2)]
            wmb = [sb("m_wmb%d" % i, [128, KC, 128], BF16, gs4) for i in range(2)]
            wa = [sb("m_wa%d" % i, [128, 8, 128], BF16, gs4) for i in range(2)]
            wb_ = [sb("m_wb%d" % i, [128, 8, 128], BF16, gs4) for i in range(2)]
            wma_b, wmb_b, wa_b, wbb_b = k.bufs(2, "wma"), k.bufs(2, "wmb"), k.bufs(2, "wa"), k.bufs(2, "wb")
            wo = [sb("m_wo%d" % i, [128, KC, 256], BF16, gs4) for i in range(2)]
            wo_b = k.bufs(2, "wo")
            sga = [sb("m_sga%d" % i, [128, 512], F32, gs4) for i in range(2)]
            sgb = [sb("m_sgb%d" % i, [128, 512], F32, gs4) for i in range(2)]
            sga_b, sgb_b = k.bufs(2, "sga"), k.bufs(2, "sgb")
            xo = [sb("m_xo%d" % i, [128, 256], F32, gs4) for i in range(4)]
            oo = [sb("m_oo%d" % i, [128, 256], F32, gs4) for i in range(4)]
            xo_b, oo_b = k.bufs(4, "xo"), k.bufs(4, "oo")

            def load_mw(j):
                b = j % 2
                k.dma("pool", wma[b][:], w_merge_d[:, j * 128:(j + 1) * 128].rearrange("(kc p) c -> p kc c", p=128), writes=[wma_b[b]])
                k.dma("pool", wmb[b][:], w_merge_d[:, D + j * 128:D + (j + 1) * 128].rearrange("(kc p) c -> p kc c", p=128), writes=[wmb_b[b]])
                k.dma("pool", wa[b][:], w_a_d[:, j * 128:(j + 1) * 128].rearrange("(kc p) c -> p kc c", p=128), writes=[wa_b[b]])
                k.dma("pool", wb_[b][:], w_b_d[:, j * 128:(j + 1) * 128].rearrange("(kc p) c -> p kc c", p=128), writes=[wbb_b[b]])

            it = 0
            for hf in range(2):
                t0 = hf * 1024
                k.dma("sp", yr[:], yretT_d[:, t0:t0 + 1024].rearrange("(hc p) t -> p hc t", p=128), reads=yretT_b, writes=[yr_b4])
                k.dma("sp", ys[:], yssmT_d[:, t0:t0 + 1024].rearrange("(hc p) t -> p hc t", p=128), reads=yssmT_b, writes=[ys_b4])
                load_mw(0)
                for j in range(KC):
                    b = j % 2
                    if j + 1 < KC:
                        load_mw(j + 1)
                    for tb in range(2):
                        pbase = 4 * (it % 2)
                        it += 1
                        tsl = slice(t0 + tb * 512, t0 + (tb + 1) * 512)
                        lsl = slice(tb * 512, (tb + 1) * 512)
                        hb_ = hT_b[(t0 + tb * 512) // 128:(t0 + tb * 512) // 128 + 4]
                        for kc in range(KC):
                            k.op("pe", lambda e: e.matmul(ps[pbase][:, :], wma[b][:, kc, :], hT[:, kc, tsl], start=(kc == 0), stop=(kc == KC - 1)),
                                 reads=[wma_b[b]] + hb_, writes=[psb[pbase]])
                        for kc in range(KC):
                            k.op("pe", lambda e: e.matmul(ps[pbase + 1][:, :], wmb[b][:, kc, :], hT[:, kc, tsl], start=(kc == 0), stop=(kc == KC - 1)),
                                 reads=[wmb_b[b]] + hb_, writes=[psb[pbase + 1]])
                        for hc in range(8):
                            k.op("pe", lambda e: e.matmul(ps[pbase + 2][:, :], wa[b][:, hc, :], yr[:, hc, lsl], start=(hc == 0), stop=(hc == 7)),
                                 reads=[wa_b[b], yr_b4], writes=[psb[pbase + 2]])
                        for hc in range(8):
                            k.op("pe", lambda e: e.matmul(ps[pbase + 3][:, :], wb_[b][:, hc, :], ys[:, hc, lsl], start=(hc == 0), stop=(hc == 7)),
                                 reads=[wbb_b[b], ys_b4], writes=[psb[pbase + 3]])
                        sb_i = it % 2
                        k.op("act", lambda e: e.activation(out=sga[sb_i][:], in_=ps[pbase][:, :], func=AF.Sigmoid), reads=[psb[pbase]], writes=[sga_b[sb_i]])
                        k.op("act", lambda e: e.activation(out=sgb[sb_i][:], in_=ps[pbase + 1][:, :], func=AF.Sigmoid), reads=[psb[pbase + 1]], writes=[sgb_b[sb_i]])
                        k.op("dve", lambda e: e.tensor_tensor(out=sga[sb_i][:], in0=sga[sb_i][:], in1=ps[pbase + 2][:, :], op=ALU.mult),
                             reads=[sga_b[sb_i], psb[pbase + 2]], writes=[sga_b[sb_i]])
                        k.op("dve", lambda e: e.tensor_tensor(out=sgb[sb_i][:], in0=sgb[sb_i][:], in1=ps[pbase + 3][:, :], op=ALU.mult),
                             reads=[sgb_b[sb_i], psb[pbase + 3]], writes=[sgb_b[sb_i]])
                        k.op("pool", lambda e: e.tensor_tensor(out=mT[:, j, lsl], in0=sga[sb_i][:], in1=sgb[sb_i][:], op=ALU.add),
                             reads=[sga_b[sb_i], sgb_b[sb_i]], writes=[mT_b[tb]])
                for nb in range(8):
                    wbi = nb % 2
                    k.dma("pool", wo[wbi][:], w_out_d[:, nb * 256:(nb + 1) * 256].rearrange("(kc p) c -> p kc c", p=128), writes=[wo_b[wbi]])
                    def ldx(it_):
                        nb_, tl_ = it_ // 8, it_ % 8
                        k.dma("sp", xo[it_ % 4][:], x_d[t0 + tl_ * 128:t0 + tl_ * 128 + 128, nb_ * 256:(nb_ + 1) * 256], writes=[xo_b[it_ % 4]])
                    if nb == 0:
                        ldx(0)
                        ldx(1)
                    for tl in range(8):
                        it_ = nb * 8 + tl
                        pb = 2 * (it_ % 2)
                        xb_ = it_ % 4
                        row0 = t0 + tl * 128
                        if it_ + 2 < 64:
                            ldx(it_ + 2)
                        for j in range(KC):
                            k.op("pe", lambda e: e.matmul(ps[pb][:, 0:256], mT[:, j, tl * 128:(tl + 1) * 128], wo[wbi][:, j, :], start=(j == 0), stop=(j == KC - 1)),
                                 reads=[mT_b[tl // 4], wo_b[wbi]], writes=[psb[pb]])
                        k.op("dve", lambda e: e.tensor_tensor(out=oo[xb_][:], in0=xo[xb_][:], in1=ps[pb][:, 0:256], op=ALU.add),
                             reads=[xo_b[xb_], psb[pb]], writes=[oo_b[xb_]])
                        k.dma("sp", x1_d[row0:row0 + 128, nb * 256:(nb + 1) * 256], oo[xb_][:], reads=[oo_b[xb_]], writes=[x1_w[row0 // 128][nb]])
        hs.close()
        k.barrier()
        if "x1" in dbg:
            with ExitStack() as sd:
                for i in range(NT):
                    tmpf = sb("dbgx1_%d" % i, [128, D], F32, sd)
                    tb_ = k.buf()
                    k.dma("sp", tmpf[:], x1_d[i * 128:(i + 1) * 128, :], reads=x1_w[i], writes=[tb_])
                    k.dma("sp", dbg["x1"][i * 128:(i + 1) * 128, :], tmpf[:], reads=[tb_])
            k.barrier()


        out_d = dout("out", [T, D])
        with ExitStack() as g5:
            st5 = sb("st5", [128, 8, 4], F32, g5)
            st5_b = k.bufs(8, "st5")
            wr = sb("wr", [128, KC, 36], BF16, g5)
            wr_b = k.buf("wr")
            k.dma("pool", wr[:], w_rt_d[:, :].rearrange("(kc p) c -> p kc c", p=128), writes=[wr_b])
            wts = sb("wts", [128, 8, NE], F32, g5)
            wts_b = k.bufs(8, "wts")
            wtsb = sb("wtsb", [128, 8, NE], BF16, g5)
            wtsb_b = k.bufs(8, "wtsb")
            Ab = sb("Ab", [128, 8, 4], BF16, g5)
            Ab_b = k.bufs(8, "Ab")
            rank = sb("rank", [128, 8, 4], F32, g5)
            rank_b = k.bufs(8, "rank")
            rt = sb("rt", [128, 8, 40], F32, g5)
            rl = sb("rl", [128, 4, 36], F32, g5)
            rt_b = k.buf("rt")
            ltso = sb("ltso", [128, 256], BF16, g5)
            iota = sb("iota", [128, CG], F32, g5)
            cst_b = k.buf("moec")
            k.dma("pool", ltso[:], moec_d[:, 0:256], writes=[cst_b])
            k.dma("sp", iota[:], moec_d[:, 256:256 + CG], reads=[cst_b], writes=[cst_b])

            def norm_tile(src, src_b, xs_ap, xs_b, junk_ap, junk_b, stc, stc_b, dst3, dst_b, nT, pbanks):
                k.op("act", lambda e: e.activation(out=junk_ap, in_=src, func=AF.Square, accum_out=stc[:, 0:1]), reads=[src_b], writes=[junk_b, stc_b])
                k.op("act", lambda e: e.activation(out=stc[:, 1:2], in_=stc[:, 0:1], func=AF.Sqrt, scale=1.0 / D, bias=EPS), reads=[stc_b], writes=[stc_b])
                k.op("dve", lambda e: e.reciprocal(out=stc[:, 1:2], in_=stc[:, 1:2]), reads=[stc_b], writes=[stc_b])
                k.op("act", lambda e: e.activation(out=xs_ap, in_=src, func=AF.Copy, scale=stc[:, 1:2]), reads=[src_b, stc_b], writes=[xs_b])
                for half in range(2):
                    pbank = ps[pbanks[half]].bitcast(BF16)
                    for j in range(8):
                        kc = half * 8 + j
                        k.op("pe", lambda e: e.transpose(out=pbank[:, j * 128:(j + 1) * 128], in_=xs_ap[:, kc * 128:(kc + 1) * 128], identity=identb[:]),
                             reads=[xs_b, identb_b], writes=[psb[pbanks[half]]])
                    k.op("dve", lambda e: e.tensor_tensor(out=dst3[:, half * 8:(half + 1) * 8, :], in0=pbank[:, :].rearrange("p (j t) -> p j t", t=128),
                                                          in1=nT[:, half * 8:(half + 1) * 8].unsqueeze(2).broadcast_to([128, 8, 128]), op=ALU.mult),
                         reads=[psb[pbanks[half]], small_b], writes=[dst_b])

            for hf in range(2):
                t0 = hf * 1024
                with ExitStack() as gm:
                    hn = sb("hn%d" % hf, [128, 8, D], BF16, gm)
                    hn_b = k.bufs(8, "hn")
                    hrt = [sb("hrt%d_%d" % (hf, i), [128, KC, 128], BF16, gm) for i in range(2)]
                    hrt_b = k.bufs(2, "hrt")
                    xt5 = [sb("xt5_%d_%d" % (hf, i), [128, D], F32, gm) for i in range(2)]
                    xt5_b = k.bufs(2, "xt5")
                    Gw = sb("Gw%d" % hf, [128, KC, FE], BF16, gm)
                    Uw = sb("Uw%d" % hf, [128, KC, FE], BF16, gm)
                    Dw = sb("Dw%d" % hf, [128, 4, D], BF16, gm)
                    Gw_b, Uw_b, Dw_b = k.buf("Gw"), k.buf("Uw"), k.buf("Dw")
                    sgT = sb("sgT%d" % hf, [128, 4, CG], BF16, gm)
                    aT = sb("aT%d" % hf, [128, 4, CG], BF16, gm)
                    sgT_b, aT_b = k.bufs(4, "sgT"), k.bufs(4, "aT")
                    xg = sb("xg%d" % hf, [128, KC * CG], BF16, gm)
                    xg3 = xg[:, :].rearrange("p (kc c) -> p kc c", c=CG)
                    accgb = xg[:, :].rearrange("p (b d) -> p b d", d=D)
                    xg_b = k.buf("xg")
                    Selg = sb("Selg%d" % hf, [128, 8, CG], BF16, gm)
                    Selg_b = k.bufs(8, "Selg")
                    SelgT = sb("SelgT%d" % hf, [128, 3, 8, 128], BF16, gm)
                    SelgT_b = k.bufs(3, "SelgT")
                    accg = sb("accg%d" % hf, [128, 3, D], F32, gm)
                    accg_b = k.bufs(3, "accg")
                    wtsg = sb("wtsg%d" % hf, [128, 3, 8], F32, gm)
                    wtsg_b = k.buf("wtsg")
                    for tl in range(8):
                        b = tl % 2
                        gt = t0 // 128 + tl
                        k.dma("sp", xt5[b][:], x1_d[gt * 128:(gt + 1) * 128, :], reads=x1_w[gt], writes=[xt5_b[b]])
                        norm_tile(xt5[b][:], xt5_b[b], hn[:, tl, :], hn_b[tl], xg[:, 0:D], xg_b, st5[:, tl, :], st5_b[tl], hrt[b][:, :, :], hrt_b[b], nffnT, (6, 7))
                        R = rt[:, tl, :]
                        for kc in range(KC):
                            k.op("pe", lambda e: e.matmul(ps[5][:, 0:36], hrt[b][:, kc, :], wr[:, kc, :], start=(kc == 0), stop=(kc == KC - 1)),
                                 reads=[hrt_b[b], wr_b], writes=[psb[5]])
                        L = rl[:, tl % 4, :]
                        rb_ = [rt_b]
                        k.op("dve", lambda e: e.tensor_tensor(out=L, in0=ps[5][:, 0:36], in1=rbias, op=ALU.add), reads=[psb[5], small_b] + rb_, writes=rb_)
                        lg, le = L[:, 0:4], L[:, 4:36]
                        gmax, ngmax, gsum, gw, m1, m2, d12, w1, w2 = [R[:, i:i + 1] for i in range(9)]
                        ohg, pen = R[:, 12:16], R[:, 16:20]
                        k.op("dve", lambda e: e.reduce_max(out=gmax, in_=lg, axis=mybir.AxisListType.X), reads=rb_, writes=rb_)
                        k.op("dve", lambda e: e.tensor_scalar(out=ngmax, in0=gmax, scalar1=-1.0, scalar2=None, op0=ALU.mult), reads=rb_, writes=rb_)
                        k.op("act", lambda e: e.activation(out=R[:, 20:24], in_=lg, func=AF.Exp, bias=ngmax, scale=1.0, accum_out=gsum), reads=rb_, writes=rb_)
                        k.op("dve", lambda e: e.reciprocal(out=gw, in_=gsum), reads=rb_, writes=rb_)
                        k.op("dve", lambda e: e.tensor_scalar(out=ohg, in0=lg, scalar1=gmax, scalar2=None, op0=ALU.is_equal), reads=rb_, writes=rb_)
                        k.op("dve", lambda e: e.tensor_scalar(out=pen, in0=ohg, scalar1=-1.0, scalar2=1e30, op0=ALU.add, op1=ALU.mult), reads=rb_, writes=rb_)
                        le3 = le.rearrange("p (g x) -> p g x", x=8)
                        k.op("dve", lambda e: e.tensor_tensor(out=le3, in0=le3, in1=pen.unsqueeze(2).broadcast_to([128, 4, 8]), op=ALU.add), reads=rb_, writes=rb_)
                        k.op("dve", lambda e: e.reduce_max(out=m1, in_=le, axis=mybir.AxisListType.X), reads=rb_, writes=rb_)
                        mk1, mk2 = wts[:, tl, :], L[:, 4:36]
                        k.op("dve", lambda e: e.tensor_scalar(out=mk1, in0=le, scalar1=m1, scalar2=None, op0=ALU.is_equal), reads=rb_, writes=rb_ + [wts_b[tl]])
                        k.op("dve", lambda e: e.scalar_tensor_tensor(out=le, in0=mk1, scalar=-1e30, in1=le, op0=ALU.mult, op1=ALU.add), reads=rb_ + [wts_b[tl]], writes=rb_)
                        k.op("dve", lambda e: e.reduce_max(out=m2, in_=le, axis=mybir.AxisListType.X), reads=rb_, writes=rb_)
                        k.op("dve", lambda e: e.tensor_scalar(out=mk2, in0=le, scalar1=m2, scalar2=None, op0=ALU.is_equal), reads=rb_, writes=rb_)
                        k.op("dve", lambda e: e.tensor_tensor(out=d12, in0=m1, in1=m2, op=ALU.subtract), reads=rb_, writes=rb_)
                        k.op("act", lambda e: e.activation(out=w1, in_=d12, func=AF.Sigmoid), reads=rb_, writes=rb_)
                        k.op("act", lambda e: e.activation(out=w2, in_=d12, func=AF.Sigmoid, scale=-1.0), reads=rb_, writes=rb_)
                        k.op("dve", lambda e: e.tensor_tensor(out=w1, in0=w1, in1=gw, op=ALU.mult), reads=rb_, writes=rb_)
                        k.op("dve", lambda e: e.tensor_tensor(out=w2, in0=w2, in1=gw, op=ALU.mult), reads=rb_, writes=rb_)
                        k.op("dve", lambda e: e.tensor_scalar(out=mk1, in0=mk1, scalar1=w1, scalar2=None, op0=ALU.mult), reads=rb_ + [wts_b[tl]], writes=[wts_b[tl]])
                        k.op("dve", lambda e: e.scalar_tensor_tensor(out=mk1, in0=mk2, scalar=w2, in1=mk1, op0=ALU.mult, op1=ALU.add), reads=rb_ + [wts_b[tl]], writes=[wts_b[tl]])
                        k.op("act", lambda e: e.copy(out=wtsb[:, tl, :], in_=wts[:, tl, :]), reads=[wts_b[tl]], writes=[wtsb_b[tl]])
                        k.op("act", lambda e: e.copy(out=Ab[:, tl, :], in_=ohg), reads=rb_, writes=[Ab_b[tl]])
                    if hf == 0 and "wts" in dbg:
                        dump("wts", wts[:, :, :].rearrange("p a b -> p (a b)"), wts_b, 256)
                    for tl in range(8):
                        k.op("pe", lambda e: e.matmul(ps[5][:, 0:4], ltso[:, 0:128], Ab[:, tl, :], start=True, stop=(tl == 0)), reads=[Ab_b[tl], cst_b], writes=[psb[5]])
                        for tp in range(tl):
                            k.op("pe", lambda e: e.matmul(ps[5][:, 0:4], ltso[:, 128:256], Ab[:, tp, :], start=False, stop=(tp == tl - 1)), reads=[Ab_b[tp], cst_b], writes=[psb[5]])
                        k.op("act", lambda e: e.copy(out=rank[:, tl, :], in_=ps[5][:, 0:4]), reads=[psb[5]], writes=[rank_b[tl]])
                    for g in range(4):
                        for tl in range(8):
                            eng = "dve" if tl % 2 == 0 else "pool"
                            k.op(eng, lambda e: e.tensor_scalar(out=Selg[:, tl, :], in0=iota[:], scalar1=rank[:, tl, g:g + 1], scalar2=rt[:, tl, 12 + g:13 + g],
                                                                op0=ALU.is_equal, op1=ALU.mult), reads=[rank_b[tl], rt_b, cst_b], writes=[Selg_b[tl]])
                        for blk in range(3):
                            bank = ps[blk].bitcast(BF16)
                            for tl in range(8):
                                k.op("pe", lambda e: e.transpose(out=bank[:, tl * 128:(tl + 1) * 128], in_=Selg[:, tl, blk * 128:(blk + 1) * 128], identity=identb[:]),
                                     reads=[Selg_b[tl], identb_b], writes=[psb[blk]])
                            k.op("act", lambda e: e.copy(out=SelgT[:, blk, :, :], in_=bank[:, :].rearrange("p (t c) -> p t c", c=128)), reads=[psb[blk]], writes=[SelgT_b[blk]])
                        for blk in range(3):
                            for tl in range(8):
                                k.op("pe", lambda e: e.matmul(ps[3][:, blk * 8:(blk + 1) * 8], Selg[:, tl, blk * 128:(blk + 1) * 128], wtsb[:, tl, g * 8:(g + 1) * 8],
                                                              start=(tl == 0), stop=(tl == 7)), reads=[Selg_b[tl], wtsb_b[tl]], writes=[psb[3]])
                        k.op("act", lambda e: e.copy(out=wtsg[:, :, :], in_=ps[3][:, 0:24].rearrange("p (b x) -> p b x", x=8)), reads=[psb[3]], writes=[wtsg_b])
                        for r4 in range(4):
                            for kcl in range(4):
                                kc = r4 * 4 + kcl
                                bank = 4 + kcl
                                for tl in range(8):
                                    k.op("pe", lambda e: e.matmul(ps[bank][:, 0:CG], hn[:, tl, kc * 128:(kc + 1) * 128], Selg[:, tl, :], start=(tl == 0), stop=(tl == 7)),
                                         reads=[hn_b[tl], Selg_b[tl]], writes=[psb[bank]])
                                if kcl % 2 == 0:
                                    k.op("act", lambda e: e.activation(out=xg3[:, kc, :], in_=ps[bank][:, 0:CG], func=AF.Copy, scale=nffnT[:, kc:kc + 1]),
                                         reads=[psb[bank], small_b, xg_b], writes=[xg_b])
                                else:
                                    k.op("dve", lambda e: e.tensor_scalar(out=xg3[:, kc, :], in0=ps[bank][:, 0:CG], scalar1=nffnT[:, kc:kc + 1], scalar2=None, op0=ALU.mult),
                                         reads=[psb[bank], small_b, xg_b], writes=[xg_b])
                        for blk in range(3):
                            k.op("pool", lambda e: e.memset(accg[:, blk, :], 0.0), writes=[accg_b[blk]])
                        for el in range(8):
                            ex = g * 8 + el
                            k.dma("pool", Gw[:], w_eg_d[ex].rearrange("(kc p) f -> p kc f", p=128), writes=[Gw_b])
                            k.dma("pool", Uw[:], w_eu_d[ex].rearrange("(kc p) f -> p kc f", p=128), writes=[Uw_b])
                            k.dma("pool", Dw[:], w_ed_d[ex].rearrange("(fc p) d -> p fc d", p=128), writes=[Dw_b])
                            for ft in range(4):
                                for kc in range(KC):
                                    k.op("pe", lambda e: e.matmul(ps[ft][:, 0:CG], Gw[:, kc, ft * 128:(ft + 1) * 128], xg3[:, kc, :], start=(kc == 0), stop=(kc == KC - 1)),
                                         reads=[Gw_b, xg_b], writes=[psb[ft]])
                                k.op("act", lambda e: e.activation(out=sgT[:, ft, :], in_=ps[ft][:, 0:CG], func=AF.Silu), reads=[psb[ft]], writes=[sgT_b[ft]])
                            for ft in range(4):
                                for kc in range(KC):
                                    k.op("pe", lambda e: e.matmul(ps[4 + ft][:, 0:CG], Uw[:, kc, ft * 128:(ft + 1) * 128], xg3[:, kc, :], start=(kc == 0), stop=(kc == KC - 1)),
                                         reads=[Uw_b, xg_b], writes=[psb[4 + ft]])
                                k.op("dve", lambda e: e.tensor_tensor(out=aT[:, ft, :], in0=sgT[:, ft, :], in1=ps[4 + ft][:, 0:CG], op=ALU.mult),
                                     reads=[psb[4 + ft], sgT_b[ft]], writes=[aT_b[ft]])
                            cnt = 0
                            for blk in range(3):
                                for nb in range(4):
                                    pb = cnt % 4
                                    cnt += 1
                                    for fc in range(4):
                                        k.op("pe", lambda e: e.matmul(ps[pb][:, :], aT[:, fc, blk * 128:(blk + 1) * 128], Dw[:, fc, nb * 512:(nb + 1) * 512],
                                                                      start=(fc == 0), stop=(fc == 3)), reads=[Dw_b] + aT_b, writes=[psb[pb]])
                                    k.op("dve", lambda e: e.scalar_tensor_tensor(out=accg[:, blk, nb * 512:(nb + 1) * 512], in0=ps[pb][:, :], scalar=wtsg[:, blk, el:el + 1],
                                                                                 in1=accg[:, blk, nb * 512:(nb + 1) * 512], op0=ALU.mult, op1=ALU.add),
                                         reads=[psb[pb], wtsg_b, accg_b[blk]], writes=[accg_b[blk]])
                        for blk in range(3):
                            k.op("act", lambda e: e.copy(out=accgb[:, blk, :], in_=accg[:, blk, :]), reads=[accg_b[blk], xg_b], writes=[xg_b])
                        cnt = 0
                        k.dma("sp", xt5[0][:], x1_d[(t0 // 128) * 128:(t0 // 128 + 1) * 128, :], reads=x1_w[t0 // 128], writes=[xt5_b[0]])
                        for tl in range(8):
                            b = tl % 2
                            gt = t0 // 128 + tl
                            if tl + 1 < 8:
                                k.dma("sp", xt5[1 - b][:], x1_d[(gt + 1) * 128:(gt + 2) * 128, :], reads=x1_w[gt + 1], writes=[xt5_b[1 - b]])
                            for nb in range(4):
                                pb = 4 + cnt % 4
                                cnt += 1
                                for blk in range(3):
                                    k.op("pe", lambda e: e.matmul(ps[pb][:, :], SelgT[:, blk, tl, :], accgb[:, blk, nb * 512:(nb + 1) * 512], start=(blk == 0), stop=(blk == 2)),
                                         reads=[SelgT_b[blk], xg_b], writes=[psb[pb]])
                                k.op("dve", lambda e: e.tensor_tensor(out=xt5[b][:, nb * 512:(nb + 1) * 512], in0=xt5[b][:, nb * 512:(nb + 1) * 512], in1=ps[pb][:, :], op=ALU.add),
                                     reads=[psb[pb], xt5_b[b]], writes=[xt5_b[b]])
                            k.dma("sp", x1_d[gt * 128:(gt + 1) * 128, :], xt5[b][:], reads=[xt5_b[b]], writes=x1_w[gt])
                k.barrier()
                gpx = ExitStack()
                acc = sb("acc%d" % hf, [128, 8, D], F32, gpx)
                acc_b = k.bufs(8, "acc")
                hh = sb("hh%d" % hf, [128, KC, 1024], BF16, gpx)
                hh_b = k.bufs(8, "hh")
                xs5 = sb("xs5_%d" % hf, [128, D], BF16, gpx)
                junk5 = sb("junk5_%d" % hf, [128, D], BF16, gpx)
                xs5_b, junk5_b = k.buf("xs5"), k.buf("junk5")
                for tl in range(8):
                    k.dma("sp", acc[:, tl, :], x1_d[t0 + tl * 128:t0 + (tl + 1) * 128, :], reads=x1_w[(t0 // 128) + tl], writes=[acc_b[tl]])
                if hf == 0 and "x2" in dbg:
                    for tl in range(8):
                        k.dma("sp", dbg["x2"][tl * 128:(tl + 1) * 128, :], acc[:, tl, :], reads=[acc_b[tl]])

                def norm_to_hh(tl, nT):
                    norm_tile(acc[:, tl, :], acc_b[tl], xs5[:], xs5_b, junk5[:], junk5_b, st5[:, tl, :], st5_b[tl],
                              hh[:, :, tl * 128:(tl + 1) * 128], hh_b[tl], nT, (6, 7))
                with ExitStack() as gp:
                    pT = sb("pT%d" % hf, [128, 2, 1024], BF16, gp)
                    pT_b = k.bufs(8, "pT")
                    pld = [sb("pld%d_%d" % (hf, i), [128, 256], F32, gp) for i in range(2)]
                    plb = [sb("plb%d_%d" % (hf, i), [128, 256], BF16, gp) for i in range(2)]
                    pld_b, plb_b = k.bufs(2, "pld"), k.bufs(2, "plb")
                    wpg = [sb("wpg%d_%d" % (hf, i), [128, KC, 256], BF16, gp) for i in range(2)]
                    wpl = [sb("wpl%d_%d" % (hf, i), [128, 2, 256], BF16, gp) for i in range(2)]
                    wpg_b, wpl_b = k.bufs(2, "wpg"), k.bufs(2, "wpl")
                    sg5 = [sb("sg5_%d_%d" % (hf, i), [128, 256], F32, gp) for i in range(2)]
                    sg5_b = k.bufs(2, "sg5")
                    nfb = sb("nfb%d" % hf, [128, D], F32, gp)
                    nfb_b = k.buf("nfb")
                    k.dma("sp", nfb[:], nfb_d[:, :], writes=[nfb_b])
                    ot = [sb("ot%d_%d" % (hf, i), [128, D], F32, gp) for i in range(2)]
                    ot_b = k.bufs(2, "ot")
                    for tl in range(8):
                        b = tl % 2
                        norm_to_hh(tl, npleT)
                        k.dma("sp", pld[b][:], p_d[t0 + tl * 128:t0 + (tl + 1) * 128, :], writes=[pld_b[b]])
                        k.op("act", lambda e: e.copy(out=plb[b][:], in_=pld[b][:]), reads=[pld_b[b]], writes=[plb_b[b]])
                        pbank = ps[5].bitcast(BF16)
                        for c2 in range(2):
                            k.op("pe", lambda e: e.transpose(out=pbank[:, c2 * 128:(c2 + 1) * 128], in_=plb[b][:, c2 * 128:(c2 + 1) * 128], identity=identb[:]),
                                 reads=[plb_b[b], identb_b], writes=[psb[5]])
                        k.op("act", lambda e: e.copy(out=pT[:, :, tl * 128:(tl + 1) * 128], in_=pbank[:, 0:256].rearrange("p (c t) -> p c t", t=128)),
                             reads=[psb[5]], writes=[pT_b[tl]])
                    cnt = 0
                    for nb in range(8):
                        wbi = nb % 2
                        k.dma("pool", wpg[wbi][:], w_pg_d[:, nb * 256:(nb + 1) * 256].rearrange("(kc p) c -> p kc c", p=128), writes=[wpg_b[wbi]])
                        k.dma("pool", wpl[wbi][:], w_ple_d[:, nb * 256:(nb + 1) * 256].rearrange("(kc p) c -> p kc c", p=128), writes=[wpl_b[wbi]])
                        for tl in range(8):
                            pb = 2 * (cnt % 2)
                            sb_i = cnt % 2
                            cnt += 1
                            for kc in range(KC):
                                k.op("pe", lambda e: e.matmul(ps[pb][:, 0:256], hh[:, kc, tl * 128:(tl + 1) * 128], wpg[wbi][:, kc, :], start=(kc == 0), stop=(kc == KC - 1)),
                                     reads=[hh_b[tl], wpg_b[wbi]], writes=[psb[pb]])
                            for c2 in range(2):
                                k.op("pe", lambda e: e.matmul(ps[pb + 1][:, 0:256], pT[:, c2, tl * 128:(tl + 1) * 128], wpl[wbi][:, c2, :], start=(c2 == 0), stop=(c2 == 1)),
                                     reads=[pT_b[tl], wpl_b[wbi]], writes=[psb[pb + 1]])
                            k.op("act", lambda e: e.activation(out=sg5[sb_i][:], in_=ps[pb][:, 0:256], func=AF.Sigmoid), reads=[psb[pb]], writes=[sg5_b[sb_i]])
                            k.op("dve", lambda e: e.tensor_tensor(out=sg5[sb_i][:], in0=sg5[sb_i][:], in1=ps[pb + 1][:, 0:256], op=ALU.mult),
                                 reads=[sg5_b[sb_i], psb[pb + 1]], writes=[sg5_b[sb_i]])
                            k.op("pool", lambda e: e.tensor_tensor(out=acc[:, tl, nb * 256:(nb + 1) * 256], in0=acc[:, tl, nb * 256:(nb + 1) * 256], in1=sg5[sb_i][:], op=ALU.add),
                                 reads=[sg5_b[sb_i], acc_b[tl]], writes=[acc_b[tl]])
                    for tl in range(8):
                        b = tl % 2
                        k.op("act", lambda e: e.activation(out=junk5[:], in_=acc[:, tl, :], func=AF.Square, accum_out=st5[:, tl, 2:3]),
                             reads=[acc_b[tl]], writes=[junk5_b, st5_b[tl]])
                        k.op("act", lambda e: e.activation(out=st5[:, tl, 3:4], in_=st5[:, tl, 2:3], func=AF.Sqrt, scale=1.0 / D, bias=EPS),
                             reads=[st5_b[tl]], writes=[st5_b[tl]])
                        k.op("dve", lambda e: e.reciprocal(out=st5[:, tl, 3:4], in_=st5[:, tl, 3:4]), reads=[st5_b[tl]], writes=[st5_b[tl]])
                        k.op("dve", lambda e: e.scalar_tensor_tensor(out=ot[b][:], in0=acc[:, tl, :], scalar=st5[:, tl, 3:4], in1=nfb[:], op0=ALU.mult, op1=ALU.mult),
                             reads=[acc_b[tl], st5_b[tl], nfb_b], writes=[ot_b[b]])
                        k.dma("sp", out_d[t0 + tl * 128:t0 + (tl + 1) * 128, :], ot[b][:], reads=[ot_b[b]])
                k.barrier()
                gpx.close()

    else:
        ms.close()
        hs.close()
    k.finish()
    cs.close()
    es.close()
    return nc


def prefix_tables(core):
    half = DH // 2
    inv_freq = (np.float32(10000.0) ** (-np.arange(half, dtype=np.float32) / np.float32(half))).astype(np.float32)
    npos = NPT * 128
    pos = np.arange(npos).astype(np.float32)
    ang = (pos[:, None] * inv_freq[None, :]).astype(np.float32).astype(np.float64)
    ropep = np.concatenate([np.cos(ang), np.sin(ang)], axis=1).astype(np.float32).reshape(NPT, 128, 128)
    g = _gammas()
    t0 = core * T
    t = np.arange(npos)
    dist = (t0 - 1 - t).astype(np.float64)
    kd = np.zeros((npos, H), np.float64)
    valid = t < t0
    for h in range(H):
        kd[valid, h] = g[h] ** dist[valid] * DH ** -0.5
    kdecp = kd.reshape(NPT, 128, H).transpose(1, 0, 2).reshape(128, NPT * H).astype(np.float32)
    return np.ascontiguousarray(ropep), np.ascontiguousarray(kdecp)


def s5_host_layout(inp):
    def st(a):
        return a.reshape(32, 2, 64).transpose(1, 2, 0).reshape(128, 32)
    lamre = st(inp["ssm_lam_re"][0])
    lamim = st(inp["ssm_lam_im"][0])
    logdt = st(np.broadcast_to(inp["ssm_log_dt"][0][:, None], (64, 64)))
    def stb(a):
        return a.reshape(32, 2, 64, 16).transpose(1, 2, 0, 3).reshape(128, 32 * 16)
    bre = stb(inp["ssm_b_re"][0])
    bim = stb(inp["ssm_b_im"][0])
    cre = stb(inp["ssm_c_re"][0].transpose(0, 2, 1))
    cim = stb(inp["ssm_c_im"][0].transpose(0, 2, 1))
    s5p = np.ascontiguousarray(np.concatenate([lamre, lamim, logdt, bre, bim, cre, cim], axis=1).astype(np.float32))
    d = inp["ssm_d"][0]
    dfull = d.reshape(8, 128).T
    dpair = np.zeros((128, 32), np.float32)
    dpair[0:32, :] = d.reshape(32, 32).T
    s5d = np.ascontiguousarray(np.concatenate([dfull, dpair], axis=1).astype(np.float32))
    return s5p, s5d


def _bf16(a):
    return np.ascontiguousarray(a).astype(ml_dtypes.bfloat16)


def make_in_maps(inp, fused=False):
    x = np.ascontiguousarray(inp["x"][0])
    nmixT = np.ascontiguousarray(inp["norm_mix"][0].reshape(KC, 128).T)
    gnw_b = np.broadcast_to(inp["ret_gn_w"][0][None, :], (128, RW))
    nffnT = inp["norm_ffn"][0].reshape(KC, 128).T
    npleT = inp["norm_ple"][0].reshape(KC, 128).T
    rb = np.broadcast_to(np.concatenate([inp["b_router_group"][0], inp["b_router_expert"][0]])[None, :], (128, 36))
    small = np.ascontiguousarray(np.concatenate([nmixT, gnw_b, nffnT, npleT, rb], axis=1).astype(np.float32))
    ident = np.eye(128, dtype=np.float32)
    s5p, s5d = s5_host_layout(inp)
    ar = np.arange(128)
    lts = (ar[:, None] < ar[None, :]).astype(np.float32)
    moec = np.ascontiguousarray(np.concatenate([lts, np.ones((128, 128), np.float32),
                                                np.broadcast_to(np.arange(CG, dtype=np.float32)[None, :], (128, CG))], axis=1))
    maps = []
    for c in range(NCORES):
        maps.append({
            "x": np.ascontiguousarray(x[c * T:(c + 1) * T]),
            "w_in": np.ascontiguousarray(inp["w_in"][0]),
            "tabs": host_tables(c),
            "small": small,
            "identb": ident,
            "s5p": s5p,
            "s5d": s5d,
            "w_glu": np.ascontiguousarray(inp["w_glu"][0]),
            "w_merge": np.ascontiguousarray(inp["w_merge"][0]),
            "w_a": np.ascontiguousarray(inp["w_branch_a"][0]),
            "w_b": np.ascontiguousarray(inp["w_branch_b"][0]),
            "w_out": np.ascontiguousarray(inp["w_out"][0]),
            "w_rt": np.ascontiguousarray(np.concatenate([inp["w_router_group"][0], inp["w_router_expert"][0]], axis=1)),
            "w_eg": np.ascontiguousarray(inp["w_exp_gate"][0]),
            "w_eu": np.ascontiguousarray(inp["w_exp_up"][0]),
            "w_ed": np.ascontiguousarray(inp["w_exp_down"][0]),
            "w_pg": np.ascontiguousarray(inp["w_ple_gate"][0]),
            "w_ple": np.ascontiguousarray(inp["w_ple"][0]),
            "p": np.ascontiguousarray(inp["p"][0, 0][c * T:(c + 1) * T]),
            "nfb": np.ascontiguousarray(np.broadcast_to(inp["norm_f"][None, :], (128, D))),
            "moec": moec,
        })
        if fused:
            ropep, kdecp = prefix_tables(c)
            maps[-1]["xfull"] = x
            maps[-1]["ropep"] = ropep
            maps[-1]["kdecp"] = kdecp
    return maps


def kernel(**inp):
    maps = make_in_maps(inp, fused=True)
    nc = build(mode="fused")
    res = run_bass_kernel_spmd(nc, maps, core_ids=list(range(NCORES)))
    return np.concatenate([r["out"] for r in res.results], axis=0)[None]
```
